# Optimizing a Trainium2 kernel written in Bass

```python
import math
import jax, jax.numpy as jnp
from jax import lax
import numpy as np


D_MODEL = 2048
BATCH = 2
SEQ = 4096
DEPTH = 1

HEAD_DIM = 64
NSA_HEADS = 16
NSA_GROUPS = 4
NSA_HPG = NSA_HEADS // NSA_GROUPS
SB_HEADS = 16
CMP_LEN = 32
CMP_STRIDE = 16
CMP_HIDDEN = 128
SEL_LEN = 64
SEL_TOPK = 16
N_LOCAL_SEL = 2
SEL_FORCE_BONUS = 1e4
WINDOW = 512
Q_BLOCK = 128
REL_BUCKETS = 32
REL_MAX_DIST = 1024
PEER_HEADS = 8
PEER_KEYS = 128
PEER_EXPERTS = PEER_KEYS * PEER_KEYS
PEER_QDIM = 256
PEER_TOPK = 16
TOKEN_CHUNK = 128
EPS = 1e-6
NEG = -1e30

NSA_Q = NSA_HEADS * HEAD_DIM
NSA_KV = NSA_GROUPS * HEAD_DIM
SB_W = SB_HEADS * HEAD_DIM
IN_COLS = NSA_Q + 6 * NSA_KV + 3 * NSA_HEADS + 3 * SB_W + 2 * D_MODEL

kernel_name = 'hybrid_nsa_stickbreak_peer_block'


def _in_splits():
    sizes = [NSA_Q] + [NSA_KV] * 6 + [3 * NSA_HEADS, SB_W, SB_W, SB_W, D_MODEL, D_MODEL]
    out, acc = [], 0
    for s in sizes[:-1]:
        acc += s
        out.append(acc)
    return out


def rmsnorm(x, g):
    xf = x.astype(jnp.float32)
    y = xf * lax.rsqrt(jnp.mean(xf * xf, axis=-1, keepdims=True) + EPS)
    return (y * g.astype(jnp.float32)).astype(x.dtype)


def rel_bucket(dist):
    dist = jnp.maximum(dist, 0)
    n_exact = REL_BUCKETS // 2
    d_f = jnp.maximum(dist, 1).astype(jnp.float32)
    large = n_exact + (jnp.log(d_f / n_exact) / math.log(REL_MAX_DIST / n_exact)
                       * (REL_BUCKETS - n_exact)).astype(jnp.int32)
    large = jnp.minimum(large, REL_BUCKETS - 1)
    return jnp.where(dist < n_exact, dist, large)


def compress_tokens(tok, pe, w1, w2):
    B, T, G, d = tok.shape
    n_cmp = (T - CMP_LEN) // CMP_STRIDE + 1
    idx = jnp.arange(n_cmp)[:, None] * CMP_STRIDE + jnp.arange(CMP_LEN)[None, :]
    blk = tok[:, idx] + pe[None, None, :, None, :]
    blk = jnp.transpose(blk, (0, 1, 3, 2, 4)).reshape(B, n_cmp, G, CMP_LEN * d)
    return jax.nn.gelu(blk @ w1) @ w2


def nsa_compressed(q, kc, vc, rel_table):
    B, T, G, Hg, d = q.shape
    n_cmp = kc.shape[1]
    blk_end = jnp.arange(n_cmp)[None, :] * CMP_STRIDE + CMP_LEN - 1
    dist = jnp.arange(T)[:, None] - blk_end
    valid = dist >= 0
    bias = jnp.transpose(rel_table[rel_bucket(dist)], (2, 0, 1)).reshape(G, Hg, T, n_cmp)
    s = jnp.einsum('btghd,bngd->bghtn', q, kc).astype(jnp.float32) * (HEAD_DIM ** -0.5) + bias
    s = jnp.where(valid, s, NEG)
    p = jnp.where(valid, jax.nn.softmax(s, axis=-1), 0.0)
    o = jnp.einsum('bghtn,bngd->btghd', p.astype(vc.dtype), vc)
    return o, p


def nsa_select_indices(p_cmp, T):
    n_cmp = p_cmp.shape[-1]
    n_sel = T // SEL_LEN
    c_start = jnp.arange(n_cmp) * CMP_STRIDE
    s_start = jnp.arange(n_sel) * SEL_LEN
    overlap = jnp.maximum(jnp.minimum(c_start[:, None] + CMP_LEN, s_start[None, :] + SEL_LEN)
                          - jnp.maximum(c_start[:, None], s_start[None, :]), 0).astype(jnp.float32) / CMP_LEN
    imp = jnp.einsum('bghtn,nj->bgtj', p_cmp, overlap)
    cur = jnp.arange(T)[:, None] // SEL_LEN
    j = jnp.arange(n_sel)[None, :]
    valid = j <= cur
    forced = (j == 0) | ((cur - j >= 0) & (cur - j < N_LOCAL_SEL))
    score = jnp.where(valid, imp + SEL_FORCE_BONUS * forced.astype(jnp.float32), -jnp.inf)
    top_s, top_i = lax.top_k(score, min(SEL_TOPK, n_sel))
    return top_i, jnp.isfinite(top_s)


def nsa_selected(q, ks, vs, sel_idx, sel_ok, rel_table):
    B, T, G, Hg, d = q.shape
    n_sel = T // SEL_LEN
    k = sel_idx.shape[-1]
    nq = T // Q_BLOCK
    kb = ks.reshape(B, n_sel, SEL_LEN, G, d).transpose(0, 3, 1, 2, 4)
    vb = vs.reshape(B, n_sel, SEL_LEN, G, d).transpose(0, 3, 1, 2, 4)
    table_g = rel_table.reshape(REL_BUCKETS, G, Hg).transpose(1, 0, 2)
    qs = q.reshape(B, nq, Q_BLOCK, G, Hg, d).transpose(1, 0, 2, 3, 4, 5)
    idx_s = sel_idx.reshape(B, G, nq, Q_BLOCK, k).transpose(2, 0, 1, 3, 4)
    ok_s = sel_ok.reshape(B, G, nq, Q_BLOCK, k).transpose(2, 0, 1, 3, 4)
    b_ix = jnp.arange(B)[:, None, None, None]
    g_ix = jnp.arange(G)[None, :, None, None]

    def body(args):
        qi, ii, oki, blk = args
        kg = kb[b_ix, g_ix, ii].reshape(B, G, Q_BLOCK, k * SEL_LEN, d)
        vg = vb[b_ix, g_ix, ii].reshape(B, G, Q_BLOCK, k * SEL_LEN, d)
        pos = (ii[..., None] * SEL_LEN + jnp.arange(SEL_LEN)).reshape(B, G, Q_BLOCK, k * SEL_LEN)
        t = blk * Q_BLOCK + jnp.arange(Q_BLOCK)
        dist = t[None, None, :, None] - pos
        mask = (dist >= 0) & jnp.repeat(oki, SEL_LEN, axis=-1)
        bias = jax.vmap(lambda tab, bk: tab[bk], in_axes=(0, 1), out_axes=1)(table_g, rel_bucket(dist))
        s = (jnp.einsum('bqghd,bgqsd->bghqs', qi, kg).astype(jnp.float32) * (HEAD_DIM ** -0.5)
             + bias.transpose(0, 1, 4, 2, 3))
        m = mask[:, :, None]
        p = jnp.where(m, jax.nn.softmax(jnp.where(m, s, NEG), axis=-1), 0.0)
        return jnp.einsum('bghqs,bgqsd->bqghd', p.astype(vg.dtype), vg)

    out = lax.map(body, (qs, idx_s, ok_s, jnp.arange(nq)))
    return out.transpose(1, 0, 2, 3, 4, 5).reshape(B, T, G, Hg, d)


def nsa_window(q, kw, vw, rel_table):
    B, T, G, Hg, d = q.shape
    nq = T // Q_BLOCK
    S = WINDOW + Q_BLOCK
    kp = jnp.pad(kw, ((0, 0), (WINDOW, 0), (0, 0), (0, 0)))
    vp = jnp.pad(vw, ((0, 0), (WINDOW, 0), (0, 0), (0, 0)))
    qs = q.reshape(B, nq, Q_BLOCK, G, Hg, d).transpose(1, 0, 2, 3, 4, 5)
    rel = jnp.arange(Q_BLOCK)[:, None] + WINDOW - jnp.arange(S)[None, :]
    bias = jnp.transpose(rel_table[rel_bucket(rel)], (2, 0, 1)).reshape(G, Hg, Q_BLOCK, S)

    def body(args):
        qi, blk = args
        start = blk * Q_BLOCK
        kk = lax.dynamic_slice_in_dim(kp, start, S, axis=1)
        vv = lax.dynamic_slice_in_dim(vp, start, S, axis=1)
        key_pos = start - WINDOW + jnp.arange(S)
        mask = (rel >= 0) & (rel < WINDOW) & (key_pos[None, :] >= 0)
        s = jnp.einsum('bqghd,bsgd->bghqs', qi, kk).astype(jnp.float32) * (HEAD_DIM ** -0.5) + bias
        p = jax.nn.softmax(jnp.where(mask, s, NEG), axis=-1)
        return jnp.einsum('bghqs,bsgd->bqghd', p.astype(vv.dtype), vv)

    out = lax.map(body, (qs, jnp.arange(nq)))
    return out.transpose(1, 0, 2, 3, 4, 5).reshape(B, T, G, Hg, d)


def stick_breaking(q, k, v):
    B, T, H, d = q.shape
    nq = T // Q_BLOCK
    qs = q.reshape(B, nq, Q_BLOCK, H, d).transpose(1, 0, 2, 3, 4)
    key_pos = jnp.arange(T)

    def body(args):
        qi, blk = args
        t = blk * Q_BLOCK + jnp.arange(Q_BLOCK)
        mask = key_pos[None, :] < t[:, None]
        z = jnp.einsum('bqhd,bshd->bhqs', qi, k).astype(jnp.float32) * (HEAD_DIM ** -0.5)
        log_beta = jax.nn.log_sigmoid(z)
        log_rem = jnp.where(mask, log_beta - z, 0.0)
        suffix = lax.cumsum(log_rem, axis=3, reverse=True) - log_rem
        a = jnp.where(mask, jnp.exp(log_beta + suffix), 0.0)
        return jnp.einsum('bhqs,bshd->bqhd', a.astype(v.dtype), v)

    out = lax.map(body, (qs, jnp.arange(nq)))
    return out.transpose(1, 0, 2, 3, 4).reshape(B, T, H * d)


def peer_ffn(x, w_q, sub_keys, u, v):
    B, T, D = x.shape
    N = B * T
    xf = x.reshape(N, D)
    q = (xf @ w_q).reshape(N, PEER_HEADS, 2, PEER_QDIM // 2)
    s = jnp.einsum('nhpc,hpkc->nhpk', q, sub_keys).astype(jnp.float32)
    top_v, top_i = lax.top_k(s, PEER_TOPK)
    cand = top_v[:, :, 0, :, None] + top_v[:, :, 1, None, :]
    cand_i = top_i[:, :, 0, :, None] * PEER_KEYS + top_i[:, :, 1, None, :]
    best_s, best_j = lax.top_k(cand.reshape(N, PEER_HEADS, PEER_TOPK * PEER_TOPK), PEER_TOPK)
    expert = jnp.take_along_axis(cand_i.reshape(N, PEER_HEADS, PEER_TOPK * PEER_TOPK), best_j, axis=-1)
    gate = jax.nn.softmax(best_s, axis=-1)
    nc = N // TOKEN_CHUNK
    E = PEER_HEADS * PEER_TOPK
    xs = (xf.reshape(nc, TOKEN_CHUNK, D), expert.reshape(nc, TOKEN_CHUNK, E), gate.reshape(nc, TOKEN_CHUNK, E))

    def body(args):
        xc, ec, gc = args
        act = jax.nn.gelu(jnp.einsum('cd,ced->ce', xc, u[ec]))
        return jnp.einsum('ce,ced->cd', (gc * act).astype(v.dtype), v[ec])

    return lax.map(body, xs).reshape(B, T, D)


def hybrid_layer(h, attn_g, w_in, ck_pe, ck_w1, ck_w2, cv_pe, cv_w1, cv_w2, rel_table,
                 w_br_nsa, w_br_sb, w_out, ffn_g, pq, psk, pu, pv):
    B, T, _ = h.shape
    a = rmsnorm(h, attn_g)
    proj = a @ w_in
    (q_n, kc, vc, ks, vs, kw, vw, g_br, q_b, k_b, v_b, g_a, g_b) = jnp.split(proj, _in_splits(), axis=-1)
    G, Hg, d = NSA_GROUPS, NSA_HPG, HEAD_DIM
    q_n = q_n.reshape(B, T, G, Hg, d)
    kv = lambda z: z.reshape(B, T, G, d)
    kc_blk = compress_tokens(kv(kc), ck_pe, ck_w1, ck_w2)
    vc_blk = compress_tokens(kv(vc), cv_pe, cv_w1, cv_w2)
    o_c, p_c = nsa_compressed(q_n, kc_blk, vc_blk, rel_table)
    sel_idx, sel_ok = nsa_select_indices(p_c, T)
    o_s = nsa_selected(q_n, kv(ks), kv(vs), sel_idx, sel_ok, rel_table)
    o_w = nsa_window(q_n, kv(kw), kv(vw), rel_table)
    gb = jax.nn.sigmoid(g_br).reshape(B, T, 3, G, Hg, 1)
    o_nsa = (gb[:, :, 0] * o_c + gb[:, :, 1] * o_s + gb[:, :, 2] * o_w).reshape(B, T, NSA_Q)
    shp = (B, T, SB_HEADS, HEAD_DIM)
    o_sb = stick_breaking(q_b.reshape(shp), k_b.reshape(shp), v_b.reshape(shp))
    merged = jax.nn.sigmoid(g_a) * (o_nsa @ w_br_nsa) + jax.nn.sigmoid(g_b) * (o_sb @ w_br_sb)
    h = h + merged @ w_out
    h = h + peer_ffn(rmsnorm(h, ffn_g), pq, psk, pu, pv)
    return h


def setup_inputs(seed: int = 0) -> dict:
    key = jax.random.key(seed)
    ks = jax.random.split(key, 20)
    f32 = jnp.float32
    nrm = lambda k, shape, s: jax.random.normal(k, shape, f32) * s
    L = DEPTH
    return {
        'x': nrm(ks[0], (BATCH, SEQ, D_MODEL), 1.0),
        'attn_norm_g': 1.0 + nrm(ks[1], (L, D_MODEL), 0.01),
        'w_in': nrm(ks[2], (L, D_MODEL, IN_COLS), D_MODEL ** -0.5),
        'cmp_k_pe': nrm(ks[3], (L, CMP_LEN, HEAD_DIM), 0.1),
        'cmp_k_w1': nrm(ks[4], (L, CMP_LEN * HEAD_DIM, CMP_HIDDEN), (CMP_LEN * HEAD_DIM) ** -0.5),
        'cmp_k_w2': nrm(ks[5], (L, CMP_HIDDEN, HEAD_DIM), CMP_HIDDEN ** -0.5),
        'cmp_v_pe': nrm(ks[6], (L, CMP_LEN, HEAD_DIM), 0.1),
        'cmp_v_w1': nrm(ks[7], (L, CMP_LEN * HEAD_DIM, CMP_HIDDEN), (CMP_LEN * HEAD_DIM) ** -0.5),
        'cmp_v_w2': nrm(ks[8], (L, CMP_HIDDEN, HEAD_DIM), CMP_HIDDEN ** -0.5),
        'rel_bias_table': nrm(ks[9], (REL_BUCKETS, NSA_HEADS), 0.2),
        'w_branch_nsa': nrm(ks[10], (L, NSA_Q, D_MODEL), NSA_Q ** -0.5),
        'w_branch_sb': nrm(ks[11], (L, SB_W, D_MODEL), SB_W ** -0.5),
        'w_out': nrm(ks[12], (L, D_MODEL, D_MODEL), D_MODEL ** -0.5),
        'ffn_norm_g': 1.0 + nrm(ks[13], (L, D_MODEL), 0.01),
        'peer_w_q': nrm(ks[14], (L, D_MODEL, PEER_HEADS * PEER_QDIM), D_MODEL ** -0.5),
        'peer_sub_keys': nrm(ks[15], (L, PEER_HEADS, 2, PEER_KEYS, PEER_QDIM // 2), (PEER_QDIM // 2) ** -0.5),
        'peer_u': nrm(ks[16], (L, PEER_EXPERTS, D_MODEL), D_MODEL ** -0.5),
        'peer_v': nrm(ks[17], (L, PEER_EXPERTS, D_MODEL), PEER_HEADS ** -0.5),
        'final_norm_g': 1.0 + nrm(ks[18], (D_MODEL,), 0.01),
    }


def reference(x, attn_norm_g, w_in, cmp_k_pe, cmp_k_w1, cmp_k_w2, cmp_v_pe, cmp_v_w1, cmp_v_w2,
              rel_bias_table, w_branch_nsa, w_branch_sb, w_out, ffn_norm_g, peer_w_q, peer_sub_keys,
              peer_u, peer_v, final_norm_g):
    h = x
    for l in range(DEPTH):
        h = hybrid_layer(h, attn_norm_g[l], w_in[l], cmp_k_pe[l], cmp_k_w1[l], cmp_k_w2[l],
                         cmp_v_pe[l], cmp_v_w1[l], cmp_v_w2[l], rel_bias_table,
                         w_branch_nsa[l], w_branch_sb[l], w_out[l], ffn_norm_g[l],
                         peer_w_q[l], peer_sub_keys[l], peer_u[l], peer_v[l])
    return rmsnorm(h, final_norm_g)
```

```python
import math
from contextlib import ExitStack

import numpy as np
import concourse.bass as bass
import concourse.mybir as mybir
from concourse.bass_utils import run_bass_kernel_spmd

F32 = mybir.dt.float32
BF16 = mybir.dt.bfloat16
I32 = mybir.dt.int32
U32 = mybir.dt.uint32
ALU = mybir.AluOpType
AF = mybir.ActivationFunctionType
AX = mybir.AxisListType

T = 4096
D = 2048
NB = 32
NS = 8
OWN = 1024
NEGM = -30000.0
EPS = 1e-6
IN_COLS = 9776
C_QN, C_KC, C_VC, C_KS, C_VS, C_KW, C_VW, C_GBR, C_QB, C_KB, C_VB, C_GA, C_GB = (
    0, 1024, 1280, 1536, 1792, 2048, 2304, 2560, 2608, 3632, 4656, 5680, 7728)

DEBUG = {}


class Sched:
    NDS = 32
    NHW = 20

    def __init__(self, nc, stack):
        self.nc = nc
        self.eng = {'pe': nc.tensor, 'dve': nc.vector, 'act': nc.scalar,
                    'pool': nc.gpsimd, 'sp': nc.sync}
        self.sem = {k: stack.enter_context(nc.semaphore('s_' + k)) for k in self.eng}
        self.cnt = {k: 0 for k in self.eng}
        self.waited = {}
        self.dsem = [stack.enter_context(nc.semaphore('d%d' % i)) for i in range(self.NDS)]
        self.dcnt = [0] * self.NDS
        self.dnext = 0
        self.dnext_sw = 0
        self.bgsem = [stack.enter_context(nc.semaphore('bg%d' % i)) for i in range(64)]
        self.bgcnt = [0] * 64
        self.bgnext = 0
        self.last_w = {}
        self.readers = {}
        self.ninst = 0

    def _wait(self, e, tok):
        if tok is None:
            return
        if tok[0] == 'e':
            _, p, n = tok
            if p == 'pe' and e == 'pe':
                return
            key = (e, 'e', p)
            if self.waited.get(key, 0) >= n:
                return
            self.eng[e].wait_ge(self.sem[p], n)
            self.waited[key] = n
        else:
            _, slot, v = tok
            key = (e, 'd', slot)
            if self.waited.get(key, 0) >= v:
                return
            self.eng[e].wait_ge(self.dsem[slot], v)
            self.waited[key] = v

    def _deps(self, e, reads, writes):
        toks = []
        for k in reads:
            toks.append(self.last_w.get(k))
        for k in writes:
            toks.append(self.last_w.get(k))
            toks.extend(self.readers.get(k, []))
        for t in toks:
            self._wait(e, t)

    def _update(self, tok, reads, writes):
        for k in reads:
            self.readers.setdefault(k, []).append(tok)
        for k in writes:
            self.last_w[k] = tok
            self.readers[k] = []

    def op(self, e, fn, reads=(), writes=()):
        self._deps(e, reads, writes)
        ins = fn(self.eng[e])
        self.cnt[e] += 1
        ins.then_inc(self.sem[e], 1)
        tok = ('e', e, self.cnt[e])
        self._update(tok, reads, writes)
        self.ninst += 1
        return tok

    def dma(self, q, out, in_, reads=(), writes=(), fn=None, **kw):
        if q == 'pool':
            slot = self.NHW + self.dnext_sw
            self.dnext_sw = (self.dnext_sw + 1) % (self.NDS - self.NHW)
        else:
            slot = self.dnext
            self.dnext = (self.dnext + 1) % self.NHW
        if self.dcnt[slot] > 0:
            self._wait(q, ('d', slot, self.dcnt[slot]))
        self._deps(q, reads, writes)
        if fn is not None:
            ins = fn(self.eng[q])
        else:
            ins = self.eng[q].dma_start(out=out, in_=in_, **kw)
        ins.then_inc(self.dsem[slot], 16)
        self.dcnt[slot] += 16
        tok = ('d', slot, self.dcnt[slot])
        self._update(tok, reads, writes)
        self.ninst += 1
        return tok

    def bg_dma(self, q, out, in_):
        slot = self.bgnext
        self.bgnext = (self.bgnext + 1) % len(self.bgsem)
        if self.bgcnt[slot] > 0:
            key = (q, 'bg', slot)
            if self.waited.get(key, 0) < self.bgcnt[slot]:
                self.eng[q].wait_ge(self.bgsem[slot], self.bgcnt[slot])
                self.waited[key] = self.bgcnt[slot]
        self.eng[q].dma_start(out=out, in_=in_).then_inc(self.bgsem[slot], 16)
        self.bgcnt[slot] += 16
        self.ninst += 1

    def wait_bg(self, engines):
        for e in engines:
            for slot in range(len(self.bgsem)):
                if self.bgcnt[slot] > 0:
                    self.eng[e].wait_ge(self.bgsem[slot], self.bgcnt[slot])

    def barrier(self):
        for e in self.eng:
            for p in self.eng:
                if p != e and self.cnt[p] > 0:
                    self._wait(e, ('e', p, self.cnt[p]))
            for s in range(self.NDS):
                if self.dcnt[s] > 0:
                    self._wait(e, ('d', s, self.dcnt[s]))
        self.last_w = {}
        self.readers = {}

    def finish(self):
        e = 'sp'
        self.wait_bg([e])
        for p in self.eng:
            if p != e and self.cnt[p] > 0:
                self._wait(e, ('e', p, self.cnt[p]))
        for s in range(self.NDS):
            if self.dcnt[s] > 0:
                self._wait(e, ('d', s, self.dcnt[s]))


def _bucket(dist):
    dist = np.maximum(dist, 0)
    d_f = np.maximum(dist, 1).astype(np.float32)
    large = 16 + (np.log(d_f / np.float32(16)) / np.float32(math.log(1024 / 16)) * np.float32(16)).astype(np.int32)
    large = np.minimum(large, 31)
    return np.where(dist < 16, dist, large).astype(np.int64)


def _bucket_jax(dist):
    return _bucket(dist)


def build_program(stages=99):
    nc = bass.Bass("TRN2", target_bir_lowering=False)
    dt_in = lambda n, s, d=F32: nc.dram_tensor(n, list(s), d, kind="ExternalInput").ap()
    dt_sc = lambda n, s, d: nc.dram_tensor(n, list(s), d, kind="Internal").ap()
    x_full = dt_in("x_full", [T, D])
    x_own = dt_in("x_own", [OWN, D])
    w_in = dt_in("w_in", [D, IN_COLS])
    g_attn = dt_in("g_attn", [D])
    g_ffn = dt_in("g_ffn", [D])
    g_fin = dt_in("g_fin", [D])
    ident_d = dt_in("ident", [128, 128])
    tri_d = dt_in("tri", [128, 128])
    abs_d = dt_in("abs", [12, 128, 16, 128])
    abw_d = dt_in("abw", [8, 128, 16, 128])
    msb_d = dt_in("msb", [4, 128, 128])
    cbias_d = dt_in("cbias", [NS, 2, 128, 16, 128])
    selc_d = dt_in("selc", [NS, 128, 64])
    ovl_d = dt_in("ovl", [2, 128, 64])
    expand_d = dt_in("expand", [NB, 64, 128])
    ckw1_d = dt_in("ckw1", [2, 2048, 128])
    ckpe_d = dt_in("ckpeT", [2, 64, 32])
    ckw2_d = dt_in("ckw2", [2, 128, 64])
    wbn_d = dt_in("wbn", [1024, D])
    wbs_d = dt_in("wbs", [1024, D])
    wout_d = dt_in("wout", [D, D])
    wq_d = dt_in("wq", [D, D])
    skT_d = dt_in("skT", [16, 128, 128])
    pu_d = dt_in("pu", [16384, D])
    pv_d = dt_in("pv", [16384, D])
    iota_d = dt_in("iota16", [128, 16])
    out_d = nc.dram_tensor("out", [OWN, D], F32, kind="ExternalOutput").ap()
    dbg = {}

    def dbg_out(name, shape, dtype=F32):
        dbg[name] = nc.dram_tensor("dbg_" + name, list(shape), dtype, kind="ExternalOutput").ap()
        return dbg[name]

    kcvT = dt_sc("kcvT", [2, 4, 64, T], BF16)
    ksT = dt_sc("ksT", [4, 64, T], BF16)
    kwT = dt_sc("kwT", [4, 64, T], BF16)
    kbT = dt_sc("kbT", [16, 64, T], BF16)
    vsw = dt_sc("vsw", [T, 512], BF16)
    vb = dt_sc("vb", [T, 1024], BF16)
    qnT = dt_sc("qnT", [16, 64, OWN], BF16)
    qbT = dt_sc("qbT", [16, 64, OWN], BF16)
    gbr_s = dt_sc("gbr_s", [OWN, 48], F32)
    gab_s = dt_sc("gab_s", [OWN, 4096], F32)
    obr_s = dt_sc("obr_s", [3, OWN, 1024], F32)
    osb_s = dt_sc("osb_s", [OWN, 1024], F32)
    h1_s = dt_sc("h1_s", [OWN, D], F32)
    pu16 = dt_sc("pu16", [16384, D], BF16)
    pv16 = dt_sc("pv16", [16384, D], BF16)

    with ExitStack() as gst:
        S = Sched(nc, gst)
        gsb = lambda n, s, d: gst.enter_context(nc.sbuf_tensor(n, list(s), d))
        gps = lambda n, s, d: gst.enter_context(nc.psum_tensor(n, list(s), d))
        pA = [gps("pA%d" % i, [128, 512], F32) for i in range(2)]
        pC = [gps("pC%d" % i, [128, 512], F32) for i in range(2)]
        pO = [gps("pO%d" % i, [128, 512], F32) for i in range(2)]
        pT = gps("pT", [128, 1024], BF16)
        pM = gps("pM", [128, 512], F32)

        identf = gsb("identf", [128, 128], F32)
        identb = gsb("identb", [128, 128], BF16)
        trib = gsb("trib", [128, 128], BF16)
        onesb = gsb("onesb", [128, 128], BF16)

        S.dma('sp', identf[:], ident_d, writes=['identf'])
        S.op('dve', lambda e: e.tensor_copy(identb[:], identf[:]), reads=['identf'], writes=['identb'])
        S.dma('pool', trib[:], tri_d, writes=['trib'])
        S.op('dve', lambda e: e.memset(onesb[:], 1.0), writes=['onesb'])

        rr = {'ev': 0}
        cast_jobs = []
        for r0 in range(0, 16384, 512):
            cast_jobs.append((pu16[r0:r0 + 512, :], pu_d[r0:r0 + 512, :]))
            cast_jobs.append((pv16[r0:r0 + 512, :], pv_d[r0:r0 + 512, :]))

        def issue_casts(n):
            for _ in range(n):
                if cast_jobs:
                    o_, i_ = cast_jobs.pop(0)
                    S.bg_dma('pool', o_, i_)

        def evac(out_ap, in_ap, reads, writes, scale=None):
            rr['ev'] += 1
            if rr['ev'] % 2 == 0:
                if scale is None:
                    S.op('dve', lambda e: e.tensor_copy(out_ap, in_ap), reads=reads, writes=writes)
                else:
                    S.op('dve', lambda e: e.tensor_scalar(out_ap, in_ap, scale, None, ALU.mult), reads=reads, writes=writes)
            else:
                S.op('act', lambda e: e.activation(out_ap, in_ap, AF.Copy, scale=(1.0 if scale is None else scale)),
                     reads=reads, writes=writes)

        def make_gfull(st, g_ap, name):
            gt = st.enter_context(nc.sbuf_tensor(name + "_gt", [128, 16], F32))
            gfull = st.enter_context(nc.sbuf_tensor(name + "_gf", [128, 16, 128], BF16))
            S.dma('sp', gt[:], g_ap.rearrange("(c p) -> p c", p=128), writes=[name + 'gt'],
                  allow_slow_non_contiguous=True)
            S.op('dve', lambda e: e.memset(gfull[:], 1.0), writes=[name + 'gf'])
            for c in range(16):
                S.op('dve', lambda e, c=c: e.tensor_scalar(gfull[:, c, :], gfull[:, c, :], gt[:, c:c + 1], None, ALU.mult),
                     reads=[name + 'gt', name + 'gf'], writes=[name + 'gf'])
            return gfull, name + 'gf'

        def norm_rows(xt, xkey, ss, rstd, junk, key):
            S.op('act', lambda e: e.activation(junk[:], xt, AF.Square, accum_out=ss[:]),
                 reads=[xkey], writes=[key + 'junk', key + 'ss'])
            S.op('dve', lambda e: e.tensor_scalar(rstd[:], ss[:], 1.0 / D, EPS, ALU.mult, ALU.add),
                 reads=[key + 'ss'], writes=[key + 'rstd'])
            S.op('act', lambda e: e.activation(rstd[:], rstd[:], AF.Sqrt), reads=[key + 'rstd'], writes=[key + 'rstd'])
            S.op('dve', lambda e: e.reciprocal(rstd[:], rstd[:]), reads=[key + 'rstd'], writes=[key + 'rstd'])

        def transposeT(xs, xskey, gfull, gfkey, dst, dstkey, col0):
            for half in range(2):
                for cc in range(8):
                    c = half * 8 + cc
                    S.op('pe', lambda e, c=c, cc=cc: e.transpose(pT[:, cc * 128:(cc + 1) * 128], xs[:, c * 128:(c + 1) * 128], identb[:]),
                         reads=[xskey, 'identb'], writes=['pT'])
                S.op('dve', lambda e, half=half: e.tensor_tensor(
                    dst[:, half * 8:(half + 1) * 8, col0:col0 + 128],
                    pT[:].rearrange("p (c t) -> p c t", c=8),
                    gfull[:, half * 8:(half + 1) * 8, :], ALU.mult),
                    reads=['pT', gfkey], writes=[dstkey])

        if stages >= 1:
            with ExitStack() as st:
                sb = lambda n, s, d: st.enter_context(nc.sbuf_tensor(n, list(s), d))
                WF = sb("p1_WF", [128, 16, 2048], BF16)
                WV = sb("p1_WV", [128, 16, 1536], BF16)
                xt = [sb("p1_xt%d" % i, [128, D], F32) for i in range(2)]
                junk = sb("p1_junk", [128, D], BF16)
                ss = sb("p1_ss", [128, 1], F32)
                rstd = sb("p1_rstd", [128, 1], F32)
                xs = sb("p1_xs", [128, D], BF16)
                aT = [sb("p1_aT%d" % i, [128, 16, 512], BF16) for i in range(2)]
                stg = [sb("p1_stg%d" % i, [128, 512], BF16) for i in range(4)]
                gfull, gfkey = make_gfull(st, g_attn, "p1")
                wv = w_in.rearrange("(c p) n -> p c n", p=128)
                fm_cols = [(C_KC, 256), (C_VC, 256), (C_KS, 256), (C_KW, 256), (C_KB, 1024)]
                o = 0
                for (c0, n) in fm_cols:
                    for c4 in range(0, 16, 4):
                        S.dma('pool', WF[:, c4:c4 + 4, o:o + n], wv[:, c4:c4 + 4, c0:c0 + n], writes=['WF'])
                    o += n
                tm_cols = [(C_VS, 256), (C_VW, 256), (C_VB, 1024)]
                o = 0
                for (c0, n) in tm_cols:
                    for c4 in range(0, 16, 4):
                        S.dma('pool', WV[:, c4:c4 + 4, o:o + n], wv[:, c4:c4 + 4, c0:c0 + n], writes=['WV'])
                    o += n
                kcv2 = kcvT.rearrange("a g d t -> a (g d) t")
                ks2 = ksT.rearrange("g d t -> (g d) t")
                kw2 = kwT.rearrange("g d t -> (g d) t")
                kb2 = kbT.rearrange("h d t -> (h d) t")
                fm_dst = [(kcv2[0], 0), (kcv2[0], 128), (kcv2[1], 0), (kcv2[1], 128),
                          (ks2, 0), (ks2, 128), (kw2, 0), (kw2, 128)] + [(kb2, 128 * i) for i in range(8)]
                nstg = 0
                for tt in range(T // 512):
                    a = aT[tt % 2]
                    akey = 'aT%d' % (tt % 2)
                    for ti in range(4):
                        r0 = tt * 512 + ti * 128
                        xi = (tt * 4 + ti) % 2
                        S.dma('sp', xt[xi][:], x_full[r0:r0 + 128, :], writes=['xt%d' % xi])
                        norm_rows(xt[xi][:], 'xt%d' % xi, ss, rstd, junk, 'p1')
                        S.op('dve', lambda e, xi=xi: e.tensor_scalar(xs[:], xt[xi][:], rstd[:, 0:1], None, ALU.mult),
                             reads=['xt%d' % xi, 'p1rstd'], writes=['xs'])
                        transposeT(xs, 'xs', gfull, gfkey, a, akey, ti * 128)
                    for bi in range(16):
                        pa = pA[bi % 2]
                        for c in range(16):
                            S.op('pe', lambda e, c=c, bi=bi, pa=pa: e.matmul(pa[:], WF[:, c, bi * 128:(bi + 1) * 128], a[:, c, :],
                                                                         start=(c == 0), stop=(c == 15)),
                                 reads=['WF', akey], writes=['pA%d' % (bi % 2)])
                        sg = stg[nstg % 4]
                        sk = 'stg%d' % (nstg % 4)
                        nstg += 1
                        evac(sg[:], pa[:], ['pA%d' % (bi % 2)], [sk])
                        dst, row0 = fm_dst[bi]
                        S.dma('sp', dst[row0:row0 + 128, tt * 512:(tt + 1) * 512], sg[:], reads=[sk], writes=['sc_fm'])
                    for ti in range(4):
                        r0 = tt * 512 + ti * 128
                        for vbk in range(3):
                            pa = pA[vbk % 2]
                            for c in range(16):
                                S.op('pe', lambda e, c=c, vbk=vbk, pa=pa, ti=ti: e.matmul(
                                    pa[:], a[:, c, ti * 128:(ti + 1) * 128], WV[:, c, vbk * 512:(vbk + 1) * 512],
                                    start=(c == 0), stop=(c == 15)),
                                    reads=['WV', akey], writes=['pA%d' % (vbk % 2)])
                            sg = stg[nstg % 4]
                            sk = 'stg%d' % (nstg % 4)
                            nstg += 1
                            evac(sg[:], pa[:], ['pA%d' % (vbk % 2)], [sk])
                            if vbk == 0:
                                S.dma('sp', vsw[r0:r0 + 128, :], sg[:], reads=[sk], writes=['sc_tm'])
                            else:
                                S.dma('sp', vb[r0:r0 + 128, (vbk - 1) * 512:vbk * 512], sg[:], reads=[sk], writes=['sc_tm'])
            S.barrier()

        if stages >= 2:
            with ExitStack() as st:
                sb = lambda n, s, d: st.enter_context(nc.sbuf_tensor(n, list(s), d))
                xt = [sb("p2_xt%d" % i, [128, D], F32) for i in range(2)]
                junk = sb("p2_junk", [128, D], BF16)
                ss = sb("p2_ss", [128, 1], F32)
                rstd = sb("p2_rstd", [128, 1], F32)
                xs = sb("p2_xs", [128, D], BF16)
                aT = sb("p2_aT", [128, 16, OWN], BF16)
                Wb = [sb("p2_W%d" % i, [128, 16, 512], BF16) for i in range(2)]
                stg = [sb("p2_stg%d" % i, [128, 512], BF16) for i in range(4)]
                stgf = [sb("p2_stgf%d" % i, [128, 512], F32) for i in range(4)]
                gfull, gfkey = make_gfull(st, g_attn, "p2")
                wv = w_in.rearrange("(c p) n -> p c n", p=128)
                for ti in range(8):
                    xi = ti % 2
                    S.dma('sp', xt[xi][:], x_own[ti * 128:(ti + 1) * 128, :], writes=['xt%d' % xi])
                    norm_rows(xt[xi][:], 'xt%d' % xi, ss, rstd, junk, 'p2')
                    S.op('dve', lambda e, xi=xi: e.tensor_scalar(xs[:], xt[xi][:], rstd[:, 0:1], None, ALU.mult),
                         reads=['xt%d' % xi, 'p2rstd'], writes=['xs'])
                    transposeT(xs, 'xs', gfull, gfkey, aT, 'aTown', ti * 128)
                qn2 = qnT.rearrange("h d t -> (h d) t")
                qb2 = qbT.rearrange("h d t -> (h d) t")
                blocks = [('fm', C_QN + 512 * i, 512, qn2, 512 * i) for i in range(2)]
                blocks += [('fm', C_QB + 512 * i, 512, qb2, 512 * i) for i in range(2)]
                blocks += [('tm', C_GA + 512 * i, 512, gab_s, 512 * i) for i in range(8)]
                blocks += [('gbr', C_GBR, 48, gbr_s, 0)]
                nstg = 0
                for bi, (kind, c0, n, dst, d0) in enumerate(blocks):
                    W = Wb[bi % 2]
                    wk = 'W%d' % (bi % 2)
                    for c4 in range(0, 16, 4):
                        S.dma('pool', W[:, c4:c4 + 4, 0:n], wv[:, c4:c4 + 4, c0:c0 + n], writes=[wk])
                    if kind == 'fm':
                        for sub in range(4):
                            for th in range(2):
                                pa = pA[(sub * 2 + th) % 2]
                                pk = 'pA%d' % ((sub * 2 + th) % 2)
                                for c in range(16):
                                    S.op('pe', lambda e, c=c, sub=sub, th=th, pa=pa, W=W: e.matmul(
                                        pa[:], W[:, c, sub * 128:(sub + 1) * 128], aT[:, c, th * 512:(th + 1) * 512],
                                        start=(c == 0), stop=(c == 15)), reads=[wk, 'aTown'], writes=[pk])
                                sg = stg[nstg % 4]
                                sk = 'stg%d' % (nstg % 4)
                                nstg += 1
                                evac(sg[:], pa[:], [pk], [sk], scale=0.125)
                                S.dma('sp', dst[d0 + sub * 128:d0 + (sub + 1) * 128, th * 512:(th + 1) * 512], sg[:],
                                      reads=[sk], writes=['sc_q'])
                    else:
                        for ti in range(8):
                            pa = pA[ti % 2]
                            pk = 'pA%d' % (ti % 2)
                            for c in range(16):
                                S.op('pe', lambda e, c=c, ti=ti, pa=pa, W=W, n=n: e.matmul(
                                    pa[:, 0:n], aT[:, c, ti * 128:(ti + 1) * 128], W[:, c, 0:n],
                                    start=(c == 0), stop=(c == 15)), reads=[wk, 'aTown'], writes=[pk])
                            sg = stgf[nstg % 4]
                            sk = 'stgf%d' % (nstg % 4)
                            nstg += 1
                            S.op('act', lambda e, sg=sg, pa=pa, n=n: e.activation(sg[:, 0:n], pa[:, 0:n], AF.Sigmoid),
                                 reads=[pk], writes=[sk])
                            if kind == 'tm':
                                S.dma('sp', dst[ti * 128:(ti + 1) * 128, d0:d0 + n], sg[:, 0:n], reads=[sk], writes=['sc_g'])
                            else:
                                S.dma('sp', dst[ti * 128:(ti + 1) * 128, :], sg[:, 0:n], reads=[sk], writes=['sc_g'])
            S.barrier()

        if 'p12' in DEBUG:
            d1 = dbg_out("ksT", [4, 64, T], BF16)
            S.dma('sp', d1, ksT, reads=[], writes=[])
            d2 = dbg_out("vb", [T, 1024], BF16)
            S.dma('sp', d2, vb, reads=[], writes=[])
            d3 = dbg_out("qnT", [16, 64, OWN], BF16)
            S.dma('sp', d3, qnT, reads=[], writes=[])
            d4 = dbg_out("gbr", [OWN, 48], F32)
            S.dma('sp', d4, gbr_s, reads=[], writes=[])
            d5 = dbg_out("gab", [OWN, 4096], F32)
            S.dma('sp', d5, gab_s, reads=[], writes=[])

        def bc(ap, shape):
            return ap.to_broadcast(list(shape))

        with ExitStack() as mid_st:
            msb_ = lambda n, s, d: mid_st.enter_context(nc.sbuf_tensor(n, list(s), d))
            kcbT = msb_("kcbT", [64, 4, 256], BF16)
            vcb = msb_("vcb", [128, 4, 2, 129], BF16)
            selT = msb_("selT", [128, 4, NS, 128], BF16)
            S.op('dve', lambda e: e.memset(kcbT[:], 0.0), writes=['kcbT'])
            S.op('dve', lambda e: e.memset(vcb[:], 0.0), writes=['vcb'])
            if stages >= 3:
                with ExitStack() as st:
                    sb = lambda n, s, d: st.enter_context(nc.sbuf_tensor(n, list(s), d))
                    kin = [sb("p3_kin%d" % i, [64, T], BF16) for i in range(2)]
                    W1 = sb("p3_W1", [64, 2, 32, 128], BF16)
                    peT = sb("p3_peT", [64, 2, 32], BF16)
                    W2 = sb("p3_W2", [128, 2, 64], BF16)
                    b1 = sb("p3_b1", [128, 2], F32)
                    uu = sb("p3_u", [128, 256], F32)
                    u2 = sb("p3_u2", [128, 256], F32)
                    sg = sb("p3_sg", [128, 256], F32)
                    gl = sb("p3_gl", [128, 256], BF16)
                    for kv in range(2):
                        S.dma('pool', W1[:, kv], ckw1_d[kv].rearrange("(l d) h -> d l h", d=64), writes=['W1'])
                        S.dma('pool', peT[:, kv, :], ckpe_d[kv], writes=['peT'])
                        S.dma('pool', W2[:, kv, :], ckw2_d[kv], writes=['W2'])
                    for g in range(4):
                        for c in range(2):
                            S.dma('pool', vcb[:, g, c, 65:129], ovl_d[c], reads=[], writes=['vcb'])
                    S.op('dve', lambda e: e.memset(vcb[:, :, :, 64:65], 1.0), writes=['vcb'])
                    for kv in range(2):
                        for l in range(32):
                            S.op('pe', lambda e, kv=kv, l=l: e.matmul(pM[:, kv:kv + 1], W1[:, kv, l, :], peT[:, kv, l:l + 1],
                                                                  start=(l == 0), stop=(l == 31)),
                                 reads=['W1', 'peT'], writes=['pM'])
                    S.op('dve', lambda e: e.tensor_copy(b1[:], pM[:, 0:2]), reads=['pM'], writes=['b1'])
                    it = 0
                    for kv in range(2):
                        for g in range(4):
                            ki = kin[it % 2]
                            kk = 'kin%d' % (it % 2)
                            pa = pA[it % 2]
                            pk = 'pA%d' % (it % 2)
                            it += 1
                            S.dma('sp', ki[:], kcvT[kv, g], writes=[kk])
                            for l in range(32):
                                S.op('pe', lambda e, kv=kv, l=l, ki=ki, pa=pa: e.matmul(
                                    pa[:, 0:255], W1[:, kv, l, :], ki[:, l:l + 4065:16], start=(l == 0), stop=(l == 31)),
                                    reads=['W1', kk], writes=[pk])
                            S.op('act', lambda e, kv=kv, pa=pa: e.activation(uu[:, 0:255], pa[:, 0:255], AF.Identity, bias=b1[:, kv:kv + 1]),
                                 reads=[pk, 'b1'], writes=['uu'])
                            S.op('dve', lambda e: e.tensor_tensor(u2[:, 0:255], uu[:, 0:255], uu[:, 0:255], ALU.mult), reads=['uu'], writes=['u2'])
                            S.op('dve', lambda e: e.tensor_scalar(u2[:, 0:255], u2[:, 0:255], 0.044715, 1.0, ALU.mult, ALU.add), reads=['u2'], writes=['u2'])
                            S.op('dve', lambda e: e.tensor_tensor(u2[:, 0:255], u2[:, 0:255], uu[:, 0:255], ALU.mult), reads=['u2', 'uu'], writes=['u2'])
                            S.op('act', lambda e: e.activation(sg[:, 0:255], u2[:, 0:255], AF.Sigmoid, scale=1.5957691216057308), reads=['u2'], writes=['sg'])
                            S.op('dve', lambda e: e.tensor_tensor(gl[:, 0:255], uu[:, 0:255], sg[:, 0:255], ALU.mult), reads=['uu', 'sg'], writes=['gl'])
                            if kv == 0:
                                S.op('pe', lambda e: e.matmul(pC[0][0:64, 0:255], W2[:, 0, :], gl[:, 0:255], start=True, stop=True),
                                     reads=['W2', 'gl'], writes=['pC0'])
                                S.op('dve', lambda e, g=g: e.tensor_copy(kcbT[:, g, 0:255], pC[0][0:64, 0:255]), reads=['pC0'], writes=['kcbT'])
                            else:
                                for c in range(2):
                                    rows = 128 if c == 0 else 127
                                    S.op('pe', lambda e, c=c, rows=rows: e.matmul(pC[c][0:rows, 0:64], gl[:, c * 128:c * 128 + rows], W2[:, 1, :],
                                                                                  start=True, stop=True),
                                         reads=['W2', 'gl'], writes=['pC%d' % c])
                                    S.op('dve', lambda e, c=c, rows=rows, g=g: e.tensor_copy(vcb[0:rows, g, c, 0:64], pC[c][0:rows, 0:64]),
                                         reads=['pC%d' % c], writes=['vcb'])
                S.barrier()

            if stages >= 4:
                with ExitStack() as st:
                    sb = lambda n, s, d: st.enter_context(nc.sbuf_tensor(n, list(s), d))
                    qg = [sb("p4_qg%d" % i, [64, 4, OWN], BF16) for i in range(2)]
                    cbt = [sb("p4_cb%d" % i, [128, 2, 4, 128], BF16) for i in range(2)]
                    E = [sb("p4_E%d" % i, [128, 512], BF16) for i in range(4)]
                    selc = sb("p4_selc", [128, NS, 64], F32)
                    rec = sb("p4_rec", [128, 4], F32)
                    oc = [sb("p4_oc%d" % i, [128, 4, 64], F32) for i in range(2)]
                    imp = sb("p4_imp", [128, 64], F32)
                    score = sb("p4_score", [128, 64], F32)
                    sc2 = sb("p4_sc2", [128, 64], F32)
                    m8a = sb("p4_m8a", [128, 8], F32)
                    m8b = sb("p4_m8b", [128, 8], F32)
                    selb = sb("p4_selb", [128, 128], F32)
                    S.op('dve', lambda e: e.memset(selb[:], 0.0), writes=['selb'])
                    S.dma('sp', selc[:], selc_d.rearrange("s q j -> q s j"), writes=['selc'])
                    it = 0
                    for g in range(4):
                        q_ = qg[g % 2]
                        qk = 'qg%d' % (g % 2)
                        S.dma('sp', q_[:], qnT[4 * g:4 * g + 4].rearrange("h d t -> d h t"), writes=[qk])
                        for s in range(NS):
                            cb = cbt[it % 2]
                            ck = 'cb%d' % (it % 2)
                            o_ = oc[it % 2]
                            ok = 'oc%d' % (it % 2)
                            it += 1
                            S.dma('pool', cb[:], cbias_d[s, :, :, 4 * g:4 * g + 4, :].rearrange("c n h q -> n c h q"), writes=[ck])
                            for c in range(2):
                                pa = pA[c]
                                pk = 'pA%d' % c
                                S.op('pe', lambda e, c=c, pa=pa, g=g, s=s, q_=q_: e.matmul(
                                    pa[:], kcbT[:, g, c * 128:(c + 1) * 128], q_[:, :, s * 128:(s + 1) * 128], start=True, stop=False),
                                    reads=['kcbT', qk], writes=[pk])
                                S.op('pe', lambda e, c=c, pa=pa, cb=cb: e.matmul(pa[:], identb[:], cb[:, c], start=False, stop=True),
                                     reads=['identb', ck], writes=[pk])
                                ei = (it % 2) * 2 + c
                                S.op('act', lambda e, pa=pa, ei=ei: e.activation(E[ei][:], pa[:], AF.Exp), reads=[pk], writes=['E%d' % ei])
                            for h in range(4):
                                po = pO[h // 2]
                                col = (h % 2) * 129
                                for c in range(2):
                                    ei = (it % 2) * 2 + c
                                    S.op('pe', lambda e, po=po, col=col, ei=ei, h=h, g=g, c=c: e.matmul(
                                        po[:, col:col + 129], E[ei][:, h * 128:(h + 1) * 128], vcb[:, g, c, :], start=(c == 0), stop=(c == 1)),
                                        reads=['E%d' % ei, 'vcb'], writes=['pO%d' % (h // 2)])
                            for h in range(4):
                                po = pO[h // 2]
                                col = (h % 2) * 129
                                S.op('dve', lambda e, po=po, col=col, h=h: e.tensor_scalar(rec[:, h:h + 1], po[:, col + 64:col + 65], 1e-30, None, ALU.max),
                                     reads=['pO%d' % (h // 2)], writes=['rec'])
                            S.op('dve', lambda e: e.reciprocal(rec[:], rec[:]), reads=['rec'], writes=['rec'])
                            for h in range(4):
                                po = pO[h // 2]
                                col = (h % 2) * 129
                                S.op('dve', lambda e, po=po, col=col, h=h, o_=o_: e.tensor_scalar(o_[:, h, :], po[:, col:col + 64], rec[:, h:h + 1], None, ALU.mult),
                                     reads=['pO%d' % (h // 2), 'rec'], writes=[ok])
                                if h == 0:
                                    S.op('dve', lambda e, po=po, col=col, h=h: e.tensor_scalar(imp[:], po[:, col + 65:col + 129], rec[:, h:h + 1], None, ALU.mult),
                                         reads=['pO%d' % (h // 2), 'rec'], writes=['imp'])
                                else:
                                    S.op('dve', lambda e, po=po, col=col, h=h: e.scalar_tensor_tensor(
                                        imp[:], po[:, col + 65:col + 129], rec[:, h:h + 1], imp[:], ALU.mult, ALU.add),
                                        reads=['pO%d' % (h // 2), 'rec', 'imp'], writes=['imp'])
                            S.dma('sp', obr_s[0, s * 128:(s + 1) * 128, g * 256:(g + 1) * 256], o_[:].rearrange("p h d -> p (h d)"), reads=[ok], writes=['sc_o'])
                            S.op('dve', lambda e, s=s: e.tensor_tensor(score[:], imp[:], selc[:, s, :], ALU.add), reads=['imp', 'selc'], writes=['score'])
                            S.op('dve', lambda e: e.max(m8a[:], score[:]), reads=['score'], writes=['m8a'])
                            S.op('dve', lambda e: e.match_replace(sc2[:], m8a[:], score[:], -3.0e38), reads=['score', 'm8a'], writes=['sc2'])
                            S.op('dve', lambda e: e.max(m8b[:], sc2[:]), reads=['sc2'], writes=['m8b'])
                            S.op('dve', lambda e: e.tensor_scalar(selb[:, 64:128], score[:], m8b[:, 7:8], -NEGM, ALU.is_ge, ALU.mult),
                                 reads=['score', 'm8b'], writes=['selb'])
                            S.op('dve', lambda e: e.tensor_scalar(selb[:, 64:128], selb[:, 64:128], NEGM, None, ALU.add), reads=['selb'], writes=['selb'])
                            S.op('pe', lambda e: e.transpose(pM[:, 0:128], selb[:], identf[:]), reads=['selb', 'identf'], writes=['pM'])
                            S.op('dve', lambda e, g=g, s=s: e.tensor_copy(selT[64:128, g, s, :], pM[64:128, 0:128]), reads=['pM'], writes=['selT'])
                S.barrier()

            def attn_phase(sel):
                with ExitStack() as st:
                    sb = lambda n, s, d: st.enter_context(nc.sbuf_tensor(n, list(s), d))
                    nm = "p5" if sel else "p6"
                    nab = 12 if sel else 8
                    AB = sb(nm + "_AB", [128, nab, 16, 128], BF16)
                    ab_src = abs_d if sel else abw_d
                    for u in range(nab):
                        S.dma('pool', AB[:, u], ab_src[u], writes=['AB'])
                    KR = 128 if sel else 64
                    kT = [sb(nm + "_kT%d" % i, [KR, T], BF16) for i in range(2)]
                    vt = [sb(nm + "_vt%d" % i, [128, NB, 65], BF16) for i in range(2)]
                    qg = [sb(nm + "_qg%d" % i, [KR, 4, OWN], BF16) for i in range(2)]
                    if sel:
                        for i in range(2):
                            S.dma('pool', kT[i][64:128, :].rearrange("j (b k) -> j b k", k=128), expand_d.rearrange("b j k -> j b k"),
                                  writes=['kT%d' % i])
                    E = [sb(nm + "_E%d" % i, [128, 512], BF16) for i in range(3)]
                    rec = sb(nm + "_rec", [128, 4], F32)
                    ot = [sb(nm + "_ot%d" % i, [128, 4, 64], F32) for i in range(4)]
                    ksrc = ksT if sel else kwT
                    voff = 0 if sel else 256
                    it = 0
                    ne = 0
                    def load_group(g):
                        k_ = kT[g % 2]
                        kk = 'kT%d' % (g % 2)
                        v_ = vt[g % 2]
                        vk = 'vt%d' % (g % 2)
                        q_ = qg[g % 2]
                        qk = 'qg%d' % (g % 2)
                        S.dma('sp', k_[0:64, :], ksrc[g], writes=[kk])
                        S.op('dve', lambda e: e.memset(v_[:, :, 64:65], 1.0), writes=[vk])
                        S.dma('sp', v_[:, :, 0:64], vsw[:, voff + g * 64:voff + (g + 1) * 64].rearrange("(b p) d -> p b d", p=128), writes=[vk])
                        S.dma('sp', q_[0:64], qnT[4 * g:4 * g + 4].rearrange("h d t -> d h t"), writes=[qk])
                        if sel:
                            for h in range(4):
                                S.op('pool', lambda e, h=h: e.tensor_copy(q_[64:128, h, :], selT[64:128, g].rearrange("j s q -> j (s q)")),
                                     reads=['selT'], writes=[qk])

                    load_group(0)
                    for g in range(4):
                        k_ = kT[g % 2]
                        kk = 'kT%d' % (g % 2)
                        v_ = vt[g % 2]
                        vk = 'vt%d' % (g % 2)
                        q_ = qg[g % 2]
                        qk = 'qg%d' % (g % 2)
                        if g + 1 < 4:
                            load_group(g + 1)
                        poh = [pO[0], pO[1], pC[0], pC[1]]
                        pokh = ['pO0', 'pO1', 'pC0', 'pC1']
                        items = []
                        for s in range(NS):
                            nu = 4 * s + 4 if sel else min(8, 4 * s + 4)
                            for u in range(nu):
                                items.append((s, u, nu))
                        srs = {}

                        def stageA(s, u, nu, idx):
                            if u == 0 and ((sel and s % 4 != 3) or ((not sel) and s % 4 == 0)):
                                issue_casts(1)
                            kb = 4 * s + 3 - u
                            pa = pA[idx % 2]
                            pk = 'pA%d' % (idx % 2)
                            S.op('pe', lambda e: e.matmul(pa[:], k_[:, kb * 128:(kb + 1) * 128], q_[:, :, s * 128:(s + 1) * 128], start=True, stop=False),
                                 reads=[kk, qk], writes=[pk])
                            S.op('pe', lambda e: e.matmul(pa[:], identb[:], AB[:, min(u, nab - 1), 4 * g:4 * g + 4, :], start=False, stop=True),
                                 reads=['identb', 'AB'], writes=[pk])
                            ei = idx % 3
                            S.op('act', lambda e: e.activation(E[ei][:], pa[:], AF.Exp), reads=[pk], writes=['E%d' % ei])

                        def stageC(s, u, nu, idx):
                            kb = 4 * s + 3 - u
                            ei = idx % 3
                            for h in range(4):
                                S.op('pe', lambda e, h=h: e.matmul(poh[h][:, 0:65], E[ei][:, h * 128:(h + 1) * 128], v_[:, kb, :],
                                                                  start=(u == 0), stop=(u == nu - 1)),
                                     reads=['E%d' % ei, vk], writes=[pokh[h]])
                            if u == nu - 1:
                                o_ = ot[s % 4]
                                ok = 'ot%d' % (s % 4)
                                for h in range(4):
                                    S.op('dve', lambda e, h=h: e.tensor_scalar(rec[:, h:h + 1], poh[h][:, 64:65], 1e-30, None, ALU.max),
                                         reads=[pokh[h]], writes=['rec'])
                                S.op('dve', lambda e: e.reciprocal(rec[:], rec[:]), reads=['rec'], writes=['rec'])
                                for h in range(4):
                                    S.op('dve', lambda e, h=h: e.tensor_scalar(o_[:, h, :], poh[h][:, 0:64], rec[:, h:h + 1], None, ALU.mult),
                                         reads=[pokh[h], 'rec'], writes=[ok])
                                S.dma('sp', obr_s[1 if sel else 2, s * 128:(s + 1) * 128, g * 256:(g + 1) * 256], o_[:].rearrange("p h d -> p (h d)"),
                                      reads=[ok], writes=['sc_o'])

                        for idx, (s, u, nu) in enumerate(items):
                            stageA(s, u, nu, idx)
                            if idx >= 1:
                                stageC(*items[idx - 1], idx - 1)
                        stageC(*items[-1], len(items) - 1)
                S.barrier()

            if stages >= 5:
                attn_phase(True)
            if stages >= 6:
                attn_phase(False)

            if stages >= 7:
                with ExitStack() as st:
                    sb = lambda n, s, d: st.enter_context(nc.sbuf_tensor(n, list(s), d))
                    kq = [sb("p7_kq%d" % i, [128, 2, T], BF16) for i in range(2)]
                    vq = [sb("p7_vq%d" % i, [128, NB, 256], BF16) for i in range(2)]
                    qq = [sb("p7_qq%d" % i, [128, 2, NS, 256], BF16) for i in range(2)]
                    msb4 = sb("p7_msb4", [128, 4, 4, 128], BF16)
                    ef = [sb("p7_ef%d" % i, [128, 512], F32) for i in range(3)]
                    spt = [sb("p7_sp%d" % i, [128, 512], BF16) for i in range(3)]
                    acc = sb("p7_acc", [128, 512], BF16)
                    wt = [sb("p7_w%d" % i, [128, 512], BF16) for i in range(2)]
                    at = [sb("p7_a%d" % i, [128, 512], BF16) for i in range(2)]
                    osbt = [sb("p7_o%d" % i, [128, 256], F32) for i in range(4)]
                    for h in range(4):
                        S.dma('pool', msb4[:, :, h, :], msb_d.rearrange("u k q -> k u q"), writes=['msb4'])
                    it = 0
                    n7 = 0
                    def load_quad(hq):
                        k_ = kq[hq % 2]
                        kk = 'kq%d' % (hq % 2)
                        v_ = vq[hq % 2]
                        vk = 'vq%d' % (hq % 2)
                        q_ = qq[hq % 2]
                        qk = 'qq%d' % (hq % 2)
                        S.dma('sp', k_[:], kbT[4 * hq:4 * hq + 4].rearrange("(p hh) d t -> (hh d) p t", hh=2), writes=[kk])
                        S.dma('sp', v_[:], vb[:, hq * 256:(hq + 1) * 256].rearrange("(b p) d -> p b d", p=128), writes=[vk])
                        S.op('dve', lambda e: e.memset(q_[:], 0.0), writes=[qk])
                        for p_ in range(2):
                            for hh in range(2):
                                S.dma('sp', q_[hh * 64:(hh + 1) * 64, p_, :, hh * 128:(hh + 1) * 128],
                                      qbT[4 * hq + 2 * p_ + hh].rearrange("d (s q) -> d s q", q=128), writes=[qk])

                    load_quad(0)
                    for hq in range(4):
                        k_ = kq[hq % 2]
                        kk = 'kq%d' % (hq % 2)
                        v_ = vq[hq % 2]
                        vk = 'vq%d' % (hq % 2)
                        q_ = qq[hq % 2]
                        qk = 'qq%d' % (hq % 2)
                        if hq + 1 < 4:
                            load_quad(hq + 1)
                        poh = [pO[0], pO[1], pC[1], pM]
                        pokh = ['pO0', 'pO1', 'pC1', 'pM']
                        pc = pC[0]
                        pck = 'pC0'
                        items = []
                        for s in range(NS):
                            for u in range(4 * s + 4):
                                items.append((s, u, 4 * s + 4))

                        def sbA(s, u, nu, idx):
                            if u == 0:
                                issue_casts(1)
                            kb = 4 * s + 3 - u
                            pa = pA[idx % 2]
                            pk = 'pA%d' % (idx % 2)
                            i3 = idx % 3
                            for p_ in range(2):
                                S.op('pe', lambda e, p_=p_: e.matmul(
                                    pa[:, p_ * 256:(p_ + 1) * 256], k_[:, p_, kb * 128:(kb + 1) * 128], q_[:, p_, s, :],
                                    start=True, stop=(u >= 4)), reads=[kk, qk], writes=[pk])
                                if u < 4:
                                    S.op('pe', lambda e, p_=p_: e.matmul(pa[:, p_ * 256:(p_ + 1) * 256], identb[:], msb4[:, u, 2 * p_:2 * p_ + 2, :],
                                                                       start=False, stop=True),
                                         reads=['identb', 'msb4'], writes=[pk])
                            S.op('act', lambda e: e.activation(ef[i3][:], pa[:], AF.Exp), reads=[pk], writes=['ef%d' % i3])
                            S.op('act', lambda e: e.activation(spt[i3][:], ef[i3][:], AF.Ln, bias=1.0), reads=['ef%d' % i3], writes=['sp%d' % i3])

                        def sbB(s, u, nu, idx):
                            i3 = idx % 3
                            i2 = idx % 2
                            S.op('pe', lambda e: e.matmul(pc[:], trib[:], spt[i3][:], start=True, stop=(u == 0)),
                                 reads=['trib', 'sp%d' % i3], writes=[pck])
                            if u > 0:
                                S.op('pe', lambda e: e.matmul(pc[:], onesb[:], acc[:], start=False, stop=True),
                                     reads=['onesb', 'acc'], writes=[pck])
                            if u < nu - 1:
                                if u == 0:
                                    S.op('pool', lambda e: e.tensor_copy(acc[:], spt[i3][:]), reads=['sp%d' % i3], writes=['acc'])
                                else:
                                    S.op('pool', lambda e: e.tensor_tensor(acc[:], acc[:], spt[i3][:], ALU.add),
                                         reads=['sp%d' % i3, 'acc'], writes=['acc'])
                            S.op('act', lambda e: e.activation(wt[i2][:], pc[:], AF.Exp, scale=-1.0), reads=[pck], writes=['w%d' % i2])
                            S.op('dve', lambda e: e.tensor_tensor(at[i2][:], ef[i3][:], wt[i2][:], ALU.mult),
                                 reads=['ef%d' % i3, 'w%d' % i2], writes=['a%d' % i2])

                        def sbC(s, u, nu, idx):
                            kb = 4 * s + 3 - u
                            i2 = idx % 2
                            for h in range(4):
                                S.op('pe', lambda e, h=h: e.matmul(
                                    poh[h][:, 0:64], at[i2][:, h * 128:(h + 1) * 128], v_[:, kb, h * 64:(h + 1) * 64],
                                    start=(u == 0), stop=(u == nu - 1)), reads=['a%d' % i2, vk], writes=[pokh[h]])
                            if u == nu - 1:
                                ob = osbt[s % 4]
                                obk = 'osbt%d' % (s % 4)
                                for h in range(4):
                                    evac(ob[:, h * 64:(h + 1) * 64], poh[h][:, 0:64], [pokh[h]], [obk])
                                S.dma('sp', osb_s[s * 128:(s + 1) * 128, hq * 256:(hq + 1) * 256], ob[:], reads=[obk], writes=['sc_osb'])

                        n_it = len(items)
                        for idx in range(n_it + 2):
                            if idx < n_it:
                                sbA(*items[idx], idx)
                            if 1 <= idx <= n_it:
                                sbB(*items[idx - 1], idx - 1)
                            if idx >= 2:
                                sbC(*items[idx - 2], idx - 2)
                S.barrier()

            if 'p37' in DEBUG:
                S.dma('sp', dbg_out("obr", [3, OWN, 1024]), obr_s)
                S.dma('sp', dbg_out("osb", [OWN, 1024]), osb_s)
                dk = dbg_out("kcbT", [64, 4, 256], BF16)
                S.dma('sp', dk, kcbT[:])
                dv = dbg_out("vcb", [128, 4, 2, 129], BF16)
                S.dma('sp', dv, vcb[:])
                dsel = dbg_out("selT", [64, 4, NS, 128], BF16)
                S.dma('sp', dsel, selT[64:128])

        if stages >= 8:
            with ExitStack() as st8:
                mTall = st8.enter_context(nc.sbuf_tensor("p8_mTall", [128, 16, OWN], BF16))
                with ExitStack() as st:
                    sb = lambda n, s, d: st.enter_context(nc.sbuf_tensor(n, list(s), d))
                    Wbn = sb("p8_Wbn", [128, 8, D], BF16)
                    Wbs = sb("p8_Wbs", [128, 8, D], BF16)
                    ob = [sb("p8_ob%d" % i, [128, 16, 64], F32) for i in range(3)]
                    osb_t = sb("p8_osb", [128, 1024], F32)
                    gbrt = sb("p8_gbr", [128, 48], F32)
                    onsa = sb("p8_onsa", [128, 16, 64], F32)
                    tmp = sb("p8_tmp", [128, 16, 64], F32)
                    onb = sb("p8_onb", [128, 1024], BF16)
                    osb16 = sb("p8_osb16", [128, 1024], BF16)
                    onT = sb("p8_onT", [128, 8, 128], BF16)
                    osT = sb("p8_osT", [128, 8, 128], BF16)
                    gab = sb("p8_gab", [128, 4096], F32)
                    m1 = sb("p8_m1", [128, 512], F32)
                    m2 = sb("p8_m2", [128, 512], F32)
                    mb = sb("p8_mb", [128, D], BF16)
                    wbn_v = wbn_d.rearrange("(c p) n -> p c n", p=128)
                    wbs_v = wbs_d.rearrange("(c p) n -> p c n", p=128)
                    for c4 in range(0, 8, 4):
                        S.dma('pool', Wbn[:, c4:c4 + 4, :], wbn_v[:, c4:c4 + 4, :], writes=['Wbn'])
                        S.dma('pool', Wbs[:, c4:c4 + 4, :], wbs_v[:, c4:c4 + 4, :], writes=['Wbs'])
                    for ti in range(8):
                        rows = slice(ti * 128, (ti + 1) * 128)
                        for i in range(3):
                            S.dma('sp', ob[i][:].rearrange("p h d -> p (h d)"), obr_s[i, rows, :], writes=['ob%d' % i])
                        S.dma('sp', osb_t[:], osb_s[rows, :], writes=['osb_t'])
                        S.dma('sp', gbrt[:], gbr_s[rows, :], writes=['gbrt'])
                        S.dma('sp', gab[:], gab_s[rows, :], writes=['gab'])
                        gv = lambda i: bc(gbrt[:, i * 16:(i + 1) * 16].unsqueeze(2), [128, 16, 64])
                        S.op('dve', lambda e: e.tensor_tensor(onsa[:], ob[0][:], gv(0), ALU.mult), reads=['ob0', 'gbrt'], writes=['onsa'])
                        S.op('dve', lambda e: e.tensor_tensor(tmp[:], ob[1][:], gv(1), ALU.mult), reads=['ob1', 'gbrt'], writes=['tmp'])
                        S.op('dve', lambda e: e.tensor_tensor(onsa[:], onsa[:], tmp[:], ALU.add), reads=['onsa', 'tmp'], writes=['onsa'])
                        S.op('dve', lambda e: e.tensor_tensor(tmp[:], ob[2][:], gv(2), ALU.mult), reads=['ob2', 'gbrt'], writes=['tmp'])
                        S.op('dve', lambda e: e.tensor_tensor(onb[:].rearrange("p (h d) -> p h d", d=64), onsa[:], tmp[:], ALU.add),
                             reads=['onsa', 'tmp'], writes=['onb'])
                        S.op('act', lambda e: e.activation(osb16[:], osb_t[:], AF.Copy), reads=['osb_t'], writes=['osb16'])
                        for (src, sk, dstT, dk) in ((onb, 'onb', onT, 'onT'), (osb16, 'osb16', osT, 'osT')):
                            for cc in range(8):
                                S.op('pe', lambda e, cc=cc, src=src: e.transpose(pT[:, cc * 128:(cc + 1) * 128], src[:, cc * 128:(cc + 1) * 128], identb[:]),
                                     reads=[sk, 'identb'], writes=['pT'])
                            S.op('dve', lambda e, dstT=dstT: e.tensor_copy(dstT[:], pT[:].rearrange("p (c t) -> p c t", c=8)),
                                 reads=['pT'], writes=[dk])
                        for cb in range(4):
                            cs = slice(cb * 512, (cb + 1) * 512)
                            for c in range(8):
                                S.op('pe', lambda e, c=c, cs=cs: e.matmul(pA[0][:], onT[:, c, :], Wbn[:, c, cs], start=(c == 0), stop=(c == 7)),
                                     reads=['onT', 'Wbn'], writes=['pA0'])
                            for c in range(8):
                                S.op('pe', lambda e, c=c, cs=cs: e.matmul(pA[1][:], osT[:, c, :], Wbs[:, c, cs], start=(c == 0), stop=(c == 7)),
                                     reads=['osT', 'Wbs'], writes=['pA1'])
                            S.op('dve', lambda e, cs=cs: e.tensor_tensor(m1[:], pA[0][:], gab[:, cs], ALU.mult), reads=['pA0', 'gab'], writes=['m1'])
                            S.op('dve', lambda e, cb=cb: e.tensor_tensor(m2[:], pA[1][:], gab[:, 2048 + cb * 512:2048 + (cb + 1) * 512], ALU.mult),
                                 reads=['pA1', 'gab'], writes=['m2'])
                            S.op('dve', lambda e, cs=cs: e.tensor_tensor(mb[:, cs], m1[:], m2[:], ALU.add), reads=['m1', 'm2'], writes=['mb'])
                        for half in range(2):
                            for cc in range(8):
                                c = half * 8 + cc
                                S.op('pe', lambda e, cc=cc, c=c: e.transpose(pT[:, cc * 128:(cc + 1) * 128], mb[:, c * 128:(c + 1) * 128], identb[:]),
                                     reads=['mb', 'identb'], writes=['pT'])
                            S.op('dve', lambda e, half=half, ti=ti: e.tensor_copy(
                                mTall[:, half * 8:(half + 1) * 8, ti * 128:(ti + 1) * 128], pT[:].rearrange("p (c t) -> p c t", c=8)),
                                reads=['pT'], writes=['mTall'])
                S.barrier()
                with ExitStack() as st:
                    sb = lambda n, s, d: st.enter_context(nc.sbuf_tensor(n, list(s), d))
                    Wout = sb("p8_Wout", [128, 16, D], BF16)
                    xo = [sb("p8_xo%d" % i, [128, D], F32) for i in range(2)]
                    h1t = [sb("p8_h1t%d" % i, [128, D], F32) for i in range(2)]
                    wo_v = wout_d.rearrange("(c p) n -> p c n", p=128)
                    for c4 in range(0, 16, 4):
                        S.dma('pool', Wout[:, c4:c4 + 4, :], wo_v[:, c4:c4 + 4, :], writes=['Wout'])
                    for ti in range(8):
                        rows = slice(ti * 128, (ti + 1) * 128)
                        x_ = xo[ti % 2]
                        xk = 'xo%d' % (ti % 2)
                        h_ = h1t[ti % 2]
                        hk = 'h1t%d' % (ti % 2)
                        S.dma('sp', x_[:], x_own[rows, :], writes=[xk])
                        for cb in range(4):
                            cs = slice(cb * 512, (cb + 1) * 512)
                            pa = pA[cb % 2]
                            pk = 'pA%d' % (cb % 2)
                            for c in range(16):
                                S.op('pe', lambda e, c=c, cs=cs, pa=pa, ti=ti: e.matmul(pa[:], mTall[:, c, ti * 128:(ti + 1) * 128], Wout[:, c, cs],
                                                                                 start=(c == 0), stop=(c == 15)),
                                     reads=['mTall', 'Wout'], writes=[pk])
                            S.op('dve', lambda e, cs=cs, pa=pa, h_=h_, x_=x_: e.tensor_tensor(h_[:, cs], pa[:], x_[:, cs], ALU.add),
                                 reads=[pk, xk], writes=[hk])
                        S.dma('sp', h1_s[rows, :], h_[:], reads=[hk], writes=['sc_h1'])
            S.barrier()

        if 'p8' in DEBUG:
            S.dma('sp', dbg_out("h1", [OWN, D]), h1_s)

        if stages >= 9:
            with ExitStack() as st:
                sb = lambda n, s, d: st.enter_context(nc.sbuf_tensor(n, list(s), d))
                Wq = sb("p9_Wq", [128, 16, D], BF16)
                skt = sb("p9_skt", [128, 16, 128], BF16)
                gffn = sb("p9_gffn", [128, D], F32)
                gfin = sb("p9_gfin", [128, D], F32)
                iota16 = sb("p9_iota", [128, 16], F32)
                h1t = sb("p9_h1t", [128, D], F32)
                junk = sb("p9_junk", [128, D], BF16)
                ss = sb("p9_ss", [128, 1], F32)
                rstd = sb("p9_rstd", [128, 1], F32)
                hn = sb("p9_hn", [128, D], F32)
                hnb = sb("p9_hnb", [128, D], BF16)
                hnT = sb("p9_hnT", [128, 16, 128], BF16)
                qTs = sb("p9_qTs", [128, 16, 128], BF16)
                sc = sb("p9_sc", [128, 16, 128], F32)
                sc2 = sb("p9_sc2", [128, 256], F32)
                tv = sb("p9_tv", [128, 16, 16], F32)
                tiu = sb("p9_tiu", [128, 16, 16], U32)
                tif = sb("p9_tif", [128, 16, 16], F32)
                cand = sb("p9_cand", [128, 8, 256], F32)
                bs = sb("p9_bs", [128, 8, 16], F32)
                bju = sb("p9_bju", [128, 8, 16], U32)
                bjf = sb("p9_bjf", [128, 8, 16], F32)
                ja = sb("p9_ja", [128, 8, 16], F32)
                jb = sb("p9_jb", [128, 8, 16], F32)
                eq = sb("p9_eq", [128, 8, 16, 16], F32)
                i0 = sb("p9_i0", [128, 8, 16], F32)
                i1 = sb("p9_i1", [128, 8, 16], F32)
                exf = sb("p9_exf", [128, 128], F32)
                exi = sb("p9_exi", [128, 128], I32)
                gm = sb("p9_gm", [128, 8], F32)
                ge = sb("p9_ge", [128, 8, 16], F32)
                gz = sb("p9_gz", [128, 8], F32)
                pre = sb("p9_pre", [128, 128], F32)
                t1 = sb("p9_t1", [128, 128], F32)
                t2 = sb("p9_t2", [128, 128], F32)
                coef = sb("p9_coef", [128, 128], F32)
                gbuf = [sb("p9_gb%d" % i, [128, D], BF16) for i in range(4)]
                DB = 16
                dgb = [sb("p9_dgb%d" % i, [128, DB, 128], BF16) for i in range(2)]
                junkb = sb("p9_junkb", [128, D], BF16)
                wq_v = wq_d.rearrange("(c p) n -> p c n", p=128)
                for c4 in range(0, 16, 4):
                    S.dma('pool', Wq[:, c4:c4 + 4, :], wq_v[:, c4:c4 + 4, :], writes=['Wq'])
                S.dma('pool', skt[:], skT_d.rearrange("a c k -> c a k"), writes=['skt'])
                S.dma('sp', gffn[:], g_ffn.partition_broadcast(128), writes=['gffn'])
                S.dma('sp', gfin[:], g_fin.partition_broadcast(128), writes=['gfin'])
                S.dma('sp', iota16[:], iota_d, writes=['iota16'])
                thr16 = sb("p9_thr16", [128, 16], F32)
                S.op('dve', lambda e: e.tensor_scalar(thr16[:], iota16[:], 16.0, 16.0, ALU.mult, ALU.add), reads=['iota16'], writes=['thr16'])
                ng = 0
                pvacc = [pO[0], pO[1], pC[0], pC[1]]
                pvk = ['pO0', 'pO1', 'pC0', 'pC1']
                issue_casts(len(cast_jobs))
                S.wait_bg(['pool'])
                h1t2 = [h1t, sb("p9_h1tb", [128, D], F32)]
                hnb2 = [hnb, sb("p9_hnbb", [128, D], BF16)]
                exi2 = [exi, sb("p9_exib", [128, 128], I32)]
                ge2 = [ge, sb("p9_geb", [128, 8, 16], F32)]
                pre2 = [pre, sb("p9_preb", [128, 128], F32)]
                coef2 = [coef, sb("p9_coefb", [128, 128], F32)]
                ssf = sb("p9_ssf", [128, 1], F32)
                rstdf = sb("p9_rstdf", [128, 1], F32)
                gbuf.extend([sb("p9_gbx%d" % i, [128, D], BF16) for i in range(2)])
                NG = len(gbuf)
                ngc = [0]

                def top16(vals, vkey, width, out_v, out_i, okey):
                    S.op('dve', lambda e: e.max(out_v[:, 0:8], vals), reads=[vkey], writes=[okey])
                    S.op('dve', lambda e: e.max_index(out_i[:, 0:8], out_v[:, 0:8], vals), reads=[vkey, okey], writes=[okey + 'i'])
                    S.op('dve', lambda e: e.match_replace(sc2[:, 0:width], out_v[:, 0:8], vals, -3.0e38), reads=[vkey, okey], writes=['sc2'])
                    S.op('dve', lambda e: e.max(out_v[:, 8:16], sc2[:, 0:width]), reads=['sc2'], writes=[okey])
                    S.op('dve', lambda e: e.max_index(out_i[:, 8:16], out_v[:, 8:16], sc2[:, 0:width]), reads=['sc2', okey], writes=[okey + 'i'])

                def front(t):
                    b = t % 2
                    h1t, exi, ge, hnb = h1t2[b], exi2[b], ge2[b], hnb2[b]
                    hk, nk, xk, gek, hbk = 'h1t%d' % b, 'hn', 'exi%d' % b, 'ge%d' % b, 'hnb%d' % b
                    rows = slice(t * 128, (t + 1) * 128)
                    S.dma('sp', h1t[:], h1_s[rows, :], writes=[hk])
                    norm_rows(h1t[:], hk, ss, rstd, junk, 'p9')
                    S.op('dve', lambda e: e.scalar_tensor_tensor(hn[:], h1t[:], rstd[:, 0:1], gffn[:], ALU.mult, ALU.mult),
                         reads=[hk, 'p9rstd', 'gffn'], writes=[nk])
                    S.op('act', lambda e: e.activation(hnb[:], hn[:], AF.Copy), reads=[nk], writes=[hbk])
                    yield
                    for half in range(2):
                        for cc in range(8):
                            c = half * 8 + cc
                            S.op('pe', lambda e, cc=cc, c=c: e.transpose(pT[:, cc * 128:(cc + 1) * 128], hnb[:, c * 128:(c + 1) * 128], identb[:]),
                                 reads=[hbk, 'identb'], writes=['pT'])
                        S.op('dve', lambda e, half=half: e.tensor_copy(hnT[:, half * 8:(half + 1) * 8, :], pT[:].rearrange("p (c t) -> p c t", c=8)),
                             reads=['pT'], writes=['hnT'])
                        yield
                    for b4 in range(4):
                        pa = pA[b4 % 2]
                        pk = 'pA%d' % (b4 % 2)
                        for bb in range(4):
                            blk = b4 * 4 + bb
                            for c in range(16):
                                S.op('pe', lambda e, c=c, blk=blk, bb=bb, pa=pa: e.matmul(
                                    pa[:, bb * 128:(bb + 1) * 128], Wq[:, c, blk * 128:(blk + 1) * 128], hnT[:, c, :],
                                    start=(c == 0), stop=(c == 15)), reads=['Wq', 'hnT'], writes=[pk])
                        evac(qTs[:, b4 * 4:(b4 + 1) * 4, :], pa[:].rearrange("p (b t) -> p b t", b=4), [pk], ['qTs'])
                        yield
                    for b4 in range(4):
                        pa = pA[b4 % 2]
                        pk = 'pA%d' % (b4 % 2)
                        for bb in range(4):
                            blk = b4 * 4 + bb
                            S.op('pe', lambda e, blk=blk, bb=bb, pa=pa: e.matmul(
                                pa[:, bb * 128:(bb + 1) * 128], qTs[:, blk, :], skt[:, blk, :], start=True, stop=True),
                                reads=['qTs', 'skt'], writes=[pk])
                        evac(sc[:, b4 * 4:(b4 + 1) * 4, :], pa[:].rearrange("p (b t) -> p b t", b=4), [pk], ['sc'])
                        yield
                    for blk in range(16):
                        top16(sc[:, blk, :], 'sc', 128, tv[:, blk, :], tiu[:, blk, :], 'tv')
                        yield
                    S.op('dve', lambda e: e.tensor_copy(tif[:], tiu[:]), reads=['tvi'], writes=['tif'])
                    tvv = tv[:].rearrange("p (h two) k -> p h two k", two=2)
                    tfv = tif[:].rearrange("p (h two) k -> p h two k", two=2)
                    S.op('dve', lambda e: e.tensor_tensor(
                        cand[:].rearrange("p h (a b) -> p h a b", b=16),
                        bc(tvv[:, :, 0, :].unsqueeze(3), [128, 8, 16, 16]),
                        bc(tvv[:, :, 1, :].unsqueeze(2), [128, 8, 16, 16]), ALU.add), reads=['tv'], writes=['cand'])
                    yield
                    for h in range(8):
                        top16(cand[:, h, :], 'cand', 256, bs[:, h, :], bju[:, h, :], 'bs')
                        yield
                    S.op('dve', lambda e: e.tensor_copy(bjf[:], bju[:]), reads=['bsi'], writes=['bjf'])
                    S.op('dve', lambda e: e.tensor_tensor(eq[:], bc(bjf[:].unsqueeze(3), [128, 8, 16, 16]),
                                                          bc(thr16[:].unsqueeze(1).unsqueeze(1), [128, 8, 16, 16]), ALU.is_ge),
                         reads=['bjf', 'thr16'], writes=['eq'])
                    S.op('dve', lambda e: e.tensor_reduce(ja[:], eq[:], AX.X, ALU.add), reads=['eq'], writes=['ja'])
                    S.op('dve', lambda e: e.scalar_tensor_tensor(jb[:], ja[:], -16.0, bjf[:], ALU.mult, ALU.add), reads=['ja', 'bjf'], writes=['jb'])
                    yield
                    iob = bc(iota16[:].unsqueeze(1).unsqueeze(1), [128, 8, 16, 16])
                    for (jx, jk, two, ix, ik) in ((ja, 'ja', 0, i0, 'i0'), (jb, 'jb', 1, i1, 'i1')):
                        S.op('dve', lambda e, jx=jx: e.tensor_tensor(eq[:], iob, bc(jx[:].unsqueeze(3), [128, 8, 16, 16]), ALU.is_equal),
                             reads=['iota16', jk], writes=['eq'])
                        S.op('dve', lambda e, two=two: e.tensor_tensor(eq[:], eq[:], bc(tfv[:, :, two, :].unsqueeze(2), [128, 8, 16, 16]), ALU.mult),
                             reads=['eq', 'tif'], writes=['eq'])
                        S.op('dve', lambda e, ix=ix: e.tensor_reduce(ix[:], eq[:], AX.X, ALU.add), reads=['eq'], writes=[ik])
                        yield
                    S.op('dve', lambda e: e.scalar_tensor_tensor(exf[:].rearrange("p (h k) -> p h k", k=16), i0[:], 128.0, i1[:], ALU.mult, ALU.add),
                         reads=['i0', 'i1'], writes=['exf'])
                    S.op('dve', lambda e: e.tensor_copy(exi[:], exf[:]), reads=['exf'], writes=[xk])
                    yield
                    S.op('dve', lambda e: e.tensor_reduce(gm[:], bs[:], AX.X, ALU.max), reads=['bs'], writes=['gm'])
                    S.op('dve', lambda e: e.tensor_tensor(ge[:], bs[:], bc(gm[:].unsqueeze(2), [128, 8, 16]), ALU.subtract), reads=['bs', 'gm'], writes=[gek])
                    S.op('act', lambda e: e.activation(ge[:], ge[:], AF.Exp), reads=[gek], writes=[gek])
                    S.op('dve', lambda e: e.tensor_reduce(gz[:], ge[:], AX.X, ALU.add), reads=[gek], writes=['gz'])
                    S.op('dve', lambda e: e.reciprocal(gz[:], gz[:]), reads=['gz'], writes=['gz'])
                    S.op('dve', lambda e: e.tensor_tensor(ge[:], ge[:], bc(gz[:].unsqueeze(2), [128, 8, 16]), ALU.mult), reads=[gek, 'gz'], writes=[gek])

                def ustep(t, slot):
                    b = t % 2
                    gb_ = gbuf[ngc[0] % NG]
                    gk = 'gb%d' % (ngc[0] % NG)
                    ngc[0] += 1
                    S.dma('pool', None, None, reads=['exi%d' % b], writes=[gk],
                          fn=lambda e: e.indirect_dma_start(
                              out=gb_[:], out_offset=None, in_=pu16[:, :],
                              in_offset=bass.IndirectOffsetOnAxis(ap=exi2[b][:, slot:slot + 1], axis=0)))
                    S.op('dve', lambda e: e.scalar_tensor_tensor(
                        junkb[:], gb_[:], 1.0, hnb2[b][:], ALU.mult, ALU.mult,
                        accum_out=pre2[b][:, slot:slot + 1]), reads=[gk, 'hnb%d' % b], writes=['junkb', 'pre%d' % b])

                def midstep(t):
                    b = t % 2
                    pre, coef, ge = pre2[b], coef2[b], ge2[b]
                    pk_, ck_ = 'pre%d' % b, 'coef%d' % b
                    S.op('dve', lambda e: e.tensor_tensor(t1[:], pre[:], pre[:], ALU.mult), reads=[pk_], writes=['t1'])
                    S.op('dve', lambda e: e.tensor_scalar(t1[:], t1[:], 0.044715, 1.0, ALU.mult, ALU.add), reads=['t1'], writes=['t1'])
                    S.op('dve', lambda e: e.tensor_tensor(t1[:], t1[:], pre[:], ALU.mult), reads=['t1', pk_], writes=['t1'])
                    S.op('act', lambda e: e.activation(t2[:], t1[:], AF.Sigmoid, scale=1.5957691216057308), reads=['t1'], writes=['t2'])
                    S.op('dve', lambda e: e.tensor_tensor(t2[:], t2[:], pre[:], ALU.mult), reads=['t2', pk_], writes=['t2'])
                    S.op('dve', lambda e: e.tensor_tensor(coef[:], t2[:], ge[:].rearrange("p h k -> p (h k)"), ALU.mult),
                         reads=['t2', 'ge%d' % b], writes=[ck_])
                    build_diag(t, 0)

                def build_diag(t, k):
                    b = t % 2
                    S.op('dve', lambda e: e.tensor_tensor(
                        dgb[k % 2][:], bc(identf[:].unsqueeze(1), [128, DB, 128]),
                        bc(coef2[b][:, k * DB:(k + 1) * DB].unsqueeze(2), [128, DB, 128]), ALU.mult),
                        reads=['identf', 'coef%d' % b], writes=['dgb%d' % (k % 2)])

                def vstep(t, slot):
                    b = t % 2
                    gb_ = gbuf[ngc[0] % NG]
                    gk = 'gb%d' % (ngc[0] % NG)
                    ngc[0] += 1
                    S.dma('pool', None, None, reads=['exi%d' % b], writes=[gk],
                          fn=lambda e: e.indirect_dma_start(
                              out=gb_[:], out_offset=None, in_=pv16[:, :],
                              in_offset=bass.IndirectOffsetOnAxis(ap=exi2[b][:, slot:slot + 1], axis=0)))
                    if slot % DB == 0 and slot + DB < 128:
                        build_diag(t, slot // DB + 1)
                    dgt = dgb[(slot // DB) % 2]
                    dk = 'dgb%d' % ((slot // DB) % 2)
                    for cb in range(4):
                        S.op('pe', lambda e, cb=cb: e.matmul(
                            pvacc[cb][:], dgt[:, slot % DB, :], gb_[:, cb * 512:(cb + 1) * 512], start=(slot == 0), stop=(slot == 127)),
                            reads=[dk, gk], writes=[pvk[cb]])

                def finstep(t):
                    b = t % 2
                    rows = slice(t * 128, (t + 1) * 128)
                    acc = h1t2[b]
                    ak = 'h1t%d' % b
                    for cb in range(4):
                        S.op('dve', lambda e, cb=cb: e.tensor_tensor(acc[:, cb * 512:(cb + 1) * 512], pvacc[cb][:], acc[:, cb * 512:(cb + 1) * 512], ALU.add),
                             reads=[pvk[cb], ak], writes=[ak])
                    norm_rows(acc[:], ak, ssf, rstdf, junk, 'p9f')
                    S.op('dve', lambda e: e.scalar_tensor_tensor(acc[:], acc[:], rstdf[:, 0:1], gfin[:], ALU.mult, ALU.mult),
                         reads=[ak, 'p9frstd', 'gfin'], writes=[ak])
                    S.dma('sp', out_d[rows, :], acc[:], reads=[ak], writes=['out'])

                for _ in front(0):
                    pass
                for slot in range(128):
                    ustep(0, slot)
                midstep(0)
                for t in range(8):
                    if t + 1 < 8:
                        fg = front(t + 1)
                        HEAD = 96
                        for slot in range(HEAD):
                            vstep(t, slot)
                            next(fg, None)
                        for _ in fg:
                            pass
                        vs_, us_ = HEAD, 0
                        while vs_ < 128 or us_ < 128:
                            if us_ < 128:
                                ustep(t + 1, us_)
                                us_ += 1
                            if vs_ < 128 and (vs_ - HEAD) * 128 < us_ * (128 - HEAD):
                                vstep(t, vs_)
                                vs_ += 1
                    else:
                        for slot in range(128):
                            vstep(t, slot)
                    finstep(t)
                    if t + 1 < 8:
                        midstep(t + 1)

        S.finish()
    return nc, dbg


def host_consts(j, rel_table):
    tab = np.asarray(rel_table, np.float32)
    k = np.arange(128)[:, None]
    q = np.arange(128)[None, :]
    abs_t = np.empty((12, 128, 16, 128), np.float32)
    for u in range(12):
        if u == 11:
            abs_t[u] = tab[31][None, :, None]
            continue
        dist = 128 * (u + j - 3) + q - k
        val = tab[_bucket(dist)]
        val = np.where((dist >= 0)[:, :, None], val, np.float32(NEGM))
        abs_t[u] = val.transpose(0, 2, 1)
    abw_t = np.empty((8, 128, 16, 128), np.float32)
    for u in range(8):
        dist = 128 * (u + j - 3) + q - k
        val = tab[_bucket(dist)]
        ok = (dist >= 0) & (dist < 512)
        val = np.where(ok[:, :, None], val, np.float32(NEGM))
        abw_t[u] = val.transpose(0, 2, 1)
    msb = np.empty((4, 128, 128), np.float32)
    for u in range(4):
        dist = 128 * (u + j - 3) + q - k
        msb[u] = np.where(dist >= 1, np.float32(0), np.float32(NEGM))
    cb = np.empty((NS, 2, 128, 16, 128), np.float32)
    selc = np.empty((NS, 128, 64), np.float32)
    for s in range(NS):
        t = (4 * s + j) * 128 + np.arange(128)
        n = np.arange(256)
        dist = t[None, :] - (16 * n[:, None] + 31)
        val = tab[_bucket(dist)]
        ok = (dist >= 0) & (n[:, None] < 255)
        val = np.where(ok[:, :, None], val, np.float32(NEGM)).transpose(0, 2, 1)
        cb[s] = val.reshape(2, 128, 16, 128)
        cur = t[:, None] // 64
        jb = np.arange(64)[None, :]
        valid = jb <= cur
        forced = (jb == 0) | ((cur - jb >= 0) & (cur - jb < 2))
        selc[s] = np.where(valid, np.where(forced, np.float32(1e4), np.float32(0)), np.float32(-1e30))
    return dict(abs=abs_t, abw=abw_t, msb=msb, cbias=cb, selc=selc)


def shared_consts():
    n_cmp = 255
    c_start = np.arange(n_cmp) * 16
    s_start = np.arange(64) * 64
    ov = np.maximum(np.minimum(c_start[:, None] + 32, s_start[None, :] + 64)
                    - np.maximum(c_start[:, None], s_start[None, :]), 0).astype(np.float32) / 32
    ovl = np.zeros((256, 64), np.float32)
    ovl[:255] = ov
    expand = np.zeros((NB, 64, 128), np.float32)
    for kb in range(NB):
        for kk in range(128):
            expand[kb, (128 * kb + kk) // 64, kk] = 1.0
    jj = np.arange(128)[:, None]
    ss = np.arange(128)[None, :]
    tri = (jj >= ss).astype(np.float32)
    iota = np.tile(np.arange(16, dtype=np.float32)[None, :], (128, 1))
    return dict(ovl=ovl.reshape(2, 128, 64), expand=expand, tri=tri, ident=np.eye(128, dtype=np.float32), iota16=iota)


def prep_inputs(inp):
    f = lambda a: np.ascontiguousarray(np.asarray(a, dtype=np.float32))
    x = f(inp['x'])
    shared = shared_consts()
    shared.update(
        w_in=f(inp['w_in'][0]), g_attn=f(inp['attn_norm_g'][0]), g_ffn=f(inp['ffn_norm_g'][0]),
        g_fin=f(inp['final_norm_g']),
        ckw1=f(np.stack([inp['cmp_k_w1'][0], inp['cmp_v_w1'][0]])),
        ckpeT=f(np.stack([np.asarray(inp['cmp_k_pe'][0]).T, np.asarray(inp['cmp_v_pe'][0]).T])),
        ckw2=f(np.stack([inp['cmp_k_w2'][0], inp['cmp_v_w2'][0]])),
        wbn=f(inp['w_branch_nsa'][0]), wbs=f(inp['w_branch_sb'][0]), wout=f(inp['w_out'][0]),
        wq=f(inp['peer_w_q'][0]),
        skT=f(np.asarray(inp['peer_sub_keys'][0]).reshape(16, 128, 128).transpose(0, 2, 1)),
        pu=f(inp['peer_u'][0]), pv=f(inp['peer_v'][0]),
    )
    per_j = [host_consts(j, inp['rel_bias_table']) for j in range(4)]
    in_maps = []
    for c in range(8):
        b, j = c // 4, c % 4
        own = np.concatenate([np.arange((4 * s + j) * 128, (4 * s + j + 1) * 128) for s in range(NS)])
        m = dict(shared)
        m.update(per_j[j])
        m['x_full'] = x[b]
        m['x_own'] = np.ascontiguousarray(x[b][own])
        in_maps.append(m)
    return in_maps


def own_index(j):
    return np.concatenate([np.arange((4 * s + j) * 128, (4 * s + j + 1) * 128) for s in range(NS)])


def kernel(**inputs):
    in_maps = prep_inputs(inputs)
    nc, dbg = build_program()
    res = run_bass_kernel_spmd(nc, in_maps, core_ids=list(range(8)))
    out = np.empty((2, T, D), np.float32)
    for c in range(8):
        b, j = c // 4, c % 4
        out[b, own_index(j)] = res.results[c]["out"]
    return out
```

```python
import math
from contextlib import ExitStack

import numpy as np
import concourse.bass as bass
import concourse.mybir as mybir
from concourse.bass_utils import run_bass_kernel_spmd

F32 = mybir.dt.float32
BF16 = mybir.dt.bfloat16
I32 = mybir.dt.int32
U32 = mybir.dt.uint32
ALU = mybir.AluOpType
AF = mybir.ActivationFunctionType
AX = mybir.AxisListType

T = 4096
D = 2048
NB = 32
NS = 8
OWN = 1024
NEGM = -30000.0
EPS = 1e-6
IN_COLS = 9776
C_QN, C_KC, C_VC, C_KS, C_VS, C_KW, C_VW, C_GBR, C_QB, C_KB, C_VB, C_GA, C_GB = (
    0, 1024, 1280, 1536, 1792, 2048, 2304, 2560, 2608, 3632, 4656, 5680, 7728)

DEBUG = {}


class Sched:
    NDS = 32
    NHW = 20

    def __init__(self, nc, stack):
        self.nc = nc
        self.eng = {'pe': nc.tensor, 'dve': nc.vector, 'act': nc.scalar,
                    'pool': nc.gpsimd, 'sp': nc.sync}
        self.sem = {k: stack.enter_context(nc.semaphore('s_' + k)) for k in self.eng}
        self.cnt = {k: 0 for k in self.eng}
        self.waited = {}
        self.dsem = [stack.enter_context(nc.semaphore('d%d' % i)) for i in range(self.NDS)]
        self.dcnt = [0] * self.NDS
        self.dnext = 0
        self.dnext_sw = 0
        self.bgsem = [stack.enter_context(nc.semaphore('bg%d' % i)) for i in range(64)]
        self.bgcnt = [0] * 64
        self.bgnext = 0
        self.last_w = {}
        self.readers = {}
        self.ninst = 0

    def _wait(self, e, tok):
        if tok is None:
            return
        if tok[0] == 'e':
            _, p, n = tok
            if p == 'pe' and e == 'pe':
                return
            key = (e, 'e', p)
            if self.waited.get(key, 0) >= n:
                return
            self.eng[e].wait_ge(self.sem[p], n)
            self.waited[key] = n
        else:
            _, slot, v = tok
            key = (e, 'd', slot)
            if self.waited.get(key, 0) >= v:
                return
            self.eng[e].wait_ge(self.dsem[slot], v)
            self.waited[key] = v

    def _deps(self, e, reads, writes):
        toks = []
        for k in reads:
            toks.append(self.last_w.get(k))
        for k in writes:
            toks.append(self.last_w.get(k))
            toks.extend(self.readers.get(k, []))
        for t in toks:
            self._wait(e, t)

    def _update(self, tok, reads, writes):
        for k in reads:
            self.readers.setdefault(k, []).append(tok)
        for k in writes:
            self.last_w[k] = tok
            self.readers[k] = []

    def op(self, e, fn, reads=(), writes=()):
        self._deps(e, reads, writes)
        ins = fn(self.eng[e])
        self.cnt[e] += 1
        ins.then_inc(self.sem[e], 1)
        tok = ('e', e, self.cnt[e])
        self._update(tok, reads, writes)
        self.ninst += 1
        return tok

    def dma(self, q, out, in_, reads=(), writes=(), fn=None, **kw):
        if q == 'pool':
            slot = self.NHW + self.dnext_sw
            self.dnext_sw = (self.dnext_sw + 1) % (self.NDS - self.NHW)
        else:
            slot = self.dnext
            self.dnext = (self.dnext + 1) % self.NHW
        if self.dcnt[slot] > 0:
            self._wait(q, ('d', slot, self.dcnt[slot]))
        self._deps(q, reads, writes)
        if fn is not None:
            ins = fn(self.eng[q])
        else:
            ins = self.eng[q].dma_start(out=out, in_=in_, **kw)
        ins.then_inc(self.dsem[slot], 16)
        self.dcnt[slot] += 16
        tok = ('d', slot, self.dcnt[slot])
        self._update(tok, reads, writes)
        self.ninst += 1
        return tok

    def bg_dma(self, q, out, in_):
        slot = self.bgnext
        self.bgnext = (self.bgnext + 1) % len(self.bgsem)
        if self.bgcnt[slot] > 0:
            key = (q, 'bg', slot)
            if self.waited.get(key, 0) < self.bgcnt[slot]:
                self.eng[q].wait_ge(self.bgsem[slot], self.bgcnt[slot])
                self.waited[key] = self.bgcnt[slot]
        self.eng[q].dma_start(out=out, in_=in_).then_inc(self.bgsem[slot], 16)
        self.bgcnt[slot] += 16
        self.ninst += 1

    def wait_bg(self, engines):
        for e in engines:
            for slot in range(len(self.bgsem)):
                if self.bgcnt[slot] > 0:
                    self.eng[e].wait_ge(self.bgsem[slot], self.bgcnt[slot])

    def barrier(self):
        for e in self.eng:
            for p in self.eng:
                if p != e and self.cnt[p] > 0:
                    self._wait(e, ('e', p, self.cnt[p]))
            for s in range(self.NDS):
                if self.dcnt[s] > 0:
                    self._wait(e, ('d', s, self.dcnt[s]))
        self.last_w = {}
        self.readers = {}

    def finish(self):
        e = 'sp'
        self.wait_bg([e])
        for p in self.eng:
            if p != e and self.cnt[p] > 0:
                self._wait(e, ('e', p, self.cnt[p]))
        for s in range(self.NDS):
            if self.dcnt[s] > 0:
                self._wait(e, ('d', s, self.dcnt[s]))


def _bucket(dist):
    dist = np.maximum(dist, 0)
    d_f = np.maximum(dist, 1).astype(np.float32)
    large = 16 + (np.log(d_f / np.float32(16)) / np.float32(math.log(1024 / 16)) * np.float32(16)).astype(np.int32)
    large = np.minimum(large, 31)
    return np.where(dist < 16, dist, large).astype(np.int64)


def _bucket_jax(dist):
    return _bucket(dist)


def build_program(stages=99):
    nc = bass.Bass("TRN2", target_bir_lowering=False)
    dt_in = lambda n, s, d=F32: nc.dram_tensor(n, list(s), d, kind="ExternalInput").ap()
    dt_sc = lambda n, s, d: nc.dram_tensor(n, list(s), d, kind="Internal").ap()
    x_full = dt_in("x_full", [T, D])
    x_own = dt_in("x_own", [OWN, D])
    w_in = dt_in("w_in", [D, IN_COLS])
    g_attn = dt_in("g_attn", [D])
    g_ffn = dt_in("g_ffn", [D])
    g_fin = dt_in("g_fin", [D])
    ident_d = dt_in("ident", [128, 128])
    tri_d = dt_in("tri", [128, 128])
    abs_d = dt_in("abs", [12, 128, 16, 128])
    abw_d = dt_in("abw", [8, 128, 16, 128])
    msb_d = dt_in("msb", [4, 128, 128])
    cbias_d = dt_in("cbias", [NS, 2, 128, 16, 128])
    selc_d = dt_in("selc", [NS, 128, 64])
    ovl_d = dt_in("ovl", [2, 128, 64])
    expand_d = dt_in("expand", [NB, 64, 128])
    ckw1_d = dt_in("ckw1", [2, 2048, 128])
    ckpe_d = dt_in("ckpeT", [2, 64, 32])
    ckw2_d = dt_in("ckw2", [2, 128, 64])
    wbn_d = dt_in("wbn", [1024, D])
    wbs_d = dt_in("wbs", [1024, D])
    wout_d = dt_in("wout", [D, D])
    wq_d = dt_in("wq", [D, D])
    skT_d = dt_in("skT", [16, 128, 128])
    pu_d = dt_in("pu", [16384, D])
    pv_d = dt_in("pv", [16384, D])
    iota_d = dt_in("iota16", [128, 16])
    out_d = nc.dram_tensor("out", [OWN, D], F32, kind="ExternalOutput").ap()
    dbg = {}

    def dbg_out(name, shape, dtype=F32):
        dbg[name] = nc.dram_tensor("dbg_" + name, list(shape), dtype, kind="ExternalOutput").ap()
        return dbg[name]

    kcvT = dt_sc("kcvT", [2, 4, 64, T], BF16)
    ksT = dt_sc("ksT", [4, 64, T], BF16)
    kwT = dt_sc("kwT", [4, 64, T], BF16)
    kbT = dt_sc("kbT", [16, 64, T], BF16)
    vsw = dt_sc("vsw", [T, 512], BF16)
    vb = dt_sc("vb", [T, 1024], BF16)
    qnT = dt_sc("qnT", [16, 64, OWN], BF16)
    qbT = dt_sc("qbT", [16, 64, OWN], BF16)
    gbr_s = dt_sc("gbr_s", [OWN, 48], F32)
    gab_s = dt_sc("gab_s", [OWN, 4096], F32)
    obr_s = dt_sc("obr_s", [3, OWN, 1024], F32)
    osb_s = dt_sc("osb_s", [OWN, 1024], F32)
    h1_s = dt_sc("h1_s", [OWN, D], F32)
    pu16 = dt_sc("pu16", [16384, D], BF16)
    pv16 = dt_sc("pv16", [16384, D], BF16)

    with ExitStack() as gst:
        S = Sched(nc, gst)
        gsb = lambda n, s, d: gst.enter_context(nc.sbuf_tensor(n, list(s), d))
        gps = lambda n, s, d: gst.enter_context(nc.psum_tensor(n, list(s), d))
        pA = [gps("pA%d" % i, [128, 512], F32) for i in range(2)]
        pC = [gps("pC%d" % i, [128, 512], F32) for i in range(2)]
        pO = [gps("pO%d" % i, [128, 512], F32) for i in range(2)]
        pT = gps("pT", [128, 1024], BF16)
        pM = gps("pM", [128, 512], F32)

        identf = gsb("identf", [128, 128], F32)
        identb = gsb("identb", [128, 128], BF16)
        trib = gsb("trib", [128, 128], BF16)
        onesb = gsb("onesb", [128, 128], BF16)

        S.dma('sp', identf[:], ident_d, writes=['identf'])
        S.op('dve', lambda e: e.tensor_copy(identb[:], identf[:]), reads=['identf'], writes=['identb'])
        S.dma('pool', trib[:], tri_d, writes=['trib'])
        S.op('dve', lambda e: e.memset(onesb[:], 1.0), writes=['onesb'])

        rr = {'ev': 0}
        cast_jobs = []
        for r0 in range(0, 16384, 512):
            cast_jobs.append((pu16[r0:r0 + 512, :], pu_d[r0:r0 + 512, :]))
            cast_jobs.append((pv16[r0:r0 + 512, :], pv_d[r0:r0 + 512, :]))

        def issue_casts(n):
            for _ in range(n):
                if cast_jobs:
                    o_, i_ = cast_jobs.pop(0)
                    S.bg_dma('pool', o_, i_)

        def evac(out_ap, in_ap, reads, writes, scale=None):
            rr['ev'] += 1
            if rr['ev'] % 2 == 0:
                if scale is None:
                    S.op('dve', lambda e: e.tensor_copy(out_ap, in_ap), reads=reads, writes=writes)
                else:
                    S.op('dve', lambda e: e.tensor_scalar(out_ap, in_ap, scale, None, ALU.mult), reads=reads, writes=writes)
            else:
                S.op('act', lambda e: e.activation(out_ap, in_ap, AF.Copy, scale=(1.0 if scale is None else scale)),
                     reads=reads, writes=writes)

        def make_gfull(st, g_ap, name):
            gt = st.enter_context(nc.sbuf_tensor(name + "_gt", [128, 16], F32))
            gfull = st.enter_context(nc.sbuf_tensor(name + "_gf", [128, 16, 128], BF16))
            S.dma('sp', gt[:], g_ap.rearrange("(c p) -> p c", p=128), writes=[name + 'gt'],
                  allow_slow_non_contiguous=True)
            S.op('dve', lambda e: e.memset(gfull[:], 1.0), writes=[name + 'gf'])
            for c in range(16):
                S.op('dve', lambda e, c=c: e.tensor_scalar(gfull[:, c, :], gfull[:, c, :], gt[:, c:c + 1], None, ALU.mult),
                     reads=[name + 'gt', name + 'gf'], writes=[name + 'gf'])
            return gfull, name + 'gf'

        def norm_rows(xt, xkey, ss, rstd, junk, key):
            S.op('act', lambda e: e.activation(junk[:], xt, AF.Square, accum_out=ss[:]),
                 reads=[xkey], writes=[key + 'junk', key + 'ss'])
            S.op('dve', lambda e: e.tensor_scalar(rstd[:], ss[:], 1.0 / D, EPS, ALU.mult, ALU.add),
                 reads=[key + 'ss'], writes=[key + 'rstd'])
            S.op('act', lambda e: e.activation(rstd[:], rstd[:], AF.Sqrt), reads=[key + 'rstd'], writes=[key + 'rstd'])
            S.op('dve', lambda e: e.reciprocal(rstd[:], rstd[:]), reads=[key + 'rstd'], writes=[key + 'rstd'])

        def transposeT(xs, xskey, gfull, gfkey, dst, dstkey, col0):
            for half in range(2):
                for cc in range(8):
                    c = half * 8 + cc
                    S.op('pe', lambda e, c=c, cc=cc: e.transpose(pT[:, cc * 128:(cc + 1) * 128], xs[:, c * 128:(c + 1) * 128], identb[:]),
                         reads=[xskey, 'identb'], writes=['pT'])
                S.op('dve', lambda e, half=half: e.tensor_tensor(
                    dst[:, half * 8:(half + 1) * 8, col0:col0 + 128],
                    pT[:].rearrange("p (c t) -> p c t", c=8),
                    gfull[:, half * 8:(half + 1) * 8, :], ALU.mult),
                    reads=['pT', gfkey], writes=[dstkey])

        if stages >= 1:
            with ExitStack() as st:
                sb = lambda n, s, d: st.enter_context(nc.sbuf_tensor(n, list(s), d))
                WF = sb("p1_WF", [128, 16, 2048], BF16)
                WV = sb("p1_WV", [128, 16, 1536], BF16)
                xt = [sb("p1_xt%d" % i, [128, D], F32) for i in range(2)]
                junk = sb("p1_junk", [128, D], BF16)
                ss = sb("p1_ss", [128, 1], F32)
                rstd = sb("p1_rstd", [128, 1], F32)
                xs = sb("p1_xs", [128, D], BF16)
                aT = [sb("p1_aT%d" % i, [128, 16, 512], BF16) for i in range(2)]
                stg = [sb("p1_stg%d" % i, [128, 512], BF16) for i in range(4)]
                gfull, gfkey = make_gfull(st, g_attn, "p1")
                wv = w_in.rearrange("(c p) n -> p c n", p=128)
                fm_cols = [(C_KC, 256), (C_VC, 256), (C_KS, 256), (C_KW, 256), (C_KB, 1024)]
                o = 0
                for gi_, (c0, n) in enumerate(fm_cols):
                    for c4 in range(0, 16, 4):
                        S.dma('pool', WF[:, c4:c4 + 4, o:o + n], wv[:, c4:c4 + 4, c0:c0 + n], writes=['WF%d' % gi_])
                    o += n
                tm_cols = [(C_VS, 256), (C_VW, 256), (C_VB, 1024)]
                o = 0
                for gi_, (c0, n) in enumerate(tm_cols):
                    for c4 in range(0, 16, 4):
                        S.dma('pool', WV[:, c4:c4 + 4, o:o + n], wv[:, c4:c4 + 4, c0:c0 + n], writes=['WV%d' % gi_])
                    o += n
                kcv2 = kcvT.rearrange("a g d t -> a (g d) t")
                ks2 = ksT.rearrange("g d t -> (g d) t")
                kw2 = kwT.rearrange("g d t -> (g d) t")
                kb2 = kbT.rearrange("h d t -> (h d) t")
                fm_dst = [(kcv2[0], 0), (kcv2[0], 128), (kcv2[1], 0), (kcv2[1], 128),
                          (ks2, 0), (ks2, 128), (kw2, 0), (kw2, 128)] + [(kb2, 128 * i) for i in range(8)]
                nstg = [0]
                npa = [0]
                fm_grp = [0, 0, 1, 1, 2, 2, 3, 3] + [4] * 8
                tm_grp = [['WV0', 'WV1'], ['WV2'], ['WV2']]

                def prepA(tt, ti):
                    r0 = tt * 512 + ti * 128
                    xi = (tt * 4 + ti) % 2
                    S.dma('sp', xt[xi][:], x_full[r0:r0 + 128, :], writes=['xt%d' % xi])
                    norm_rows(xt[xi][:], 'xt%d' % xi, ss, rstd, junk, 'p1')
                    S.op('dve', lambda e: e.tensor_scalar(xs[:], xt[xi][:], rstd[:, 0:1], None, ALU.mult),
                         reads=['xt%d' % xi, 'p1rstd'], writes=['xs'])

                def prepB(tt, ti):
                    transposeT(xs, 'xs', gfull, gfkey, aT[tt % 2], 'aT%d' % (tt % 2), ti * 128)

                def fm_group(tt, bi):
                    a = aT[tt % 2]
                    akey = 'aT%d' % (tt % 2)
                    pi = npa[0] % 2
                    npa[0] += 1
                    pa = pA[pi]
                    for c in range(16):
                        S.op('pe', lambda e, c=c: e.matmul(pa[:], WF[:, c, bi * 128:(bi + 1) * 128], a[:, c, :],
                                                          start=(c == 0), stop=(c == 15)),
                             reads=['WF%d' % fm_grp[bi], akey], writes=['pA%d' % pi])
                    sg = stg[nstg[0] % 4]
                    sk = 'stg%d' % (nstg[0] % 4)
                    nstg[0] += 1
                    evac(sg[:], pa[:], ['pA%d' % pi], [sk])
                    dst, row0 = fm_dst[bi]
                    S.dma('sp', dst[row0:row0 + 128, tt * 512:(tt + 1) * 512], sg[:], reads=[sk], writes=['sc_fm'])

                def tm_group(tt, ti, vbk):
                    a = aT[tt % 2]
                    akey = 'aT%d' % (tt % 2)
                    r0 = tt * 512 + ti * 128
                    pi = npa[0] % 2
                    npa[0] += 1
                    pa = pA[pi]
                    for c in range(16):
                        S.op('pe', lambda e, c=c: e.matmul(
                            pa[:], a[:, c, ti * 128:(ti + 1) * 128], WV[:, c, vbk * 512:(vbk + 1) * 512],
                            start=(c == 0), stop=(c == 15)),
                            reads=tm_grp[vbk] + [akey], writes=['pA%d' % pi])
                    sg = stg[nstg[0] % 4]
                    sk = 'stg%d' % (nstg[0] % 4)
                    nstg[0] += 1
                    evac(sg[:], pa[:], ['pA%d' % pi], [sk])
                    if vbk == 0:
                        S.dma('sp', vsw[r0:r0 + 128, :], sg[:], reads=[sk], writes=['sc_tm'])
                    else:
                        S.dma('sp', vb[r0:r0 + 128, (vbk - 1) * 512:vbk * 512], sg[:], reads=[sk], writes=['sc_tm'])

                for ti in range(4):
                    prepA(0, ti)
                    prepB(0, ti)
                NT = T // 512
                for tt in range(NT):
                    groups = [(lambda bi=bi: fm_group(tt, bi)) for bi in range(16)]
                    groups += [(lambda ti=ti, vbk=vbk: tm_group(tt, ti, vbk)) for ti in range(4) for vbk in range(3)]
                    posA = {0: 0, 7: 1, 14: 2, 21: 3}
                    posB = {4: 0, 11: 1, 18: 2, 25: 3}
                    for gi, grp in enumerate(groups):
                        if tt + 1 < NT and gi in posA:
                            prepA(tt + 1, posA[gi])
                        if tt + 1 < NT and gi in posB:
                            prepB(tt + 1, posB[gi])
                        grp()
            S.barrier()

        if stages >= 2:
            with ExitStack() as st:
                sb = lambda n, s, d: st.enter_context(nc.sbuf_tensor(n, list(s), d))
                xt = [sb("p2_xt%d" % i, [128, D], F32) for i in range(2)]
                junk = sb("p2_junk", [128, D], BF16)
                ss = sb("p2_ss", [128, 1], F32)
                rstd = sb("p2_rstd", [128, 1], F32)
                xs = sb("p2_xs", [128, D], BF16)
                aT = sb("p2_aT", [128, 16, OWN], BF16)
                Wb = [sb("p2_W%d" % i, [128, 16, 512], BF16) for i in range(2)]
                stg = [sb("p2_stg%d" % i, [128, 512], BF16) for i in range(4)]
                stgf = [sb("p2_stgf%d" % i, [128, 512], F32) for i in range(4)]
                gfull, gfkey = make_gfull(st, g_attn, "p2")
                wv = w_in.rearrange("(c p) n -> p c n", p=128)
                for ti in range(8):
                    xi = ti % 2
                    S.dma('sp', xt[xi][:], x_own[ti * 128:(ti + 1) * 128, :], writes=['xt%d' % xi])
                    norm_rows(xt[xi][:], 'xt%d' % xi, ss, rstd, junk, 'p2')
                    S.op('dve', lambda e, xi=xi: e.tensor_scalar(xs[:], xt[xi][:], rstd[:, 0:1], None, ALU.mult),
                         reads=['xt%d' % xi, 'p2rstd'], writes=['xs'])
                    transposeT(xs, 'xs', gfull, gfkey, aT, 'aTown', ti * 128)
                qn2 = qnT.rearrange("h d t -> (h d) t")
                qb2 = qbT.rearrange("h d t -> (h d) t")
                blocks = [('fm', C_QN + 512 * i, 512, qn2, 512 * i) for i in range(2)]
                blocks += [('fm', C_QB + 512 * i, 512, qb2, 512 * i) for i in range(2)]
                blocks += [('tm', C_GA + 512 * i, 512, gab_s, 512 * i) for i in range(8)]
                blocks += [('gbr', C_GBR, 48, gbr_s, 0)]
                nstg = 0
                for bi, (kind, c0, n, dst, d0) in enumerate(blocks):
                    W = Wb[bi % 2]
                    wk = 'W%d' % (bi % 2)
                    for c4 in range(0, 16, 4):
                        S.dma('pool', W[:, c4:c4 + 4, 0:n], wv[:, c4:c4 + 4, c0:c0 + n], writes=[wk])
                    if kind == 'fm':
                        for sub in range(4):
                            for th in range(2):
                                pa = pA[(sub * 2 + th) % 2]
                                pk = 'pA%d' % ((sub * 2 + th) % 2)
                                for c in range(16):
                                    S.op('pe', lambda e, c=c, sub=sub, th=th, pa=pa, W=W: e.matmul(
                                        pa[:], W[:, c, sub * 128:(sub + 1) * 128], aT[:, c, th * 512:(th + 1) * 512],
                                        start=(c == 0), stop=(c == 15)), reads=[wk, 'aTown'], writes=[pk])
                                sg = stg[nstg % 4]
                                sk = 'stg%d' % (nstg % 4)
                                nstg += 1
                                evac(sg[:], pa[:], [pk], [sk], scale=0.125)
                                S.dma('sp', dst[d0 + sub * 128:d0 + (sub + 1) * 128, th * 512:(th + 1) * 512], sg[:],
                                      reads=[sk], writes=['sc_q'])
                    else:
                        for ti in range(8):
                            pa = pA[ti % 2]
                            pk = 'pA%d' % (ti % 2)
                            for c in range(16):
                                S.op('pe', lambda e, c=c, ti=ti, pa=pa, W=W, n=n: e.matmul(
                                    pa[:, 0:n], aT[:, c, ti * 128:(ti + 1) * 128], W[:, c, 0:n],
                                    start=(c == 0), stop=(c == 15)), reads=[wk, 'aTown'], writes=[pk])
                            sg = stgf[nstg % 4]
                            sk = 'stgf%d' % (nstg % 4)
                            nstg += 1
                            S.op('act', lambda e, sg=sg, pa=pa, n=n: e.activation(sg[:, 0:n], pa[:, 0:n], AF.Sigmoid),
                                 reads=[pk], writes=[sk])
                            if kind == 'tm':
                                S.dma('sp', dst[ti * 128:(ti + 1) * 128, d0:d0 + n], sg[:, 0:n], reads=[sk], writes=['sc_g'])
                            else:
                                S.dma('sp', dst[ti * 128:(ti + 1) * 128, :], sg[:, 0:n], reads=[sk], writes=['sc_g'])
            S.barrier()

        if 'p12' in DEBUG:
            d1 = dbg_out("ksT", [4, 64, T], BF16)
            S.dma('sp', d1, ksT, reads=[], writes=[])
            d2 = dbg_out("vb", [T, 1024], BF16)
            S.dma('sp', d2, vb, reads=[], writes=[])
            d3 = dbg_out("qnT", [16, 64, OWN], BF16)
            S.dma('sp', d3, qnT, reads=[], writes=[])
            d4 = dbg_out("gbr", [OWN, 48], F32)
            S.dma('sp', d4, gbr_s, reads=[], writes=[])
            d5 = dbg_out("gab", [OWN, 4096], F32)
            S.dma('sp', d5, gab_s, reads=[], writes=[])

        def bc(ap, shape):
            return ap.to_broadcast(list(shape))

        with ExitStack() as mid_st:
            msb_ = lambda n, s, d: mid_st.enter_context(nc.sbuf_tensor(n, list(s), d))
            kcbT = msb_("kcbT", [64, 4, 256], BF16)
            vcb = msb_("vcb", [128, 4, 2, 129], BF16)
            selT = msb_("selT", [128, 4, NS, 128], BF16)
            S.op('dve', lambda e: e.memset(kcbT[:], 0.0), writes=['kcbT'])
            S.op('dve', lambda e: e.memset(vcb[:], 0.0), writes=['vcb'])
            if stages >= 3:
                with ExitStack() as st:
                    sb = lambda n, s, d: st.enter_context(nc.sbuf_tensor(n, list(s), d))
                    kin = [sb("p3_kin%d" % i, [64, T], BF16) for i in range(2)]
                    W1 = sb("p3_W1", [64, 2, 32, 128], BF16)
                    peT = sb("p3_peT", [64, 2, 32], BF16)
                    W2 = sb("p3_W2", [128, 2, 64], BF16)
                    b1 = sb("p3_b1", [128, 2], F32)
                    uu = sb("p3_u", [128, 256], F32)
                    u2 = sb("p3_u2", [128, 256], F32)
                    sg = sb("p3_sg", [128, 256], F32)
                    gl = sb("p3_gl", [128, 256], BF16)
                    for kv in range(2):
                        S.dma('pool', W1[:, kv], ckw1_d[kv].rearrange("(l d) h -> d l h", d=64), writes=['W1'])
                        S.dma('pool', peT[:, kv, :], ckpe_d[kv], writes=['peT'])
                        S.dma('pool', W2[:, kv, :], ckw2_d[kv], writes=['W2'])
                    for g in range(4):
                        for c in range(2):
                            S.dma('pool', vcb[:, g, c, 65:129], ovl_d[c], reads=[], writes=['vcb'])
                    S.op('dve', lambda e: e.memset(vcb[:, :, :, 64:65], 1.0), writes=['vcb'])
                    for kv in range(2):
                        for l in range(32):
                            S.op('pe', lambda e, kv=kv, l=l: e.matmul(pM[:, kv:kv + 1], W1[:, kv, l, :], peT[:, kv, l:l + 1],
                                                                  start=(l == 0), stop=(l == 31)),
                                 reads=['W1', 'peT'], writes=['pM'])
                    S.op('dve', lambda e: e.tensor_copy(b1[:], pM[:, 0:2]), reads=['pM'], writes=['b1'])
                    it = 0
                    for kv in range(2):
                        for g in range(4):
                            ki = kin[it % 2]
                            kk = 'kin%d' % (it % 2)
                            pa = pA[it % 2]
                            pk = 'pA%d' % (it % 2)
                            it += 1
                            S.dma('sp', ki[:], kcvT[kv, g], writes=[kk])
                            for l in range(32):
                                S.op('pe', lambda e, kv=kv, l=l, ki=ki, pa=pa: e.matmul(
                                    pa[:, 0:255], W1[:, kv, l, :], ki[:, l:l + 4065:16], start=(l == 0), stop=(l == 31)),
                                    reads=['W1', kk], writes=[pk])
                            S.op('act', lambda e, kv=kv, pa=pa: e.activation(uu[:, 0:255], pa[:, 0:255], AF.Identity, bias=b1[:, kv:kv + 1]),
                                 reads=[pk, 'b1'], writes=['uu'])
                            S.op('dve', lambda e: e.tensor_tensor(u2[:, 0:255], uu[:, 0:255], uu[:, 0:255], ALU.mult), reads=['uu'], writes=['u2'])
                            S.op('dve', lambda e: e.tensor_scalar(u2[:, 0:255], u2[:, 0:255], 0.044715, 1.0, ALU.mult, ALU.add), reads=['u2'], writes=['u2'])
                            S.op('dve', lambda e: e.tensor_tensor(u2[:, 0:255], u2[:, 0:255], uu[:, 0:255], ALU.mult), reads=['u2', 'uu'], writes=['u2'])
                            S.op('act', lambda e: e.activation(sg[:, 0:255], u2[:, 0:255], AF.Sigmoid, scale=1.5957691216057308), reads=['u2'], writes=['sg'])
                            S.op('dve', lambda e: e.tensor_tensor(gl[:, 0:255], uu[:, 0:255], sg[:, 0:255], ALU.mult), reads=['uu', 'sg'], writes=['gl'])
                            if kv == 0:
                                S.op('pe', lambda e: e.matmul(pC[0][0:64, 0:255], W2[:, 0, :], gl[:, 0:255], start=True, stop=True),
                                     reads=['W2', 'gl'], writes=['pC0'])
                                S.op('dve', lambda e, g=g: e.tensor_copy(kcbT[:, g, 0:255], pC[0][0:64, 0:255]), reads=['pC0'], writes=['kcbT'])
                            else:
                                for c in range(2):
                                    rows = 128 if c == 0 else 127
                                    S.op('pe', lambda e, c=c, rows=rows: e.matmul(pC[c][0:rows, 0:64], gl[:, c * 128:c * 128 + rows], W2[:, 1, :],
                                                                                  start=True, stop=True),
                                         reads=['W2', 'gl'], writes=['pC%d' % c])
                                    S.op('dve', lambda e, c=c, rows=rows, g=g: e.tensor_copy(vcb[0:rows, g, c, 0:64], pC[c][0:rows, 0:64]),
                                         reads=['pC%d' % c], writes=['vcb'])
                S.barrier()

            if stages >= 4:
                with ExitStack() as st:
                    sb = lambda n, s, d: st.enter_context(nc.sbuf_tensor(n, list(s), d))
                    qg = [sb("p4_qg%d" % i, [64, 4, OWN], BF16) for i in range(2)]
                    cbt = [sb("p4_cb%d" % i, [128, 2, 4, 128], BF16) for i in range(2)]
                    E = [sb("p4_E%d" % i, [128, 512], BF16) for i in range(4)]
                    selc = sb("p4_selc", [128, NS, 64], F32)
                    rec = sb("p4_rec", [128, 4], F32)
                    oc = [sb("p4_oc%d" % i, [128, 4, 64], F32) for i in range(2)]
                    imp = sb("p4_imp", [128, 64], F32)
                    score = sb("p4_score", [128, 64], F32)
                    sc2 = sb("p4_sc2", [128, 64], F32)
                    m8a = sb("p4_m8a", [128, 8], F32)
                    m8b = sb("p4_m8b", [128, 8], F32)
                    selb = sb("p4_selb", [128, 128], F32)
                    S.op('dve', lambda e: e.memset(selb[:], 0.0), writes=['selb'])
                    S.dma('sp', selc[:], selc_d.rearrange("s q j -> q s j"), writes=['selc'])
                    it = 0
                    for g in range(4):
                        q_ = qg[g % 2]
                        qk = 'qg%d' % (g % 2)
                        S.dma('sp', q_[:], qnT[4 * g:4 * g + 4].rearrange("h d t -> d h t"), writes=[qk])
                        for s in range(NS):
                            cb = cbt[it % 2]
                            ck = 'cb%d' % (it % 2)
                            o_ = oc[it % 2]
                            ok = 'oc%d' % (it % 2)
                            it += 1
                            S.dma('pool', cb[:], cbias_d[s, :, :, 4 * g:4 * g + 4, :].rearrange("c n h q -> n c h q"), writes=[ck])
                            for c in range(2):
                                pa = pA[c]
                                pk = 'pA%d' % c
                                S.op('pe', lambda e, c=c, pa=pa, g=g, s=s, q_=q_: e.matmul(
                                    pa[:], kcbT[:, g, c * 128:(c + 1) * 128], q_[:, :, s * 128:(s + 1) * 128], start=True, stop=False),
                                    reads=['kcbT', qk], writes=[pk])
                                S.op('pe', lambda e, c=c, pa=pa, cb=cb: e.matmul(pa[:], identb[:], cb[:, c], start=False, stop=True),
                                     reads=['identb', ck], writes=[pk])
                                ei = (it % 2) * 2 + c
                                S.op('act', lambda e, pa=pa, ei=ei: e.activation(E[ei][:], pa[:], AF.Exp), reads=[pk], writes=['E%d' % ei])
                            for h in range(4):
                                po = pO[h // 2]
                                col = (h % 2) * 129
                                for c in range(2):
                                    ei = (it % 2) * 2 + c
                                    S.op('pe', lambda e, po=po, col=col, ei=ei, h=h, g=g, c=c: e.matmul(
                                        po[:, col:col + 129], E[ei][:, h * 128:(h + 1) * 128], vcb[:, g, c, :], start=(c == 0), stop=(c == 1)),
                                        reads=['E%d' % ei, 'vcb'], writes=['pO%d' % (h // 2)])
                            for h in range(4):
                                po = pO[h // 2]
                                col = (h % 2) * 129
                                S.op('dve', lambda e, po=po, col=col, h=h: e.tensor_scalar(rec[:, h:h + 1], po[:, col + 64:col + 65], 1e-30, None, ALU.max),
                                     reads=['pO%d' % (h // 2)], writes=['rec'])
                            S.op('dve', lambda e: e.reciprocal(rec[:], rec[:]), reads=['rec'], writes=['rec'])
                            for h in range(4):
                                po = pO[h // 2]
                                col = (h % 2) * 129
                                S.op('dve', lambda e, po=po, col=col, h=h, o_=o_: e.tensor_scalar(o_[:, h, :], po[:, col:col + 64], rec[:, h:h + 1], None, ALU.mult),
                                     reads=['pO%d' % (h // 2), 'rec'], writes=[ok])
                                if h == 0:
                                    S.op('dve', lambda e, po=po, col=col, h=h: e.tensor_scalar(imp[:], po[:, col + 65:col + 129], rec[:, h:h + 1], None, ALU.mult),
                                         reads=['pO%d' % (h // 2), 'rec'], writes=['imp'])
                                else:
                                    S.op('dve', lambda e, po=po, col=col, h=h: e.scalar_tensor_tensor(
                                        imp[:], po[:, col + 65:col + 129], rec[:, h:h + 1], imp[:], ALU.mult, ALU.add),
                                        reads=['pO%d' % (h // 2), 'rec', 'imp'], writes=['imp'])
                            S.dma('sp', obr_s[0, s * 128:(s + 1) * 128, g * 256:(g + 1) * 256], o_[:].rearrange("p h d -> p (h d)"), reads=[ok], writes=['sc_o'])
                            S.op('dve', lambda e, s=s: e.tensor_tensor(score[:], imp[:], selc[:, s, :], ALU.add), reads=['imp', 'selc'], writes=['score'])
                            S.op('dve', lambda e: e.max(m8a[:], score[:]), reads=['score'], writes=['m8a'])
                            S.op('dve', lambda e: e.match_replace(sc2[:], m8a[:], score[:], -3.0e38), reads=['score', 'm8a'], writes=['sc2'])
                            S.op('dve', lambda e: e.max(m8b[:], sc2[:]), reads=['sc2'], writes=['m8b'])
                            S.op('dve', lambda e: e.tensor_scalar(selb[:, 64:128], score[:], m8b[:, 7:8], -NEGM, ALU.is_ge, ALU.mult),
                                 reads=['score', 'm8b'], writes=['selb'])
                            S.op('dve', lambda e: e.tensor_scalar(selb[:, 64:128], selb[:, 64:128], NEGM, None, ALU.add), reads=['selb'], writes=['selb'])
                            S.op('pe', lambda e: e.transpose(pM[:, 0:128], selb[:], identf[:]), reads=['selb', 'identf'], writes=['pM'])
                            S.op('dve', lambda e, g=g, s=s: e.tensor_copy(selT[64:128, g, s, :], pM[64:128, 0:128]), reads=['pM'], writes=['selT'])
                S.barrier()

            def attn_phase(sel):
                with ExitStack() as st:
                    sb = lambda n, s, d: st.enter_context(nc.sbuf_tensor(n, list(s), d))
                    nm = "p5" if sel else "p6"
                    nab = 12 if sel else 8
                    AB = sb(nm + "_AB", [128, nab, 16, 128], BF16)
                    ab_src = abs_d if sel else abw_d
                    for u in range(nab):
                        S.dma('pool', AB[:, u], ab_src[u], writes=['AB'])
                    KR = 128 if sel else 64
                    kT = [sb(nm + "_kT%d" % i, [KR, T], BF16) for i in range(2)]
                    vt = [sb(nm + "_vt%d" % i, [128, NB, 65], BF16) for i in range(2)]
                    qg = [sb(nm + "_qg%d" % i, [KR, 4, OWN], BF16) for i in range(2)]
                    if sel:
                        for i in range(2):
                            S.dma('pool', kT[i][64:128, :].rearrange("j (b k) -> j b k", k=128), expand_d.rearrange("b j k -> j b k"),
                                  writes=['kT%d' % i])
                    E = [sb(nm + "_E%d" % i, [128, 512], BF16) for i in range(3)]
                    rec = sb(nm + "_rec", [128, 4], F32)
                    ot = [sb(nm + "_ot%d" % i, [128, 4, 64], F32) for i in range(4)]
                    ksrc = ksT if sel else kwT
                    voff = 0 if sel else 256
                    it = 0
                    ne = 0
                    def load_group(g):
                        k_ = kT[g % 2]
                        kk = 'kT%d' % (g % 2)
                        v_ = vt[g % 2]
                        vk = 'vt%d' % (g % 2)
                        q_ = qg[g % 2]
                        qk = 'qg%d' % (g % 2)
                        S.dma('sp', k_[0:64, :], ksrc[g], writes=[kk])
                        S.op('dve', lambda e: e.memset(v_[:, :, 64:65], 1.0), writes=[vk])
                        S.dma('sp', v_[:, :, 0:64], vsw[:, voff + g * 64:voff + (g + 1) * 64].rearrange("(b p) d -> p b d", p=128), writes=[vk])
                        S.dma('sp', q_[0:64], qnT[4 * g:4 * g + 4].rearrange("h d t -> d h t"), writes=[qk])
                        if sel:
                            for h in range(4):
                                S.op('pool', lambda e, h=h: e.tensor_copy(q_[64:128, h, :], selT[64:128, g].rearrange("j s q -> j (s q)")),
                                     reads=['selT'], writes=[qk])

                    load_group(0)
                    for g in range(4):
                        k_ = kT[g % 2]
                        kk = 'kT%d' % (g % 2)
                        v_ = vt[g % 2]
                        vk = 'vt%d' % (g % 2)
                        q_ = qg[g % 2]
                        qk = 'qg%d' % (g % 2)
                        if g + 1 < 4:
                            load_group(g + 1)
                        poh = [pO[0], pO[1], pC[0], pC[1]]
                        pokh = ['pO0', 'pO1', 'pC0', 'pC1']
                        items = []
                        for s in range(NS):
                            nu = 4 * s + 4 if sel else min(8, 4 * s + 4)
                            for u in range(nu):
                                items.append((s, u, nu))
                        srs = {}

                        def stageA(s, u, nu, idx):
                            if u == 0 and ((sel and s % 4 != 3) or ((not sel) and s % 4 == 0)):
                                issue_casts(1)
                            kb = 4 * s + 3 - u
                            pa = pA[idx % 2]
                            pk = 'pA%d' % (idx % 2)
                            S.op('pe', lambda e: e.matmul(pa[:], k_[:, kb * 128:(kb + 1) * 128], q_[:, :, s * 128:(s + 1) * 128], start=True, stop=False),
                                 reads=[kk, qk], writes=[pk])
                            S.op('pe', lambda e: e.matmul(pa[:], identb[:], AB[:, min(u, nab - 1), 4 * g:4 * g + 4, :], start=False, stop=True),
                                 reads=['identb', 'AB'], writes=[pk])
                            ei = idx % 3
                            S.op('act', lambda e: e.activation(E[ei][:], pa[:], AF.Exp), reads=[pk], writes=['E%d' % ei])

                        def stageC(s, u, nu, idx):
                            kb = 4 * s + 3 - u
                            ei = idx % 3
                            for h in range(4):
                                S.op('pe', lambda e, h=h: e.matmul(poh[h][:, 0:65], E[ei][:, h * 128:(h + 1) * 128], v_[:, kb, :],
                                                                  start=(u == 0), stop=(u == nu - 1)),
                                     reads=['E%d' % ei, vk], writes=[pokh[h]])
                            if u == nu - 1:
                                o_ = ot[s % 4]
                                ok = 'ot%d' % (s % 4)
                                for h in range(4):
                                    S.op('dve', lambda e, h=h: e.tensor_scalar(rec[:, h:h + 1], poh[h][:, 64:65], 1e-30, None, ALU.max),
                                         reads=[pokh[h]], writes=['rec'])
                                S.op('dve', lambda e: e.reciprocal(rec[:], rec[:]), reads=['rec'], writes=['rec'])
                                for h in range(4):
                                    S.op('dve', lambda e, h=h: e.tensor_scalar(o_[:, h, :], poh[h][:, 0:64], rec[:, h:h + 1], None, ALU.mult),
                                         reads=[pokh[h], 'rec'], writes=[ok])
                                S.dma('sp', obr_s[1 if sel else 2, s * 128:(s + 1) * 128, g * 256:(g + 1) * 256], o_[:].rearrange("p h d -> p (h d)"),
                                      reads=[ok], writes=['sc_o'])

                        for idx, (s, u, nu) in enumerate(items):
                            stageA(s, u, nu, idx)
                            if idx >= 1:
                                stageC(*items[idx - 1], idx - 1)
                        stageC(*items[-1], len(items) - 1)
                S.barrier()

            if stages >= 5:
                attn_phase(True)
            if stages >= 6:
                attn_phase(False)

            if stages >= 7:
                with ExitStack() as st:
                    sb = lambda n, s, d: st.enter_context(nc.sbuf_tensor(n, list(s), d))
                    kq = [sb("p7_kq%d" % i, [128, 2, T], BF16) for i in range(2)]
                    vq = [sb("p7_vq%d" % i, [128, NB, 256], BF16) for i in range(2)]
                    qq = [sb("p7_qq%d" % i, [128, 2, NS, 256], BF16) for i in range(2)]
                    msb4 = sb("p7_msb4", [128, 4, 4, 128], BF16)
                    ef = [sb("p7_ef%d" % i, [128, 512], F32) for i in range(3)]
                    spt = [sb("p7_sp%d" % i, [128, 512], BF16) for i in range(3)]
                    acc = sb("p7_acc", [128, 512], BF16)
                    wt = [sb("p7_w%d" % i, [128, 512], BF16) for i in range(2)]
                    at = [sb("p7_a%d" % i, [128, 512], BF16) for i in range(2)]
                    osbt = [sb("p7_o%d" % i, [128, 256], F32) for i in range(4)]
                    for h in range(4):
                        S.dma('pool', msb4[:, :, h, :], msb_d.rearrange("u k q -> k u q"), writes=['msb4'])
                    it = 0
                    n7 = 0
                    def load_quad(hq):
                        k_ = kq[hq % 2]
                        kk = 'kq%d' % (hq % 2)
                        v_ = vq[hq % 2]
                        vk = 'vq%d' % (hq % 2)
                        q_ = qq[hq % 2]
                        qk = 'qq%d' % (hq % 2)
                        S.dma('sp', k_[:], kbT[4 * hq:4 * hq + 4].rearrange("(p hh) d t -> (hh d) p t", hh=2), writes=[kk])
                        S.dma('sp', v_[:], vb[:, hq * 256:(hq + 1) * 256].rearrange("(b p) d -> p b d", p=128), writes=[vk])
                        S.op('dve', lambda e: e.memset(q_[:], 0.0), writes=[qk])
                        for p_ in range(2):
                            for hh in range(2):
                                S.dma('sp', q_[hh * 64:(hh + 1) * 64, p_, :, hh * 128:(hh + 1) * 128],
                                      qbT[4 * hq + 2 * p_ + hh].rearrange("d (s q) -> d s q", q=128), writes=[qk])

                    load_quad(0)
                    for hq in range(4):
                        k_ = kq[hq % 2]
                        kk = 'kq%d' % (hq % 2)
                        v_ = vq[hq % 2]
                        vk = 'vq%d' % (hq % 2)
                        q_ = qq[hq % 2]
                        qk = 'qq%d' % (hq % 2)
                        if hq + 1 < 4:
                            load_quad(hq + 1)
                        poh = [pO[0], pO[1], pC[1], pM]
                        pokh = ['pO0', 'pO1', 'pC1', 'pM']
                        pc = pC[0]
                        pck = 'pC0'
                        items = []
                        for s in range(NS):
                            for u in range(4 * s + 4):
                                items.append((s, u, 4 * s + 4))

                        def sbA(s, u, nu, idx):
                            if u == 0:
                                issue_casts(1)
                            kb = 4 * s + 3 - u
                            pa = pA[idx % 2]
                            pk = 'pA%d' % (idx % 2)
                            i3 = idx % 3
                            for p_ in range(2):
                                S.op('pe', lambda e, p_=p_: e.matmul(
                                    pa[:, p_ * 256:(p_ + 1) * 256], k_[:, p_, kb * 128:(kb + 1) * 128], q_[:, p_, s, :],
                                    start=True, stop=(u >= 4)), reads=[kk, qk], writes=[pk])
                                if u < 4:
                                    S.op('pe', lambda e, p_=p_: e.matmul(pa[:, p_ * 256:(p_ + 1) * 256], identb[:], msb4[:, u, 2 * p_:2 * p_ + 2, :],
                                                                       start=False, stop=True),
                                         reads=['identb', 'msb4'], writes=[pk])
                            S.op('act', lambda e: e.activation(ef[i3][:], pa[:], AF.Exp), reads=[pk], writes=['ef%d' % i3])
                            S.op('act', lambda e: e.activation(spt[i3][:], ef[i3][:], AF.Ln, bias=1.0), reads=['ef%d' % i3], writes=['sp%d' % i3])

                        def sbB(s, u, nu, idx):
                            i3 = idx % 3
                            i2 = idx % 2
                            S.op('pe', lambda e: e.matmul(pc[:], trib[:], spt[i3][:], start=True, stop=(u == 0)),
                                 reads=['trib', 'sp%d' % i3], writes=[pck])
                            if u > 0:
                                S.op('pe', lambda e: e.matmul(pc[:], onesb[:], acc[:], start=False, stop=True),
                                     reads=['onesb', 'acc'], writes=[pck])
                            if u < nu - 1:
                                if u == 0:
                                    S.op('pool', lambda e: e.tensor_copy(acc[:], spt[i3][:]), reads=['sp%d' % i3], writes=['acc'])
                                else:
                                    S.op('pool', lambda e: e.tensor_tensor(acc[:], acc[:], spt[i3][:], ALU.add),
                                         reads=['sp%d' % i3, 'acc'], writes=['acc'])
                            S.op('act', lambda e: e.activation(wt[i2][:], pc[:], AF.Exp, scale=-1.0), reads=[pck], writes=['w%d' % i2])
                            S.op('dve', lambda e: e.tensor_tensor(at[i2][:], ef[i3][:], wt[i2][:], ALU.mult),
                                 reads=['ef%d' % i3, 'w%d' % i2], writes=['a%d' % i2])

                        def sbC(s, u, nu, idx):
                            kb = 4 * s + 3 - u
                            i2 = idx % 2
                            for h in range(4):
                                S.op('pe', lambda e, h=h: e.matmul(
                                    poh[h][:, 0:64], at[i2][:, h * 128:(h + 1) * 128], v_[:, kb, h * 64:(h + 1) * 64],
                                    start=(u == 0), stop=(u == nu - 1)), reads=['a%d' % i2, vk], writes=[pokh[h]])
                            if u == nu - 1:
                                ob = osbt[s % 4]
                                obk = 'osbt%d' % (s % 4)
                                for h in range(4):
                                    evac(ob[:, h * 64:(h + 1) * 64], poh[h][:, 0:64], [pokh[h]], [obk])
                                S.dma('sp', osb_s[s * 128:(s + 1) * 128, hq * 256:(hq + 1) * 256], ob[:], reads=[obk], writes=['sc_osb'])

                        n_it = len(items)
                        for idx in range(n_it + 2):
                            if idx < n_it:
                                sbA(*items[idx], idx)
                            if 1 <= idx <= n_it:
                                sbB(*items[idx - 1], idx - 1)
                            if idx >= 2:
                                sbC(*items[idx - 2], idx - 2)
                S.barrier()

            if 'p37' in DEBUG:
                S.dma('sp', dbg_out("obr", [3, OWN, 1024]), obr_s)
                S.dma('sp', dbg_out("osb", [OWN, 1024]), osb_s)
                dk = dbg_out("kcbT", [64, 4, 256], BF16)
                S.dma('sp', dk, kcbT[:])
                dv = dbg_out("vcb", [128, 4, 2, 129], BF16)
                S.dma('sp', dv, vcb[:])
                dsel = dbg_out("selT", [64, 4, NS, 128], BF16)
                S.dma('sp', dsel, selT[64:128])

        if stages >= 8:
            with ExitStack() as st8:
                mTall = st8.enter_context(nc.sbuf_tensor("p8_mTall", [128, 16, OWN], BF16))
                with ExitStack() as st:
                    sb = lambda n, s, d: st.enter_context(nc.sbuf_tensor(n, list(s), d))
                    Wbn = sb("p8_Wbn", [128, 8, D], BF16)
                    Wbs = sb("p8_Wbs", [128, 8, D], BF16)
                    ob2 = [[sb("p8_ob%d_%d" % (i, j), [128, 16, 64], F32) for i in range(3)] for j in range(2)]
                    osb_t2 = [sb("p8_osb%d" % j, [128, 1024], F32) for j in range(2)]
                    gbrt2 = [sb("p8_gbr%d" % j, [128, 48], F32) for j in range(2)]
                    onsa = sb("p8_onsa", [128, 16, 64], F32)
                    tmp = sb("p8_tmp", [128, 16, 64], F32)
                    onb = sb("p8_onb", [128, 1024], BF16)
                    osb16 = sb("p8_osb16", [128, 1024], BF16)
                    onT = sb("p8_onT", [128, 8, 128], BF16)
                    osT = sb("p8_osT", [128, 8, 128], BF16)
                    gab2 = [sb("p8_gab%d" % j, [128, 4096], F32) for j in range(2)]
                    m1 = sb("p8_m1", [128, 512], F32)
                    m2 = sb("p8_m2", [128, 512], F32)
                    mb = sb("p8_mb", [128, D], BF16)
                    wbn_v = wbn_d.rearrange("(c p) n -> p c n", p=128)
                    wbs_v = wbs_d.rearrange("(c p) n -> p c n", p=128)
                    for c4 in range(0, 8, 4):
                        S.dma('pool', Wbn[:, c4:c4 + 4, :], wbn_v[:, c4:c4 + 4, :], writes=['Wbn'])
                        S.dma('pool', Wbs[:, c4:c4 + 4, :], wbs_v[:, c4:c4 + 4, :], writes=['Wbs'])
                    for ti in range(8):
                        rows = slice(ti * 128, (ti + 1) * 128)
                        jb_ = ti % 2
                        ob, osb_t, gbrt, gab = ob2[jb_], osb_t2[jb_], gbrt2[jb_], gab2[jb_]
                        kx = '_%d' % jb_
                        for i in range(3):
                            S.dma('sp', ob[i][:].rearrange("p h d -> p (h d)"), obr_s[i, rows, :], writes=['ob%d' % i + kx])
                        S.dma('sp', osb_t[:], osb_s[rows, :], writes=['osb_t' + kx])
                        S.dma('sp', gbrt[:], gbr_s[rows, :], writes=['gbrt' + kx])
                        S.dma('sp', gab[:], gab_s[rows, :], writes=['gab' + kx])
                        gv = lambda i: bc(gbrt[:, i * 16:(i + 1) * 16].unsqueeze(2), [128, 16, 64])
                        S.op('dve', lambda e: e.tensor_tensor(onsa[:], ob[0][:], gv(0), ALU.mult), reads=['ob0' + kx, 'gbrt' + kx], writes=['onsa'])
                        S.op('dve', lambda e: e.tensor_tensor(tmp[:], ob[1][:], gv(1), ALU.mult), reads=['ob1' + kx, 'gbrt' + kx], writes=['tmp'])
                        S.op('dve', lambda e: e.tensor_tensor(onsa[:], onsa[:], tmp[:], ALU.add), reads=['onsa', 'tmp'], writes=['onsa'])
                        S.op('dve', lambda e: e.tensor_tensor(tmp[:], ob[2][:], gv(2), ALU.mult), reads=['ob2' + kx, 'gbrt' + kx], writes=['tmp'])
                        S.op('dve', lambda e: e.tensor_tensor(onb[:].rearrange("p (h d) -> p h d", d=64), onsa[:], tmp[:], ALU.add),
                             reads=['onsa', 'tmp'], writes=['onb'])
                        S.op('act', lambda e: e.activation(osb16[:], osb_t[:], AF.Copy), reads=['osb_t' + kx], writes=['osb16'])
                        for (src, sk, dstT, dk) in ((onb, 'onb', onT, 'onT'), (osb16, 'osb16', osT, 'osT')):
                            for cc in range(8):
                                S.op('pe', lambda e, cc=cc, src=src: e.transpose(pT[:, cc * 128:(cc + 1) * 128], src[:, cc * 128:(cc + 1) * 128], identb[:]),
                                     reads=[sk, 'identb'], writes=['pT'])
                            S.op('dve', lambda e, dstT=dstT: e.tensor_copy(dstT[:], pT[:].rearrange("p (c t) -> p c t", c=8)),
                                 reads=['pT'], writes=[dk])
                        for cb in range(4):
                            cs = slice(cb * 512, (cb + 1) * 512)
                            for c in range(8):
                                S.op('pe', lambda e, c=c, cs=cs: e.matmul(pA[0][:], onT[:, c, :], Wbn[:, c, cs], start=(c == 0), stop=(c == 7)),
                                     reads=['onT', 'Wbn'], writes=['pA0'])
                            for c in range(8):
                                S.op('pe', lambda e, c=c, cs=cs: e.matmul(pA[1][:], osT[:, c, :], Wbs[:, c, cs], start=(c == 0), stop=(c == 7)),
                                     reads=['osT', 'Wbs'], writes=['pA1'])
                            S.op('dve', lambda e, cs=cs: e.tensor_tensor(m1[:], pA[0][:], gab[:, cs], ALU.mult), reads=['pA0', 'gab' + kx], writes=['m1'])
                            S.op('dve', lambda e, cb=cb: e.tensor_tensor(m2[:], pA[1][:], gab[:, 2048 + cb * 512:2048 + (cb + 1) * 512], ALU.mult),
                                 reads=['pA1', 'gab' + kx], writes=['m2'])
                            S.op('dve', lambda e, cs=cs: e.tensor_tensor(mb[:, cs], m1[:], m2[:], ALU.add), reads=['m1', 'm2'], writes=['mb'])
                        for half in range(2):
                            for cc in range(8):
                                c = half * 8 + cc
                                S.op('pe', lambda e, cc=cc, c=c: e.transpose(pT[:, cc * 128:(cc + 1) * 128], mb[:, c * 128:(c + 1) * 128], identb[:]),
                                     reads=['mb', 'identb'], writes=['pT'])
                            S.op('dve', lambda e, half=half, ti=ti: e.tensor_copy(
                                mTall[:, half * 8:(half + 1) * 8, ti * 128:(ti + 1) * 128], pT[:].rearrange("p (c t) -> p c t", c=8)),
                                reads=['pT'], writes=['mTall'])
                S.barrier()
                with ExitStack() as st:
                    sb = lambda n, s, d: st.enter_context(nc.sbuf_tensor(n, list(s), d))
                    Wout = sb("p8_Wout", [128, 16, D], BF16)
                    xo = [sb("p8_xo%d" % i, [128, D], F32) for i in range(2)]
                    h1t = [sb("p8_h1t%d" % i, [128, D], F32) for i in range(2)]
                    wo_v = wout_d.rearrange("(c p) n -> p c n", p=128)
                    for c4 in range(0, 16, 4):
                        S.dma('pool', Wout[:, c4:c4 + 4, :], wo_v[:, c4:c4 + 4, :], writes=['Wout'])
                    for ti in range(8):
                        rows = slice(ti * 128, (ti + 1) * 128)
                        x_ = xo[ti % 2]
                        xk = 'xo%d' % (ti % 2)
                        h_ = h1t[ti % 2]
                        hk = 'h1t%d' % (ti % 2)
                        S.dma('sp', x_[:], x_own[rows, :], writes=[xk])
                        for cb in range(4):
                            cs = slice(cb * 512, (cb + 1) * 512)
                            pa = pA[cb % 2]
                            pk = 'pA%d' % (cb % 2)
                            for c in range(16):
                                S.op('pe', lambda e, c=c, cs=cs, pa=pa, ti=ti: e.matmul(pa[:], mTall[:, c, ti * 128:(ti + 1) * 128], Wout[:, c, cs],
                                                                                 start=(c == 0), stop=(c == 15)),
                                     reads=['mTall', 'Wout'], writes=[pk])
                            S.op('dve', lambda e, cs=cs, pa=pa, h_=h_, x_=x_: e.tensor_tensor(h_[:, cs], pa[:], x_[:, cs], ALU.add),
                                 reads=[pk, xk], writes=[hk])
                        S.dma('sp', h1_s[rows, :], h_[:], reads=[hk], writes=['sc_h1'])
            S.barrier()

        if 'p8' in DEBUG:
            S.dma('sp', dbg_out("h1", [OWN, D]), h1_s)

        if stages >= 9:
            with ExitStack() as st:
                sb = lambda n, s, d: st.enter_context(nc.sbuf_tensor(n, list(s), d))
                Wq = sb("p9_Wq", [128, 16, D], BF16)
                skt = sb("p9_skt", [128, 16, 128], BF16)
                gffn = sb("p9_gffn", [128, D], F32)
                gfin = sb("p9_gfin", [128, D], F32)
                iota16 = sb("p9_iota", [128, 16], F32)
                h1t = sb("p9_h1t", [128, D], F32)
                junk = sb("p9_junk", [128, D], BF16)
                ss = sb("p9_ss", [128, 1], F32)
                rstd = sb("p9_rstd", [128, 1], F32)
                hn = sb("p9_hn", [128, D], F32)
                hnb = sb("p9_hnb", [128, D], BF16)
                hnT = sb("p9_hnT", [128, 16, 128], BF16)
                qTs = sb("p9_qTs", [128, 16, 128], BF16)
                sc = sb("p9_sc", [128, 16, 128], F32)
                sc2 = sb("p9_sc2", [128, 256], F32)
                tv = sb("p9_tv", [128, 16, 16], F32)
                tiu = sb("p9_tiu", [128, 16, 16], U32)
                tif = sb("p9_tif", [128, 16, 16], F32)
                cand = sb("p9_cand", [128, 8, 256], F32)
                bs = sb("p9_bs", [128, 8, 16], F32)
                bju = sb("p9_bju", [128, 8, 16], U32)
                bjf = sb("p9_bjf", [128, 8, 16], F32)
                ja = sb("p9_ja", [128, 8, 16], F32)
                jb = sb("p9_jb", [128, 8, 16], F32)
                eq = sb("p9_eq", [128, 8, 16, 16], F32)
                i0 = sb("p9_i0", [128, 8, 16], F32)
                i1 = sb("p9_i1", [128, 8, 16], F32)
                exf = sb("p9_exf", [128, 128], F32)
                exi = sb("p9_exi", [128, 128], I32)
                gm = sb("p9_gm", [128, 8], F32)
                ge = sb("p9_ge", [128, 8, 16], F32)
                gz = sb("p9_gz", [128, 8], F32)
                pre = sb("p9_pre", [128, 128], F32)
                t1 = sb("p9_t1", [128, 128], F32)
                t2 = sb("p9_t2", [128, 128], F32)
                coef = sb("p9_coef", [128, 128], F32)
                gbuf = [sb("p9_gb%d" % i, [128, D], BF16) for i in range(4)]
                DB = 16
                dgb = [sb("p9_dgb%d" % i, [128, DB, 128], BF16) for i in range(2)]
                junkb = sb("p9_junkb", [128, D], BF16)
                wq_v = wq_d.rearrange("(c p) n -> p c n", p=128)
                for c4 in range(0, 16, 4):
                    S.dma('pool', Wq[:, c4:c4 + 4, :], wq_v[:, c4:c4 + 4, :], writes=['Wq'])
                S.dma('pool', skt[:], skT_d.rearrange("a c k -> c a k"), writes=['skt'])
                S.dma('sp', gffn[:], g_ffn.partition_broadcast(128), writes=['gffn'])
                S.dma('sp', gfin[:], g_fin.partition_broadcast(128), writes=['gfin'])
                S.dma('sp', iota16[:], iota_d, writes=['iota16'])
                thr16 = sb("p9_thr16", [128, 16], F32)
                S.op('dve', lambda e: e.tensor_scalar(thr16[:], iota16[:], 16.0, 16.0, ALU.mult, ALU.add), reads=['iota16'], writes=['thr16'])
                ng = 0
                pvacc = [pO[0], pO[1], pC[0], pC[1]]
                pvk = ['pO0', 'pO1', 'pC0', 'pC1']
                issue_casts(len(cast_jobs))
                S.wait_bg(['pool'])
                h1t2 = [h1t, sb("p9_h1tb", [128, D], F32)]
                hnb2 = [hnb, sb("p9_hnbb", [128, D], BF16)]
                exi2 = [exi, sb("p9_exib", [128, 128], I32)]
                ge2 = [ge, sb("p9_geb", [128, 8, 16], F32)]
                pre2 = [pre, sb("p9_preb", [128, 128], F32)]
                coef2 = [coef, sb("p9_coefb", [128, 128], F32)]
                ssf = sb("p9_ssf", [128, 1], F32)
                rstdf = sb("p9_rstdf", [128, 1], F32)
                gbuf.extend([sb("p9_gbx%d" % i, [128, D], BF16) for i in range(2)])
                NG = len(gbuf)
                ngc = [0]

                def top16(vals, vkey, width, out_v, out_i, okey):
                    S.op('dve', lambda e: e.max(out_v[:, 0:8], vals), reads=[vkey], writes=[okey])
                    S.op('dve', lambda e: e.max_index(out_i[:, 0:8], out_v[:, 0:8], vals), reads=[vkey, okey], writes=[okey + 'i'])
                    S.op('dve', lambda e: e.match_replace(sc2[:, 0:width], out_v[:, 0:8], vals, -3.0e38), reads=[vkey, okey], writes=['sc2'])
                    S.op('dve', lambda e: e.max(out_v[:, 8:16], sc2[:, 0:width]), reads=['sc2'], writes=[okey])
                    S.op('dve', lambda e: e.max_index(out_i[:, 8:16], out_v[:, 8:16], sc2[:, 0:width]), reads=['sc2', okey], writes=[okey + 'i'])

                def front(t):
                    b = t % 2
                    h1t, exi, ge, hnb = h1t2[b], exi2[b], ge2[b], hnb2[b]
                    hk, nk, xk, gek, hbk = 'h1t%d' % b, 'hn', 'exi%d' % b, 'ge%d' % b, 'hnb%d' % b
                    rows = slice(t * 128, (t + 1) * 128)
                    S.dma('sp', h1t[:], h1_s[rows, :], writes=[hk])
                    norm_rows(h1t[:], hk, ss, rstd, junk, 'p9')
                    S.op('dve', lambda e: e.scalar_tensor_tensor(hn[:], h1t[:], rstd[:, 0:1], gffn[:], ALU.mult, ALU.mult),
                         reads=[hk, 'p9rstd', 'gffn'], writes=[nk])
                    S.op('act', lambda e: e.activation(hnb[:], hn[:], AF.Copy), reads=[nk], writes=[hbk])
                    yield
                    for half in range(2):
                        for cc in range(8):
                            c = half * 8 + cc
                            S.op('pe', lambda e, cc=cc, c=c: e.transpose(pT[:, cc * 128:(cc + 1) * 128], hnb[:, c * 128:(c + 1) * 128], identb[:]),
                                 reads=[hbk, 'identb'], writes=['pT'])
                        S.op('dve', lambda e, half=half: e.tensor_copy(hnT[:, half * 8:(half + 1) * 8, :], pT[:].rearrange("p (c t) -> p c t", c=8)),
                             reads=['pT'], writes=['hnT'])
                        yield
                    for b4 in range(4):
                        pa = pA[b4 % 2]
                        pk = 'pA%d' % (b4 % 2)
                        for bb in range(4):
                            blk = b4 * 4 + bb
                            for c in range(16):
                                S.op('pe', lambda e, c=c, blk=blk, bb=bb, pa=pa: e.matmul(
                                    pa[:, bb * 128:(bb + 1) * 128], Wq[:, c, blk * 128:(blk + 1) * 128], hnT[:, c, :],
                                    start=(c == 0), stop=(c == 15)), reads=['Wq', 'hnT'], writes=[pk])
                        evac(qTs[:, b4 * 4:(b4 + 1) * 4, :], pa[:].rearrange("p (b t) -> p b t", b=4), [pk], ['qTs'])
                        yield
                    for b4 in range(4):
                        pa = pA[b4 % 2]
                        pk = 'pA%d' % (b4 % 2)
                        for bb in range(4):
                            blk = b4 * 4 + bb
                            S.op('pe', lambda e, blk=blk, bb=bb, pa=pa: e.matmul(
                                pa[:, bb * 128:(bb + 1) * 128], qTs[:, blk, :], skt[:, blk, :], start=True, stop=True),
                                reads=['qTs', 'skt'], writes=[pk])
                        evac(sc[:, b4 * 4:(b4 + 1) * 4, :], pa[:].rearrange("p (b t) -> p b t", b=4), [pk], ['sc'])
                        yield
                    for blk in range(16):
                        top16(sc[:, blk, :], 'sc', 128, tv[:, blk, :], tiu[:, blk, :], 'tv')
                        yield
                    S.op('dve', lambda e: e.tensor_copy(tif[:], tiu[:]), reads=['tvi'], writes=['tif'])
                    tvv = tv[:].rearrange("p (h two) k -> p h two k", two=2)
                    tfv = tif[:].rearrange("p (h two) k -> p h two k", two=2)
                    S.op('dve', lambda e: e.tensor_tensor(
                        cand[:].rearrange("p h (a b) -> p h a b", b=16),
                        bc(tvv[:, :, 0, :].unsqueeze(3), [128, 8, 16, 16]),
                        bc(tvv[:, :, 1, :].unsqueeze(2), [128, 8, 16, 16]), ALU.add), reads=['tv'], writes=['cand'])
                    yield
                    for h in range(8):
                        top16(cand[:, h, :], 'cand', 256, bs[:, h, :], bju[:, h, :], 'bs')
                        yield
                    S.op('dve', lambda e: e.tensor_copy(bjf[:], bju[:]), reads=['bsi'], writes=['bjf'])
                    S.op('dve', lambda e: e.tensor_tensor(eq[:], bc(bjf[:].unsqueeze(3), [128, 8, 16, 16]),
                                                          bc(thr16[:].unsqueeze(1).unsqueeze(1), [128, 8, 16, 16]), ALU.is_ge),
                         reads=['bjf', 'thr16'], writes=['eq'])
                    S.op('dve', lambda e: e.tensor_reduce(ja[:], eq[:], AX.X, ALU.add), reads=['eq'], writes=['ja'])
                    S.op('dve', lambda e: e.scalar_tensor_tensor(jb[:], ja[:], -16.0, bjf[:], ALU.mult, ALU.add), reads=['ja', 'bjf'], writes=['jb'])
                    yield
                    iob = bc(iota16[:].unsqueeze(1).unsqueeze(1), [128, 8, 16, 16])
                    for (jx, jk, two, ix, ik) in ((ja, 'ja', 0, i0, 'i0'), (jb, 'jb', 1, i1, 'i1')):
                        S.op('dve', lambda e, jx=jx: e.tensor_tensor(eq[:], iob, bc(jx[:].unsqueeze(3), [128, 8, 16, 16]), ALU.is_equal),
                             reads=['iota16', jk], writes=['eq'])
                        S.op('dve', lambda e, two=two: e.tensor_tensor(eq[:], eq[:], bc(tfv[:, :, two, :].unsqueeze(2), [128, 8, 16, 16]), ALU.mult),
                             reads=['eq', 'tif'], writes=['eq'])
                        S.op('dve', lambda e, ix=ix: e.tensor_reduce(ix[:], eq[:], AX.X, ALU.add), reads=['eq'], writes=[ik])
                        yield
                    S.op('dve', lambda e: e.scalar_tensor_tensor(exf[:].rearrange("p (h k) -> p h k", k=16), i0[:], 128.0, i1[:], ALU.mult, ALU.add),
                         reads=['i0', 'i1'], writes=['exf'])
                    S.op('dve', lambda e: e.tensor_copy(exi[:], exf[:]), reads=['exf'], writes=[xk])
                    yield
                    S.op('dve', lambda e: e.tensor_reduce(gm[:], bs[:], AX.X, ALU.max), reads=['bs'], writes=['gm'])
                    S.op('dve', lambda e: e.tensor_tensor(ge[:], bs[:], bc(gm[:].unsqueeze(2), [128, 8, 16]), ALU.subtract), reads=['bs', 'gm'], writes=[gek])
                    S.op('act', lambda e: e.activation(ge[:], ge[:], AF.Exp), reads=[gek], writes=[gek])
                    S.op('dve', lambda e: e.tensor_reduce(gz[:], ge[:], AX.X, ALU.add), reads=[gek], writes=['gz'])
                    S.op('dve', lambda e: e.reciprocal(gz[:], gz[:]), reads=['gz'], writes=['gz'])
                    S.op('dve', lambda e: e.tensor_tensor(ge[:], ge[:], bc(gz[:].unsqueeze(2), [128, 8, 16]), ALU.mult), reads=[gek, 'gz'], writes=[gek])

                def ustep(t, slot):
                    b = t % 2
                    gb_ = gbuf[ngc[0] % NG]
                    gk = 'gb%d' % (ngc[0] % NG)
                    ngc[0] += 1
                    S.dma('pool', None, None, reads=['exi%d' % b], writes=[gk],
                          fn=lambda e: e.indirect_dma_start(
                              out=gb_[:], out_offset=None, in_=pu16[:, :],
                              in_offset=bass.IndirectOffsetOnAxis(ap=exi2[b][:, slot:slot + 1], axis=0)))
                    S.op('dve', lambda e: e.scalar_tensor_tensor(
                        junkb[:], gb_[:], 1.0, hnb2[b][:], ALU.mult, ALU.mult,
                        accum_out=pre2[b][:, slot:slot + 1]), reads=[gk, 'hnb%d' % b], writes=['junkb', 'pre%d' % b])

                def midstep(t):
                    b = t % 2
                    pre, coef, ge = pre2[b], coef2[b], ge2[b]
                    pk_, ck_ = 'pre%d' % b, 'coef%d' % b
                    S.op('dve', lambda e: e.tensor_tensor(t1[:], pre[:], pre[:], ALU.mult), reads=[pk_], writes=['t1'])
                    S.op('dve', lambda e: e.tensor_scalar(t1[:], t1[:], 0.044715, 1.0, ALU.mult, ALU.add), reads=['t1'], writes=['t1'])
                    S.op('dve', lambda e: e.tensor_tensor(t1[:], t1[:], pre[:], ALU.mult), reads=['t1', pk_], writes=['t1'])
                    S.op('act', lambda e: e.activation(t2[:], t1[:], AF.Sigmoid, scale=1.5957691216057308), reads=['t1'], writes=['t2'])
                    S.op('dve', lambda e: e.tensor_tensor(t2[:], t2[:], pre[:], ALU.mult), reads=['t2', pk_], writes=['t2'])
                    S.op('dve', lambda e: e.tensor_tensor(coef[:], t2[:], ge[:].rearrange("p h k -> p (h k)"), ALU.mult),
                         reads=['t2', 'ge%d' % b], writes=[ck_])
                    build_diag(t, 0)

                def build_diag(t, k):
                    b = t % 2
                    S.op('dve', lambda e: e.tensor_tensor(
                        dgb[k % 2][:], bc(identf[:].unsqueeze(1), [128, DB, 128]),
                        bc(coef2[b][:, k * DB:(k + 1) * DB].unsqueeze(2), [128, DB, 128]), ALU.mult),
                        reads=['identf', 'coef%d' % b], writes=['dgb%d' % (k % 2)])

                def vstep(t, slot):
                    b = t % 2
                    gb_ = gbuf[ngc[0] % NG]
                    gk = 'gb%d' % (ngc[0] % NG)
                    ngc[0] += 1
                    S.dma('pool', None, None, reads=['exi%d' % b], writes=[gk],
                          fn=lambda e: e.indirect_dma_start(
                              out=gb_[:], out_offset=None, in_=pv16[:, :],
                              in_offset=bass.IndirectOffsetOnAxis(ap=exi2[b][:, slot:slot + 1], axis=0)))
                    if slot % DB == 0 and slot + DB < 128:
                        build_diag(t, slot // DB + 1)
                    dgt = dgb[(slot // DB) % 2]
                    dk = 'dgb%d' % ((slot // DB) % 2)
                    for cb in range(4):
                        S.op('pe', lambda e, cb=cb: e.matmul(
                            pvacc[cb][:], dgt[:, slot % DB, :], gb_[:, cb * 512:(cb + 1) * 512], start=(slot == 0), stop=(slot == 127)),
                            reads=[dk, gk], writes=[pvk[cb]])

                def finstep(t):
                    b = t % 2
                    rows = slice(t * 128, (t + 1) * 128)
                    acc = h1t2[b]
                    ak = 'h1t%d' % b
                    for cb in range(4):
                        S.op('dve', lambda e, cb=cb: e.tensor_tensor(acc[:, cb * 512:(cb + 1) * 512], pvacc[cb][:], acc[:, cb * 512:(cb + 1) * 512], ALU.add),
                             reads=[pvk[cb], ak], writes=[ak])
                    norm_rows(acc[:], ak, ssf, rstdf, junk, 'p9f')
                    S.op('dve', lambda e: e.scalar_tensor_tensor(acc[:], acc[:], rstdf[:, 0:1], gfin[:], ALU.mult, ALU.mult),
                         reads=[ak, 'p9frstd', 'gfin'], writes=[ak])
                    S.dma('sp', out_d[rows, :], acc[:], reads=[ak], writes=['out'])

                for _ in front(0):
                    pass
                for slot in range(128):
                    ustep(0, slot)
                midstep(0)
                for t in range(8):
                    if t + 1 < 8:
                        fg = front(t + 1)
                        HEAD = 96
                        for slot in range(HEAD):
                            vstep(t, slot)
                            next(fg, None)
                        for _ in fg:
                            pass
                        vs_, us_ = HEAD, 0
                        while vs_ < 128 or us_ < 128:
                            if us_ < 128:
                                ustep(t + 1, us_)
                                us_ += 1
                            if vs_ < 128 and (vs_ - HEAD) * 128 < us_ * (128 - HEAD):
                                vstep(t, vs_)
                                vs_ += 1
                    else:
                        for slot in range(128):
                            vstep(t, slot)
                    finstep(t)
                    if t + 1 < 8:
                        midstep(t + 1)

        S.finish()
    return nc, dbg


def host_consts(j, rel_table):
    tab = np.asarray(rel_table, np.float32)
    k = np.arange(128)[:, None]
    q = np.arange(128)[None, :]
    abs_t = np.empty((12, 128, 16, 128), np.float32)
    for u in range(12):
        if u == 11:
            abs_t[u] = tab[31][None, :, None]
            continue
        dist = 128 * (u + j - 3) + q - k
        val = tab[_bucket(dist)]
        val = np.where((dist >= 0)[:, :, None], val, np.float32(NEGM))
        abs_t[u] = val.transpose(0, 2, 1)
    abw_t = np.empty((8, 128, 16, 128), np.float32)
    for u in range(8):
        dist = 128 * (u + j - 3) + q - k
        val = tab[_bucket(dist)]
        ok = (dist >= 0) & (dist < 512)
        val = np.where(ok[:, :, None], val, np.float32(NEGM))
        abw_t[u] = val.transpose(0, 2, 1)
    msb = np.empty((4, 128, 128), np.float32)
    for u in range(4):
        dist = 128 * (u + j - 3) + q - k
        msb[u] = np.where(dist >= 1, np.float32(0), np.float32(NEGM))
    cb = np.empty((NS, 2, 128, 16, 128), np.float32)
    selc = np.empty((NS, 128, 64), np.float32)
    for s in range(NS):
        t = (4 * s + j) * 128 + np.arange(128)
        n = np.arange(256)
        dist = t[None, :] - (16 * n[:, None] + 31)
        val = tab[_bucket(dist)]
        ok = (dist >= 0) & (n[:, None] < 255)
        val = np.where(ok[:, :, None], val, np.float32(NEGM)).transpose(0, 2, 1)
        cb[s] = val.reshape(2, 128, 16, 128)
        cur = t[:, None] // 64
        jb = np.arange(64)[None, :]
        valid = jb <= cur
        forced = (jb == 0) | ((cur - jb >= 0) & (cur - jb < 2))
        selc[s] = np.where(valid, np.where(forced, np.float32(1e4), np.float32(0)), np.float32(-1e30))
    return dict(abs=abs_t, abw=abw_t, msb=msb, cbias=cb, selc=selc)


def shared_consts():
    n_cmp = 255
    c_start = np.arange(n_cmp) * 16
    s_start = np.arange(64) * 64
    ov = np.maximum(np.minimum(c_start[:, None] + 32, s_start[None, :] + 64)
                    - np.maximum(c_start[:, None], s_start[None, :]), 0).astype(np.float32) / 32
    ovl = np.zeros((256, 64), np.float32)
    ovl[:255] = ov
    expand = np.zeros((NB, 64, 128), np.float32)
    for kb in range(NB):
        for kk in range(128):
            expand[kb, (128 * kb + kk) // 64, kk] = 1.0
    jj = np.arange(128)[:, None]
    ss = np.arange(128)[None, :]
    tri = (jj >= ss).astype(np.float32)
    iota = np.tile(np.arange(16, dtype=np.float32)[None, :], (128, 1))
    return dict(ovl=ovl.reshape(2, 128, 64), expand=expand, tri=tri, ident=np.eye(128, dtype=np.float32), iota16=iota)


def prep_inputs(inp):
    f = lambda a: np.ascontiguousarray(np.asarray(a, dtype=np.float32))
    x = f(inp['x'])
    shared = shared_consts()
    shared.update(
        w_in=f(inp['w_in'][0]), g_attn=f(inp['attn_norm_g'][0]), g_ffn=f(inp['ffn_norm_g'][0]),
        g_fin=f(inp['final_norm_g']),
        ckw1=f(np.stack([inp['cmp_k_w1'][0], inp['cmp_v_w1'][0]])),
        ckpeT=f(np.stack([np.asarray(inp['cmp_k_pe'][0]).T, np.asarray(inp['cmp_v_pe'][0]).T])),
        ckw2=f(np.stack([inp['cmp_k_w2'][0], inp['cmp_v_w2'][0]])),
        wbn=f(inp['w_branch_nsa'][0]), wbs=f(inp['w_branch_sb'][0]), wout=f(inp['w_out'][0]),
        wq=f(inp['peer_w_q'][0]),
        skT=f(np.asarray(inp['peer_sub_keys'][0]).reshape(16, 128, 128).transpose(0, 2, 1)),
        pu=f(inp['peer_u'][0]), pv=f(inp['peer_v'][0]),
    )
    per_j = [host_consts(j, inp['rel_bias_table']) for j in range(4)]
    in_maps = []
    for c in range(8):
        b, j = c // 4, c % 4
        own = np.concatenate([np.arange((4 * s + j) * 128, (4 * s + j + 1) * 128) for s in range(NS)])
        m = dict(shared)
        m.update(per_j[j])
        m['x_full'] = x[b]
        m['x_own'] = np.ascontiguousarray(x[b][own])
        in_maps.append(m)
    return in_maps


def own_index(j):
    return np.concatenate([np.arange((4 * s + j) * 128, (4 * s + j + 1) * 128) for s in range(NS)])


def kernel(**inputs):
    in_maps = prep_inputs(inputs)
    nc, dbg = build_program()
    res = run_bass_kernel_spmd(nc, in_maps, core_ids=list(range(8)))
    out = np.empty((2, T, D), np.float32)
    for c in range(8):
        b, j = c // 4, c % 4
        out[b, own_index(j)] = res.results[c]["out"]
    return out
```

```python
import math
from contextlib import ExitStack

import numpy as np
import concourse.bass as bass
import concourse.mybir as mybir
from concourse.bass_utils import run_bass_kernel_spmd

F32 = mybir.dt.float32
BF16 = mybir.dt.bfloat16
I32 = mybir.dt.int32
U32 = mybir.dt.uint32
ALU = mybir.AluOpType
AF = mybir.ActivationFunctionType
AX = mybir.AxisListType

T = 4096
D = 2048
NB = 32
NS = 8
OWN = 1024
NEGM = -30000.0
EPS = 1e-6
IN_COLS = 9776
C_QN, C_KC, C_VC, C_KS, C_VS, C_KW, C_VW, C_GBR, C_QB, C_KB, C_VB, C_GA, C_GB = (
    0, 1024, 1280, 1536, 1792, 2048, 2304, 2560, 2608, 3632, 4656, 5680, 7728)

DEBUG = {}


class Sched:
    NDS = 32
    NHW = 20

    def __init__(self, nc, stack):
        self.nc = nc
        self.eng = {'pe': nc.tensor, 'dve': nc.vector, 'act': nc.scalar,
                    'pool': nc.gpsimd, 'sp': nc.sync}
        self.sem = {k: stack.enter_context(nc.semaphore('s_' + k)) for k in self.eng}
        self.cnt = {k: 0 for k in self.eng}
        self.waited = {}
        self.dsem = [stack.enter_context(nc.semaphore('d%d' % i)) for i in range(self.NDS)]
        self.dcnt = [0] * self.NDS
        self.dnext = 0
        self.dnext_sw = 0
        self.bgsem = [stack.enter_context(nc.semaphore('bg%d' % i)) for i in range(64)]
        self.bgcnt = [0] * 64
        self.bgnext = 0
        self.last_w = {}
        self.readers = {}
        self.ninst = 0

    def _wait(self, e, tok):
        if tok is None:
            return
        if tok[0] == 'e':
            _, p, n = tok
            if p == 'pe' and e == 'pe':
                return
            key = (e, 'e', p)
            if self.waited.get(key, 0) >= n:
                return
            self.eng[e].wait_ge(self.sem[p], n)
            self.waited[key] = n
        else:
            _, slot, v = tok
            key = (e, 'd', slot)
            if self.waited.get(key, 0) >= v:
                return
            self.eng[e].wait_ge(self.dsem[slot], v)
            self.waited[key] = v

    def _deps(self, e, reads, writes):
        toks = []
        for k in reads:
            toks.append(self.last_w.get(k))
        for k in writes:
            toks.append(self.last_w.get(k))
            toks.extend(self.readers.get(k, []))
        for t in toks:
            self._wait(e, t)

    def _update(self, tok, reads, writes):
        for k in reads:
            self.readers.setdefault(k, []).append(tok)
        for k in writes:
            self.last_w[k] = tok
            self.readers[k] = []

    def op(self, e, fn, reads=(), writes=()):
        self._deps(e, reads, writes)
        ins = fn(self.eng[e])
        self.cnt[e] += 1
        ins.then_inc(self.sem[e], 1)
        tok = ('e', e, self.cnt[e])
        self._update(tok, reads, writes)
        self.ninst += 1
        return tok

    def dma(self, q, out, in_, reads=(), writes=(), fn=None, **kw):
        if q == 'pool':
            slot = self.NHW + self.dnext_sw
            self.dnext_sw = (self.dnext_sw + 1) % (self.NDS - self.NHW)
        else:
            slot = self.dnext
            self.dnext = (self.dnext + 1) % self.NHW
        if self.dcnt[slot] > 0:
            self._wait(q, ('d', slot, self.dcnt[slot]))
        self._deps(q, reads, writes)
        if fn is not None:
            ins = fn(self.eng[q])
        else:
            ins = self.eng[q].dma_start(out=out, in_=in_, **kw)
        ins.then_inc(self.dsem[slot], 16)
        self.dcnt[slot] += 16
        tok = ('d', slot, self.dcnt[slot])
        self._update(tok, reads, writes)
        self.ninst += 1
        return tok

    def bg_dma(self, q, out, in_):
        slot = self.bgnext
        self.bgnext = (self.bgnext + 1) % len(self.bgsem)
        if self.bgcnt[slot] > 0:
            key = (q, 'bg', slot)
            if self.waited.get(key, 0) < self.bgcnt[slot]:
                self.eng[q].wait_ge(self.bgsem[slot], self.bgcnt[slot])
                self.waited[key] = self.bgcnt[slot]
        self.eng[q].dma_start(out=out, in_=in_).then_inc(self.bgsem[slot], 16)
        self.bgcnt[slot] += 16
        self.ninst += 1

    def wait_bg(self, engines):
        for e in engines:
            for slot in range(len(self.bgsem)):
                if self.bgcnt[slot] > 0:
                    self.eng[e].wait_ge(self.bgsem[slot], self.bgcnt[slot])

    def barrier(self):
        for e in self.eng:
            for p in self.eng:
                if p != e and self.cnt[p] > 0:
                    self._wait(e, ('e', p, self.cnt[p]))
            for s in range(self.NDS):
                if self.dcnt[s] > 0:
                    self._wait(e, ('d', s, self.dcnt[s]))
        self.last_w = {}
        self.readers = {}

    def finish(self):
        e = 'sp'
        self.wait_bg([e])
        for p in self.eng:
            if p != e and self.cnt[p] > 0:
                self._wait(e, ('e', p, self.cnt[p]))
        for s in range(self.NDS):
            if self.dcnt[s] > 0:
                self._wait(e, ('d', s, self.dcnt[s]))


def _bucket(dist):
    dist = np.maximum(dist, 0)
    d_f = np.maximum(dist, 1).astype(np.float32)
    large = 16 + (np.log(d_f / np.float32(16)) / np.float32(math.log(1024 / 16)) * np.float32(16)).astype(np.int32)
    large = np.minimum(large, 31)
    return np.where(dist < 16, dist, large).astype(np.int64)


def _bucket_jax(dist):
    return _bucket(dist)


def build_program(stages=99):
    nc = bass.Bass("TRN2", target_bir_lowering=False)
    dt_in = lambda n, s, d=F32: nc.dram_tensor(n, list(s), d, kind="ExternalInput").ap()
    dt_sc = lambda n, s, d: nc.dram_tensor(n, list(s), d, kind="Internal").ap()
    x_full = dt_in("x_full", [T, D])
    x_own = dt_in("x_own", [OWN, D])
    w_in = dt_in("w_in", [D, IN_COLS])
    g_attn = dt_in("g_attn", [D])
    g_ffn = dt_in("g_ffn", [D])
    g_fin = dt_in("g_fin", [D])
    ident_d = dt_in("ident", [128, 128])
    tri_d = dt_in("tri", [128, 128])
    abs_d = dt_in("abs", [12, 128, 16, 128])
    abw_d = dt_in("abw", [8, 128, 16, 128])
    msb_d = dt_in("msb", [4, 128, 128])
    cbias_d = dt_in("cbias", [NS, 2, 128, 16, 128])
    selc_d = dt_in("selc", [NS, 128, 64])
    ovl_d = dt_in("ovl", [2, 128, 64])
    expand_d = dt_in("expand", [NB, 64, 128])
    ckw1_d = dt_in("ckw1", [2, 2048, 128])
    ckpe_d = dt_in("ckpeT", [2, 64, 32])
    ckw2_d = dt_in("ckw2", [2, 128, 64])
    wbn_d = dt_in("wbn", [1024, D])
    wbs_d = dt_in("wbs", [1024, D])
    wout_d = dt_in("wout", [D, D])
    wq_d = dt_in("wq", [D, D])
    skT_d = dt_in("skT", [16, 128, 128])
    pu_d = dt_in("pu", [16384, D])
    pv_d = dt_in("pv", [16384, D])
    iota_d = dt_in("iota16", [128, 16])
    out_d = nc.dram_tensor("out", [OWN, D], F32, kind="ExternalOutput").ap()
    dbg = {}

    def dbg_out(name, shape, dtype=F32):
        dbg[name] = nc.dram_tensor("dbg_" + name, list(shape), dtype, kind="ExternalOutput").ap()
        return dbg[name]

    kcvT = dt_sc("kcvT", [2, 4, 64, T], BF16)
    ksT = dt_sc("ksT", [4, 64, T], BF16)
    kwT = dt_sc("kwT", [4, 64, T], BF16)
    kbT = dt_sc("kbT", [16, 64, T], BF16)
    vsw = dt_sc("vsw", [T, 512], BF16)
    vb = dt_sc("vb", [T, 1024], BF16)
    qnT = dt_sc("qnT", [16, 64, OWN], BF16)
    qbT = dt_sc("qbT", [16, 64, OWN], BF16)
    gbr_s = dt_sc("gbr_s", [OWN, 48], F32)
    gab_s = dt_sc("gab_s", [OWN, 4096], F32)
    obr_s = dt_sc("obr_s", [3, OWN, 1024], F32)
    osb_s = dt_sc("osb_s", [OWN, 1024], F32)
    h1_s = dt_sc("h1_s", [OWN, D], F32)
    pu16 = dt_sc("pu16", [16384, D], BF16)
    pv16 = dt_sc("pv16", [16384, D], BF16)

    with ExitStack() as gst:
        S = Sched(nc, gst)
        gsb = lambda n, s, d: gst.enter_context(nc.sbuf_tensor(n, list(s), d))
        gps = lambda n, s, d: gst.enter_context(nc.psum_tensor(n, list(s), d))
        pA = [gps("pA%d" % i, [128, 512], F32) for i in range(2)]
        pC = [gps("pC%d" % i, [128, 512], F32) for i in range(2)]
        pO = [gps("pO%d" % i, [128, 512], F32) for i in range(2)]
        pT = gps("pT", [128, 1024], BF16)
        pM = gps("pM", [128, 512], F32)

        identf = gsb("identf", [128, 128], F32)
        identb = gsb("identb", [128, 128], BF16)
        trib = gsb("trib", [128, 128], BF16)
        onesb = gsb("onesb", [128, 128], BF16)

        S.dma('sp', identf[:], ident_d, writes=['identf'])
        S.op('dve', lambda e: e.tensor_copy(identb[:], identf[:]), reads=['identf'], writes=['identb'])
        S.dma('pool', trib[:], tri_d, writes=['trib'])
        S.op('dve', lambda e: e.memset(onesb[:], 1.0), writes=['onesb'])

        rr = {'ev': 0}
        cast_jobs = []
        for r0 in range(0, 16384, 512):
            cast_jobs.append((pu16[r0:r0 + 512, :], pu_d[r0:r0 + 512, :]))
            cast_jobs.append((pv16[r0:r0 + 512, :], pv_d[r0:r0 + 512, :]))

        def issue_casts(n):
            for _ in range(n):
                if cast_jobs:
                    o_, i_ = cast_jobs.pop(0)
                    S.bg_dma('pool', o_, i_)

        def evac(out_ap, in_ap, reads, writes, scale=None):
            rr['ev'] += 1
            if rr['ev'] % 2 == 0:
                if scale is None:
                    S.op('dve', lambda e: e.tensor_copy(out_ap, in_ap), reads=reads, writes=writes)
                else:
                    S.op('dve', lambda e: e.tensor_scalar(out_ap, in_ap, scale, None, ALU.mult), reads=reads, writes=writes)
            else:
                S.op('act', lambda e: e.activation(out_ap, in_ap, AF.Copy, scale=(1.0 if scale is None else scale)),
                     reads=reads, writes=writes)

        def make_gfull(st, g_ap, name):
            gt = st.enter_context(nc.sbuf_tensor(name + "_gt", [128, 16], F32))
            gfull = st.enter_context(nc.sbuf_tensor(name + "_gf", [128, 16, 128], BF16))
            S.dma('sp', gt[:], g_ap.rearrange("(c p) -> p c", p=128), writes=[name + 'gt'],
                  allow_slow_non_contiguous=True)
            S.op('dve', lambda e: e.memset(gfull[:], 1.0), writes=[name + 'gf'])
            for c in range(16):
                S.op('dve', lambda e, c=c: e.tensor_scalar(gfull[:, c, :], gfull[:, c, :], gt[:, c:c + 1], None, ALU.mult),
                     reads=[name + 'gt', name + 'gf'], writes=[name + 'gf'])
            return gfull, name + 'gf'

        def norm_rows(xt, xkey, ss, rstd, junk, key):
            S.op('act', lambda e: e.activation(junk[:], xt, AF.Square, accum_out=ss[:]),
                 reads=[xkey], writes=[key + 'junk', key + 'ss'])
            S.op('dve', lambda e: e.tensor_scalar(rstd[:], ss[:], 1.0 / D, EPS, ALU.mult, ALU.add),
                 reads=[key + 'ss'], writes=[key + 'rstd'])
            S.op('act', lambda e: e.activation(rstd[:], rstd[:], AF.Sqrt), reads=[key + 'rstd'], writes=[key + 'rstd'])
            S.op('dve', lambda e: e.reciprocal(rstd[:], rstd[:]), reads=[key + 'rstd'], writes=[key + 'rstd'])

        def transposeT(xs, xskey, gfull, gfkey, dst, dstkey, col0):
            for half in range(2):
                for cc in range(8):
                    c = half * 8 + cc
                    S.op('pe', lambda e, c=c, cc=cc: e.transpose(pT[:, cc * 128:(cc + 1) * 128], xs[:, c * 128:(c + 1) * 128], identb[:]),
                         reads=[xskey, 'identb'], writes=['pT'])
                S.op('dve', lambda e, half=half: e.tensor_tensor(
                    dst[:, half * 8:(half + 1) * 8, col0:col0 + 128],
                    pT[:].rearrange("p (c t) -> p c t", c=8),
                    gfull[:, half * 8:(half + 1) * 8, :], ALU.mult),
                    reads=['pT', gfkey], writes=[dstkey])

        if stages >= 1:
            with ExitStack() as st:
                sb = lambda n, s, d: st.enter_context(nc.sbuf_tensor(n, list(s), d))
                WF = sb("p1_WF", [128, 16, 2048], BF16)
                WV = sb("p1_WV", [128, 16, 1536], BF16)
                xt = [sb("p1_xt%d" % i, [128, D], F32) for i in range(2)]
                junk = sb("p1_junk", [128, D], BF16)
                ss = sb("p1_ss", [128, 1], F32)
                rstd = sb("p1_rstd", [128, 1], F32)
                xs = sb("p1_xs", [128, D], BF16)
                aT = [sb("p1_aT%d" % i, [128, 16, 512], BF16) for i in range(2)]
                stg = [sb("p1_stg%d" % i, [128, 512], BF16) for i in range(4)]
                gfull, gfkey = make_gfull(st, g_attn, "p1")
                wv = w_in.rearrange("(c p) n -> p c n", p=128)
                fm_cols = [(C_KC, 256), (C_VC, 256), (C_KS, 256), (C_KW, 256), (C_KB, 1024)]
                o = 0
                for gi_, (c0, n) in enumerate(fm_cols):
                    for c4 in range(0, 16, 4):
                        S.dma('pool', WF[:, c4:c4 + 4, o:o + n], wv[:, c4:c4 + 4, c0:c0 + n], writes=['WF%d' % gi_])
                    o += n
                tm_cols = [(C_VS, 256), (C_VW, 256), (C_VB, 1024)]
                o = 0
                for gi_, (c0, n) in enumerate(tm_cols):
                    for c4 in range(0, 16, 4):
                        S.dma('pool', WV[:, c4:c4 + 4, o:o + n], wv[:, c4:c4 + 4, c0:c0 + n], writes=['WV%d' % gi_])
                    o += n
                kcv2 = kcvT.rearrange("a g d t -> a (g d) t")
                ks2 = ksT.rearrange("g d t -> (g d) t")
                kw2 = kwT.rearrange("g d t -> (g d) t")
                kb2 = kbT.rearrange("h d t -> (h d) t")
                fm_dst = [(kcv2[0], 0), (kcv2[0], 128), (kcv2[1], 0), (kcv2[1], 128),
                          (ks2, 0), (ks2, 128), (kw2, 0), (kw2, 128)] + [(kb2, 128 * i) for i in range(8)]
                nstg = [0]
                npa = [0]
                fm_grp = [0, 0, 1, 1, 2, 2, 3, 3] + [4] * 8
                tm_grp = [['WV0', 'WV1'], ['WV2'], ['WV2']]

                def prepA(tt, ti):
                    r0 = tt * 512 + ti * 128
                    xi = (tt * 4 + ti) % 2
                    S.dma('sp', xt[xi][:], x_full[r0:r0 + 128, :], writes=['xt%d' % xi])
                    norm_rows(xt[xi][:], 'xt%d' % xi, ss, rstd, junk, 'p1')
                    S.op('dve', lambda e: e.tensor_scalar(xs[:], xt[xi][:], rstd[:, 0:1], None, ALU.mult),
                         reads=['xt%d' % xi, 'p1rstd'], writes=['xs'])

                def prepB(tt, ti):
                    transposeT(xs, 'xs', gfull, gfkey, aT[tt % 2], 'aT%d' % (tt % 2), ti * 128)

                def fm_group(tt, bi):
                    a = aT[tt % 2]
                    akey = 'aT%d' % (tt % 2)
                    pi = npa[0] % 2
                    npa[0] += 1
                    pa = pA[pi]
                    for c in range(16):
                        S.op('pe', lambda e, c=c: e.matmul(pa[:], WF[:, c, bi * 128:(bi + 1) * 128], a[:, c, :],
                                                          start=(c == 0), stop=(c == 15)),
                             reads=['WF%d' % fm_grp[bi], akey], writes=['pA%d' % pi])
                    sg = stg[nstg[0] % 4]
                    sk = 'stg%d' % (nstg[0] % 4)
                    nstg[0] += 1
                    evac(sg[:], pa[:], ['pA%d' % pi], [sk])
                    dst, row0 = fm_dst[bi]
                    S.dma('sp', dst[row0:row0 + 128, tt * 512:(tt + 1) * 512], sg[:], reads=[sk], writes=['sc_fm'])

                def tm_group(tt, ti, vbk):
                    a = aT[tt % 2]
                    akey = 'aT%d' % (tt % 2)
                    r0 = tt * 512 + ti * 128
                    pi = npa[0] % 2
                    npa[0] += 1
                    pa = pA[pi]
                    for c in range(16):
                        S.op('pe', lambda e, c=c: e.matmul(
                            pa[:], a[:, c, ti * 128:(ti + 1) * 128], WV[:, c, vbk * 512:(vbk + 1) * 512],
                            start=(c == 0), stop=(c == 15)),
                            reads=tm_grp[vbk] + [akey], writes=['pA%d' % pi])
                    sg = stg[nstg[0] % 4]
                    sk = 'stg%d' % (nstg[0] % 4)
                    nstg[0] += 1
                    evac(sg[:], pa[:], ['pA%d' % pi], [sk])
                    if vbk == 0:
                        S.dma('sp', vsw[r0:r0 + 128, :], sg[:], reads=[sk], writes=['sc_tm'])
                    else:
                        S.dma('sp', vb[r0:r0 + 128, (vbk - 1) * 512:vbk * 512], sg[:], reads=[sk], writes=['sc_tm'])

                for ti in range(4):
                    prepA(0, ti)
                    prepB(0, ti)
                NT = T // 512
                for tt in range(NT):
                    groups = [(lambda bi=bi: fm_group(tt, bi)) for bi in range(16)]
                    groups += [(lambda ti=ti, vbk=vbk: tm_group(tt, ti, vbk)) for ti in range(4) for vbk in range(3)]
                    posA = {0: 0, 7: 1, 14: 2, 21: 3}
                    posB = {4: 0, 11: 1, 18: 2, 25: 3}
                    for gi, grp in enumerate(groups):
                        if tt + 1 < NT and gi in posA:
                            prepA(tt + 1, posA[gi])
                        if tt + 1 < NT and gi in posB:
                            prepB(tt + 1, posB[gi])
                        grp()
            S.barrier()

        if stages >= 2:
            with ExitStack() as st:
                sb = lambda n, s, d: st.enter_context(nc.sbuf_tensor(n, list(s), d))
                xt = [sb("p2_xt%d" % i, [128, D], F32) for i in range(2)]
                junk = sb("p2_junk", [128, D], BF16)
                ss = sb("p2_ss", [128, 1], F32)
                rstd = sb("p2_rstd", [128, 1], F32)
                xs = sb("p2_xs", [128, D], BF16)
                aT = sb("p2_aT", [128, 16, OWN], BF16)
                Wb = [sb("p2_W%d" % i, [128, 16, 512], BF16) for i in range(2)]
                stg = [sb("p2_stg%d" % i, [128, 512], BF16) for i in range(4)]
                stgf = [sb("p2_stgf%d" % i, [128, 512], F32) for i in range(4)]
                gfull, gfkey = make_gfull(st, g_attn, "p2")
                wv = w_in.rearrange("(c p) n -> p c n", p=128)
                for ti in range(8):
                    xi = ti % 2
                    S.dma('sp', xt[xi][:], x_own[ti * 128:(ti + 1) * 128, :], writes=['xt%d' % xi])
                    norm_rows(xt[xi][:], 'xt%d' % xi, ss, rstd, junk, 'p2')
                    S.op('dve', lambda e, xi=xi: e.tensor_scalar(xs[:], xt[xi][:], rstd[:, 0:1], None, ALU.mult),
                         reads=['xt%d' % xi, 'p2rstd'], writes=['xs'])
                    transposeT(xs, 'xs', gfull, gfkey, aT, 'aTown', ti * 128)
                qn2 = qnT.rearrange("h d t -> (h d) t")
                qb2 = qbT.rearrange("h d t -> (h d) t")
                blocks = [('fm', C_QN + 512 * i, 512, qn2, 512 * i) for i in range(2)]
                blocks += [('fm', C_QB + 512 * i, 512, qb2, 512 * i) for i in range(2)]
                blocks += [('tm', C_GA + 512 * i, 512, gab_s, 512 * i) for i in range(8)]
                blocks += [('gbr', C_GBR, 48, gbr_s, 0)]
                nstg = 0
                for bi, (kind, c0, n, dst, d0) in enumerate(blocks):
                    W = Wb[bi % 2]
                    wk = 'W%d' % (bi % 2)
                    for c4 in range(0, 16, 4):
                        S.dma('pool', W[:, c4:c4 + 4, 0:n], wv[:, c4:c4 + 4, c0:c0 + n], writes=[wk])
                    if kind == 'fm':
                        for sub in range(4):
                            for th in range(2):
                                pa = pA[(sub * 2 + th) % 2]
                                pk = 'pA%d' % ((sub * 2 + th) % 2)
                                for c in range(16):
                                    S.op('pe', lambda e, c=c, sub=sub, th=th, pa=pa, W=W: e.matmul(
                                        pa[:], W[:, c, sub * 128:(sub + 1) * 128], aT[:, c, th * 512:(th + 1) * 512],
                                        start=(c == 0), stop=(c == 15)), reads=[wk, 'aTown'], writes=[pk])
                                sg = stg[nstg % 4]
                                sk = 'stg%d' % (nstg % 4)
                                nstg += 1
                                evac(sg[:], pa[:], [pk], [sk], scale=0.125)
                                S.dma('sp', dst[d0 + sub * 128:d0 + (sub + 1) * 128, th * 512:(th + 1) * 512], sg[:],
                                      reads=[sk], writes=['sc_q'])
                    else:
                        for ti in range(8):
                            pa = pA[ti % 2]
                            pk = 'pA%d' % (ti % 2)
                            for c in range(16):
                                S.op('pe', lambda e, c=c, ti=ti, pa=pa, W=W, n=n: e.matmul(
                                    pa[:, 0:n], aT[:, c, ti * 128:(ti + 1) * 128], W[:, c, 0:n],
                                    start=(c == 0), stop=(c == 15)), reads=[wk, 'aTown'], writes=[pk])
                            sg = stgf[nstg % 4]
                            sk = 'stgf%d' % (nstg % 4)
                            nstg += 1
                            S.op('act', lambda e, sg=sg, pa=pa, n=n: e.activation(sg[:, 0:n], pa[:, 0:n], AF.Sigmoid),
                                 reads=[pk], writes=[sk])
                            if kind == 'tm':
                                S.dma('sp', dst[ti * 128:(ti + 1) * 128, d0:d0 + n], sg[:, 0:n], reads=[sk], writes=['sc_g'])
                            else:
                                S.dma('sp', dst[ti * 128:(ti + 1) * 128, :], sg[:, 0:n], reads=[sk], writes=['sc_g'])
            S.barrier()

        if 'p12' in DEBUG:
            d1 = dbg_out("ksT", [4, 64, T], BF16)
            S.dma('sp', d1, ksT, reads=[], writes=[])
            d2 = dbg_out("vb", [T, 1024], BF16)
            S.dma('sp', d2, vb, reads=[], writes=[])
            d3 = dbg_out("qnT", [16, 64, OWN], BF16)
            S.dma('sp', d3, qnT, reads=[], writes=[])
            d4 = dbg_out("gbr", [OWN, 48], F32)
            S.dma('sp', d4, gbr_s, reads=[], writes=[])
            d5 = dbg_out("gab", [OWN, 4096], F32)
            S.dma('sp', d5, gab_s, reads=[], writes=[])

        def bc(ap, shape):
            return ap.to_broadcast(list(shape))

        with ExitStack() as mid_st:
            msb_ = lambda n, s, d: mid_st.enter_context(nc.sbuf_tensor(n, list(s), d))
            kcbT = msb_("kcbT", [64, 4, 256], BF16)
            vcb = msb_("vcb", [128, 4, 2, 129], BF16)
            selT = msb_("selT", [128, 4, NS, 128], BF16)
            S.op('dve', lambda e: e.memset(kcbT[:], 0.0), writes=['kcbT'])
            S.op('dve', lambda e: e.memset(vcb[:], 0.0), writes=['vcb'])
            if stages >= 3:
                with ExitStack() as st:
                    sb = lambda n, s, d: st.enter_context(nc.sbuf_tensor(n, list(s), d))
                    kin = [sb("p3_kin%d" % i, [64, T], BF16) for i in range(2)]
                    W1 = sb("p3_W1", [64, 2, 32, 128], BF16)
                    peT = sb("p3_peT", [64, 2, 32], BF16)
                    W2 = sb("p3_W2", [128, 2, 64], BF16)
                    b1 = sb("p3_b1", [128, 2], F32)
                    uu = sb("p3_u", [128, 256], F32)
                    u2 = sb("p3_u2", [128, 256], F32)
                    sg = sb("p3_sg", [128, 256], F32)
                    gl = sb("p3_gl", [128, 256], BF16)
                    for kv in range(2):
                        S.dma('pool', W1[:, kv], ckw1_d[kv].rearrange("(l d) h -> d l h", d=64), writes=['W1'])
                        S.dma('pool', peT[:, kv, :], ckpe_d[kv], writes=['peT'])
                        S.dma('pool', W2[:, kv, :], ckw2_d[kv], writes=['W2'])
                    for g in range(4):
                        for c in range(2):
                            S.dma('pool', vcb[:, g, c, 65:129], ovl_d[c], reads=[], writes=['vcb'])
                    S.op('dve', lambda e: e.memset(vcb[:, :, :, 64:65], 1.0), writes=['vcb'])
                    for kv in range(2):
                        for l in range(32):
                            S.op('pe', lambda e, kv=kv, l=l: e.matmul(pM[:, kv:kv + 1], W1[:, kv, l, :], peT[:, kv, l:l + 1],
                                                                  start=(l == 0), stop=(l == 31)),
                                 reads=['W1', 'peT'], writes=['pM'])
                    S.op('dve', lambda e: e.tensor_copy(b1[:], pM[:, 0:2]), reads=['pM'], writes=['b1'])
                    gl2 = [gl, sb("p3_gl2", [128, 256], BF16)]

                    def head3(it):
                        kv, g = it // 4, it % 4
                        ki = kin[it % 2]
                        kk = 'kin%d' % (it % 2)
                        pa = pA[it % 2]
                        pk = 'pA%d' % (it % 2)
                        gl_ = gl2[it % 2]
                        glk = 'gl%d' % (it % 2)
                        S.dma('sp', ki[:], kcvT[kv, g], writes=[kk])
                        for l in range(32):
                            S.op('pe', lambda e, l=l: e.matmul(
                                pa[:, 0:255], W1[:, kv, l, :], ki[:, l:l + 4065:16], start=(l == 0), stop=(l == 31)),
                                reads=['W1', kk], writes=[pk])
                        S.op('act', lambda e: e.activation(uu[:, 0:255], pa[:, 0:255], AF.Identity, bias=b1[:, kv:kv + 1]),
                             reads=[pk, 'b1'], writes=['uu'])
                        S.op('dve', lambda e: e.tensor_tensor(u2[:, 0:255], uu[:, 0:255], uu[:, 0:255], ALU.mult), reads=['uu'], writes=['u2'])
                        S.op('dve', lambda e: e.tensor_scalar(u2[:, 0:255], u2[:, 0:255], 0.044715, 1.0, ALU.mult, ALU.add), reads=['u2'], writes=['u2'])
                        S.op('dve', lambda e: e.tensor_tensor(u2[:, 0:255], u2[:, 0:255], uu[:, 0:255], ALU.mult), reads=['u2', 'uu'], writes=['u2'])
                        S.op('act', lambda e: e.activation(sg[:, 0:255], u2[:, 0:255], AF.Sigmoid, scale=1.5957691216057308), reads=['u2'], writes=['sg'])
                        S.op('dve', lambda e: e.tensor_tensor(gl_[:, 0:255], uu[:, 0:255], sg[:, 0:255], ALU.mult), reads=['uu', 'sg'], writes=[glk])

                    def tail3(it):
                        kv, g = it // 4, it % 4
                        gl_ = gl2[it % 2]
                        glk = 'gl%d' % (it % 2)
                        if kv == 0:
                            S.op('pe', lambda e: e.matmul(pC[0][0:64, 0:255], W2[:, 0, :], gl_[:, 0:255], start=True, stop=True),
                                 reads=['W2', glk], writes=['pC0'])
                            S.op('dve', lambda e: e.tensor_copy(kcbT[:, g, 0:255], pC[0][0:64, 0:255]), reads=['pC0'], writes=['kcbT'])
                        else:
                            for c in range(2):
                                rows = 128 if c == 0 else 127
                                S.op('pe', lambda e, c=c, rows=rows: e.matmul(pC[c][0:rows, 0:64], gl_[:, c * 128:c * 128 + rows], W2[:, 1, :],
                                                                              start=True, stop=True),
                                     reads=['W2', glk], writes=['pC%d' % c])
                                S.op('dve', lambda e, c=c, rows=rows: e.tensor_copy(vcb[0:rows, g, c, 0:64], pC[c][0:rows, 0:64]),
                                     reads=['pC%d' % c], writes=['vcb'])

                    for it in range(8):
                        head3(it)
                        if it >= 1:
                            tail3(it - 1)
                    tail3(7)
                S.barrier()

            if stages >= 4:
                with ExitStack() as st:
                    sb = lambda n, s, d: st.enter_context(nc.sbuf_tensor(n, list(s), d))
                    qg = [sb("p4_qg%d" % i, [64, 4, OWN], BF16) for i in range(2)]
                    cbt = [sb("p4_cb%d" % i, [128, 2, 4, 128], BF16) for i in range(2)]
                    E = [sb("p4_E%d" % i, [128, 512], BF16) for i in range(4)]
                    selc = sb("p4_selc", [128, NS, 64], F32)
                    rec = sb("p4_rec", [128, 4], F32)
                    oc = [sb("p4_oc%d" % i, [128, 4, 64], F32) for i in range(2)]
                    imp = sb("p4_imp", [128, 64], F32)
                    score = sb("p4_score", [128, 64], F32)
                    sc2 = sb("p4_sc2", [128, 64], F32)
                    m8a = sb("p4_m8a", [128, 8], F32)
                    m8b = sb("p4_m8b", [128, 8], F32)
                    selb = sb("p4_selb", [128, 128], F32)
                    S.op('dve', lambda e: e.memset(selb[:], 0.0), writes=['selb'])
                    S.dma('sp', selc[:], selc_d.rearrange("s q j -> q s j"), writes=['selc'])
                    it = 0
                    for g in range(4):
                        q_ = qg[g % 2]
                        qk = 'qg%d' % (g % 2)
                        S.dma('sp', q_[:], qnT[4 * g:4 * g + 4].rearrange("h d t -> d h t"), writes=[qk])
                        for s in range(NS):
                            cb = cbt[it % 2]
                            ck = 'cb%d' % (it % 2)
                            o_ = oc[it % 2]
                            ok = 'oc%d' % (it % 2)
                            it += 1
                            S.dma('pool', cb[:], cbias_d[s, :, :, 4 * g:4 * g + 4, :].rearrange("c n h q -> n c h q"), writes=[ck])
                            for c in range(2):
                                pa = pA[c]
                                pk = 'pA%d' % c
                                S.op('pe', lambda e, c=c, pa=pa, g=g, s=s, q_=q_: e.matmul(
                                    pa[:], kcbT[:, g, c * 128:(c + 1) * 128], q_[:, :, s * 128:(s + 1) * 128], start=True, stop=False),
                                    reads=['kcbT', qk], writes=[pk])
                                S.op('pe', lambda e, c=c, pa=pa, cb=cb: e.matmul(pa[:], identb[:], cb[:, c], start=False, stop=True),
                                     reads=['identb', ck], writes=[pk])
                                ei = (it % 2) * 2 + c
                                S.op('act', lambda e, pa=pa, ei=ei: e.activation(E[ei][:], pa[:], AF.Exp), reads=[pk], writes=['E%d' % ei])
                            for h in range(4):
                                po = pO[h // 2]
                                col = (h % 2) * 129
                                for c in range(2):
                                    ei = (it % 2) * 2 + c
                                    S.op('pe', lambda e, po=po, col=col, ei=ei, h=h, g=g, c=c: e.matmul(
                                        po[:, col:col + 129], E[ei][:, h * 128:(h + 1) * 128], vcb[:, g, c, :], start=(c == 0), stop=(c == 1)),
                                        reads=['E%d' % ei, 'vcb'], writes=['pO%d' % (h // 2)])
                            for h in range(4):
                                po = pO[h // 2]
                                col = (h % 2) * 129
                                S.op('dve', lambda e, po=po, col=col, h=h: e.tensor_scalar(rec[:, h:h + 1], po[:, col + 64:col + 65], 1e-30, None, ALU.max),
                                     reads=['pO%d' % (h // 2)], writes=['rec'])
                            S.op('dve', lambda e: e.reciprocal(rec[:], rec[:]), reads=['rec'], writes=['rec'])
                            for h in range(4):
                                po = pO[h // 2]
                                col = (h % 2) * 129
                                S.op('dve', lambda e, po=po, col=col, h=h, o_=o_: e.tensor_scalar(o_[:, h, :], po[:, col:col + 64], rec[:, h:h + 1], None, ALU.mult),
                                     reads=['pO%d' % (h // 2), 'rec'], writes=[ok])
                                if h == 0:
                                    S.op('dve', lambda e, po=po, col=col, h=h: e.tensor_scalar(imp[:], po[:, col + 65:col + 129], rec[:, h:h + 1], None, ALU.mult),
                                         reads=['pO%d' % (h // 2), 'rec'], writes=['imp'])
                                else:
                                    S.op('dve', lambda e, po=po, col=col, h=h: e.scalar_tensor_tensor(
                                        imp[:], po[:, col + 65:col + 129], rec[:, h:h + 1], imp[:], ALU.mult, ALU.add),
                                        reads=['pO%d' % (h // 2), 'rec', 'imp'], writes=['imp'])
                            S.dma('sp', obr_s[0, s * 128:(s + 1) * 128, g * 256:(g + 1) * 256], o_[:].rearrange("p h d -> p (h d)"), reads=[ok], writes=['sc_o'])
                            S.op('dve', lambda e, s=s: e.tensor_tensor(score[:], imp[:], selc[:, s, :], ALU.add), reads=['imp', 'selc'], writes=['score'])
                            S.op('dve', lambda e: e.max(m8a[:], score[:]), reads=['score'], writes=['m8a'])
                            S.op('dve', lambda e: e.match_replace(sc2[:], m8a[:], score[:], -3.0e38), reads=['score', 'm8a'], writes=['sc2'])
                            S.op('dve', lambda e: e.max(m8b[:], sc2[:]), reads=['sc2'], writes=['m8b'])
                            S.op('dve', lambda e: e.tensor_scalar(selb[:, 64:128], score[:], m8b[:, 7:8], -NEGM, ALU.is_ge, ALU.mult),
                                 reads=['score', 'm8b'], writes=['selb'])
                            S.op('dve', lambda e: e.tensor_scalar(selb[:, 64:128], selb[:, 64:128], NEGM, None, ALU.add), reads=['selb'], writes=['selb'])
                            S.op('pe', lambda e: e.transpose(pM[:, 0:128], selb[:], identf[:]), reads=['selb', 'identf'], writes=['pM'])
                            S.op('dve', lambda e, g=g, s=s: e.tensor_copy(selT[64:128, g, s, :], pM[64:128, 0:128]), reads=['pM'], writes=['selT'])
                S.barrier()

            def attn_phase(sel):
                with ExitStack() as st:
                    sb = lambda n, s, d: st.enter_context(nc.sbuf_tensor(n, list(s), d))
                    nm = "p5" if sel else "p6"
                    nab = 12 if sel else 8
                    AB = sb(nm + "_AB", [128, nab, 16, 128], BF16)
                    ab_src = abs_d if sel else abw_d
                    for u in range(nab):
                        S.dma('pool', AB[:, u], ab_src[u], writes=['AB'])
                    KR = 128 if sel else 64
                    kT = [sb(nm + "_kT%d" % i, [KR, T], BF16) for i in range(2)]
                    vt = [sb(nm + "_vt%d" % i, [128, NB, 65], BF16) for i in range(2)]
                    qg = [sb(nm + "_qg%d" % i, [KR, 4, OWN], BF16) for i in range(2)]
                    if sel:
                        for i in range(2):
                            S.dma('pool', kT[i][64:128, :].rearrange("j (b k) -> j b k", k=128), expand_d.rearrange("b j k -> j b k"),
                                  writes=['kT%d' % i])
                    E = [sb(nm + "_E%d" % i, [128, 512], BF16) for i in range(3)]
                    rec = sb(nm + "_rec", [128, 4], F32)
                    ot = [sb(nm + "_ot%d" % i, [128, 4, 64], F32) for i in range(4)]
                    ksrc = ksT if sel else kwT
                    voff = 0 if sel else 256
                    it = 0
                    ne = 0
                    def load_group(g):
                        k_ = kT[g % 2]
                        kk = 'kT%d' % (g % 2)
                        v_ = vt[g % 2]
                        vk = 'vt%d' % (g % 2)
                        q_ = qg[g % 2]
                        qk = 'qg%d' % (g % 2)
                        S.dma('sp', k_[0:64, :], ksrc[g], writes=[kk])
                        S.op('dve', lambda e: e.memset(v_[:, :, 64:65], 1.0), writes=[vk])
                        S.dma('sp', v_[:, :, 0:64], vsw[:, voff + g * 64:voff + (g + 1) * 64].rearrange("(b p) d -> p b d", p=128), writes=[vk])
                        S.dma('sp', q_[0:64], qnT[4 * g:4 * g + 4].rearrange("h d t -> d h t"), writes=[qk])
                        if sel:
                            for h in range(4):
                                S.op('pool', lambda e, h=h: e.tensor_copy(q_[64:128, h, :], selT[64:128, g].rearrange("j s q -> j (s q)")),
                                     reads=['selT'], writes=[qk])

                    load_group(0)
                    for g in range(4):
                        k_ = kT[g % 2]
                        kk = 'kT%d' % (g % 2)
                        v_ = vt[g % 2]
                        vk = 'vt%d' % (g % 2)
                        q_ = qg[g % 2]
                        qk = 'qg%d' % (g % 2)
                        if g + 1 < 4:
                            load_group(g + 1)
                        poh = [pO[0], pO[1], pC[0], pC[1]]
                        pokh = ['pO0', 'pO1', 'pC0', 'pC1']
                        items = []
                        for s in range(NS):
                            nu = 4 * s + 4 if sel else min(8, 4 * s + 4)
                            for u in range(nu):
                                items.append((s, u, nu))
                        srs = {}

                        def stageA(s, u, nu, idx):
                            if u == 0 and ((sel and s % 4 != 3) or ((not sel) and s % 4 == 0)):
                                issue_casts(1)
                            kb = 4 * s + 3 - u
                            pa = pA[idx % 2]
                            pk = 'pA%d' % (idx % 2)
                            S.op('pe', lambda e: e.matmul(pa[:], k_[:, kb * 128:(kb + 1) * 128], q_[:, :, s * 128:(s + 1) * 128], start=True, stop=False),
                                 reads=[kk, qk], writes=[pk])
                            S.op('pe', lambda e: e.matmul(pa[:], identb[:], AB[:, min(u, nab - 1), 4 * g:4 * g + 4, :], start=False, stop=True),
                                 reads=['identb', 'AB'], writes=[pk])
                            ei = idx % 3
                            S.op('act', lambda e: e.activation(E[ei][:], pa[:], AF.Exp), reads=[pk], writes=['E%d' % ei])

                        def stageC(s, u, nu, idx):
                            kb = 4 * s + 3 - u
                            ei = idx % 3
                            for h in range(4):
                                S.op('pe', lambda e, h=h: e.matmul(poh[h][:, 0:65], E[ei][:, h * 128:(h + 1) * 128], v_[:, kb, :],
                                                                  start=(u == 0), stop=(u == nu - 1)),
                                     reads=['E%d' % ei, vk], writes=[pokh[h]])
                            if u == nu - 1:
                                o_ = ot[s % 4]
                                ok = 'ot%d' % (s % 4)
                                for h in range(4):
                                    S.op('dve', lambda e, h=h: e.tensor_scalar(rec[:, h:h + 1], poh[h][:, 64:65], 1e-30, None, ALU.max),
                                         reads=[pokh[h]], writes=['rec'])
                                S.op('dve', lambda e: e.reciprocal(rec[:], rec[:]), reads=['rec'], writes=['rec'])
                                for h in range(4):
                                    S.op('dve', lambda e, h=h: e.tensor_scalar(o_[:, h, :], poh[h][:, 0:64], rec[:, h:h + 1], None, ALU.mult),
                                         reads=[pokh[h], 'rec'], writes=[ok])
                                S.dma('sp', obr_s[1 if sel else 2, s * 128:(s + 1) * 128, g * 256:(g + 1) * 256], o_[:].rearrange("p h d -> p (h d)"),
                                      reads=[ok], writes=['sc_o'])

                        for idx, (s, u, nu) in enumerate(items):
                            stageA(s, u, nu, idx)
                            if idx >= 1:
                                stageC(*items[idx - 1], idx - 1)
                        stageC(*items[-1], len(items) - 1)
                S.barrier()

            if stages >= 5:
                attn_phase(True)
            if stages >= 6:
                attn_phase(False)

            if stages >= 7:
                with ExitStack() as st:
                    sb = lambda n, s, d: st.enter_context(nc.sbuf_tensor(n, list(s), d))
                    kq = [sb("p7_kq%d" % i, [128, 2, T], BF16) for i in range(2)]
                    vq = [sb("p7_vq%d" % i, [128, NB, 256], BF16) for i in range(2)]
                    qq = [sb("p7_qq%d" % i, [128, 2, NS, 256], BF16) for i in range(2)]
                    msb4 = sb("p7_msb4", [128, 4, 4, 128], BF16)
                    ef = [sb("p7_ef%d" % i, [128, 512], F32) for i in range(3)]
                    spt = [sb("p7_sp%d" % i, [128, 512], BF16) for i in range(3)]
                    acc = sb("p7_acc", [128, 512], BF16)
                    wt = [sb("p7_w%d" % i, [128, 512], BF16) for i in range(2)]
                    at = [sb("p7_a%d" % i, [128, 512], BF16) for i in range(2)]
                    osbt = [sb("p7_o%d" % i, [128, 256], F32) for i in range(4)]
                    for h in range(4):
                        S.dma('pool', msb4[:, :, h, :], msb_d.rearrange("u k q -> k u q"), writes=['msb4'])
                    it = 0
                    n7 = 0
                    def load_quad(hq):
                        k_ = kq[hq % 2]
                        kk = 'kq%d' % (hq % 2)
                        v_ = vq[hq % 2]
                        vk = 'vq%d' % (hq % 2)
                        q_ = qq[hq % 2]
                        qk = 'qq%d' % (hq % 2)
                        S.dma('sp', k_[:], kbT[4 * hq:4 * hq + 4].rearrange("(p hh) d t -> (hh d) p t", hh=2), writes=[kk])
                        S.dma('sp', v_[:], vb[:, hq * 256:(hq + 1) * 256].rearrange("(b p) d -> p b d", p=128), writes=[vk])
                        S.op('dve', lambda e: e.memset(q_[:], 0.0), writes=[qk])
                        for p_ in range(2):
                            for hh in range(2):
                                S.dma('sp', q_[hh * 64:(hh + 1) * 64, p_, :, hh * 128:(hh + 1) * 128],
                                      qbT[4 * hq + 2 * p_ + hh].rearrange("d (s q) -> d s q", q=128), writes=[qk])

                    load_quad(0)
                    for hq in range(4):
                        k_ = kq[hq % 2]
                        kk = 'kq%d' % (hq % 2)
                        v_ = vq[hq % 2]
                        vk = 'vq%d' % (hq % 2)
                        q_ = qq[hq % 2]
                        qk = 'qq%d' % (hq % 2)
                        if hq + 1 < 4:
                            load_quad(hq + 1)
                        poh = [pO[0], pO[1], pC[1], pM]
                        pokh = ['pO0', 'pO1', 'pC1', 'pM']
                        pc = pC[0]
                        pck = 'pC0'
                        items = []
                        for s in range(NS):
                            for u in range(4 * s + 4):
                                items.append((s, u, 4 * s + 4))

                        def sbA(s, u, nu, idx):
                            if u == 0:
                                issue_casts(1)
                            kb = 4 * s + 3 - u
                            pa = pA[idx % 2]
                            pk = 'pA%d' % (idx % 2)
                            i3 = idx % 3
                            for p_ in range(2):
                                S.op('pe', lambda e, p_=p_: e.matmul(
                                    pa[:, p_ * 256:(p_ + 1) * 256], k_[:, p_, kb * 128:(kb + 1) * 128], q_[:, p_, s, :],
                                    start=True, stop=(u >= 4)), reads=[kk, qk], writes=[pk])
                                if u < 4:
                                    S.op('pe', lambda e, p_=p_: e.matmul(pa[:, p_ * 256:(p_ + 1) * 256], identb[:], msb4[:, u, 2 * p_:2 * p_ + 2, :],
                                                                       start=False, stop=True),
                                         reads=['identb', 'msb4'], writes=[pk])
                            S.op('act', lambda e: e.activation(ef[i3][:], pa[:], AF.Exp), reads=[pk], writes=['ef%d' % i3])
                            S.op('act', lambda e: e.activation(spt[i3][:], ef[i3][:], AF.Ln, bias=1.0), reads=['ef%d' % i3], writes=['sp%d' % i3])

                        def sbB(s, u, nu, idx):
                            i3 = idx % 3
                            i2 = idx % 2
                            S.op('pe', lambda e: e.matmul(pc[:], trib[:], spt[i3][:], start=True, stop=(u == 0)),
                                 reads=['trib', 'sp%d' % i3], writes=[pck])
                            if u > 0:
                                S.op('pe', lambda e: e.matmul(pc[:], onesb[:], acc[:], start=False, stop=True),
                                     reads=['onesb', 'acc'], writes=[pck])
                            if u < nu - 1:
                                if u == 0:
                                    S.op('pool', lambda e: e.tensor_copy(acc[:], spt[i3][:]), reads=['sp%d' % i3], writes=['acc'])
                                else:
                                    S.op('pool', lambda e: e.tensor_tensor(acc[:], acc[:], spt[i3][:], ALU.add),
                                         reads=['sp%d' % i3, 'acc'], writes=['acc'])
                            S.op('act', lambda e: e.activation(wt[i2][:], pc[:], AF.Exp, scale=-1.0), reads=[pck], writes=['w%d' % i2])
                            S.op('dve', lambda e: e.tensor_tensor(at[i2][:], ef[i3][:], wt[i2][:], ALU.mult),
                                 reads=['ef%d' % i3, 'w%d' % i2], writes=['a%d' % i2])

                        def sbC(s, u, nu, idx):
                            kb = 4 * s + 3 - u
                            i2 = idx % 2
                            for h in range(4):
                                S.op('pe', lambda e, h=h: e.matmul(
                                    poh[h][:, 0:64], at[i2][:, h * 128:(h + 1) * 128], v_[:, kb, h * 64:(h + 1) * 64],
                                    start=(u == 0), stop=(u == nu - 1)), reads=['a%d' % i2, vk], writes=[pokh[h]])
                            if u == nu - 1:
                                ob = osbt[s % 4]
                                obk = 'osbt%d' % (s % 4)
                                for h in range(4):
                                    evac(ob[:, h * 64:(h + 1) * 64], poh[h][:, 0:64], [pokh[h]], [obk])
                                S.dma('sp', osb_s[s * 128:(s + 1) * 128, hq * 256:(hq + 1) * 256], ob[:], reads=[obk], writes=['sc_osb'])

                        n_it = len(items)
                        for idx in range(n_it + 2):
                            if idx < n_it:
                                sbA(*items[idx], idx)
                            if 1 <= idx <= n_it:
                                sbB(*items[idx - 1], idx - 1)
                            if idx >= 2:
                                sbC(*items[idx - 2], idx - 2)
                S.barrier()

            if 'p37' in DEBUG:
                S.dma('sp', dbg_out("obr", [3, OWN, 1024]), obr_s)
                S.dma('sp', dbg_out("osb", [OWN, 1024]), osb_s)
                dk = dbg_out("kcbT", [64, 4, 256], BF16)
                S.dma('sp', dk, kcbT[:])
                dv = dbg_out("vcb", [128, 4, 2, 129], BF16)
                S.dma('sp', dv, vcb[:])
                dsel = dbg_out("selT", [64, 4, NS, 128], BF16)
                S.dma('sp', dsel, selT[64:128])

        if stages >= 8:
            with ExitStack() as st8:
                mTall = st8.enter_context(nc.sbuf_tensor("p8_mTall", [128, 16, OWN], BF16))
                with ExitStack() as st:
                    sb = lambda n, s, d: st.enter_context(nc.sbuf_tensor(n, list(s), d))
                    Wbn = sb("p8_Wbn", [128, 8, D], BF16)
                    Wbs = sb("p8_Wbs", [128, 8, D], BF16)
                    ob2 = [[sb("p8_ob%d_%d" % (i, j), [128, 16, 64], F32) for i in range(3)] for j in range(2)]
                    osb_t2 = [sb("p8_osb%d" % j, [128, 1024], F32) for j in range(2)]
                    gbrt2 = [sb("p8_gbr%d" % j, [128, 48], F32) for j in range(2)]
                    onsa = sb("p8_onsa", [128, 16, 64], F32)
                    tmp = sb("p8_tmp", [128, 16, 64], F32)
                    onb = sb("p8_onb", [128, 1024], BF16)
                    osb16 = sb("p8_osb16", [128, 1024], BF16)
                    onT = sb("p8_onT", [128, 8, 128], BF16)
                    osT = sb("p8_osT", [128, 8, 128], BF16)
                    gab2 = [sb("p8_gab%d" % j, [128, 4096], F32) for j in range(2)]
                    m1 = sb("p8_m1", [128, 512], F32)
                    m2 = sb("p8_m2", [128, 512], F32)
                    mb = sb("p8_mb", [128, D], BF16)
                    wbn_v = wbn_d.rearrange("(c p) n -> p c n", p=128)
                    wbs_v = wbs_d.rearrange("(c p) n -> p c n", p=128)
                    for c4 in range(0, 8, 4):
                        S.dma('pool', Wbn[:, c4:c4 + 4, :], wbn_v[:, c4:c4 + 4, :], writes=['Wbn'])
                        S.dma('pool', Wbs[:, c4:c4 + 4, :], wbs_v[:, c4:c4 + 4, :], writes=['Wbs'])
                    for ti in range(8):
                        rows = slice(ti * 128, (ti + 1) * 128)
                        jb_ = ti % 2
                        ob, osb_t, gbrt, gab = ob2[jb_], osb_t2[jb_], gbrt2[jb_], gab2[jb_]
                        kx = '_%d' % jb_
                        for i in range(3):
                            S.dma('sp', ob[i][:].rearrange("p h d -> p (h d)"), obr_s[i, rows, :], writes=['ob%d' % i + kx])
                        S.dma('sp', osb_t[:], osb_s[rows, :], writes=['osb_t' + kx])
                        S.dma('sp', gbrt[:], gbr_s[rows, :], writes=['gbrt' + kx])
                        S.dma('sp', gab[:], gab_s[rows, :], writes=['gab' + kx])
                        gv = lambda i: bc(gbrt[:, i * 16:(i + 1) * 16].unsqueeze(2), [128, 16, 64])
                        S.op('dve', lambda e: e.tensor_tensor(onsa[:], ob[0][:], gv(0), ALU.mult), reads=['ob0' + kx, 'gbrt' + kx], writes=['onsa'])
                        S.op('dve', lambda e: e.tensor_tensor(tmp[:], ob[1][:], gv(1), ALU.mult), reads=['ob1' + kx, 'gbrt' + kx], writes=['tmp'])
                        S.op('dve', lambda e: e.tensor_tensor(onsa[:], onsa[:], tmp[:], ALU.add), reads=['onsa', 'tmp'], writes=['onsa'])
                        S.op('dve', lambda e: e.tensor_tensor(tmp[:], ob[2][:], gv(2), ALU.mult), reads=['ob2' + kx, 'gbrt' + kx], writes=['tmp'])
                        S.op('dve', lambda e: e.tensor_tensor(onb[:].rearrange("p (h d) -> p h d", d=64), onsa[:], tmp[:], ALU.add),
                             reads=['onsa', 'tmp'], writes=['onb'])
                        S.op('act', lambda e: e.activation(osb16[:], osb_t[:], AF.Copy), reads=['osb_t' + kx], writes=['osb16'])
                        for (src, sk, dstT, dk) in ((onb, 'onb', onT, 'onT'), (osb16, 'osb16', osT, 'osT')):
                            for cc in range(8):
                                S.op('pe', lambda e, cc=cc, src=src: e.transpose(pT[:, cc * 128:(cc + 1) * 128], src[:, cc * 128:(cc + 1) * 128], identb[:]),
                                     reads=[sk, 'identb'], writes=['pT'])
                            S.op('dve', lambda e, dstT=dstT: e.tensor_copy(dstT[:], pT[:].rearrange("p (c t) -> p c t", c=8)),
                                 reads=['pT'], writes=[dk])
                        for cb in range(4):
                            cs = slice(cb * 512, (cb + 1) * 512)
                            for c in range(8):
                                S.op('pe', lambda e, c=c, cs=cs: e.matmul(pA[0][:], onT[:, c, :], Wbn[:, c, cs], start=(c == 0), stop=(c == 7)),
                                     reads=['onT', 'Wbn'], writes=['pA0'])
                            for c in range(8):
                                S.op('pe', lambda e, c=c, cs=cs: e.matmul(pA[1][:], osT[:, c, :], Wbs[:, c, cs], start=(c == 0), stop=(c == 7)),
                                     reads=['osT', 'Wbs'], writes=['pA1'])
                            S.op('dve', lambda e, cs=cs: e.tensor_tensor(m1[:], pA[0][:], gab[:, cs], ALU.mult), reads=['pA0', 'gab' + kx], writes=['m1'])
                            S.op('dve', lambda e, cb=cb: e.tensor_tensor(m2[:], pA[1][:], gab[:, 2048 + cb * 512:2048 + (cb + 1) * 512], ALU.mult),
                                 reads=['pA1', 'gab' + kx], writes=['m2'])
                            S.op('dve', lambda e, cs=cs: e.tensor_tensor(mb[:, cs], m1[:], m2[:], ALU.add), reads=['m1', 'm2'], writes=['mb'])
                        for half in range(2):
                            for cc in range(8):
                                c = half * 8 + cc
                                S.op('pe', lambda e, cc=cc, c=c: e.transpose(pT[:, cc * 128:(cc + 1) * 128], mb[:, c * 128:(c + 1) * 128], identb[:]),
                                     reads=['mb', 'identb'], writes=['pT'])
                            S.op('dve', lambda e, half=half, ti=ti: e.tensor_copy(
                                mTall[:, half * 8:(half + 1) * 8, ti * 128:(ti + 1) * 128], pT[:].rearrange("p (c t) -> p c t", c=8)),
                                reads=['pT'], writes=['mTall'])
                S.barrier()
                with ExitStack() as st:
                    sb = lambda n, s, d: st.enter_context(nc.sbuf_tensor(n, list(s), d))
                    Wout = sb("p8_Wout", [128, 16, D], BF16)
                    xo = [sb("p8_xo%d" % i, [128, D], F32) for i in range(2)]
                    h1t = [sb("p8_h1t%d" % i, [128, D], F32) for i in range(2)]
                    wo_v = wout_d.rearrange("(c p) n -> p c n", p=128)
                    for c4 in range(0, 16, 4):
                        S.dma('pool', Wout[:, c4:c4 + 4, :], wo_v[:, c4:c4 + 4, :], writes=['Wout'])
                    for ti in range(8):
                        rows = slice(ti * 128, (ti + 1) * 128)
                        x_ = xo[ti % 2]
                        xk = 'xo%d' % (ti % 2)
                        h_ = h1t[ti % 2]
                        hk = 'h1t%d' % (ti % 2)
                        S.dma('sp', x_[:], x_own[rows, :], writes=[xk])
                        for cb in range(4):
                            cs = slice(cb * 512, (cb + 1) * 512)
                            pa = pA[cb % 2]
                            pk = 'pA%d' % (cb % 2)
                            for c in range(16):
                                S.op('pe', lambda e, c=c, cs=cs, pa=pa, ti=ti: e.matmul(pa[:], mTall[:, c, ti * 128:(ti + 1) * 128], Wout[:, c, cs],
                                                                                 start=(c == 0), stop=(c == 15)),
                                     reads=['mTall', 'Wout'], writes=[pk])
                            S.op('dve', lambda e, cs=cs, pa=pa, h_=h_, x_=x_: e.tensor_tensor(h_[:, cs], pa[:], x_[:, cs], ALU.add),
                                 reads=[pk, xk], writes=[hk])
                        S.dma('sp', h1_s[rows, :], h_[:], reads=[hk], writes=['sc_h1'])
            S.barrier()

        if 'p8' in DEBUG:
            S.dma('sp', dbg_out("h1", [OWN, D]), h1_s)

        if stages >= 9:
            with ExitStack() as st:
                sb = lambda n, s, d: st.enter_context(nc.sbuf_tensor(n, list(s), d))
                Wq = sb("p9_Wq", [128, 16, D], BF16)
                skt = sb("p9_skt", [128, 16, 128], BF16)
                gffn = sb("p9_gffn", [128, D], F32)
                gfin = sb("p9_gfin", [128, D], F32)
                iota16 = sb("p9_iota", [128, 16], F32)
                h1t = sb("p9_h1t", [128, D], F32)
                junk = sb("p9_junk", [128, D], BF16)
                ss = sb("p9_ss", [128, 1], F32)
                rstd = sb("p9_rstd", [128, 1], F32)
                hn = sb("p9_hn", [128, D], F32)
                hnb = sb("p9_hnb", [128, D], BF16)
                hnT = sb("p9_hnT", [128, 16, 128], BF16)
                qTs = sb("p9_qTs", [128, 16, 128], BF16)
                sc = sb("p9_sc", [128, 16, 128], F32)
                sc2 = sb("p9_sc2", [128, 256], F32)
                tv = sb("p9_tv", [128, 16, 16], F32)
                tiu = sb("p9_tiu", [128, 16, 16], U32)
                tif = sb("p9_tif", [128, 16, 16], F32)
                cand = sb("p9_cand", [128, 8, 256], F32)
                bs = sb("p9_bs", [128, 8, 16], F32)
                bju = sb("p9_bju", [128, 8, 16], U32)
                bjf = sb("p9_bjf", [128, 8, 16], F32)
                ja = sb("p9_ja", [128, 8, 16], F32)
                jb = sb("p9_jb", [128, 8, 16], F32)
                eq = sb("p9_eq", [128, 8, 16, 16], F32)
                i0 = sb("p9_i0", [128, 8, 16], F32)
                i1 = sb("p9_i1", [128, 8, 16], F32)
                exf = sb("p9_exf", [128, 128], F32)
                exi = sb("p9_exi", [128, 128], I32)
                gm = sb("p9_gm", [128, 8], F32)
                ge = sb("p9_ge", [128, 8, 16], F32)
                gz = sb("p9_gz", [128, 8], F32)
                pre = sb("p9_pre", [128, 128], F32)
                t1 = sb("p9_t1", [128, 128], F32)
                t2 = sb("p9_t2", [128, 128], F32)
                coef = sb("p9_coef", [128, 128], F32)
                gbuf = [sb("p9_gb%d" % i, [128, D], BF16) for i in range(4)]
                DB = 16
                dgb = [sb("p9_dgb%d" % i, [128, DB, 128], BF16) for i in range(2)]
                junkb = sb("p9_junkb", [128, D], BF16)
                wq_v = wq_d.rearrange("(c p) n -> p c n", p=128)
                for c4 in range(0, 16, 4):
                    S.dma('pool', Wq[:, c4:c4 + 4, :], wq_v[:, c4:c4 + 4, :], writes=['Wq'])
                S.dma('pool', skt[:], skT_d.rearrange("a c k -> c a k"), writes=['skt'])
                S.dma('sp', gffn[:], g_ffn.partition_broadcast(128), writes=['gffn'])
                S.dma('sp', gfin[:], g_fin.partition_broadcast(128), writes=['gfin'])
                S.dma('sp', iota16[:], iota_d, writes=['iota16'])
                thr16 = sb("p9_thr16", [128, 16], F32)
                S.op('dve', lambda e: e.tensor_scalar(thr16[:], iota16[:], 16.0, 16.0, ALU.mult, ALU.add), reads=['iota16'], writes=['thr16'])
                ng = 0
                pvacc = [pO[0], pO[1], pC[0], pC[1]]
                pvk = ['pO0', 'pO1', 'pC0', 'pC1']
                issue_casts(len(cast_jobs))
                S.wait_bg(['pool'])
                h1t2 = [h1t, sb("p9_h1tb", [128, D], F32)]
                hnb2 = [hnb, sb("p9_hnbb", [128, D], BF16)]
                exi2 = [exi, sb("p9_exib", [128, 128], I32)]
                ge2 = [ge, sb("p9_geb", [128, 8, 16], F32)]
                pre2 = [pre, sb("p9_preb", [128, 128], F32)]
                coef2 = [coef, sb("p9_coefb", [128, 128], F32)]
                ssf = sb("p9_ssf", [128, 1], F32)
                rstdf = sb("p9_rstdf", [128, 1], F32)
                gbuf.extend([sb("p9_gbx%d" % i, [128, D], BF16) for i in range(2)])
                NG = len(gbuf)
                ngc = [0]

                def top16(vals, vkey, width, out_v, out_i, okey):
                    S.op('dve', lambda e: e.max(out_v[:, 0:8], vals), reads=[vkey], writes=[okey])
                    S.op('dve', lambda e: e.max_index(out_i[:, 0:8], out_v[:, 0:8], vals), reads=[vkey, okey], writes=[okey + 'i'])
                    S.op('dve', lambda e: e.match_replace(sc2[:, 0:width], out_v[:, 0:8], vals, -3.0e38), reads=[vkey, okey], writes=['sc2'])
                    S.op('dve', lambda e: e.max(out_v[:, 8:16], sc2[:, 0:width]), reads=['sc2'], writes=[okey])
                    S.op('dve', lambda e: e.max_index(out_i[:, 8:16], out_v[:, 8:16], sc2[:, 0:width]), reads=['sc2', okey], writes=[okey + 'i'])

                def front(t):
                    b = t % 2
                    h1t, exi, ge, hnb = h1t2[b], exi2[b], ge2[b], hnb2[b]
                    hk, nk, xk, gek, hbk = 'h1t%d' % b, 'hn', 'exi%d' % b, 'ge%d' % b, 'hnb%d' % b
                    rows = slice(t * 128, (t + 1) * 128)
                    S.dma('sp', h1t[:], h1_s[rows, :], writes=[hk])
                    norm_rows(h1t[:], hk, ss, rstd, junk, 'p9')
                    S.op('dve', lambda e: e.scalar_tensor_tensor(hn[:], h1t[:], rstd[:, 0:1], gffn[:], ALU.mult, ALU.mult),
                         reads=[hk, 'p9rstd', 'gffn'], writes=[nk])
                    S.op('act', lambda e: e.activation(hnb[:], hn[:], AF.Copy), reads=[nk], writes=[hbk])
                    yield
                    for half in range(2):
                        for cc in range(8):
                            c = half * 8 + cc
                            S.op('pe', lambda e, cc=cc, c=c: e.transpose(pT[:, cc * 128:(cc + 1) * 128], hnb[:, c * 128:(c + 1) * 128], identb[:]),
                                 reads=[hbk, 'identb'], writes=['pT'])
                        S.op('dve', lambda e, half=half: e.tensor_copy(hnT[:, half * 8:(half + 1) * 8, :], pT[:].rearrange("p (c t) -> p c t", c=8)),
                             reads=['pT'], writes=['hnT'])
                        yield
                    for b4 in range(4):
                        pa = pA[b4 % 2]
                        pk = 'pA%d' % (b4 % 2)
                        for bb in range(4):
                            blk = b4 * 4 + bb
                            for c in range(16):
                                S.op('pe', lambda e, c=c, blk=blk, bb=bb, pa=pa: e.matmul(
                                    pa[:, bb * 128:(bb + 1) * 128], Wq[:, c, blk * 128:(blk + 1) * 128], hnT[:, c, :],
                                    start=(c == 0), stop=(c == 15)), reads=['Wq', 'hnT'], writes=[pk])
                        evac(qTs[:, b4 * 4:(b4 + 1) * 4, :], pa[:].rearrange("p (b t) -> p b t", b=4), [pk], ['qTs'])
                        yield
                    for b4 in range(4):
                        pa = pA[b4 % 2]
                        pk = 'pA%d' % (b4 % 2)
                        for bb in range(4):
                            blk = b4 * 4 + bb
                            S.op('pe', lambda e, blk=blk, bb=bb, pa=pa: e.matmul(
                                pa[:, bb * 128:(bb + 1) * 128], qTs[:, blk, :], skt[:, blk, :], start=True, stop=True),
                                reads=['qTs', 'skt'], writes=[pk])
                        evac(sc[:, b4 * 4:(b4 + 1) * 4, :], pa[:].rearrange("p (b t) -> p b t", b=4), [pk], ['sc'])
                        yield
                    for blk in range(16):
                        top16(sc[:, blk, :], 'sc', 128, tv[:, blk, :], tiu[:, blk, :], 'tv')
                        yield
                    S.op('dve', lambda e: e.tensor_copy(tif[:], tiu[:]), reads=['tvi'], writes=['tif'])
                    tvv = tv[:].rearrange("p (h two) k -> p h two k", two=2)
                    tfv = tif[:].rearrange("p (h two) k -> p h two k", two=2)
                    S.op('dve', lambda e: e.tensor_tensor(
                        cand[:].rearrange("p h (a b) -> p h a b", b=16),
                        bc(tvv[:, :, 0, :].unsqueeze(3), [128, 8, 16, 16]),
                        bc(tvv[:, :, 1, :].unsqueeze(2), [128, 8, 16, 16]), ALU.add), reads=['tv'], writes=['cand'])
                    yield
                    for h in range(8):
                        top16(cand[:, h, :], 'cand', 256, bs[:, h, :], bju[:, h, :], 'bs')
                        yield
                    S.op('dve', lambda e: e.tensor_copy(bjf[:], bju[:]), reads=['bsi'], writes=['bjf'])
                    S.op('dve', lambda e: e.tensor_tensor(eq[:], bc(bjf[:].unsqueeze(3), [128, 8, 16, 16]),
                                                          bc(thr16[:].unsqueeze(1).unsqueeze(1), [128, 8, 16, 16]), ALU.is_ge),
                         reads=['bjf', 'thr16'], writes=['eq'])
                    S.op('dve', lambda e: e.tensor_reduce(ja[:], eq[:], AX.X, ALU.add), reads=['eq'], writes=['ja'])
                    S.op('dve', lambda e: e.scalar_tensor_tensor(jb[:], ja[:], -16.0, bjf[:], ALU.mult, ALU.add), reads=['ja', 'bjf'], writes=['jb'])
                    yield
                    iob = bc(iota16[:].unsqueeze(1).unsqueeze(1), [128, 8, 16, 16])
                    for (jx, jk, two, ix, ik) in ((ja, 'ja', 0, i0, 'i0'), (jb, 'jb', 1, i1, 'i1')):
                        S.op('dve', lambda e, jx=jx: e.tensor_tensor(eq[:], iob, bc(jx[:].unsqueeze(3), [128, 8, 16, 16]), ALU.is_equal),
                             reads=['iota16', jk], writes=['eq'])
                        S.op('dve', lambda e, two=two: e.tensor_tensor(eq[:], eq[:], bc(tfv[:, :, two, :].unsqueeze(2), [128, 8, 16, 16]), ALU.mult),
                             reads=['eq', 'tif'], writes=['eq'])
                        S.op('dve', lambda e, ix=ix: e.tensor_reduce(ix[:], eq[:], AX.X, ALU.add), reads=['eq'], writes=[ik])
                        yield
                    S.op('dve', lambda e: e.scalar_tensor_tensor(exf[:].rearrange("p (h k) -> p h k", k=16), i0[:], 128.0, i1[:], ALU.mult, ALU.add),
                         reads=['i0', 'i1'], writes=['exf'])
                    S.op('dve', lambda e: e.tensor_copy(exi[:], exf[:]), reads=['exf'], writes=[xk])
                    yield
                    S.op('dve', lambda e: e.tensor_reduce(gm[:], bs[:], AX.X, ALU.max), reads=['bs'], writes=['gm'])
                    S.op('dve', lambda e: e.tensor_tensor(ge[:], bs[:], bc(gm[:].unsqueeze(2), [128, 8, 16]), ALU.subtract), reads=['bs', 'gm'], writes=[gek])
                    S.op('act', lambda e: e.activation(ge[:], ge[:], AF.Exp), reads=[gek], writes=[gek])
                    S.op('dve', lambda e: e.tensor_reduce(gz[:], ge[:], AX.X, ALU.add), reads=[gek], writes=['gz'])
                    S.op('dve', lambda e: e.reciprocal(gz[:], gz[:]), reads=['gz'], writes=['gz'])
                    S.op('dve', lambda e: e.tensor_tensor(ge[:], ge[:], bc(gz[:].unsqueeze(2), [128, 8, 16]), ALU.mult), reads=[gek, 'gz'], writes=[gek])

                def ustep(t, slot):
                    b = t % 2
                    gb_ = gbuf[ngc[0] % NG]
                    gk = 'gb%d' % (ngc[0] % NG)
                    ngc[0] += 1
                    S.dma('pool', None, None, reads=['exi%d' % b], writes=[gk],
                          fn=lambda e: e.indirect_dma_start(
                              out=gb_[:], out_offset=None, in_=pu16[:, :],
                              in_offset=bass.IndirectOffsetOnAxis(ap=exi2[b][:, slot:slot + 1], axis=0)))
                    S.op('dve', lambda e: e.scalar_tensor_tensor(
                        junkb[:], gb_[:], 1.0, hnb2[b][:], ALU.mult, ALU.mult,
                        accum_out=pre2[b][:, slot:slot + 1]), reads=[gk, 'hnb%d' % b], writes=['junkb', 'pre%d' % b])

                def midstep(t):
                    b = t % 2
                    pre, coef, ge = pre2[b], coef2[b], ge2[b]
                    pk_, ck_ = 'pre%d' % b, 'coef%d' % b
                    S.op('dve', lambda e: e.tensor_tensor(t1[:], pre[:], pre[:], ALU.mult), reads=[pk_], writes=['t1'])
                    S.op('dve', lambda e: e.tensor_scalar(t1[:], t1[:], 0.044715, 1.0, ALU.mult, ALU.add), reads=['t1'], writes=['t1'])
                    S.op('dve', lambda e: e.tensor_tensor(t1[:], t1[:], pre[:], ALU.mult), reads=['t1', pk_], writes=['t1'])
                    S.op('act', lambda e: e.activation(t2[:], t1[:], AF.Sigmoid, scale=1.5957691216057308), reads=['t1'], writes=['t2'])
                    S.op('dve', lambda e: e.tensor_tensor(t2[:], t2[:], pre[:], ALU.mult), reads=['t2', pk_], writes=['t2'])
                    S.op('dve', lambda e: e.tensor_tensor(coef[:], t2[:], ge[:].rearrange("p h k -> p (h k)"), ALU.mult),
                         reads=['t2', 'ge%d' % b], writes=[ck_])
                    build_diag(t, 0)

                def build_diag(t, k):
                    b = t % 2
                    S.op('dve', lambda e: e.tensor_tensor(
                        dgb[k % 2][:], bc(identf[:].unsqueeze(1), [128, DB, 128]),
                        bc(coef2[b][:, k * DB:(k + 1) * DB].unsqueeze(2), [128, DB, 128]), ALU.mult),
                        reads=['identf', 'coef%d' % b], writes=['dgb%d' % (k % 2)])

                def vstep(t, slot):
                    b = t % 2
                    gb_ = gbuf[ngc[0] % NG]
                    gk = 'gb%d' % (ngc[0] % NG)
                    ngc[0] += 1
                    S.dma('pool', None, None, reads=['exi%d' % b], writes=[gk],
                          fn=lambda e: e.indirect_dma_start(
                              out=gb_[:], out_offset=None, in_=pv16[:, :],
                              in_offset=bass.IndirectOffsetOnAxis(ap=exi2[b][:, slot:slot + 1], axis=0)))
                    if slot % DB == 0 and slot + DB < 128:
                        build_diag(t, slot // DB + 1)
                    dgt = dgb[(slot // DB) % 2]
                    dk = 'dgb%d' % ((slot // DB) % 2)
                    for cb in range(4):
                        S.op('pe', lambda e, cb=cb: e.matmul(
                            pvacc[cb][:], dgt[:, slot % DB, :], gb_[:, cb * 512:(cb + 1) * 512], start=(slot == 0), stop=(slot == 127)),
                            reads=[dk, gk], writes=[pvk[cb]])

                def finstep(t):
                    b = t % 2
                    rows = slice(t * 128, (t + 1) * 128)
                    acc = h1t2[b]
                    ak = 'h1t%d' % b
                    for cb in range(4):
                        S.op('dve', lambda e, cb=cb: e.tensor_tensor(acc[:, cb * 512:(cb + 1) * 512], pvacc[cb][:], acc[:, cb * 512:(cb + 1) * 512], ALU.add),
                             reads=[pvk[cb], ak], writes=[ak])
                    norm_rows(acc[:], ak, ssf, rstdf, junk, 'p9f')
                    S.op('dve', lambda e: e.scalar_tensor_tensor(acc[:], acc[:], rstdf[:, 0:1], gfin[:], ALU.mult, ALU.mult),
                         reads=[ak, 'p9frstd', 'gfin'], writes=[ak])
                    S.dma('sp', out_d[rows, :], acc[:], reads=[ak], writes=['out'])

                for _ in front(0):
                    pass
                for slot in range(128):
                    ustep(0, slot)
                midstep(0)
                for t in range(8):
                    if t + 1 < 8:
                        fg = front(t + 1)
                        HEAD = 96
                        for slot in range(HEAD):
                            vstep(t, slot)
                            next(fg, None)
                        for _ in fg:
                            pass
                        vs_, us_ = HEAD, 0
                        while vs_ < 128 or us_ < 128:
                            if us_ < 128:
                                ustep(t + 1, us_)
                                us_ += 1
                            if vs_ < 128 and (vs_ - HEAD) * 128 < us_ * (128 - HEAD):
                                vstep(t, vs_)
                                vs_ += 1
                    else:
                        for slot in range(128):
                            vstep(t, slot)
                    finstep(t)
                    if t + 1 < 8:
                        midstep(t + 1)

        S.finish()
    return nc, dbg


def host_consts(j, rel_table):
    tab = np.asarray(rel_table, np.float32)
    k = np.arange(128)[:, None]
    q = np.arange(128)[None, :]
    abs_t = np.empty((12, 128, 16, 128), np.float32)
    for u in range(12):
        if u == 11:
            abs_t[u] = tab[31][None, :, None]
            continue
        dist = 128 * (u + j - 3) + q - k
        val = tab[_bucket(dist)]
        val = np.where((dist >= 0)[:, :, None], val, np.float32(NEGM))
        abs_t[u] = val.transpose(0, 2, 1)
    abw_t = np.empty((8, 128, 16, 128), np.float32)
    for u in range(8):
        dist = 128 * (u + j - 3) + q - k
        val = tab[_bucket(dist)]
        ok = (dist >= 0) & (dist < 512)
        val = np.where(ok[:, :, None], val, np.float32(NEGM))
        abw_t[u] = val.transpose(0, 2, 1)
    msb = np.empty((4, 128, 128), np.float32)
    for u in range(4):
        dist = 128 * (u + j - 3) + q - k
        msb[u] = np.where(dist >= 1, np.float32(0), np.float32(NEGM))
    cb = np.empty((NS, 2, 128, 16, 128), np.float32)
    selc = np.empty((NS, 128, 64), np.float32)
    for s in range(NS):
        t = (4 * s + j) * 128 + np.arange(128)
        n = np.arange(256)
        dist = t[None, :] - (16 * n[:, None] + 31)
        val = tab[_bucket(dist)]
        ok = (dist >= 0) & (n[:, None] < 255)
        val = np.where(ok[:, :, None], val, np.float32(NEGM)).transpose(0, 2, 1)
        cb[s] = val.reshape(2, 128, 16, 128)
        cur = t[:, None] // 64
        jb = np.arange(64)[None, :]
        valid = jb <= cur
        forced = (jb == 0) | ((cur - jb >= 0) & (cur - jb < 2))
        selc[s] = np.where(valid, np.where(forced, np.float32(1e4), np.float32(0)), np.float32(-1e30))
    return dict(abs=abs_t, abw=abw_t, msb=msb, cbias=cb, selc=selc)


def shared_consts():
    n_cmp = 255
    c_start = np.arange(n_cmp) * 16
    s_start = np.arange(64) * 64
    ov = np.maximum(np.minimum(c_start[:, None] + 32, s_start[None, :] + 64)
                    - np.maximum(c_start[:, None], s_start[None, :]), 0).astype(np.float32) / 32
    ovl = np.zeros((256, 64), np.float32)
    ovl[:255] = ov
    expand = np.zeros((NB, 64, 128), np.float32)
    for kb in range(NB):
        for kk in range(128):
            expand[kb, (128 * kb + kk) // 64, kk] = 1.0
    jj = np.arange(128)[:, None]
    ss = np.arange(128)[None, :]
    tri = (jj >= ss).astype(np.float32)
    iota = np.tile(np.arange(16, dtype=np.float32)[None, :], (128, 1))
    return dict(ovl=ovl.reshape(2, 128, 64), expand=expand, tri=tri, ident=np.eye(128, dtype=np.float32), iota16=iota)


def prep_inputs(inp):
    f = lambda a: np.ascontiguousarray(np.asarray(a, dtype=np.float32))
    x = f(inp['x'])
    shared = shared_consts()
    shared.update(
        w_in=f(inp['w_in'][0]), g_attn=f(inp['attn_norm_g'][0]), g_ffn=f(inp['ffn_norm_g'][0]),
        g_fin=f(inp['final_norm_g']),
        ckw1=f(np.stack([inp['cmp_k_w1'][0], inp['cmp_v_w1'][0]])),
        ckpeT=f(np.stack([np.asarray(inp['cmp_k_pe'][0]).T, np.asarray(inp['cmp_v_pe'][0]).T])),
        ckw2=f(np.stack([inp['cmp_k_w2'][0], inp['cmp_v_w2'][0]])),
        wbn=f(inp['w_branch_nsa'][0]), wbs=f(inp['w_branch_sb'][0]), wout=f(inp['w_out'][0]),
        wq=f(inp['peer_w_q'][0]),
        skT=f(np.asarray(inp['peer_sub_keys'][0]).reshape(16, 128, 128).transpose(0, 2, 1)),
        pu=f(inp['peer_u'][0]), pv=f(inp['peer_v'][0]),
    )
    per_j = [host_consts(j, inp['rel_bias_table']) for j in range(4)]
    in_maps = []
    for c in range(8):
        b, j = c // 4, c % 4
        own = np.concatenate([np.arange((4 * s + j) * 128, (4 * s + j + 1) * 128) for s in range(NS)])
        m = dict(shared)
        m.update(per_j[j])
        m['x_full'] = x[b]
        m['x_own'] = np.ascontiguousarray(x[b][own])
        in_maps.append(m)
    return in_maps


def own_index(j):
    return np.concatenate([np.arange((4 * s + j) * 128, (4 * s + j + 1) * 128) for s in range(NS)])


def kernel(**inputs):
    in_maps = prep_inputs(inputs)
    nc, dbg = build_program()
    res = run_bass_kernel_spmd(nc, in_maps, core_ids=list(range(8)))
    out = np.empty((2, T, D), np.float32)
    for c in range(8):
        b, j = c // 4, c % 4
        out[b, own_index(j)] = res.results[c]["out"]
    return out
```

```python
import math
from contextlib import ExitStack

import numpy as np
import concourse.bass as bass
import concourse.mybir as mybir
from concourse.bass_utils import run_bass_kernel_spmd

F32 = mybir.dt.float32
BF16 = mybir.dt.bfloat16
I32 = mybir.dt.int32
U32 = mybir.dt.uint32
ALU = mybir.AluOpType
AF = mybir.ActivationFunctionType
AX = mybir.AxisListType

T = 4096
D = 2048
NB = 32
NS = 8
OWN = 1024
NEGM = -30000.0
EPS = 1e-6
IN_COLS = 9776
C_QN, C_KC, C_VC, C_KS, C_VS, C_KW, C_VW, C_GBR, C_QB, C_KB, C_VB, C_GA, C_GB = (
    0, 1024, 1280, 1536, 1792, 2048, 2304, 2560, 2608, 3632, 4656, 5680, 7728)

DEBUG = {}


class Sched:
    NDS = 32
    NHW = 20

    def __init__(self, nc, stack):
        self.nc = nc
        self.eng = {'pe': nc.tensor, 'dve': nc.vector, 'act': nc.scalar,
                    'pool': nc.gpsimd, 'sp': nc.sync}
        self.sem = {k: stack.enter_context(nc.semaphore('s_' + k)) for k in self.eng}
        self.cnt = {k: 0 for k in self.eng}
        self.waited = {}
        self.dsem = [stack.enter_context(nc.semaphore('d%d' % i)) for i in range(self.NDS)]
        self.dcnt = [0] * self.NDS
        self.dnext = 0
        self.dnext_sw = 0
        self.bgsem = [stack.enter_context(nc.semaphore('bg%d' % i)) for i in range(64)]
        self.bgcnt = [0] * 64
        self.bgnext = 0
        self.last_w = {}
        self.readers = {}
        self.ninst = 0

    def _wait(self, e, tok):
        if tok is None:
            return
        if tok[0] == 'e':
            _, p, n = tok
            if p == 'pe' and e == 'pe':
                return
            key = (e, 'e', p)
            if self.waited.get(key, 0) >= n:
                return
            self.eng[e].wait_ge(self.sem[p], n)
            self.waited[key] = n
        else:
            _, slot, v = tok
            key = (e, 'd', slot)
            if self.waited.get(key, 0) >= v:
                return
            self.eng[e].wait_ge(self.dsem[slot], v)
            self.waited[key] = v

    def _deps(self, e, reads, writes):
        toks = []
        for k in reads:
            toks.append(self.last_w.get(k))
        for k in writes:
            toks.append(self.last_w.get(k))
            toks.extend(self.readers.get(k, []))
        for t in toks:
            self._wait(e, t)

    def _update(self, tok, reads, writes):
        for k in reads:
            self.readers.setdefault(k, []).append(tok)
        for k in writes:
            self.last_w[k] = tok
            self.readers[k] = []

    def op(self, e, fn, reads=(), writes=()):
        self._deps(e, reads, writes)
        ins = fn(self.eng[e])
        self.cnt[e] += 1
        ins.then_inc(self.sem[e], 1)
        tok = ('e', e, self.cnt[e])
        self._update(tok, reads, writes)
        self.ninst += 1
        return tok

    def dma(self, q, out, in_, reads=(), writes=(), fn=None, **kw):
        if q == 'pool':
            slot = self.NHW + self.dnext_sw
            self.dnext_sw = (self.dnext_sw + 1) % (self.NDS - self.NHW)
        else:
            slot = self.dnext
            self.dnext = (self.dnext + 1) % self.NHW
        if self.dcnt[slot] > 0:
            self._wait(q, ('d', slot, self.dcnt[slot]))
        self._deps(q, reads, writes)
        if fn is not None:
            ins = fn(self.eng[q])
        else:
            ins = self.eng[q].dma_start(out=out, in_=in_, **kw)
        ins.then_inc(self.dsem[slot], 16)
        self.dcnt[slot] += 16
        tok = ('d', slot, self.dcnt[slot])
        self._update(tok, reads, writes)
        self.ninst += 1
        return tok

    def bg_dma(self, q, out, in_):
        slot = self.bgnext
        self.bgnext = (self.bgnext + 1) % len(self.bgsem)
        if self.bgcnt[slot] > 0:
            key = (q, 'bg', slot)
            if self.waited.get(key, 0) < self.bgcnt[slot]:
                self.eng[q].wait_ge(self.bgsem[slot], self.bgcnt[slot])
                self.waited[key] = self.bgcnt[slot]
        self.eng[q].dma_start(out=out, in_=in_).then_inc(self.bgsem[slot], 16)
        self.bgcnt[slot] += 16
        self.ninst += 1

    def wait_bg(self, engines):
        for e in engines:
            for slot in range(len(self.bgsem)):
                if self.bgcnt[slot] > 0:
                    self.eng[e].wait_ge(self.bgsem[slot], self.bgcnt[slot])

    def barrier(self):
        for e in self.eng:
            for p in self.eng:
                if p != e and self.cnt[p] > 0:
                    self._wait(e, ('e', p, self.cnt[p]))
            for s in range(self.NDS):
                if self.dcnt[s] > 0:
                    self._wait(e, ('d', s, self.dcnt[s]))
        self.last_w = {}
        self.readers = {}

    def finish(self):
        e = 'sp'
        self.wait_bg([e])
        for p in self.eng:
            if p != e and self.cnt[p] > 0:
                self._wait(e, ('e', p, self.cnt[p]))
        for s in range(self.NDS):
            if self.dcnt[s] > 0:
                self._wait(e, ('d', s, self.dcnt[s]))


def _bucket(dist):
    dist = np.maximum(dist, 0)
    d_f = np.maximum(dist, 1).astype(np.float32)
    large = 16 + (np.log(d_f / np.float32(16)) / np.float32(math.log(1024 / 16)) * np.float32(16)).astype(np.int32)
    large = np.minimum(large, 31)
    return np.where(dist < 16, dist, large).astype(np.int64)


def _bucket_jax(dist):
    return _bucket(dist)


def build_program(stages=99):
    nc = bass.Bass("TRN2", target_bir_lowering=False)
    dt_in = lambda n, s, d=F32: nc.dram_tensor(n, list(s), d, kind="ExternalInput").ap()
    dt_sc = lambda n, s, d: nc.dram_tensor(n, list(s), d, kind="Internal").ap()
    x_full = dt_in("x_full", [T, D])
    x_own = dt_in("x_own", [OWN, D])
    w_in = dt_in("w_in", [D, IN_COLS])
    g_attn = dt_in("g_attn", [D])
    g_ffn = dt_in("g_ffn", [D])
    g_fin = dt_in("g_fin", [D])
    ident_d = dt_in("ident", [128, 128])
    tri_d = dt_in("tri", [128, 128])
    abs_d = dt_in("abs", [12, 128, 16, 128])
    abw_d = dt_in("abw", [8, 128, 16, 128])
    msb_d = dt_in("msb", [4, 128, 128])
    cbias_d = dt_in("cbias", [NS, 2, 128, 16, 128])
    selc_d = dt_in("selc", [NS, 128, 64])
    ovl_d = dt_in("ovl", [2, 128, 64])
    expand_d = dt_in("expand", [NB, 64, 128])
    ckw1_d = dt_in("ckw1", [2, 2048, 128])
    ckpe_d = dt_in("ckpeT", [2, 64, 32])
    ckw2_d = dt_in("ckw2", [2, 128, 64])
    wbn_d = dt_in("wbn", [1024, D])
    wbs_d = dt_in("wbs", [1024, D])
    wout_d = dt_in("wout", [D, D])
    wq_d = dt_in("wq", [D, D])
    skT_d = dt_in("skT", [16, 128, 128])
    pu_d = dt_in("pu", [16384, D])
    pv_d = dt_in("pv", [16384, D])
    iota_d = dt_in("iota16", [128, 16])
    out_d = nc.dram_tensor("out", [OWN, D], F32, kind="ExternalOutput").ap()
    dbg = {}

    def dbg_out(name, shape, dtype=F32):
        dbg[name] = nc.dram_tensor("dbg_" + name, list(shape), dtype, kind="ExternalOutput").ap()
        return dbg[name]

    kcvT = dt_sc("kcvT", [2, 4, 64, T], BF16)
    ksT = dt_sc("ksT", [4, 64, T], BF16)
    kwT = dt_sc("kwT", [4, 64, T], BF16)
    kbT = dt_sc("kbT", [16, 64, T], BF16)
    vsw = dt_sc("vsw", [T, 512], BF16)
    vb = dt_sc("vb", [T, 1024], BF16)
    qnT = dt_sc("qnT", [16, 64, OWN], BF16)
    qbT = dt_sc("qbT", [16, 64, OWN], BF16)
    gbr_s = dt_sc("gbr_s", [OWN, 48], F32)
    gab_s = dt_sc("gab_s", [OWN, 4096], F32)
    obr_s = dt_sc("obr_s", [3, OWN, 1024], F32)
    osb_s = dt_sc("osb_s", [OWN, 1024], F32)
    h1_s = dt_sc("h1_s", [OWN, D], F32)
    pu16 = dt_sc("pu16", [16384, D], BF16)
    pv16 = dt_sc("pv16", [16384, D], BF16)

    with ExitStack() as gst:
        S = Sched(nc, gst)
        gsb = lambda n, s, d: gst.enter_context(nc.sbuf_tensor(n, list(s), d))
        gps = lambda n, s, d: gst.enter_context(nc.psum_tensor(n, list(s), d))
        pA = [gps("pA%d" % i, [128, 512], F32) for i in range(2)]
        pC = [gps("pC%d" % i, [128, 512], F32) for i in range(2)]
        pO = [gps("pO%d" % i, [128, 512], F32) for i in range(2)]
        pT = gps("pT", [128, 1024], BF16)
        pM = gps("pM", [128, 512], F32)

        identf = gsb("identf", [128, 128], F32)
        identb = gsb("identb", [128, 128], BF16)
        trib = gsb("trib", [128, 128], BF16)
        onesb = gsb("onesb", [128, 128], BF16)

        S.dma('sp', identf[:], ident_d, writes=['identf'])
        S.op('dve', lambda e: e.tensor_copy(identb[:], identf[:]), reads=['identf'], writes=['identb'])
        S.dma('pool', trib[:], tri_d, writes=['trib'])
        S.op('dve', lambda e: e.memset(onesb[:], 1.0), writes=['onesb'])

        rr = {'ev': 0}
        cast_jobs = []
        for r0 in range(0, 16384, 512):
            cast_jobs.append((pu16[r0:r0 + 512, :], pu_d[r0:r0 + 512, :]))
            cast_jobs.append((pv16[r0:r0 + 512, :], pv_d[r0:r0 + 512, :]))

        def issue_casts(n):
            for _ in range(n):
                if cast_jobs:
                    o_, i_ = cast_jobs.pop(0)
                    S.bg_dma('pool', o_, i_)

        def evac(out_ap, in_ap, reads, writes, scale=None):
            rr['ev'] += 1
            if rr['ev'] % 2 == 0:
                if scale is None:
                    S.op('dve', lambda e: e.tensor_copy(out_ap, in_ap), reads=reads, writes=writes)
                else:
                    S.op('dve', lambda e: e.tensor_scalar(out_ap, in_ap, scale, None, ALU.mult), reads=reads, writes=writes)
            else:
                S.op('act', lambda e: e.activation(out_ap, in_ap, AF.Copy, scale=(1.0 if scale is None else scale)),
                     reads=reads, writes=writes)

        def make_gfull(st, g_ap, name):
            gt = st.enter_context(nc.sbuf_tensor(name + "_gt", [128, 16], F32))
            gfull = st.enter_context(nc.sbuf_tensor(name + "_gf", [128, 16, 128], BF16))
            S.dma('sp', gt[:], g_ap.rearrange("(c p) -> p c", p=128), writes=[name + 'gt'],
                  allow_slow_non_contiguous=True)
            S.op('dve', lambda e: e.memset(gfull[:], 1.0), writes=[name + 'gf'])
            for c in range(16):
                S.op('dve', lambda e, c=c: e.tensor_scalar(gfull[:, c, :], gfull[:, c, :], gt[:, c:c + 1], None, ALU.mult),
                     reads=[name + 'gt', name + 'gf'], writes=[name + 'gf'])
            return gfull, name + 'gf'

        def norm_rows(xt, xkey, ss, rstd, junk, key, jkey=None):
            S.op('act', lambda e: e.activation(junk[:], xt, AF.Square, accum_out=ss[:]),
                 reads=[xkey], writes=[jkey if jkey else key + 'junk', key + 'ss'])
            S.op('dve', lambda e: e.tensor_scalar(rstd[:], ss[:], 1.0 / D, EPS, ALU.mult, ALU.add),
                 reads=[key + 'ss'], writes=[key + 'rstd'])
            S.op('act', lambda e: e.activation(rstd[:], rstd[:], AF.Sqrt), reads=[key + 'rstd'], writes=[key + 'rstd'])
            S.op('dve', lambda e: e.reciprocal(rstd[:], rstd[:]), reads=[key + 'rstd'], writes=[key + 'rstd'])

        def transposeT(xs, xskey, gfull, gfkey, dst, dstkey, col0):
            for half in range(2):
                for cc in range(8):
                    c = half * 8 + cc
                    S.op('pe', lambda e, c=c, cc=cc: e.transpose(pT[:, cc * 128:(cc + 1) * 128], xs[:, c * 128:(c + 1) * 128], identb[:]),
                         reads=[xskey, 'identb'], writes=['pT'])
                S.op('dve', lambda e, half=half: e.tensor_tensor(
                    dst[:, half * 8:(half + 1) * 8, col0:col0 + 128],
                    pT[:].rearrange("p (c t) -> p c t", c=8),
                    gfull[:, half * 8:(half + 1) * 8, :], ALU.mult),
                    reads=['pT', gfkey], writes=[dstkey])

        if stages >= 1:
            with ExitStack() as st:
                sb = lambda n, s, d: st.enter_context(nc.sbuf_tensor(n, list(s), d))
                WF = sb("p1_WF", [128, 16, 2048], BF16)
                WV = sb("p1_WV", [128, 16, 1536], BF16)
                xt = [sb("p1_xt%d" % i, [128, D], F32) for i in range(2)]
                junk = sb("p1_junk", [128, D], BF16)
                ss = sb("p1_ss", [128, 1], F32)
                rstd = sb("p1_rstd", [128, 1], F32)
                xs = sb("p1_xs", [128, D], BF16)
                aT = [sb("p1_aT%d" % i, [128, 16, 512], BF16) for i in range(2)]
                stg = [sb("p1_stg%d" % i, [128, 512], BF16) for i in range(4)]
                gfull, gfkey = make_gfull(st, g_attn, "p1")
                wv = w_in.rearrange("(c p) n -> p c n", p=128)
                fm_cols = [(C_KC, 256), (C_VC, 256), (C_KS, 256), (C_KW, 256), (C_KB, 1024)]
                o = 0
                for gi_, (c0, n) in enumerate(fm_cols):
                    for c4 in range(0, 16, 4):
                        S.dma('pool', WF[:, c4:c4 + 4, o:o + n], wv[:, c4:c4 + 4, c0:c0 + n], writes=['WF%d' % gi_])
                    o += n
                tm_cols = [(C_VS, 256), (C_VW, 256), (C_VB, 1024)]
                o = 0
                for gi_, (c0, n) in enumerate(tm_cols):
                    for c4 in range(0, 16, 4):
                        S.dma('pool', WV[:, c4:c4 + 4, o:o + n], wv[:, c4:c4 + 4, c0:c0 + n], writes=['WV%d' % gi_])
                    o += n
                kcv2 = kcvT.rearrange("a g d t -> a (g d) t")
                ks2 = ksT.rearrange("g d t -> (g d) t")
                kw2 = kwT.rearrange("g d t -> (g d) t")
                kb2 = kbT.rearrange("h d t -> (h d) t")
                fm_dst = [(kcv2[0], 0), (kcv2[0], 128), (kcv2[1], 0), (kcv2[1], 128),
                          (ks2, 0), (ks2, 128), (kw2, 0), (kw2, 128)] + [(kb2, 128 * i) for i in range(8)]
                nstg = [0]
                npa = [0]
                fm_grp = [0, 0, 1, 1, 2, 2, 3, 3] + [4] * 8
                tm_grp = [['WV0', 'WV1'], ['WV2'], ['WV2']]

                def prepA(tt, ti):
                    r0 = tt * 512 + ti * 128
                    xi = (tt * 4 + ti) % 2
                    S.dma('sp', xt[xi][:], x_full[r0:r0 + 128, :], writes=['xt%d' % xi])
                    norm_rows(xt[xi][:], 'xt%d' % xi, ss, rstd, junk, 'p1')
                    S.op('dve', lambda e: e.tensor_scalar(xs[:], xt[xi][:], rstd[:, 0:1], None, ALU.mult),
                         reads=['xt%d' % xi, 'p1rstd'], writes=['xs'])

                def prepB(tt, ti):
                    transposeT(xs, 'xs', gfull, gfkey, aT[tt % 2], 'aT%d' % (tt % 2), ti * 128)

                def fm_group(tt, bi):
                    a = aT[tt % 2]
                    akey = 'aT%d' % (tt % 2)
                    pi = npa[0] % 2
                    npa[0] += 1
                    pa = pA[pi]
                    for c in range(16):
                        S.op('pe', lambda e, c=c: e.matmul(pa[:], WF[:, c, bi * 128:(bi + 1) * 128], a[:, c, :],
                                                          start=(c == 0), stop=(c == 15)),
                             reads=['WF%d' % fm_grp[bi], akey], writes=['pA%d' % pi])
                    sg = stg[nstg[0] % 4]
                    sk = 'stg%d' % (nstg[0] % 4)
                    nstg[0] += 1
                    evac(sg[:], pa[:], ['pA%d' % pi], [sk])
                    dst, row0 = fm_dst[bi]
                    S.dma('sp', dst[row0:row0 + 128, tt * 512:(tt + 1) * 512], sg[:], reads=[sk], writes=['sc_fm'])

                def tm_group(tt, ti, vbk):
                    a = aT[tt % 2]
                    akey = 'aT%d' % (tt % 2)
                    r0 = tt * 512 + ti * 128
                    pi = npa[0] % 2
                    npa[0] += 1
                    pa = pA[pi]
                    for c in range(16):
                        S.op('pe', lambda e, c=c: e.matmul(
                            pa[:], a[:, c, ti * 128:(ti + 1) * 128], WV[:, c, vbk * 512:(vbk + 1) * 512],
                            start=(c == 0), stop=(c == 15)),
                            reads=tm_grp[vbk] + [akey], writes=['pA%d' % pi])
                    sg = stg[nstg[0] % 4]
                    sk = 'stg%d' % (nstg[0] % 4)
                    nstg[0] += 1
                    evac(sg[:], pa[:], ['pA%d' % pi], [sk])
                    if vbk == 0:
                        S.dma('sp', vsw[r0:r0 + 128, :], sg[:], reads=[sk], writes=['sc_tm'])
                    else:
                        S.dma('sp', vb[r0:r0 + 128, (vbk - 1) * 512:vbk * 512], sg[:], reads=[sk], writes=['sc_tm'])

                for ti in range(4):
                    prepA(0, ti)
                    prepB(0, ti)
                NT = T // 512
                for tt in range(NT):
                    groups = [(lambda bi=bi: fm_group(tt, bi)) for bi in range(16)]
                    groups += [(lambda ti=ti, vbk=vbk: tm_group(tt, ti, vbk)) for ti in range(4) for vbk in range(3)]
                    posA = {0: 0, 7: 1, 14: 2, 21: 3}
                    posB = {4: 0, 11: 1, 18: 2, 25: 3}
                    for gi, grp in enumerate(groups):
                        if tt + 1 < NT and gi in posA:
                            prepA(tt + 1, posA[gi])
                        if tt + 1 < NT and gi in posB:
                            prepB(tt + 1, posB[gi])
                        grp()
            S.barrier()

        if stages >= 2:
            with ExitStack() as st:
                sb = lambda n, s, d: st.enter_context(nc.sbuf_tensor(n, list(s), d))
                xt = [sb("p2_xt%d" % i, [128, D], F32) for i in range(2)]
                junk = sb("p2_junk", [128, D], BF16)
                ss = sb("p2_ss", [128, 1], F32)
                rstd = sb("p2_rstd", [128, 1], F32)
                xs = sb("p2_xs", [128, D], BF16)
                aT = sb("p2_aT", [128, 16, OWN], BF16)
                Wb = [sb("p2_W%d" % i, [128, 16, 512], BF16) for i in range(2)]
                stg = [sb("p2_stg%d" % i, [128, 512], BF16) for i in range(4)]
                stgf = [sb("p2_stgf%d" % i, [128, 512], F32) for i in range(4)]
                gfull, gfkey = make_gfull(st, g_attn, "p2")
                wv = w_in.rearrange("(c p) n -> p c n", p=128)
                for ti in range(8):
                    xi = ti % 2
                    S.dma('sp', xt[xi][:], x_own[ti * 128:(ti + 1) * 128, :], writes=['xt%d' % xi])
                    norm_rows(xt[xi][:], 'xt%d' % xi, ss, rstd, junk, 'p2')
                    S.op('dve', lambda e, xi=xi: e.tensor_scalar(xs[:], xt[xi][:], rstd[:, 0:1], None, ALU.mult),
                         reads=['xt%d' % xi, 'p2rstd'], writes=['xs'])
                    transposeT(xs, 'xs', gfull, gfkey, aT, 'aTown', ti * 128)
                qn2 = qnT.rearrange("h d t -> (h d) t")
                qb2 = qbT.rearrange("h d t -> (h d) t")
                blocks = [('fm', C_QN + 512 * i, 512, qn2, 512 * i) for i in range(2)]
                blocks += [('fm', C_QB + 512 * i, 512, qb2, 512 * i) for i in range(2)]
                blocks += [('tm', C_GA + 512 * i, 512, gab_s, 512 * i) for i in range(8)]
                blocks += [('gbr', C_GBR, 48, gbr_s, 0)]
                nstg = 0
                for bi, (kind, c0, n, dst, d0) in enumerate(blocks):
                    W = Wb[bi % 2]
                    wk = 'W%d' % (bi % 2)
                    for c4 in range(0, 16, 4):
                        S.dma('pool', W[:, c4:c4 + 4, 0:n], wv[:, c4:c4 + 4, c0:c0 + n], writes=[wk])
                    if kind == 'fm':
                        for sub in range(4):
                            for th in range(2):
                                pa = pA[(sub * 2 + th) % 2]
                                pk = 'pA%d' % ((sub * 2 + th) % 2)
                                for c in range(16):
                                    S.op('pe', lambda e, c=c, sub=sub, th=th, pa=pa, W=W: e.matmul(
                                        pa[:], W[:, c, sub * 128:(sub + 1) * 128], aT[:, c, th * 512:(th + 1) * 512],
                                        start=(c == 0), stop=(c == 15)), reads=[wk, 'aTown'], writes=[pk])
                                sg = stg[nstg % 4]
                                sk = 'stg%d' % (nstg % 4)
                                nstg += 1
                                evac(sg[:], pa[:], [pk], [sk], scale=0.125)
                                S.dma('sp', dst[d0 + sub * 128:d0 + (sub + 1) * 128, th * 512:(th + 1) * 512], sg[:],
                                      reads=[sk], writes=['sc_q'])
                    else:
                        for ti in range(8):
                            pa = pA[ti % 2]
                            pk = 'pA%d' % (ti % 2)
                            for c in range(16):
                                S.op('pe', lambda e, c=c, ti=ti, pa=pa, W=W, n=n: e.matmul(
                                    pa[:, 0:n], aT[:, c, ti * 128:(ti + 1) * 128], W[:, c, 0:n],
                                    start=(c == 0), stop=(c == 15)), reads=[wk, 'aTown'], writes=[pk])
                            sg = stgf[nstg % 4]
                            sk = 'stgf%d' % (nstg % 4)
                            nstg += 1
                            S.op('act', lambda e, sg=sg, pa=pa, n=n: e.activation(sg[:, 0:n], pa[:, 0:n], AF.Sigmoid),
                                 reads=[pk], writes=[sk])
                            if kind == 'tm':
                                S.dma('sp', dst[ti * 128:(ti + 1) * 128, d0:d0 + n], sg[:, 0:n], reads=[sk], writes=['sc_g'])
                            else:
                                S.dma('sp', dst[ti * 128:(ti + 1) * 128, :], sg[:, 0:n], reads=[sk], writes=['sc_g'])
            S.barrier()

        if 'p12' in DEBUG:
            d1 = dbg_out("ksT", [4, 64, T], BF16)
            S.dma('sp', d1, ksT, reads=[], writes=[])
            d2 = dbg_out("vb", [T, 1024], BF16)
            S.dma('sp', d2, vb, reads=[], writes=[])
            d3 = dbg_out("qnT", [16, 64, OWN], BF16)
            S.dma('sp', d3, qnT, reads=[], writes=[])
            d4 = dbg_out("gbr", [OWN, 48], F32)
            S.dma('sp', d4, gbr_s, reads=[], writes=[])
            d5 = dbg_out("gab", [OWN, 4096], F32)
            S.dma('sp', d5, gab_s, reads=[], writes=[])

        def bc(ap, shape):
            return ap.to_broadcast(list(shape))

        with ExitStack() as mid_st:
            msb_ = lambda n, s, d: mid_st.enter_context(nc.sbuf_tensor(n, list(s), d))
            kcbT = msb_("kcbT", [64, 4, 256], BF16)
            vcb = msb_("vcb", [128, 4, 2, 129], BF16)
            selT = msb_("selT", [128, 4, NS, 128], BF16)
            S.op('dve', lambda e: e.memset(kcbT[:], 0.0), writes=['kcbT'])
            S.op('dve', lambda e: e.memset(vcb[:], 0.0), writes=['vcb'])
            if stages >= 3:
                with ExitStack() as st:
                    sb = lambda n, s, d: st.enter_context(nc.sbuf_tensor(n, list(s), d))
                    kin = [sb("p3_kin%d" % i, [64, T], BF16) for i in range(2)]
                    W1 = sb("p3_W1", [64, 2, 32, 128], BF16)
                    peT = sb("p3_peT", [64, 2, 32], BF16)
                    W2 = sb("p3_W2", [128, 2, 64], BF16)
                    b1 = sb("p3_b1", [128, 2], F32)
                    uu = sb("p3_u", [128, 256], F32)
                    u2 = sb("p3_u2", [128, 256], F32)
                    sg = sb("p3_sg", [128, 256], F32)
                    gl = sb("p3_gl", [128, 256], BF16)
                    for kv in range(2):
                        S.dma('pool', W1[:, kv], ckw1_d[kv].rearrange("(l d) h -> d l h", d=64), writes=['W1'])
                        S.dma('pool', peT[:, kv, :], ckpe_d[kv], writes=['peT'])
                        S.dma('pool', W2[:, kv, :], ckw2_d[kv], writes=['W2'])
                    for g in range(4):
                        for c in range(2):
                            S.dma('pool', vcb[:, g, c, 65:129], ovl_d[c], reads=[], writes=['vcb'])
                    S.op('dve', lambda e: e.memset(vcb[:, :, :, 64:65], 1.0), writes=['vcb'])
                    for kv in range(2):
                        for l in range(32):
                            S.op('pe', lambda e, kv=kv, l=l: e.matmul(pM[:, kv:kv + 1], W1[:, kv, l, :], peT[:, kv, l:l + 1],
                                                                  start=(l == 0), stop=(l == 31)),
                                 reads=['W1', 'peT'], writes=['pM'])
                    S.op('dve', lambda e: e.tensor_copy(b1[:], pM[:, 0:2]), reads=['pM'], writes=['b1'])
                    gl2 = [gl, sb("p3_gl2", [128, 256], BF16)]

                    def head3(it):
                        kv, g = it // 4, it % 4
                        ki = kin[it % 2]
                        kk = 'kin%d' % (it % 2)
                        pa = pA[it % 2]
                        pk = 'pA%d' % (it % 2)
                        gl_ = gl2[it % 2]
                        glk = 'gl%d' % (it % 2)
                        S.dma('sp', ki[:], kcvT[kv, g], writes=[kk])
                        for l in range(32):
                            S.op('pe', lambda e, l=l: e.matmul(
                                pa[:, 0:255], W1[:, kv, l, :], ki[:, l:l + 4065:16], start=(l == 0), stop=(l == 31)),
                                reads=['W1', kk], writes=[pk])
                        S.op('act', lambda e: e.activation(uu[:, 0:255], pa[:, 0:255], AF.Identity, bias=b1[:, kv:kv + 1]),
                             reads=[pk, 'b1'], writes=['uu'])
                        S.op('dve', lambda e: e.tensor_tensor(u2[:, 0:255], uu[:, 0:255], uu[:, 0:255], ALU.mult), reads=['uu'], writes=['u2'])
                        S.op('dve', lambda e: e.tensor_scalar(u2[:, 0:255], u2[:, 0:255], 0.044715, 1.0, ALU.mult, ALU.add), reads=['u2'], writes=['u2'])
                        S.op('dve', lambda e: e.tensor_tensor(u2[:, 0:255], u2[:, 0:255], uu[:, 0:255], ALU.mult), reads=['u2', 'uu'], writes=['u2'])
                        S.op('act', lambda e: e.activation(sg[:, 0:255], u2[:, 0:255], AF.Sigmoid, scale=1.5957691216057308), reads=['u2'], writes=['sg'])
                        S.op('dve', lambda e: e.tensor_tensor(gl_[:, 0:255], uu[:, 0:255], sg[:, 0:255], ALU.mult), reads=['uu', 'sg'], writes=[glk])

                    def tail3(it):
                        kv, g = it // 4, it % 4
                        gl_ = gl2[it % 2]
                        glk = 'gl%d' % (it % 2)
                        if kv == 0:
                            S.op('pe', lambda e: e.matmul(pC[0][0:64, 0:255], W2[:, 0, :], gl_[:, 0:255], start=True, stop=True),
                                 reads=['W2', glk], writes=['pC0'])
                            S.op('dve', lambda e: e.tensor_copy(kcbT[:, g, 0:255], pC[0][0:64, 0:255]), reads=['pC0'], writes=['kcbT'])
                        else:
                            for c in range(2):
                                rows = 128 if c == 0 else 127
                                S.op('pe', lambda e, c=c, rows=rows: e.matmul(pC[c][0:rows, 0:64], gl_[:, c * 128:c * 128 + rows], W2[:, 1, :],
                                                                              start=True, stop=True),
                                     reads=['W2', glk], writes=['pC%d' % c])
                                S.op('dve', lambda e, c=c, rows=rows: e.tensor_copy(vcb[0:rows, g, c, 0:64], pC[c][0:rows, 0:64]),
                                     reads=['pC%d' % c], writes=['vcb'])

                    for it in range(8):
                        head3(it)
                        if it >= 1:
                            tail3(it - 1)
                    tail3(7)
                S.barrier()

            if stages >= 4:
                with ExitStack() as st:
                    sb = lambda n, s, d: st.enter_context(nc.sbuf_tensor(n, list(s), d))
                    qg = [sb("p4_qg%d" % i, [64, 4, OWN], BF16) for i in range(2)]
                    cbt = [sb("p4_cb%d" % i, [128, 2, 4, 128], BF16) for i in range(2)]
                    E = [sb("p4_E%d" % i, [128, 512], BF16) for i in range(4)]
                    selc = sb("p4_selc", [128, NS, 64], F32)
                    rec = sb("p4_rec", [128, 4], F32)
                    oc = [sb("p4_oc%d" % i, [128, 4, 64], F32) for i in range(2)]
                    imp = sb("p4_imp", [128, 64], F32)
                    score = sb("p4_score", [128, 64], F32)
                    sc2 = sb("p4_sc2", [128, 64], F32)
                    m8a = sb("p4_m8a", [128, 8], F32)
                    m8b = sb("p4_m8b", [128, 8], F32)
                    selb = sb("p4_selb", [128, 128], F32)
                    S.op('dve', lambda e: e.memset(selb[:], 0.0), writes=['selb'])
                    S.dma('sp', selc[:], selc_d.rearrange("s q j -> q s j"), writes=['selc'])
                    it = 0
                    for g in range(4):
                        q_ = qg[g % 2]
                        qk = 'qg%d' % (g % 2)
                        S.dma('sp', q_[:], qnT[4 * g:4 * g + 4].rearrange("h d t -> d h t"), writes=[qk])
                        for s in range(NS):
                            cb = cbt[it % 2]
                            ck = 'cb%d' % (it % 2)
                            o_ = oc[it % 2]
                            ok = 'oc%d' % (it % 2)
                            it += 1
                            S.dma('pool', cb[:], cbias_d[s, :, :, 4 * g:4 * g + 4, :].rearrange("c n h q -> n c h q"), writes=[ck])
                            for c in range(2):
                                pa = pA[c]
                                pk = 'pA%d' % c
                                S.op('pe', lambda e, c=c, pa=pa, g=g, s=s, q_=q_: e.matmul(
                                    pa[:], kcbT[:, g, c * 128:(c + 1) * 128], q_[:, :, s * 128:(s + 1) * 128], start=True, stop=False),
                                    reads=['kcbT', qk], writes=[pk])
                                S.op('pe', lambda e, c=c, pa=pa, cb=cb: e.matmul(pa[:], identb[:], cb[:, c], start=False, stop=True),
                                     reads=['identb', ck], writes=[pk])
                                ei = (it % 2) * 2 + c
                                S.op('act', lambda e, pa=pa, ei=ei: e.activation(E[ei][:], pa[:], AF.Exp), reads=[pk], writes=['E%d' % ei])
                            for h in range(4):
                                po = pO[h // 2]
                                col = (h % 2) * 129
                                for c in range(2):
                                    ei = (it % 2) * 2 + c
                                    S.op('pe', lambda e, po=po, col=col, ei=ei, h=h, g=g, c=c: e.matmul(
                                        po[:, col:col + 129], E[ei][:, h * 128:(h + 1) * 128], vcb[:, g, c, :], start=(c == 0), stop=(c == 1)),
                                        reads=['E%d' % ei, 'vcb'], writes=['pO%d' % (h // 2)])
                            for h in range(4):
                                po = pO[h // 2]
                                col = (h % 2) * 129
                                S.op('dve', lambda e, po=po, col=col, h=h: e.tensor_scalar(rec[:, h:h + 1], po[:, col + 64:col + 65], 1e-30, None, ALU.max),
                                     reads=['pO%d' % (h // 2)], writes=['rec'])
                            S.op('dve', lambda e: e.reciprocal(rec[:], rec[:]), reads=['rec'], writes=['rec'])
                            for h in range(4):
                                po = pO[h // 2]
                                col = (h % 2) * 129
                                S.op('dve', lambda e, po=po, col=col, h=h, o_=o_: e.tensor_scalar(o_[:, h, :], po[:, col:col + 64], rec[:, h:h + 1], None, ALU.mult),
                                     reads=['pO%d' % (h // 2), 'rec'], writes=[ok])
                                if h == 0:
                                    S.op('dve', lambda e, po=po, col=col, h=h: e.tensor_scalar(imp[:], po[:, col + 65:col + 129], rec[:, h:h + 1], None, ALU.mult),
                                         reads=['pO%d' % (h // 2), 'rec'], writes=['imp'])
                                else:
                                    S.op('dve', lambda e, po=po, col=col, h=h: e.scalar_tensor_tensor(
                                        imp[:], po[:, col + 65:col + 129], rec[:, h:h + 1], imp[:], ALU.mult, ALU.add),
                                        reads=['pO%d' % (h // 2), 'rec', 'imp'], writes=['imp'])
                            S.dma('sp', obr_s[0, s * 128:(s + 1) * 128, g * 256:(g + 1) * 256], o_[:].rearrange("p h d -> p (h d)"), reads=[ok], writes=['sc_o'])
                            S.op('dve', lambda e, s=s: e.tensor_tensor(score[:], imp[:], selc[:, s, :], ALU.add), reads=['imp', 'selc'], writes=['score'])
                            S.op('dve', lambda e: e.max(m8a[:], score[:]), reads=['score'], writes=['m8a'])
                            S.op('dve', lambda e: e.match_replace(sc2[:], m8a[:], score[:], -3.0e38), reads=['score', 'm8a'], writes=['sc2'])
                            S.op('dve', lambda e: e.max(m8b[:], sc2[:]), reads=['sc2'], writes=['m8b'])
                            S.op('dve', lambda e: e.tensor_scalar(selb[:, 64:128], score[:], m8b[:, 7:8], -NEGM, ALU.is_ge, ALU.mult),
                                 reads=['score', 'm8b'], writes=['selb'])
                            S.op('dve', lambda e: e.tensor_scalar(selb[:, 64:128], selb[:, 64:128], NEGM, None, ALU.add), reads=['selb'], writes=['selb'])
                            S.op('pe', lambda e: e.transpose(pM[:, 0:128], selb[:], identf[:]), reads=['selb', 'identf'], writes=['pM'])
                            S.op('dve', lambda e, g=g, s=s: e.tensor_copy(selT[64:128, g, s, :], pM[64:128, 0:128]), reads=['pM'], writes=['selT'])
                S.barrier()

            def attn_phase(sel):
                with ExitStack() as st:
                    sb = lambda n, s, d: st.enter_context(nc.sbuf_tensor(n, list(s), d))
                    nm = "p5" if sel else "p6"
                    nab = 12 if sel else 8
                    AB = sb(nm + "_AB", [128, nab, 16, 128], BF16)
                    ab_src = abs_d if sel else abw_d
                    for u in range(nab):
                        S.dma('pool', AB[:, u], ab_src[u], writes=['AB'])
                    KR = 128 if sel else 64
                    kT = [sb(nm + "_kT%d" % i, [KR, T], BF16) for i in range(2)]
                    vt = [sb(nm + "_vt%d" % i, [128, NB, 65], BF16) for i in range(2)]
                    qg = [sb(nm + "_qg%d" % i, [KR, 4, OWN], BF16) for i in range(2)]
                    if sel:
                        for i in range(2):
                            S.dma('pool', kT[i][64:128, :].rearrange("j (b k) -> j b k", k=128), expand_d.rearrange("b j k -> j b k"),
                                  writes=['kT%d' % i])
                    E = [sb(nm + "_E%d" % i, [128, 512], BF16) for i in range(3)]
                    rec = sb(nm + "_rec", [128, 4], F32)
                    ot = [sb(nm + "_ot%d" % i, [128, 4, 64], F32) for i in range(4)]
                    ksrc = ksT if sel else kwT
                    voff = 0 if sel else 256
                    it = 0
                    ne = 0
                    def load_group(g):
                        k_ = kT[g % 2]
                        kk = 'kT%d' % (g % 2)
                        v_ = vt[g % 2]
                        vk = 'vt%d' % (g % 2)
                        q_ = qg[g % 2]
                        qk = 'qg%d' % (g % 2)
                        S.dma('sp', k_[0:64, :], ksrc[g], writes=[kk])
                        S.op('dve', lambda e: e.memset(v_[:, :, 64:65], 1.0), writes=[vk])
                        S.dma('sp', v_[:, :, 0:64], vsw[:, voff + g * 64:voff + (g + 1) * 64].rearrange("(b p) d -> p b d", p=128), writes=[vk])
                        S.dma('sp', q_[0:64], qnT[4 * g:4 * g + 4].rearrange("h d t -> d h t"), writes=[qk])
                        if sel:
                            for h in range(4):
                                S.op('pool', lambda e, h=h: e.tensor_copy(q_[64:128, h, :], selT[64:128, g].rearrange("j s q -> j (s q)")),
                                     reads=['selT'], writes=[qk])

                    load_group(0)
                    for g in range(4):
                        k_ = kT[g % 2]
                        kk = 'kT%d' % (g % 2)
                        v_ = vt[g % 2]
                        vk = 'vt%d' % (g % 2)
                        q_ = qg[g % 2]
                        qk = 'qg%d' % (g % 2)
                        if g + 1 < 4:
                            load_group(g + 1)
                        poh = [pO[0], pO[1], pC[0], pC[1]]
                        pokh = ['pO0', 'pO1', 'pC0', 'pC1']
                        items = []
                        for s in range(NS):
                            nu = 4 * s + 4 if sel else min(8, 4 * s + 4)
                            for u in range(nu):
                                items.append((s, u, nu))
                        srs = {}

                        def stageA(s, u, nu, idx):
                            if u == 0 and ((sel and s % 4 != 3) or ((not sel) and s % 4 == 0)):
                                issue_casts(1)
                            kb = 4 * s + 3 - u
                            pa = pA[idx % 2]
                            pk = 'pA%d' % (idx % 2)
                            S.op('pe', lambda e: e.matmul(pa[:], k_[:, kb * 128:(kb + 1) * 128], q_[:, :, s * 128:(s + 1) * 128], start=True, stop=False),
                                 reads=[kk, qk], writes=[pk])
                            S.op('pe', lambda e: e.matmul(pa[:], identb[:], AB[:, min(u, nab - 1), 4 * g:4 * g + 4, :], start=False, stop=True),
                                 reads=['identb', 'AB'], writes=[pk])
                            ei = idx % 3
                            S.op('act', lambda e: e.activation(E[ei][:], pa[:], AF.Exp), reads=[pk], writes=['E%d' % ei])

                        def stageC(s, u, nu, idx):
                            kb = 4 * s + 3 - u
                            ei = idx % 3
                            for h in range(4):
                                S.op('pe', lambda e, h=h: e.matmul(poh[h][:, 0:65], E[ei][:, h * 128:(h + 1) * 128], v_[:, kb, :],
                                                                  start=(u == 0), stop=(u == nu - 1)),
                                     reads=['E%d' % ei, vk], writes=[pokh[h]])
                            if u == nu - 1:
                                o_ = ot[s % 4]
                                ok = 'ot%d' % (s % 4)
                                for h in range(4):
                                    S.op('dve', lambda e, h=h: e.tensor_scalar(rec[:, h:h + 1], poh[h][:, 64:65], 1e-30, None, ALU.max),
                                         reads=[pokh[h]], writes=['rec'])
                                S.op('dve', lambda e: e.reciprocal(rec[:], rec[:]), reads=['rec'], writes=['rec'])
                                for h in range(4):
                                    S.op('dve', lambda e, h=h: e.tensor_scalar(o_[:, h, :], poh[h][:, 0:64], rec[:, h:h + 1], None, ALU.mult),
                                         reads=[pokh[h], 'rec'], writes=[ok])
                                S.dma('sp', obr_s[1 if sel else 2, s * 128:(s + 1) * 128, g * 256:(g + 1) * 256], o_[:].rearrange("p h d -> p (h d)"),
                                      reads=[ok], writes=['sc_o'])

                        for idx, (s, u, nu) in enumerate(items):
                            stageA(s, u, nu, idx)
                            if idx >= 1:
                                stageC(*items[idx - 1], idx - 1)
                        stageC(*items[-1], len(items) - 1)
                S.barrier()

            if stages >= 5:
                attn_phase(True)
            if stages >= 6:
                attn_phase(False)

            if stages >= 7:
                with ExitStack() as st:
                    sb = lambda n, s, d: st.enter_context(nc.sbuf_tensor(n, list(s), d))
                    kq = [sb("p7_kq%d" % i, [128, 2, T], BF16) for i in range(2)]
                    vq = [sb("p7_vq%d" % i, [128, NB, 256], BF16) for i in range(2)]
                    qq = [sb("p7_qq%d" % i, [128, 2, NS, 256], BF16) for i in range(2)]
                    msb4 = sb("p7_msb4", [128, 4, 4, 128], BF16)
                    ef = [sb("p7_ef%d" % i, [128, 512], F32) for i in range(3)]
                    spt = [sb("p7_sp%d" % i, [128, 512], BF16) for i in range(3)]
                    acc = sb("p7_acc", [128, 512], BF16)
                    wt = [sb("p7_w%d" % i, [128, 512], BF16) for i in range(2)]
                    at = [sb("p7_a%d" % i, [128, 512], BF16) for i in range(2)]
                    osbt = [sb("p7_o%d" % i, [128, 256], F32) for i in range(4)]
                    for h in range(4):
                        S.dma('pool', msb4[:, :, h, :], msb_d.rearrange("u k q -> k u q"), writes=['msb4'])
                    it = 0
                    n7 = 0
                    def load_quad(hq):
                        k_ = kq[hq % 2]
                        kk = 'kq%d' % (hq % 2)
                        v_ = vq[hq % 2]
                        vk = 'vq%d' % (hq % 2)
                        q_ = qq[hq % 2]
                        qk = 'qq%d' % (hq % 2)
                        S.dma('sp', k_[:], kbT[4 * hq:4 * hq + 4].rearrange("(p hh) d t -> (hh d) p t", hh=2), writes=[kk])
                        S.dma('sp', v_[:], vb[:, hq * 256:(hq + 1) * 256].rearrange("(b p) d -> p b d", p=128), writes=[vk])
                        S.op('dve', lambda e: e.memset(q_[:], 0.0), writes=[qk])
                        for p_ in range(2):
                            for hh in range(2):
                                S.dma('sp', q_[hh * 64:(hh + 1) * 64, p_, :, hh * 128:(hh + 1) * 128],
                                      qbT[4 * hq + 2 * p_ + hh].rearrange("d (s q) -> d s q", q=128), writes=[qk])

                    load_quad(0)
                    for hq in range(4):
                        k_ = kq[hq % 2]
                        kk = 'kq%d' % (hq % 2)
                        v_ = vq[hq % 2]
                        vk = 'vq%d' % (hq % 2)
                        q_ = qq[hq % 2]
                        qk = 'qq%d' % (hq % 2)
                        if hq + 1 < 4:
                            load_quad(hq + 1)
                        poh = [pO[0], pO[1], pC[1], pM]
                        pokh = ['pO0', 'pO1', 'pC1', 'pM']
                        pc = pC[0]
                        pck = 'pC0'
                        items = []
                        for s in range(NS):
                            for u in range(4 * s + 4):
                                items.append((s, u, 4 * s + 4))

                        def sbA(s, u, nu, idx):
                            if u == 0:
                                issue_casts(1)
                            kb = 4 * s + 3 - u
                            pa = pA[idx % 2]
                            pk = 'pA%d' % (idx % 2)
                            i3 = idx % 3
                            for p_ in range(2):
                                S.op('pe', lambda e, p_=p_: e.matmul(
                                    pa[:, p_ * 256:(p_ + 1) * 256], k_[:, p_, kb * 128:(kb + 1) * 128], q_[:, p_, s, :],
                                    start=True, stop=(u >= 4)), reads=[kk, qk], writes=[pk])
                                if u < 4:
                                    S.op('pe', lambda e, p_=p_: e.matmul(pa[:, p_ * 256:(p_ + 1) * 256], identb[:], msb4[:, u, 2 * p_:2 * p_ + 2, :],
                                                                       start=False, stop=True),
                                         reads=['identb', 'msb4'], writes=[pk])
                            S.op('act', lambda e: e.activation(ef[i3][:], pa[:], AF.Exp), reads=[pk], writes=['ef%d' % i3])
                            S.op('act', lambda e: e.activation(spt[i3][:], ef[i3][:], AF.Ln, bias=1.0), reads=['ef%d' % i3], writes=['sp%d' % i3])

                        def sbB(s, u, nu, idx):
                            i3 = idx % 3
                            i2 = idx % 2
                            S.op('pe', lambda e: e.matmul(pc[:], trib[:], spt[i3][:], start=True, stop=(u == 0)),
                                 reads=['trib', 'sp%d' % i3], writes=[pck])
                            if u > 0:
                                S.op('pe', lambda e: e.matmul(pc[:], onesb[:], acc[:], start=False, stop=True),
                                     reads=['onesb', 'acc'], writes=[pck])
                            if u < nu - 1:
                                if u == 0:
                                    S.op('pool', lambda e: e.tensor_copy(acc[:], spt[i3][:]), reads=['sp%d' % i3], writes=['acc'])
                                else:
                                    S.op('pool', lambda e: e.tensor_tensor(acc[:], acc[:], spt[i3][:], ALU.add),
                                         reads=['sp%d' % i3, 'acc'], writes=['acc'])
                            S.op('act', lambda e: e.activation(wt[i2][:], pc[:], AF.Exp, scale=-1.0), reads=[pck], writes=['w%d' % i2])
                            S.op('dve', lambda e: e.tensor_tensor(at[i2][:], ef[i3][:], wt[i2][:], ALU.mult),
                                 reads=['ef%d' % i3, 'w%d' % i2], writes=['a%d' % i2])

                        def sbC(s, u, nu, idx):
                            kb = 4 * s + 3 - u
                            i2 = idx % 2
                            for h in range(4):
                                S.op('pe', lambda e, h=h: e.matmul(
                                    poh[h][:, 0:64], at[i2][:, h * 128:(h + 1) * 128], v_[:, kb, h * 64:(h + 1) * 64],
                                    start=(u == 0), stop=(u == nu - 1)), reads=['a%d' % i2, vk], writes=[pokh[h]])
                            if u == nu - 1:
                                ob = osbt[s % 4]
                                obk = 'osbt%d' % (s % 4)
                                for h in range(4):
                                    evac(ob[:, h * 64:(h + 1) * 64], poh[h][:, 0:64], [pokh[h]], [obk])
                                S.dma('sp', osb_s[s * 128:(s + 1) * 128, hq * 256:(hq + 1) * 256], ob[:], reads=[obk], writes=['sc_osb'])

                        n_it = len(items)
                        for idx in range(n_it + 2):
                            if idx < n_it:
                                sbA(*items[idx], idx)
                            if 1 <= idx <= n_it:
                                sbB(*items[idx - 1], idx - 1)
                            if idx >= 2:
                                sbC(*items[idx - 2], idx - 2)
                S.barrier()

            if 'p37' in DEBUG:
                S.dma('sp', dbg_out("obr", [3, OWN, 1024]), obr_s)
                S.dma('sp', dbg_out("osb", [OWN, 1024]), osb_s)
                dk = dbg_out("kcbT", [64, 4, 256], BF16)
                S.dma('sp', dk, kcbT[:])
                dv = dbg_out("vcb", [128, 4, 2, 129], BF16)
                S.dma('sp', dv, vcb[:])
                dsel = dbg_out("selT", [64, 4, NS, 128], BF16)
                S.dma('sp', dsel, selT[64:128])

        if stages >= 8:
            with ExitStack() as st8:
                mTall = st8.enter_context(nc.sbuf_tensor("p8_mTall", [128, 16, OWN], BF16))
                with ExitStack() as st:
                    sb = lambda n, s, d: st.enter_context(nc.sbuf_tensor(n, list(s), d))
                    Wbn = sb("p8_Wbn", [128, 8, D], BF16)
                    Wbs = sb("p8_Wbs", [128, 8, D], BF16)
                    ob2 = [[sb("p8_ob%d_%d" % (i, j), [128, 16, 64], F32) for i in range(3)] for j in range(2)]
                    osb_t2 = [sb("p8_osb%d" % j, [128, 1024], F32) for j in range(2)]
                    gbrt2 = [sb("p8_gbr%d" % j, [128, 48], F32) for j in range(2)]
                    onsa = sb("p8_onsa", [128, 16, 64], F32)
                    tmp = sb("p8_tmp", [128, 16, 64], F32)
                    onb = sb("p8_onb", [128, 1024], BF16)
                    osb16 = sb("p8_osb16", [128, 1024], BF16)
                    onT = sb("p8_onT", [128, 8, 128], BF16)
                    osT = sb("p8_osT", [128, 8, 128], BF16)
                    gab2 = [sb("p8_gab%d" % j, [128, 4096], F32) for j in range(2)]
                    m1 = sb("p8_m1", [128, 512], F32)
                    m2 = sb("p8_m2", [128, 512], F32)
                    mb = sb("p8_mb", [128, D], BF16)
                    wbn_v = wbn_d.rearrange("(c p) n -> p c n", p=128)
                    wbs_v = wbs_d.rearrange("(c p) n -> p c n", p=128)
                    for c4 in range(0, 8, 4):
                        S.dma('pool', Wbn[:, c4:c4 + 4, :], wbn_v[:, c4:c4 + 4, :], writes=['Wbn'])
                        S.dma('pool', Wbs[:, c4:c4 + 4, :], wbs_v[:, c4:c4 + 4, :], writes=['Wbs'])
                    for ti in range(8):
                        rows = slice(ti * 128, (ti + 1) * 128)
                        jb_ = ti % 2
                        ob, osb_t, gbrt, gab = ob2[jb_], osb_t2[jb_], gbrt2[jb_], gab2[jb_]
                        kx = '_%d' % jb_
                        for i in range(3):
                            S.dma('sp', ob[i][:].rearrange("p h d -> p (h d)"), obr_s[i, rows, :], writes=['ob%d' % i + kx])
                        S.dma('sp', osb_t[:], osb_s[rows, :], writes=['osb_t' + kx])
                        S.dma('sp', gbrt[:], gbr_s[rows, :], writes=['gbrt' + kx])
                        S.dma('sp', gab[:], gab_s[rows, :], writes=['gab' + kx])
                        gv = lambda i: bc(gbrt[:, i * 16:(i + 1) * 16].unsqueeze(2), [128, 16, 64])
                        S.op('dve', lambda e: e.tensor_tensor(onsa[:], ob[0][:], gv(0), ALU.mult), reads=['ob0' + kx, 'gbrt' + kx], writes=['onsa'])
                        S.op('dve', lambda e: e.tensor_tensor(tmp[:], ob[1][:], gv(1), ALU.mult), reads=['ob1' + kx, 'gbrt' + kx], writes=['tmp'])
                        S.op('dve', lambda e: e.tensor_tensor(onsa[:], onsa[:], tmp[:], ALU.add), reads=['onsa', 'tmp'], writes=['onsa'])
                        S.op('dve', lambda e: e.tensor_tensor(tmp[:], ob[2][:], gv(2), ALU.mult), reads=['ob2' + kx, 'gbrt' + kx], writes=['tmp'])
                        S.op('dve', lambda e: e.tensor_tensor(onb[:].rearrange("p (h d) -> p h d", d=64), onsa[:], tmp[:], ALU.add),
                             reads=['onsa', 'tmp'], writes=['onb'])
                        S.op('act', lambda e: e.activation(osb16[:], osb_t[:], AF.Copy), reads=['osb_t' + kx], writes=['osb16'])
                        for (src, sk, dstT, dk) in ((onb, 'onb', onT, 'onT'), (osb16, 'osb16', osT, 'osT')):
                            for cc in range(8):
                                S.op('pe', lambda e, cc=cc, src=src: e.transpose(pT[:, cc * 128:(cc + 1) * 128], src[:, cc * 128:(cc + 1) * 128], identb[:]),
                                     reads=[sk, 'identb'], writes=['pT'])
                            S.op('dve', lambda e, dstT=dstT: e.tensor_copy(dstT[:], pT[:].rearrange("p (c t) -> p c t", c=8)),
                                 reads=['pT'], writes=[dk])
                        for cb in range(4):
                            cs = slice(cb * 512, (cb + 1) * 512)
                            for c in range(8):
                                S.op('pe', lambda e, c=c, cs=cs: e.matmul(pA[0][:], onT[:, c, :], Wbn[:, c, cs], start=(c == 0), stop=(c == 7)),
                                     reads=['onT', 'Wbn'], writes=['pA0'])
                            for c in range(8):
                                S.op('pe', lambda e, c=c, cs=cs: e.matmul(pA[1][:], osT[:, c, :], Wbs[:, c, cs], start=(c == 0), stop=(c == 7)),
                                     reads=['osT', 'Wbs'], writes=['pA1'])
                            S.op('dve', lambda e, cs=cs: e.tensor_tensor(m1[:], pA[0][:], gab[:, cs], ALU.mult), reads=['pA0', 'gab' + kx], writes=['m1'])
                            S.op('dve', lambda e, cb=cb: e.tensor_tensor(m2[:], pA[1][:], gab[:, 2048 + cb * 512:2048 + (cb + 1) * 512], ALU.mult),
                                 reads=['pA1', 'gab' + kx], writes=['m2'])
                            S.op('dve', lambda e, cs=cs: e.tensor_tensor(mb[:, cs], m1[:], m2[:], ALU.add), reads=['m1', 'm2'], writes=['mb'])
                        for half in range(2):
                            for cc in range(8):
                                c = half * 8 + cc
                                S.op('pe', lambda e, cc=cc, c=c: e.transpose(pT[:, cc * 128:(cc + 1) * 128], mb[:, c * 128:(c + 1) * 128], identb[:]),
                                     reads=['mb', 'identb'], writes=['pT'])
                            S.op('dve', lambda e, half=half, ti=ti: e.tensor_copy(
                                mTall[:, half * 8:(half + 1) * 8, ti * 128:(ti + 1) * 128], pT[:].rearrange("p (c t) -> p c t", c=8)),
                                reads=['pT'], writes=['mTall'])
                S.barrier()
                with ExitStack() as st:
                    sb = lambda n, s, d: st.enter_context(nc.sbuf_tensor(n, list(s), d))
                    Wout = sb("p8_Wout", [128, 16, D], BF16)
                    xo = [sb("p8_xo%d" % i, [128, D], F32) for i in range(2)]
                    h1t = [sb("p8_h1t%d" % i, [128, D], F32) for i in range(2)]
                    wo_v = wout_d.rearrange("(c p) n -> p c n", p=128)
                    for c4 in range(0, 16, 4):
                        S.dma('pool', Wout[:, c4:c4 + 4, :], wo_v[:, c4:c4 + 4, :], writes=['Wout'])
                    for ti in range(8):
                        rows = slice(ti * 128, (ti + 1) * 128)
                        x_ = xo[ti % 2]
                        xk = 'xo%d' % (ti % 2)
                        h_ = h1t[ti % 2]
                        hk = 'h1t%d' % (ti % 2)
                        S.dma('sp', x_[:], x_own[rows, :], writes=[xk])
                        for cb in range(4):
                            cs = slice(cb * 512, (cb + 1) * 512)
                            pa = pA[cb % 2]
                            pk = 'pA%d' % (cb % 2)
                            for c in range(16):
                                S.op('pe', lambda e, c=c, cs=cs, pa=pa, ti=ti: e.matmul(pa[:], mTall[:, c, ti * 128:(ti + 1) * 128], Wout[:, c, cs],
                                                                                 start=(c == 0), stop=(c == 15)),
                                     reads=['mTall', 'Wout'], writes=[pk])
                            S.op('dve', lambda e, cs=cs, pa=pa, h_=h_, x_=x_: e.tensor_tensor(h_[:, cs], pa[:], x_[:, cs], ALU.add),
                                 reads=[pk, xk], writes=[hk])
                        S.dma('sp', h1_s[rows, :], h_[:], reads=[hk], writes=['sc_h1'])
            S.barrier()

        if 'p8' in DEBUG:
            S.dma('sp', dbg_out("h1", [OWN, D]), h1_s)

        if stages >= 9:
            with ExitStack() as st:
                sb = lambda n, s, d: st.enter_context(nc.sbuf_tensor(n, list(s), d))
                Wq = sb("p9_Wq", [128, 16, D], BF16)
                skt = sb("p9_skt", [128, 16, 128], BF16)
                gffn = sb("p9_gffn", [128, D], F32)
                gfin = sb("p9_gfin", [128, D], F32)
                iota16 = sb("p9_iota", [128, 16], F32)
                h1t = sb("p9_h1t", [128, D], F32)
                ss = sb("p9_ss", [128, 1], F32)
                rstd = sb("p9_rstd", [128, 1], F32)
                hn = sb("p9_hn", [128, D], F32)
                hnb = sb("p9_hnb", [128, D], BF16)
                hnT = sb("p9_hnT", [128, 16, 128], BF16)
                qTs = sb("p9_qTs", [128, 16, 128], BF16)
                sc = sb("p9_sc", [128, 16, 128], F32)
                sc2 = sb("p9_sc2", [128, 256], F32)
                tv = sb("p9_tv", [128, 16, 16], F32)
                tiu = sb("p9_tiu", [128, 16, 16], U32)
                tif = sb("p9_tif", [128, 16, 16], F32)
                cand = sb("p9_cand", [128, 8, 256], F32)
                bs = sb("p9_bs", [128, 8, 16], F32)
                bju = sb("p9_bju", [128, 8, 16], U32)
                bjf = sb("p9_bjf", [128, 8, 16], F32)
                ja = sb("p9_ja", [128, 8, 16], F32)
                jb = sb("p9_jb", [128, 8, 16], F32)
                eq = sb("p9_eq", [128, 8, 16, 16], BF16)
                i0 = sb("p9_i0", [128, 8, 16], F32)
                i1 = sb("p9_i1", [128, 8, 16], F32)
                exf = sb("p9_exf", [128, 128], F32)
                exi = sb("p9_exi", [128, 128], I32)
                gm = sb("p9_gm", [128, 8], F32)
                ge = sb("p9_ge", [128, 8, 16], F32)
                gz = sb("p9_gz", [128, 8], F32)
                pre = sb("p9_pre", [128, 128], F32)
                t1 = sb("p9_t1", [128, 128], F32)
                t2 = sb("p9_t2", [128, 128], F32)
                coef = sb("p9_coef", [128, 128], F32)
                gbuf = [sb("p9_gb%d" % i, [128, D], BF16) for i in range(4)]
                DB = 16
                dgb = [sb("p9_dgb%d" % i, [128, DB, 128], BF16) for i in range(2)]
                junkb = sb("p9_junkb", [128, D], BF16)
                wq_v = wq_d.rearrange("(c p) n -> p c n", p=128)
                for c4 in range(0, 16, 4):
                    S.dma('pool', Wq[:, c4:c4 + 4, :], wq_v[:, c4:c4 + 4, :], writes=['Wq'])
                S.dma('pool', skt[:], skT_d.rearrange("a c k -> c a k"), writes=['skt'])
                S.dma('sp', gffn[:], g_ffn.partition_broadcast(128), writes=['gffn'])
                S.dma('sp', gfin[:], g_fin.partition_broadcast(128), writes=['gfin'])
                S.dma('sp', iota16[:], iota_d, writes=['iota16'])
                thr16 = sb("p9_thr16", [128, 16], F32)
                S.op('dve', lambda e: e.tensor_scalar(thr16[:], iota16[:], 16.0, 16.0, ALU.mult, ALU.add), reads=['iota16'], writes=['thr16'])
                ng = 0
                pvacc = [pO[0], pO[1], pC[0], pC[1]]
                pvk = ['pO0', 'pO1', 'pC0', 'pC1']
                issue_casts(len(cast_jobs))
                S.wait_bg(['pool'])
                NGX = 5
                h1t2 = [h1t, sb("p9_h1tb", [128, D], F32)]
                hnb2 = [hnb, sb("p9_hnbb", [128, D], BF16)]
                exi2 = [exi, sb("p9_exib", [128, 128], I32)]
                ge2 = [ge, sb("p9_geb", [128, 8, 16], F32)]
                pre2 = [pre, sb("p9_preb", [128, 128], F32)]
                coef2 = [coef, sb("p9_coefb", [128, 128], F32)]
                ssf = sb("p9_ssf", [128, 1], F32)
                rstdf = sb("p9_rstdf", [128, 1], F32)
                gbuf.extend([sb("p9_gbx%d" % i, [128, D], BF16) for i in range(NGX)])
                NG = len(gbuf)
                ngc = [0]

                def top16(vals, vkey, width, out_v, out_i, okey):
                    S.op('dve', lambda e: e.max(out_v[:, 0:8], vals), reads=[vkey], writes=[okey])
                    S.op('dve', lambda e: e.max_index(out_i[:, 0:8], out_v[:, 0:8], vals), reads=[vkey, okey], writes=[okey + 'i'])
                    S.op('dve', lambda e: e.match_replace(sc2[:, 0:width], out_v[:, 0:8], vals, -3.0e38), reads=[vkey, okey], writes=['sc2'])
                    S.op('dve', lambda e: e.max(out_v[:, 8:16], sc2[:, 0:width]), reads=['sc2'], writes=[okey])
                    S.op('dve', lambda e: e.max_index(out_i[:, 8:16], out_v[:, 8:16], sc2[:, 0:width]), reads=['sc2', okey], writes=[okey + 'i'])

                def front(t):
                    b = t % 2
                    h1t, exi, ge, hnb = h1t2[b], exi2[b], ge2[b], hnb2[b]
                    hk, nk, xk, gek, hbk = 'h1t%d' % b, 'hn', 'exi%d' % b, 'ge%d' % b, 'hnb%d' % b
                    rows = slice(t * 128, (t + 1) * 128)
                    S.dma('sp', h1t[:], h1_s[rows, :], writes=[hk])
                    norm_rows(h1t[:], hk, ss, rstd, hnb, 'p9', jkey=hbk)
                    S.op('dve', lambda e: e.scalar_tensor_tensor(hn[:], h1t[:], rstd[:, 0:1], gffn[:], ALU.mult, ALU.mult),
                         reads=[hk, 'p9rstd', 'gffn'], writes=[nk])
                    S.op('act', lambda e: e.activation(hnb[:], hn[:], AF.Copy), reads=[nk], writes=[hbk])
                    yield
                    for half in range(2):
                        for cc in range(8):
                            c = half * 8 + cc
                            S.op('pe', lambda e, cc=cc, c=c: e.transpose(pT[:, cc * 128:(cc + 1) * 128], hnb[:, c * 128:(c + 1) * 128], identb[:]),
                                 reads=[hbk, 'identb'], writes=['pT'])
                        S.op('dve', lambda e, half=half: e.tensor_copy(hnT[:, half * 8:(half + 1) * 8, :], pT[:].rearrange("p (c t) -> p c t", c=8)),
                             reads=['pT'], writes=['hnT'])
                        yield
                    for b4 in range(4):
                        pa = pA[b4 % 2]
                        pk = 'pA%d' % (b4 % 2)
                        for bb in range(4):
                            blk = b4 * 4 + bb
                            for c in range(16):
                                S.op('pe', lambda e, c=c, blk=blk, bb=bb, pa=pa: e.matmul(
                                    pa[:, bb * 128:(bb + 1) * 128], Wq[:, c, blk * 128:(blk + 1) * 128], hnT[:, c, :],
                                    start=(c == 0), stop=(c == 15)), reads=['Wq', 'hnT'], writes=[pk])
                        evac(qTs[:, b4 * 4:(b4 + 1) * 4, :], pa[:].rearrange("p (b t) -> p b t", b=4), [pk], ['qTs'])
                        yield
                    for b4 in range(4):
                        pa = pA[b4 % 2]
                        pk = 'pA%d' % (b4 % 2)
                        for bb in range(4):
                            blk = b4 * 4 + bb
                            S.op('pe', lambda e, blk=blk, bb=bb, pa=pa: e.matmul(
                                pa[:, bb * 128:(bb + 1) * 128], qTs[:, blk, :], skt[:, blk, :], start=True, stop=True),
                                reads=['qTs', 'skt'], writes=[pk])
                        evac(sc[:, b4 * 4:(b4 + 1) * 4, :], pa[:].rearrange("p (b t) -> p b t", b=4), [pk], ['sc'])
                        yield
                    for blk in range(16):
                        top16(sc[:, blk, :], 'sc', 128, tv[:, blk, :], tiu[:, blk, :], 'tv')
                        yield
                    S.op('dve', lambda e: e.tensor_copy(tif[:], tiu[:]), reads=['tvi'], writes=['tif'])
                    tvv = tv[:].rearrange("p (h two) k -> p h two k", two=2)
                    tfv = tif[:].rearrange("p (h two) k -> p h two k", two=2)
                    S.op('dve', lambda e: e.tensor_tensor(
                        cand[:].rearrange("p h (a b) -> p h a b", b=16),
                        bc(tvv[:, :, 0, :].unsqueeze(3), [128, 8, 16, 16]),
                        bc(tvv[:, :, 1, :].unsqueeze(2), [128, 8, 16, 16]), ALU.add), reads=['tv'], writes=['cand'])
                    yield
                    for h in range(8):
                        top16(cand[:, h, :], 'cand', 256, bs[:, h, :], bju[:, h, :], 'bs')
                        yield
                    S.op('dve', lambda e: e.tensor_copy(bjf[:], bju[:]), reads=['bsi'], writes=['bjf'])
                    S.op('dve', lambda e: e.tensor_tensor(eq[:], bc(bjf[:].unsqueeze(3), [128, 8, 16, 16]),
                                                          bc(thr16[:].unsqueeze(1).unsqueeze(1), [128, 8, 16, 16]), ALU.is_ge),
                         reads=['bjf', 'thr16'], writes=['eq'])
                    S.op('dve', lambda e: e.tensor_reduce(ja[:], eq[:], AX.X, ALU.add), reads=['eq'], writes=['ja'])
                    S.op('dve', lambda e: e.scalar_tensor_tensor(jb[:], ja[:], -16.0, bjf[:], ALU.mult, ALU.add), reads=['ja', 'bjf'], writes=['jb'])
                    yield
                    iob = bc(iota16[:].unsqueeze(1).unsqueeze(1), [128, 8, 16, 16])
                    for (jx, jk, two, ix, ik) in ((ja, 'ja', 0, i0, 'i0'), (jb, 'jb', 1, i1, 'i1')):
                        S.op('dve', lambda e, jx=jx: e.tensor_tensor(eq[:], iob, bc(jx[:].unsqueeze(3), [128, 8, 16, 16]), ALU.is_equal),
                             reads=['iota16', jk], writes=['eq'])
                        S.op('dve', lambda e, two=two: e.tensor_tensor(eq[:], eq[:], bc(tfv[:, :, two, :].unsqueeze(2), [128, 8, 16, 16]), ALU.mult),
                             reads=['eq', 'tif'], writes=['eq'])
                        S.op('dve', lambda e, ix=ix: e.tensor_reduce(ix[:], eq[:], AX.X, ALU.add), reads=['eq'], writes=[ik])
                        yield
                    S.op('dve', lambda e: e.scalar_tensor_tensor(exf[:].rearrange("p (h k) -> p h k", k=16), i0[:], 128.0, i1[:], ALU.mult, ALU.add),
                         reads=['i0', 'i1'], writes=['exf'])
                    S.op('dve', lambda e: e.tensor_copy(exi[:], exf[:]), reads=['exf'], writes=[xk])
                    yield
                    S.op('dve', lambda e: e.tensor_reduce(gm[:], bs[:], AX.X, ALU.max), reads=['bs'], writes=['gm'])
                    S.op('dve', lambda e: e.tensor_tensor(ge[:], bs[:], bc(gm[:].unsqueeze(2), [128, 8, 16]), ALU.subtract), reads=['bs', 'gm'], writes=[gek])
                    S.op('act', lambda e: e.activation(ge[:], ge[:], AF.Exp), reads=[gek], writes=[gek])
                    S.op('dve', lambda e: e.tensor_reduce(gz[:], ge[:], AX.X, ALU.add), reads=[gek], writes=['gz'])
                    S.op('dve', lambda e: e.reciprocal(gz[:], gz[:]), reads=['gz'], writes=['gz'])
                    S.op('dve', lambda e: e.tensor_tensor(ge[:], ge[:], bc(gz[:].unsqueeze(2), [128, 8, 16]), ALU.mult), reads=[gek, 'gz'], writes=[gek])

                def ustep(t, slot):
                    b = t % 2
                    gb_ = gbuf[ngc[0] % NG]
                    gk = 'gb%d' % (ngc[0] % NG)
                    ngc[0] += 1
                    S.dma('pool', None, None, reads=['exi%d' % b], writes=[gk],
                          fn=lambda e: e.indirect_dma_start(
                              out=gb_[:], out_offset=None, in_=pu16[:, :],
                              in_offset=bass.IndirectOffsetOnAxis(ap=exi2[b][:, slot:slot + 1], axis=0)))
                    S.op('dve', lambda e: e.scalar_tensor_tensor(
                        junkb[:], gb_[:], 1.0, hnb2[b][:], ALU.mult, ALU.mult,
                        accum_out=pre2[b][:, slot:slot + 1]), reads=[gk, 'hnb%d' % b], writes=['junkb', 'pre%d' % b])

                def midstep(t):
                    b = t % 2
                    pre, coef, ge = pre2[b], coef2[b], ge2[b]
                    pk_, ck_ = 'pre%d' % b, 'coef%d' % b
                    S.op('dve', lambda e: e.tensor_tensor(t1[:], pre[:], pre[:], ALU.mult), reads=[pk_], writes=['t1'])
                    S.op('dve', lambda e: e.tensor_scalar(t1[:], t1[:], 0.044715, 1.0, ALU.mult, ALU.add), reads=['t1'], writes=['t1'])
                    S.op('dve', lambda e: e.tensor_tensor(t1[:], t1[:], pre[:], ALU.mult), reads=['t1', pk_], writes=['t1'])
                    S.op('act', lambda e: e.activation(t2[:], t1[:], AF.Sigmoid, scale=1.5957691216057308), reads=['t1'], writes=['t2'])
                    S.op('dve', lambda e: e.tensor_tensor(t2[:], t2[:], pre[:], ALU.mult), reads=['t2', pk_], writes=['t2'])
                    S.op('dve', lambda e: e.tensor_tensor(coef[:], t2[:], ge[:].rearrange("p h k -> p (h k)"), ALU.mult),
                         reads=['t2', 'ge%d' % b], writes=[ck_])
                    build_diag(t, 0)

                def build_diag(t, k):
                    b = t % 2
                    S.op('dve', lambda e: e.tensor_tensor(
                        dgb[k % 2][:], bc(identf[:].unsqueeze(1), [128, DB, 128]),
                        bc(coef2[b][:, k * DB:(k + 1) * DB].unsqueeze(2), [128, DB, 128]), ALU.mult),
                        reads=['identf', 'coef%d' % b], writes=['dgb%d' % (k % 2)])

                def vstep(t, slot):
                    b = t % 2
                    gb_ = gbuf[ngc[0] % NG]
                    gk = 'gb%d' % (ngc[0] % NG)
                    ngc[0] += 1
                    S.dma('pool', None, None, reads=['exi%d' % b], writes=[gk],
                          fn=lambda e: e.indirect_dma_start(
                              out=gb_[:], out_offset=None, in_=pv16[:, :],
                              in_offset=bass.IndirectOffsetOnAxis(ap=exi2[b][:, slot:slot + 1], axis=0)))
                    if slot % DB == 0 and slot + DB < 128:
                        build_diag(t, slot // DB + 1)
                    dgt = dgb[(slot // DB) % 2]
                    dk = 'dgb%d' % ((slot // DB) % 2)
                    for cb in range(4):
                        S.op('pe', lambda e, cb=cb: e.matmul(
                            pvacc[cb][:], dgt[:, slot % DB, :], gb_[:, cb * 512:(cb + 1) * 512], start=(slot == 0), stop=(slot == 127)),
                            reads=[dk, gk], writes=[pvk[cb]])

                def finstep(t):
                    b = t % 2
                    rows = slice(t * 128, (t + 1) * 128)
                    acc = h1t2[b]
                    ak = 'h1t%d' % b
                    for cb in range(4):
                        S.op('dve', lambda e, cb=cb: e.tensor_tensor(acc[:, cb * 512:(cb + 1) * 512], pvacc[cb][:], acc[:, cb * 512:(cb + 1) * 512], ALU.add),
                             reads=[pvk[cb], ak], writes=[ak])
                    norm_rows(acc[:], ak, ssf, rstdf, hnb2[b], 'p9f', jkey='hnb%d' % b)
                    S.op('dve', lambda e: e.scalar_tensor_tensor(acc[:], acc[:], rstdf[:, 0:1], gfin[:], ALU.mult, ALU.mult),
                         reads=[ak, 'p9frstd', 'gfin'], writes=[ak])
                    S.dma('sp', out_d[rows, :], acc[:], reads=[ak], writes=['out'])

                for _ in front(0):
                    pass
                for slot in range(128):
                    ustep(0, slot)
                midstep(0)
                for t in range(8):
                    if t + 1 < 8:
                        fg = front(t + 1)
                        HEAD = 96
                        for slot in range(HEAD):
                            vstep(t, slot)
                            next(fg, None)
                        for _ in fg:
                            pass
                        vs_, us_ = HEAD, 0
                        while vs_ < 128 or us_ < 128:
                            if us_ < 128:
                                ustep(t + 1, us_)
                                us_ += 1
                            if vs_ < 128 and (vs_ - HEAD) * 128 < us_ * (128 - HEAD):
                                vstep(t, vs_)
                                vs_ += 1
                    else:
                        for slot in range(128):
                            vstep(t, slot)
                    finstep(t)
                    if t + 1 < 8:
                        midstep(t + 1)

        S.finish()
    return nc, dbg


def host_consts(j, rel_table):
    tab = np.asarray(rel_table, np.float32)
    k = np.arange(128)[:, None]
    q = np.arange(128)[None, :]
    abs_t = np.empty((12, 128, 16, 128), np.float32)
    for u in range(12):
        if u == 11:
            abs_t[u] = tab[31][None, :, None]
            continue
        dist = 128 * (u + j - 3) + q - k
        val = tab[_bucket(dist)]
        val = np.where((dist >= 0)[:, :, None], val, np.float32(NEGM))
        abs_t[u] = val.transpose(0, 2, 1)
    abw_t = np.empty((8, 128, 16, 128), np.float32)
    for u in range(8):
        dist = 128 * (u + j - 3) + q - k
        val = tab[_bucket(dist)]
        ok = (dist >= 0) & (dist < 512)
        val = np.where(ok[:, :, None], val, np.float32(NEGM))
        abw_t[u] = val.transpose(0, 2, 1)
    msb = np.empty((4, 128, 128), np.float32)
    for u in range(4):
        dist = 128 * (u + j - 3) + q - k
        msb[u] = np.where(dist >= 1, np.float32(0), np.float32(NEGM))
    cb = np.empty((NS, 2, 128, 16, 128), np.float32)
    selc = np.empty((NS, 128, 64), np.float32)
    for s in range(NS):
        t = (4 * s + j) * 128 + np.arange(128)
        n = np.arange(256)
        dist = t[None, :] - (16 * n[:, None] + 31)
        val = tab[_bucket(dist)]
        ok = (dist >= 0) & (n[:, None] < 255)
        val = np.where(ok[:, :, None], val, np.float32(NEGM)).transpose(0, 2, 1)
        cb[s] = val.reshape(2, 128, 16, 128)
        cur = t[:, None] // 64
        jb = np.arange(64)[None, :]
        valid = jb <= cur
        forced = (jb == 0) | ((cur - jb >= 0) & (cur - jb < 2))
        selc[s] = np.where(valid, np.where(forced, np.float32(1e4), np.float32(0)), np.float32(-1e30))
    return dict(abs=abs_t, abw=abw_t, msb=msb, cbias=cb, selc=selc)


def shared_consts():
    n_cmp = 255
    c_start = np.arange(n_cmp) * 16
    s_start = np.arange(64) * 64
    ov = np.maximum(np.minimum(c_start[:, None] + 32, s_start[None, :] + 64)
                    - np.maximum(c_start[:, None], s_start[None, :]), 0).astype(np.float32) / 32
    ovl = np.zeros((256, 64), np.float32)
    ovl[:255] = ov
    expand = np.zeros((NB, 64, 128), np.float32)
    for kb in range(NB):
        for kk in range(128):
            expand[kb, (128 * kb + kk) // 64, kk] = 1.0
    jj = np.arange(128)[:, None]
    ss = np.arange(128)[None, :]
    tri = (jj >= ss).astype(np.float32)
    iota = np.tile(np.arange(16, dtype=np.float32)[None, :], (128, 1))
    return dict(ovl=ovl.reshape(2, 128, 64), expand=expand, tri=tri, ident=np.eye(128, dtype=np.float32), iota16=iota)


def prep_inputs(inp):
    f = lambda a: np.ascontiguousarray(np.asarray(a, dtype=np.float32))
    x = f(inp['x'])
    shared = shared_consts()
    shared.update(
        w_in=f(inp['w_in'][0]), g_attn=f(inp['attn_norm_g'][0]), g_ffn=f(inp['ffn_norm_g'][0]),
        g_fin=f(inp['final_norm_g']),
        ckw1=f(np.stack([inp['cmp_k_w1'][0], inp['cmp_v_w1'][0]])),
        ckpeT=f(np.stack([np.asarray(inp['cmp_k_pe'][0]).T, np.asarray(inp['cmp_v_pe'][0]).T])),
        ckw2=f(np.stack([inp['cmp_k_w2'][0], inp['cmp_v_w2'][0]])),
        wbn=f(inp['w_branch_nsa'][0]), wbs=f(inp['w_branch_sb'][0]), wout=f(inp['w_out'][0]),
        wq=f(inp['peer_w_q'][0]),
        skT=f(np.asarray(inp['peer_sub_keys'][0]).reshape(16, 128, 128).transpose(0, 2, 1)),
        pu=f(inp['peer_u'][0]), pv=f(inp['peer_v'][0]),
    )
    per_j = [host_consts(j, inp['rel_bias_table']) for j in range(4)]
    in_maps = []
    for c in range(8):
        b, j = c // 4, c % 4
        own = np.concatenate([np.arange((4 * s + j) * 128, (4 * s + j + 1) * 128) for s in range(NS)])
        m = dict(shared)
        m.update(per_j[j])
        m['x_full'] = x[b]
        m['x_own'] = np.ascontiguousarray(x[b][own])
        in_maps.append(m)
    return in_maps


def own_index(j):
    return np.concatenate([np.arange((4 * s + j) * 128, (4 * s + j + 1) * 128) for s in range(NS)])


def kernel(**inputs):
    in_maps = prep_inputs(inputs)
    nc, dbg = build_program()
    res = run_bass_kernel_spmd(nc, in_maps, core_ids=list(range(8)))
    out = np.empty((2, T, D), np.float32)
    for c in range(8):
        b, j = c // 4, c % 4
        out[b, own_index(j)] = res.results[c]["out"]
    return out
```

```python
import math
from contextlib import ExitStack

import numpy as np
import concourse.bass as bass
import concourse.mybir as mybir
from concourse.bass_utils import run_bass_kernel_spmd

F32 = mybir.dt.float32
BF16 = mybir.dt.bfloat16
I32 = mybir.dt.int32
U32 = mybir.dt.uint32
ALU = mybir.AluOpType
AF = mybir.ActivationFunctionType
AX = mybir.AxisListType

T = 4096
D = 2048
NB = 32
NS = 8
OWN = 1024
NEGM = -30000.0
EPS = 1e-6
IN_COLS = 9776
C_QN, C_KC, C_VC, C_KS, C_VS, C_KW, C_VW, C_GBR, C_QB, C_KB, C_VB, C_GA, C_GB = (
    0, 1024, 1280, 1536, 1792, 2048, 2304, 2560, 2608, 3632, 4656, 5680, 7728)

DEBUG = {}


class Sched:
    NDS = 29
    NHW = 20

    def __init__(self, nc, stack):
        self.nc = nc
        self.eng = {'pe': nc.tensor, 'dve': nc.vector, 'act': nc.scalar,
                    'pool': nc.gpsimd, 'sp': nc.sync}
        self.sem = {k: stack.enter_context(nc.semaphore('s_' + k)) for k in self.eng}
        self.cnt = {k: 0 for k in self.eng}
        self.waited = {}
        self.dsem = [stack.enter_context(nc.semaphore('d%d' % i)) for i in range(self.NDS)]
        self.dcnt = [0] * self.NDS
        self.dnext = 0
        self.dnext_sw = 0
        self.bgsem = [stack.enter_context(nc.semaphore('bg%d' % i)) for i in range(64)]
        self.bgcnt = [0] * 64
        self.bgnext = 0
        self.last_w = {}
        self.readers = {}
        self.ninst = 0

    def _wait(self, e, tok):
        if tok is None:
            return
        if tok[0] == 'e':
            _, p, n = tok
            if p == 'pe' and e == 'pe':
                return
            key = (e, 'e', p)
            if self.waited.get(key, 0) >= n:
                return
            self.eng[e].wait_ge(self.sem[p], n)
            self.waited[key] = n
        else:
            _, slot, v = tok
            key = (e, 'd', slot)
            if self.waited.get(key, 0) >= v:
                return
            self.eng[e].wait_ge(self.dsem[slot], v)
            self.waited[key] = v

    def _deps(self, e, reads, writes):
        toks = []
        for k in reads:
            toks.append(self.last_w.get(k))
        for k in writes:
            toks.append(self.last_w.get(k))
            toks.extend(self.readers.get(k, []))
        for t in toks:
            self._wait(e, t)

    def _update(self, tok, reads, writes):
        for k in reads:
            self.readers.setdefault(k, []).append(tok)
        for k in writes:
            self.last_w[k] = tok
            self.readers[k] = []

    def op(self, e, fn, reads=(), writes=()):
        self._deps(e, reads, writes)
        ins = fn(self.eng[e])
        self.cnt[e] += 1
        ins.then_inc(self.sem[e], 1)
        tok = ('e', e, self.cnt[e])
        self._update(tok, reads, writes)
        self.ninst += 1
        return tok

    def dma(self, q, out, in_, reads=(), writes=(), fn=None, **kw):
        if q == 'pool':
            slot = self.NHW + self.dnext_sw
            self.dnext_sw = (self.dnext_sw + 1) % (self.NDS - self.NHW)
        else:
            slot = self.dnext
            self.dnext = (self.dnext + 1) % self.NHW
        if self.dcnt[slot] > 0:
            self._wait(q, ('d', slot, self.dcnt[slot]))
        self._deps(q, reads, writes)
        if fn is not None:
            ins = fn(self.eng[q])
        else:
            ins = self.eng[q].dma_start(out=out, in_=in_, **kw)
        ins.then_inc(self.dsem[slot], 16)
        self.dcnt[slot] += 16
        tok = ('d', slot, self.dcnt[slot])
        self._update(tok, reads, writes)
        self.ninst += 1
        return tok

    def bg_dma(self, q, out, in_):
        slot = self.bgnext
        self.bgnext = (self.bgnext + 1) % len(self.bgsem)
        if self.bgcnt[slot] > 0:
            key = (q, 'bg', slot)
            if self.waited.get(key, 0) < self.bgcnt[slot]:
                self.eng[q].wait_ge(self.bgsem[slot], self.bgcnt[slot])
                self.waited[key] = self.bgcnt[slot]
        self.eng[q].dma_start(out=out, in_=in_).then_inc(self.bgsem[slot], 16)
        self.bgcnt[slot] += 16
        self.ninst += 1

    def wait_bg(self, engines):
        for e in engines:
            for slot in range(len(self.bgsem)):
                if self.bgcnt[slot] > 0:
                    self.eng[e].wait_ge(self.bgsem[slot], self.bgcnt[slot])

    def barrier(self):
        for e in self.eng:
            for p in self.eng:
                if p != e and self.cnt[p] > 0:
                    self._wait(e, ('e', p, self.cnt[p]))
            for s in range(self.NDS):
                if self.dcnt[s] > 0:
                    self._wait(e, ('d', s, self.dcnt[s]))
        self.last_w = {}
        self.readers = {}

    def finish(self):
        e = 'sp'
        self.wait_bg([e])
        for p in self.eng:
            if p != e and self.cnt[p] > 0:
                self._wait(e, ('e', p, self.cnt[p]))
        for s in range(self.NDS):
            if self.dcnt[s] > 0:
                self._wait(e, ('d', s, self.dcnt[s]))


def _bucket(dist):
    dist = np.maximum(dist, 0)
    d_f = np.maximum(dist, 1).astype(np.float32)
    large = 16 + (np.log(d_f / np.float32(16)) / np.float32(math.log(1024 / 16)) * np.float32(16)).astype(np.int32)
    large = np.minimum(large, 31)
    return np.where(dist < 16, dist, large).astype(np.int64)


def _bucket_jax(dist):
    return _bucket(dist)


def build_program(stages=99):
    nc = bass.Bass("TRN2", target_bir_lowering=False)
    dt_in = lambda n, s, d=F32: nc.dram_tensor(n, list(s), d, kind="ExternalInput").ap()
    dt_sc = lambda n, s, d: nc.dram_tensor(n, list(s), d, kind="Internal").ap()
    x_full = dt_in("x_full", [T, D])
    x_own = dt_in("x_own", [OWN, D])
    w_in = dt_in("w_in", [D, IN_COLS])
    g_attn = dt_in("g_attn", [D])
    g_ffn = dt_in("g_ffn", [D])
    g_fin = dt_in("g_fin", [D])
    ident_d = dt_in("ident", [128, 128])
    tri_d = dt_in("tri", [128, 128])
    abs_d = dt_in("abs", [12, 128, 16, 128])
    abw_d = dt_in("abw", [8, 128, 16, 128])
    msb_d = dt_in("msb", [4, 128, 128])
    cbias_d = dt_in("cbias", [NS, 2, 128, 16, 128])
    selc_d = dt_in("selc", [NS, 128, 64])
    ovl_d = dt_in("ovl", [2, 128, 64])
    expand_d = dt_in("expand", [NB, 64, 128])
    ckw1_d = dt_in("ckw1", [2, 2048, 128])
    ckpe_d = dt_in("ckpeT", [2, 64, 32])
    ckw2_d = dt_in("ckw2", [2, 128, 64])
    wbn_d = dt_in("wbn", [1024, D])
    wbs_d = dt_in("wbs", [1024, D])
    wout_d = dt_in("wout", [D, D])
    wq_d = dt_in("wq", [D, D])
    skT_d = dt_in("skT", [16, 128, 128])
    pu_d = dt_in("pu", [16384, D])
    pv_d = dt_in("pv", [16384, D])
    iota_d = dt_in("iota16", [128, 16])
    out_d = nc.dram_tensor("out", [OWN, D], F32, kind="ExternalOutput").ap()
    dbg = {}

    def dbg_out(name, shape, dtype=F32):
        dbg[name] = nc.dram_tensor("dbg_" + name, list(shape), dtype, kind="ExternalOutput").ap()
        return dbg[name]

    kcvT = dt_sc("kcvT", [2, 4, 64, T], BF16)
    ksT = dt_sc("ksT", [4, 64, T], BF16)
    kwT = dt_sc("kwT", [4, 64, T], BF16)
    kbT = dt_sc("kbT", [16, 64, T], BF16)
    vsw = dt_sc("vsw", [T, 512], BF16)
    vb = dt_sc("vb", [T, 1024], BF16)
    qnT = dt_sc("qnT", [16, 64, OWN], BF16)
    qbT = dt_sc("qbT", [16, 64, OWN], BF16)
    gbr_s = dt_sc("gbr_s", [OWN, 48], F32)
    gab_s = dt_sc("gab_s", [OWN, 4096], F32)
    obr_s = dt_sc("obr_s", [3, OWN, 1024], F32)
    osb_s = dt_sc("osb_s", [OWN, 1024], F32)
    h1_s = dt_sc("h1_s", [OWN, D], F32)
    pu16 = dt_sc("pu16", [16384, D], BF16)
    pv16 = dt_sc("pv16", [16384, D], BF16)

    with ExitStack() as gst:
        S = Sched(nc, gst)
        gsb = lambda n, s, d: gst.enter_context(nc.sbuf_tensor(n, list(s), d))
        gps = lambda n, s, d: gst.enter_context(nc.psum_tensor(n, list(s), d))
        pA = [gps("pA%d" % i, [128, 512], F32) for i in range(2)]
        pC = [gps("pC%d" % i, [128, 512], F32) for i in range(2)]
        pO = [gps("pO%d" % i, [128, 512], F32) for i in range(2)]
        pT = gps("pT", [128, 1024], BF16)
        pM = gps("pM", [128, 512], F32)

        identf = gsb("identf", [128, 128], F32)
        identb = gsb("identb", [128, 128], BF16)
        trib = gsb("trib", [128, 128], BF16)
        onesb = gsb("onesb", [128, 128], BF16)

        S.dma('sp', identf[:], ident_d, writes=['identf'])
        S.op('dve', lambda e: e.tensor_copy(identb[:], identf[:]), reads=['identf'], writes=['identb'])
        S.dma('pool', trib[:], tri_d, writes=['trib'])
        S.op('dve', lambda e: e.memset(onesb[:], 1.0), writes=['onesb'])

        rr = {'ev': 0}
        cast_jobs = []
        for r0 in range(0, 16384, 512):
            cast_jobs.append((pu16[r0:r0 + 512, :], pu_d[r0:r0 + 512, :]))
            cast_jobs.append((pv16[r0:r0 + 512, :], pv_d[r0:r0 + 512, :]))

        def issue_casts(n):
            for _ in range(n):
                if cast_jobs:
                    o_, i_ = cast_jobs.pop(0)
                    S.bg_dma('pool', o_, i_)

        def evac(out_ap, in_ap, reads, writes, scale=None):
            rr['ev'] += 1
            if rr['ev'] % 2 == 0:
                if scale is None:
                    S.op('dve', lambda e: e.tensor_copy(out_ap, in_ap), reads=reads, writes=writes)
                else:
                    S.op('dve', lambda e: e.tensor_scalar(out_ap, in_ap, scale, None, ALU.mult), reads=reads, writes=writes)
            else:
                S.op('act', lambda e: e.activation(out_ap, in_ap, AF.Copy, scale=(1.0 if scale is None else scale)),
                     reads=reads, writes=writes)

        def make_gfull(st, g_ap, name):
            gt = st.enter_context(nc.sbuf_tensor(name + "_gt", [128, 16], F32))
            gfull = st.enter_context(nc.sbuf_tensor(name + "_gf", [128, 16, 128], BF16))
            S.dma('sp', gt[:], g_ap.rearrange("(c p) -> p c", p=128), writes=[name + 'gt'],
                  allow_slow_non_contiguous=True)
            S.op('dve', lambda e: e.memset(gfull[:], 1.0), writes=[name + 'gf'])
            for c in range(16):
                S.op('dve', lambda e, c=c: e.tensor_scalar(gfull[:, c, :], gfull[:, c, :], gt[:, c:c + 1], None, ALU.mult),
                     reads=[name + 'gt', name + 'gf'], writes=[name + 'gf'])
            return gfull, name + 'gf'

        def norm_rows(xt, xkey, ss, rstd, junk, key, jkey=None):
            S.op('act', lambda e: e.activation(junk[:], xt, AF.Square, accum_out=ss[:]),
                 reads=[xkey], writes=[jkey if jkey else key + 'junk', key + 'ss'])
            S.op('dve', lambda e: e.tensor_scalar(rstd[:], ss[:], 1.0 / D, EPS, ALU.mult, ALU.add),
                 reads=[key + 'ss'], writes=[key + 'rstd'])
            S.op('act', lambda e: e.activation(rstd[:], rstd[:], AF.Sqrt), reads=[key + 'rstd'], writes=[key + 'rstd'])
            S.op('dve', lambda e: e.reciprocal(rstd[:], rstd[:]), reads=[key + 'rstd'], writes=[key + 'rstd'])

        def transposeT(xs, xskey, gfull, gfkey, dst, dstkey, col0):
            for half in range(2):
                for cc in range(8):
                    c = half * 8 + cc
                    S.op('pe', lambda e, c=c, cc=cc: e.transpose(pT[:, cc * 128:(cc + 1) * 128], xs[:, c * 128:(c + 1) * 128], identb[:]),
                         reads=[xskey, 'identb'], writes=['pT'])
                S.op('dve', lambda e, half=half: e.tensor_tensor(
                    dst[:, half * 8:(half + 1) * 8, col0:col0 + 128],
                    pT[:].rearrange("p (c t) -> p c t", c=8),
                    gfull[:, half * 8:(half + 1) * 8, :], ALU.mult),
                    reads=['pT', gfkey], writes=[dstkey])

        if stages >= 1:
            with ExitStack() as st:
                sb = lambda n, s, d: st.enter_context(nc.sbuf_tensor(n, list(s), d))
                WF = sb("p1_WF", [128, 16, 2048], BF16)
                WV = sb("p1_WV", [128, 16, 1536], BF16)
                xt = [sb("p1_xt%d" % i, [128, D], F32) for i in range(2)]
                junk = sb("p1_junk", [128, D], BF16)
                ss = sb("p1_ss", [128, 1], F32)
                rstd = sb("p1_rstd", [128, 1], F32)
                xs = sb("p1_xs", [128, D], BF16)
                aT = [sb("p1_aT%d" % i, [128, 16, 512], BF16) for i in range(2)]
                stg = [sb("p1_stg%d" % i, [128, 512], BF16) for i in range(4)]
                gfull, gfkey = make_gfull(st, g_attn, "p1")
                wv = w_in.rearrange("(c p) n -> p c n", p=128)
                fm_cols = [(C_KC, 256), (C_VC, 256), (C_KS, 256), (C_KW, 256), (C_KB, 1024)]
                o = 0
                for gi_, (c0, n) in enumerate(fm_cols):
                    for c4 in range(0, 16, 4):
                        S.dma('pool', WF[:, c4:c4 + 4, o:o + n], wv[:, c4:c4 + 4, c0:c0 + n], writes=['WF%d' % gi_])
                    o += n
                tm_cols = [(C_VS, 256), (C_VW, 256), (C_VB, 1024)]
                o = 0
                for gi_, (c0, n) in enumerate(tm_cols):
                    for c4 in range(0, 16, 4):
                        S.dma('pool', WV[:, c4:c4 + 4, o:o + n], wv[:, c4:c4 + 4, c0:c0 + n], writes=['WV%d' % gi_])
                    o += n
                kcv2 = kcvT.rearrange("a g d t -> a (g d) t")
                ks2 = ksT.rearrange("g d t -> (g d) t")
                kw2 = kwT.rearrange("g d t -> (g d) t")
                kb2 = kbT.rearrange("h d t -> (h d) t")
                fm_dst = [(kcv2[0], 0), (kcv2[0], 128), (kcv2[1], 0), (kcv2[1], 128),
                          (ks2, 0), (ks2, 128), (kw2, 0), (kw2, 128)] + [(kb2, 128 * i) for i in range(8)]
                nstg = [0]
                npa = [0]
                fm_grp = [0, 0, 1, 1, 2, 2, 3, 3] + [4] * 8
                tm_grp = [['WV0', 'WV1'], ['WV2'], ['WV2']]

                def prepA(tt, ti):
                    r0 = tt * 512 + ti * 128
                    xi = (tt * 4 + ti) % 2
                    S.dma('sp', xt[xi][:], x_full[r0:r0 + 128, :], writes=['xt%d' % xi])
                    norm_rows(xt[xi][:], 'xt%d' % xi, ss, rstd, junk, 'p1')
                    S.op('dve', lambda e: e.tensor_scalar(xs[:], xt[xi][:], rstd[:, 0:1], None, ALU.mult),
                         reads=['xt%d' % xi, 'p1rstd'], writes=['xs'])

                def prepB(tt, ti):
                    transposeT(xs, 'xs', gfull, gfkey, aT[tt % 2], 'aT%d' % (tt % 2), ti * 128)

                def fm_group(tt, bi):
                    a = aT[tt % 2]
                    akey = 'aT%d' % (tt % 2)
                    pi = npa[0] % 2
                    npa[0] += 1
                    pa = pA[pi]
                    for c in range(16):
                        S.op('pe', lambda e, c=c: e.matmul(pa[:], WF[:, c, bi * 128:(bi + 1) * 128], a[:, c, :],
                                                          start=(c == 0), stop=(c == 15)),
                             reads=['WF%d' % fm_grp[bi], akey], writes=['pA%d' % pi])
                    sg = stg[nstg[0] % 4]
                    sk = 'stg%d' % (nstg[0] % 4)
                    nstg[0] += 1
                    evac(sg[:], pa[:], ['pA%d' % pi], [sk])
                    dst, row0 = fm_dst[bi]
                    S.dma('sp', dst[row0:row0 + 128, tt * 512:(tt + 1) * 512], sg[:], reads=[sk], writes=['sc_fm'])

                def tm_group(tt, ti, vbk):
                    a = aT[tt % 2]
                    akey = 'aT%d' % (tt % 2)
                    r0 = tt * 512 + ti * 128
                    pi = npa[0] % 2
                    npa[0] += 1
                    pa = pA[pi]
                    for c in range(16):
                        S.op('pe', lambda e, c=c: e.matmul(
                            pa[:], a[:, c, ti * 128:(ti + 1) * 128], WV[:, c, vbk * 512:(vbk + 1) * 512],
                            start=(c == 0), stop=(c == 15)),
                            reads=tm_grp[vbk] + [akey], writes=['pA%d' % pi])
                    sg = stg[nstg[0] % 4]
                    sk = 'stg%d' % (nstg[0] % 4)
                    nstg[0] += 1
                    evac(sg[:], pa[:], ['pA%d' % pi], [sk])
                    if vbk == 0:
                        S.dma('sp', vsw[r0:r0 + 128, :], sg[:], reads=[sk], writes=['sc_tm'])
                    else:
                        S.dma('sp', vb[r0:r0 + 128, (vbk - 1) * 512:vbk * 512], sg[:], reads=[sk], writes=['sc_tm'])

                for ti in range(4):
                    prepA(0, ti)
                    prepB(0, ti)
                NT = T // 512
                for tt in range(NT):
                    groups = [(lambda bi=bi: fm_group(tt, bi)) for bi in range(16)]
                    groups += [(lambda ti=ti, vbk=vbk: tm_group(tt, ti, vbk)) for ti in range(4) for vbk in range(3)]
                    posA = {0: 0, 7: 1, 14: 2, 21: 3}
                    posB = {4: 0, 11: 1, 18: 2, 25: 3}
                    for gi, grp in enumerate(groups):
                        if tt + 1 < NT and gi in posA:
                            prepA(tt + 1, posA[gi])
                        if tt + 1 < NT and gi in posB:
                            prepB(tt + 1, posB[gi])
                        grp()
            S.barrier()

        if stages >= 2:
            with ExitStack() as st:
                sb = lambda n, s, d: st.enter_context(nc.sbuf_tensor(n, list(s), d))
                xt = [sb("p2_xt%d" % i, [128, D], F32) for i in range(2)]
                junk = sb("p2_junk", [128, D], BF16)
                ss = sb("p2_ss", [128, 1], F32)
                rstd = sb("p2_rstd", [128, 1], F32)
                xs = sb("p2_xs", [128, D], BF16)
                aT = sb("p2_aT", [128, 16, OWN], BF16)
                Wb = [sb("p2_W%d" % i, [128, 16, 512], BF16) for i in range(2)]
                stg = [sb("p2_stg%d" % i, [128, 512], BF16) for i in range(4)]
                stgf = [sb("p2_stgf%d" % i, [128, 512], F32) for i in range(4)]
                gfull, gfkey = make_gfull(st, g_attn, "p2")
                wv = w_in.rearrange("(c p) n -> p c n", p=128)
                for ti in range(8):
                    xi = ti % 2
                    S.dma('sp', xt[xi][:], x_own[ti * 128:(ti + 1) * 128, :], writes=['xt%d' % xi])
                    norm_rows(xt[xi][:], 'xt%d' % xi, ss, rstd, junk, 'p2')
                    S.op('dve', lambda e, xi=xi: e.tensor_scalar(xs[:], xt[xi][:], rstd[:, 0:1], None, ALU.mult),
                         reads=['xt%d' % xi, 'p2rstd'], writes=['xs'])
                    transposeT(xs, 'xs', gfull, gfkey, aT, 'aTown%d' % ti, ti * 128)
                qn2 = qnT.rearrange("h d t -> (h d) t")
                qb2 = qbT.rearrange("h d t -> (h d) t")
                blocks = [('fm', C_QN + 512 * i, 512, qn2, 512 * i) for i in range(2)]
                blocks += [('fm', C_QB + 512 * i, 512, qb2, 512 * i) for i in range(2)]
                blocks += [('tm', C_GA + 512 * i, 512, gab_s, 512 * i) for i in range(8)]
                blocks += [('gbr', C_GBR, 48, gbr_s, 0)]
                nstg = 0
                for bi, (kind, c0, n, dst, d0) in enumerate(blocks):
                    W = Wb[bi % 2]
                    wk = 'W%d' % (bi % 2)
                    for c4 in range(0, 16, 4):
                        S.dma('pool', W[:, c4:c4 + 4, 0:n], wv[:, c4:c4 + 4, c0:c0 + n], writes=[wk])
                    if kind == 'fm':
                        for sub in range(4):
                            for th in range(2):
                                pa = pA[(sub * 2 + th) % 2]
                                pk = 'pA%d' % ((sub * 2 + th) % 2)
                                for c in range(16):
                                    S.op('pe', lambda e, c=c, sub=sub, th=th, pa=pa, W=W: e.matmul(
                                        pa[:], W[:, c, sub * 128:(sub + 1) * 128], aT[:, c, th * 512:(th + 1) * 512],
                                        start=(c == 0), stop=(c == 15)), reads=[wk] + ['aTown%d' % (4 * th + q4) for q4 in range(4)], writes=[pk])
                                sg = stg[nstg % 4]
                                sk = 'stg%d' % (nstg % 4)
                                nstg += 1
                                evac(sg[:], pa[:], [pk], [sk], scale=0.125)
                                S.dma('sp', dst[d0 + sub * 128:d0 + (sub + 1) * 128, th * 512:(th + 1) * 512], sg[:],
                                      reads=[sk], writes=['sc_q'])
                    else:
                        for ti in range(8):
                            pa = pA[ti % 2]
                            pk = 'pA%d' % (ti % 2)
                            for c in range(16):
                                S.op('pe', lambda e, c=c, ti=ti, pa=pa, W=W, n=n: e.matmul(
                                    pa[:, 0:n], aT[:, c, ti * 128:(ti + 1) * 128], W[:, c, 0:n],
                                    start=(c == 0), stop=(c == 15)), reads=[wk, 'aTown%d' % ti], writes=[pk])
                            sg = stgf[nstg % 4]
                            sk = 'stgf%d' % (nstg % 4)
                            nstg += 1
                            S.op('act', lambda e, sg=sg, pa=pa, n=n: e.activation(sg[:, 0:n], pa[:, 0:n], AF.Sigmoid),
                                 reads=[pk], writes=[sk])
                            if kind == 'tm':
                                S.dma('sp', dst[ti * 128:(ti + 1) * 128, d0:d0 + n], sg[:, 0:n], reads=[sk], writes=['sc_g'])
                            else:
                                S.dma('sp', dst[ti * 128:(ti + 1) * 128, :], sg[:, 0:n], reads=[sk], writes=['sc_g'])
            S.barrier()

        if 'p12' in DEBUG:
            d1 = dbg_out("ksT", [4, 64, T], BF16)
            S.dma('sp', d1, ksT, reads=[], writes=[])
            d2 = dbg_out("vb", [T, 1024], BF16)
            S.dma('sp', d2, vb, reads=[], writes=[])
            d3 = dbg_out("qnT", [16, 64, OWN], BF16)
            S.dma('sp', d3, qnT, reads=[], writes=[])
            d4 = dbg_out("gbr", [OWN, 48], F32)
            S.dma('sp', d4, gbr_s, reads=[], writes=[])
            d5 = dbg_out("gab", [OWN, 4096], F32)
            S.dma('sp', d5, gab_s, reads=[], writes=[])

        def bc(ap, shape):
            return ap.to_broadcast(list(shape))

        with ExitStack() as mid_st:
            msb_ = lambda n, s, d: mid_st.enter_context(nc.sbuf_tensor(n, list(s), d))
            kcbT = msb_("kcbT", [64, 4, 256], BF16)
            vcb = msb_("vcb", [128, 4, 2, 129], BF16)
            selT = msb_("selT", [128, 4, NS, 128], BF16)
            S.op('dve', lambda e: e.memset(kcbT[:], 0.0), writes=['kcbT'])
            S.op('dve', lambda e: e.memset(vcb[:], 0.0), writes=['vcb'])
            if stages >= 3:
                with ExitStack() as st:
                    sb = lambda n, s, d: st.enter_context(nc.sbuf_tensor(n, list(s), d))
                    kin = [sb("p3_kin%d" % i, [64, T], BF16) for i in range(2)]
                    W1 = sb("p3_W1", [64, 2, 32, 128], BF16)
                    peT = sb("p3_peT", [64, 2, 32], BF16)
                    W2 = sb("p3_W2", [128, 2, 64], BF16)
                    b1 = sb("p3_b1", [128, 2], F32)
                    uu = sb("p3_u", [128, 256], F32)
                    u2 = sb("p3_u2", [128, 256], F32)
                    sg = sb("p3_sg", [128, 256], F32)
                    gl = sb("p3_gl", [128, 256], BF16)
                    for kv in range(2):
                        S.dma('pool', W1[:, kv], ckw1_d[kv].rearrange("(l d) h -> d l h", d=64), writes=['W1'])
                        S.dma('pool', peT[:, kv, :], ckpe_d[kv], writes=['peT'])
                        S.dma('pool', W2[:, kv, :], ckw2_d[kv], writes=['W2'])
                    for g in range(4):
                        for c in range(2):
                            S.dma('pool', vcb[:, g, c, 65:129], ovl_d[c], reads=[], writes=['vcb'])
                    S.op('dve', lambda e: e.memset(vcb[:, :, :, 64:65], 1.0), writes=['vcb'])
                    for kv in range(2):
                        for l in range(32):
                            S.op('pe', lambda e, kv=kv, l=l: e.matmul(pM[:, kv:kv + 1], W1[:, kv, l, :], peT[:, kv, l:l + 1],
                                                                  start=(l == 0), stop=(l == 31)),
                                 reads=['W1', 'peT'], writes=['pM'])
                    S.op('dve', lambda e: e.tensor_copy(b1[:], pM[:, 0:2]), reads=['pM'], writes=['b1'])
                    gl2 = [gl, sb("p3_gl2", [128, 256], BF16)]

                    def head3(it):
                        kv, g = it // 4, it % 4
                        ki = kin[it % 2]
                        kk = 'kin%d' % (it % 2)
                        pa = pA[it % 2]
                        pk = 'pA%d' % (it % 2)
                        gl_ = gl2[it % 2]
                        glk = 'gl%d' % (it % 2)
                        S.dma('sp', ki[:], kcvT[kv, g], writes=[kk])
                        for l in range(32):
                            S.op('pe', lambda e, l=l: e.matmul(
                                pa[:, 0:255], W1[:, kv, l, :], ki[:, l:l + 4065:16], start=(l == 0), stop=(l == 31)),
                                reads=['W1', kk], writes=[pk])
                        S.op('act', lambda e: e.activation(uu[:, 0:255], pa[:, 0:255], AF.Identity, bias=b1[:, kv:kv + 1]),
                             reads=[pk, 'b1'], writes=['uu'])
                        S.op('dve', lambda e: e.tensor_tensor(u2[:, 0:255], uu[:, 0:255], uu[:, 0:255], ALU.mult), reads=['uu'], writes=['u2'])
                        S.op('dve', lambda e: e.tensor_scalar(u2[:, 0:255], u2[:, 0:255], 0.044715, 1.0, ALU.mult, ALU.add), reads=['u2'], writes=['u2'])
                        S.op('dve', lambda e: e.tensor_tensor(u2[:, 0:255], u2[:, 0:255], uu[:, 0:255], ALU.mult), reads=['u2', 'uu'], writes=['u2'])
                        S.op('act', lambda e: e.activation(sg[:, 0:255], u2[:, 0:255], AF.Sigmoid, scale=1.5957691216057308), reads=['u2'], writes=['sg'])
                        S.op('dve', lambda e: e.tensor_tensor(gl_[:, 0:255], uu[:, 0:255], sg[:, 0:255], ALU.mult), reads=['uu', 'sg'], writes=[glk])

                    def tail3(it):
                        kv, g = it // 4, it % 4
                        gl_ = gl2[it % 2]
                        glk = 'gl%d' % (it % 2)
                        if kv == 0:
                            S.op('pe', lambda e: e.matmul(pC[0][0:64, 0:255], W2[:, 0, :], gl_[:, 0:255], start=True, stop=True),
                                 reads=['W2', glk], writes=['pC0'])
                            S.op('dve', lambda e: e.tensor_copy(kcbT[:, g, 0:255], pC[0][0:64, 0:255]), reads=['pC0'], writes=['kcbT'])
                        else:
                            for c in range(2):
                                rows = 128 if c == 0 else 127
                                S.op('pe', lambda e, c=c, rows=rows: e.matmul(pC[c][0:rows, 0:64], gl_[:, c * 128:c * 128 + rows], W2[:, 1, :],
                                                                              start=True, stop=True),
                                     reads=['W2', glk], writes=['pC%d' % c])
                                S.op('dve', lambda e, c=c, rows=rows: e.tensor_copy(vcb[0:rows, g, c, 0:64], pC[c][0:rows, 0:64]),
                                     reads=['pC%d' % c], writes=['vcb'])

                    for it in range(8):
                        head3(it)
                        if it >= 1:
                            tail3(it - 1)
                    tail3(7)
                S.barrier()

            if stages >= 4:
                with ExitStack() as st:
                    sb = lambda n, s, d: st.enter_context(nc.sbuf_tensor(n, list(s), d))
                    qg = [sb("p4_qg%d" % i, [64, 4, OWN], BF16) for i in range(2)]
                    cbt = [sb("p4_cb%d" % i, [128, 2, 4, 128], BF16) for i in range(2)]
                    E = [sb("p4_E%d" % i, [128, 512], BF16) for i in range(4)]
                    selc = sb("p4_selc", [128, NS, 64], F32)
                    rec = sb("p4_rec", [128, 4], F32)
                    oc = [sb("p4_oc%d" % i, [128, 4, 64], F32) for i in range(2)]
                    imp = sb("p4_imp", [128, 64], F32)
                    score = sb("p4_score", [128, 64], F32)
                    sc2 = sb("p4_sc2", [128, 64], F32)
                    m8a = sb("p4_m8a", [128, 8], F32)
                    m8b = sb("p4_m8b", [128, 8], F32)
                    selb = sb("p4_selb", [128, 128], F32)
                    S.op('dve', lambda e: e.memset(selb[:], 0.0), writes=['selb'])
                    S.dma('sp', selc[:], selc_d.rearrange("s q j -> q s j"), writes=['selc'])
                    it = 0
                    for g in range(4):
                        q_ = qg[g % 2]
                        qk = 'qg%d' % (g % 2)
                        S.dma('sp', q_[:], qnT[4 * g:4 * g + 4].rearrange("h d t -> d h t"), writes=[qk])
                        for s in range(NS):
                            cb = cbt[it % 2]
                            ck = 'cb%d' % (it % 2)
                            o_ = oc[it % 2]
                            ok = 'oc%d' % (it % 2)
                            it += 1
                            S.dma('pool', cb[:], cbias_d[s, :, :, 4 * g:4 * g + 4, :].rearrange("c n h q -> n c h q"), writes=[ck])
                            for c in range(2):
                                pa = pA[c]
                                pk = 'pA%d' % c
                                S.op('pe', lambda e, c=c, pa=pa, g=g, s=s, q_=q_: e.matmul(
                                    pa[:], kcbT[:, g, c * 128:(c + 1) * 128], q_[:, :, s * 128:(s + 1) * 128], start=True, stop=False),
                                    reads=['kcbT', qk], writes=[pk])
                                S.op('pe', lambda e, c=c, pa=pa, cb=cb: e.matmul(pa[:], identb[:], cb[:, c], start=False, stop=True),
                                     reads=['identb', ck], writes=[pk])
                                ei = (it % 2) * 2 + c
                                S.op('act', lambda e, pa=pa, ei=ei: e.activation(E[ei][:], pa[:], AF.Exp), reads=[pk], writes=['E%d' % ei])
                            for h in range(4):
                                po = pO[h // 2]
                                col = (h % 2) * 129
                                for c in range(2):
                                    ei = (it % 2) * 2 + c
                                    S.op('pe', lambda e, po=po, col=col, ei=ei, h=h, g=g, c=c: e.matmul(
                                        po[:, col:col + 129], E[ei][:, h * 128:(h + 1) * 128], vcb[:, g, c, :], start=(c == 0), stop=(c == 1)),
                                        reads=['E%d' % ei, 'vcb'], writes=['pO%d' % (h // 2)])
                            for h in range(4):
                                po = pO[h // 2]
                                col = (h % 2) * 129
                                S.op('dve', lambda e, po=po, col=col, h=h: e.tensor_scalar(rec[:, h:h + 1], po[:, col + 64:col + 65], 1e-30, None, ALU.max),
                                     reads=['pO%d' % (h // 2)], writes=['rec'])
                            S.op('dve', lambda e: e.reciprocal(rec[:], rec[:]), reads=['rec'], writes=['rec'])
                            for h in range(4):
                                po = pO[h // 2]
                                col = (h % 2) * 129
                                S.op('dve', lambda e, po=po, col=col, h=h, o_=o_: e.tensor_scalar(o_[:, h, :], po[:, col:col + 64], rec[:, h:h + 1], None, ALU.mult),
                                     reads=['pO%d' % (h // 2), 'rec'], writes=[ok])
                                if h == 0:
                                    S.op('dve', lambda e, po=po, col=col, h=h: e.tensor_scalar(imp[:], po[:, col + 65:col + 129], rec[:, h:h + 1], None, ALU.mult),
                                         reads=['pO%d' % (h // 2), 'rec'], writes=['imp'])
                                else:
                                    S.op('dve', lambda e, po=po, col=col, h=h: e.scalar_tensor_tensor(
                                        imp[:], po[:, col + 65:col + 129], rec[:, h:h + 1], imp[:], ALU.mult, ALU.add),
                                        reads=['pO%d' % (h // 2), 'rec', 'imp'], writes=['imp'])
                            S.dma('sp', obr_s[0, s * 128:(s + 1) * 128, g * 256:(g + 1) * 256], o_[:].rearrange("p h d -> p (h d)"), reads=[ok], writes=['sc_o'])
                            S.op('dve', lambda e, s=s: e.tensor_tensor(score[:], imp[:], selc[:, s, :], ALU.add), reads=['imp', 'selc'], writes=['score'])
                            S.op('dve', lambda e: e.max(m8a[:], score[:]), reads=['score'], writes=['m8a'])
                            S.op('dve', lambda e: e.match_replace(sc2[:], m8a[:], score[:], -3.0e38), reads=['score', 'm8a'], writes=['sc2'])
                            S.op('dve', lambda e: e.max(m8b[:], sc2[:]), reads=['sc2'], writes=['m8b'])
                            S.op('dve', lambda e: e.tensor_scalar(selb[:, 64:128], score[:], m8b[:, 7:8], -NEGM, ALU.is_ge, ALU.mult),
                                 reads=['score', 'm8b'], writes=['selb'])
                            S.op('dve', lambda e: e.tensor_scalar(selb[:, 64:128], selb[:, 64:128], NEGM, None, ALU.add), reads=['selb'], writes=['selb'])
                            S.op('pe', lambda e: e.transpose(pM[:, 0:128], selb[:], identf[:]), reads=['selb', 'identf'], writes=['pM'])
                            S.op('dve', lambda e, g=g, s=s: e.tensor_copy(selT[64:128, g, s, :], pM[64:128, 0:128]), reads=['pM'], writes=['selT'])
                S.barrier()

            def attn_phase(sel):
                with ExitStack() as st:
                    sb = lambda n, s, d: st.enter_context(nc.sbuf_tensor(n, list(s), d))
                    nm = "p5" if sel else "p6"
                    nab = 12 if sel else 8
                    AB = sb(nm + "_AB", [128, nab, 16, 128], BF16)
                    ab_src = abs_d if sel else abw_d
                    for u in range(nab):
                        S.dma('pool', AB[:, u], ab_src[u], writes=['AB'])
                    KR = 128 if sel else 64
                    kT = [sb(nm + "_kT%d" % i, [KR, T], BF16) for i in range(2)]
                    vt = [sb(nm + "_vt%d" % i, [128, NB, 65], BF16) for i in range(2)]
                    qg = [sb(nm + "_qg%d" % i, [KR, 4, OWN], BF16) for i in range(2)]
                    if sel:
                        for i in range(2):
                            S.dma('pool', kT[i][64:128, :].rearrange("j (b k) -> j b k", k=128), expand_d.rearrange("b j k -> j b k"),
                                  writes=['kT%d' % i])
                    E = [sb(nm + "_E%d" % i, [128, 512], BF16) for i in range(3)]
                    rec = sb(nm + "_rec", [128, 4], F32)
                    ot = [sb(nm + "_ot%d" % i, [128, 4, 64], F32) for i in range(4)]
                    ksrc = ksT if sel else kwT
                    voff = 0 if sel else 256
                    it = 0
                    ne = 0
                    def load_group(g):
                        k_ = kT[g % 2]
                        kk = 'kT%d' % (g % 2)
                        v_ = vt[g % 2]
                        vk = 'vt%d' % (g % 2)
                        q_ = qg[g % 2]
                        qk = 'qg%d' % (g % 2)
                        S.dma('sp', k_[0:64, :], ksrc[g], writes=[kk])
                        S.op('dve', lambda e: e.memset(v_[:, :, 64:65], 1.0), writes=[vk])
                        S.dma('sp', v_[:, :, 0:64], vsw[:, voff + g * 64:voff + (g + 1) * 64].rearrange("(b p) d -> p b d", p=128), writes=[vk])
                        S.dma('sp', q_[0:64], qnT[4 * g:4 * g + 4].rearrange("h d t -> d h t"), writes=[qk])
                        if sel:
                            for h in range(4):
                                S.op('pool', lambda e, h=h: e.tensor_copy(q_[64:128, h, :], selT[64:128, g].rearrange("j s q -> j (s q)")),
                                     reads=['selT'], writes=[qk])

                    load_group(0)
                    for g in range(4):
                        k_ = kT[g % 2]
                        kk = 'kT%d' % (g % 2)
                        v_ = vt[g % 2]
                        vk = 'vt%d' % (g % 2)
                        q_ = qg[g % 2]
                        qk = 'qg%d' % (g % 2)
                        if g + 1 < 4:
                            load_group(g + 1)
                        poh = [pO[0], pO[1], pC[0], pC[1]]
                        pokh = ['pO0', 'pO1', 'pC0', 'pC1']
                        items = []
                        for s in range(NS):
                            nu = 4 * s + 4 if sel else min(8, 4 * s + 4)
                            for u in range(nu):
                                items.append((s, u, nu))
                        srs = {}

                        def stageA(s, u, nu, idx):
                            if u == 0 and ((sel and s % 4 != 3) or ((not sel) and s % 4 == 0)):
                                issue_casts(1)
                            kb = 4 * s + 3 - u
                            pa = pA[idx % 2]
                            pk = 'pA%d' % (idx % 2)
                            S.op('pe', lambda e: e.matmul(pa[:], k_[:, kb * 128:(kb + 1) * 128], q_[:, :, s * 128:(s + 1) * 128], start=True, stop=False),
                                 reads=[kk, qk], writes=[pk])
                            S.op('pe', lambda e: e.matmul(pa[:], identb[:], AB[:, min(u, nab - 1), 4 * g:4 * g + 4, :], start=False, stop=True),
                                 reads=['identb', 'AB'], writes=[pk])
                            ei = idx % 3
                            S.op('act', lambda e: e.activation(E[ei][:], pa[:], AF.Exp), reads=[pk], writes=['E%d' % ei])

                        def stageC(s, u, nu, idx):
                            kb = 4 * s + 3 - u
                            ei = idx % 3
                            for h in range(4):
                                S.op('pe', lambda e, h=h: e.matmul(poh[h][:, 0:65], E[ei][:, h * 128:(h + 1) * 128], v_[:, kb, :],
                                                                  start=(u == 0), stop=(u == nu - 1)),
                                     reads=['E%d' % ei, vk], writes=[pokh[h]])
                            if u == nu - 1:
                                o_ = ot[s % 4]
                                ok = 'ot%d' % (s % 4)
                                for h in range(4):
                                    S.op('dve', lambda e, h=h: e.tensor_scalar(rec[:, h:h + 1], poh[h][:, 64:65], 1e-30, None, ALU.max),
                                         reads=[pokh[h]], writes=['rec'])
                                S.op('dve', lambda e: e.reciprocal(rec[:], rec[:]), reads=['rec'], writes=['rec'])
                                for h in range(4):
                                    S.op('dve', lambda e, h=h: e.tensor_scalar(o_[:, h, :], poh[h][:, 0:64], rec[:, h:h + 1], None, ALU.mult),
                                         reads=[pokh[h], 'rec'], writes=[ok])
                                S.dma('sp', obr_s[1 if sel else 2, s * 128:(s + 1) * 128, g * 256:(g + 1) * 256], o_[:].rearrange("p h d -> p (h d)"),
                                      reads=[ok], writes=['sc_o'])

                        for idx, (s, u, nu) in enumerate(items):
                            stageA(s, u, nu, idx)
                            if idx >= 1:
                                stageC(*items[idx - 1], idx - 1)
                        stageC(*items[-1], len(items) - 1)
                S.barrier()

            if stages >= 5:
                attn_phase(True)
            if stages >= 6:
                attn_phase(False)

            if stages >= 7:
                with ExitStack() as st:
                    sb = lambda n, s, d: st.enter_context(nc.sbuf_tensor(n, list(s), d))
                    kq = [sb("p7_kq%d" % i, [128, 2, T], BF16) for i in range(2)]
                    vq = [sb("p7_vq%d" % i, [128, NB, 256], BF16) for i in range(2)]
                    qq = [sb("p7_qq%d" % i, [128, 2, NS, 256], BF16) for i in range(2)]
                    msb4 = sb("p7_msb4", [128, 4, 4, 128], BF16)
                    ef = [sb("p7_ef%d" % i, [128, 512], F32) for i in range(3)]
                    spt = [sb("p7_sp%d" % i, [128, 512], BF16) for i in range(3)]
                    acc = sb("p7_acc", [128, 512], BF16)
                    wt = [sb("p7_w%d" % i, [128, 512], BF16) for i in range(2)]
                    at = [sb("p7_a%d" % i, [128, 512], BF16) for i in range(2)]
                    osbt = [sb("p7_o%d" % i, [128, 256], F32) for i in range(4)]
                    for h in range(4):
                        S.dma('pool', msb4[:, :, h, :], msb_d.rearrange("u k q -> k u q"), writes=['msb4'])
                    it = 0
                    n7 = 0
                    def load_quad(hq):
                        k_ = kq[hq % 2]
                        kk = 'kq%d' % (hq % 2)
                        v_ = vq[hq % 2]
                        vk = 'vq%d' % (hq % 2)
                        q_ = qq[hq % 2]
                        qk = 'qq%d' % (hq % 2)
                        S.dma('sp', k_[:], kbT[4 * hq:4 * hq + 4].rearrange("(p hh) d t -> (hh d) p t", hh=2), writes=[kk])
                        S.dma('sp', v_[:], vb[:, hq * 256:(hq + 1) * 256].rearrange("(b p) d -> p b d", p=128), writes=[vk])
                        S.op('dve', lambda e: e.memset(q_[:], 0.0), writes=[qk])
                        for p_ in range(2):
                            for hh in range(2):
                                S.dma('sp', q_[hh * 64:(hh + 1) * 64, p_, :, hh * 128:(hh + 1) * 128],
                                      qbT[4 * hq + 2 * p_ + hh].rearrange("d (s q) -> d s q", q=128), writes=[qk])

                    load_quad(0)
                    for hq in range(4):
                        k_ = kq[hq % 2]
                        kk = 'kq%d' % (hq % 2)
                        v_ = vq[hq % 2]
                        vk = 'vq%d' % (hq % 2)
                        q_ = qq[hq % 2]
                        qk = 'qq%d' % (hq % 2)
                        if hq + 1 < 4:
                            load_quad(hq + 1)
                        poh = [pO[0], pO[1], pC[1], pM]
                        pokh = ['pO0', 'pO1', 'pC1', 'pM']
                        pc = pC[0]
                        pck = 'pC0'
                        items = []
                        for s in range(NS):
                            for u in range(4 * s + 4):
                                items.append((s, u, 4 * s + 4))

                        def sbA(s, u, nu, idx):
                            if u == 0:
                                issue_casts(1)
                            kb = 4 * s + 3 - u
                            pa = pA[idx % 2]
                            pk = 'pA%d' % (idx % 2)
                            i3 = idx % 3
                            for p_ in range(2):
                                S.op('pe', lambda e, p_=p_: e.matmul(
                                    pa[:, p_ * 256:(p_ + 1) * 256], k_[:, p_, kb * 128:(kb + 1) * 128], q_[:, p_, s, :],
                                    start=True, stop=(u >= 4)), reads=[kk, qk], writes=[pk])
                                if u < 4:
                                    S.op('pe', lambda e, p_=p_: e.matmul(pa[:, p_ * 256:(p_ + 1) * 256], identb[:], msb4[:, u, 2 * p_:2 * p_ + 2, :],
                                                                       start=False, stop=True),
                                         reads=['identb', 'msb4'], writes=[pk])
                            S.op('act', lambda e: e.activation(ef[i3][:], pa[:], AF.Exp), reads=[pk], writes=['ef%d' % i3])
                            S.op('act', lambda e: e.activation(spt[i3][:], ef[i3][:], AF.Ln, bias=1.0), reads=['ef%d' % i3], writes=['sp%d' % i3])

                        def sbB(s, u, nu, idx):
                            i3 = idx % 3
                            i2 = idx % 2
                            S.op('pe', lambda e: e.matmul(pc[:], trib[:], spt[i3][:], start=True, stop=(u == 0)),
                                 reads=['trib', 'sp%d' % i3], writes=[pck])
                            if u > 0:
                                S.op('pe', lambda e: e.matmul(pc[:], onesb[:], acc[:], start=False, stop=True),
                                     reads=['onesb', 'acc'], writes=[pck])
                            if u < nu - 1:
                                if u == 0:
                                    S.op('pool', lambda e: e.tensor_copy(acc[:], spt[i3][:]), reads=['sp%d' % i3], writes=['acc'])
                                else:
                                    S.op('pool', lambda e: e.tensor_tensor(acc[:], acc[:], spt[i3][:], ALU.add),
                                         reads=['sp%d' % i3, 'acc'], writes=['acc'])
                            S.op('act', lambda e: e.activation(wt[i2][:], pc[:], AF.Exp, scale=-1.0), reads=[pck], writes=['w%d' % i2])
                            S.op('dve', lambda e: e.tensor_tensor(at[i2][:], ef[i3][:], wt[i2][:], ALU.mult),
                                 reads=['ef%d' % i3, 'w%d' % i2], writes=['a%d' % i2])

                        def sbC(s, u, nu, idx):
                            kb = 4 * s + 3 - u
                            i2 = idx % 2
                            for h in range(4):
                                S.op('pe', lambda e, h=h: e.matmul(
                                    poh[h][:, 0:64], at[i2][:, h * 128:(h + 1) * 128], v_[:, kb, h * 64:(h + 1) * 64],
                                    start=(u == 0), stop=(u == nu - 1)), reads=['a%d' % i2, vk], writes=[pokh[h]])
                            if u == nu - 1:
                                ob = osbt[s % 4]
                                obk = 'osbt%d' % (s % 4)
                                for h in range(4):
                                    evac(ob[:, h * 64:(h + 1) * 64], poh[h][:, 0:64], [pokh[h]], [obk])
                                S.dma('sp', osb_s[s * 128:(s + 1) * 128, hq * 256:(hq + 1) * 256], ob[:], reads=[obk], writes=['sc_osb'])

                        n_it = len(items)
                        for idx in range(n_it + 2):
                            if idx < n_it:
                                sbA(*items[idx], idx)
                            if 1 <= idx <= n_it:
                                sbB(*items[idx - 1], idx - 1)
                            if idx >= 2:
                                sbC(*items[idx - 2], idx - 2)
                S.barrier()

            if 'p37' in DEBUG:
                S.dma('sp', dbg_out("obr", [3, OWN, 1024]), obr_s)
                S.dma('sp', dbg_out("osb", [OWN, 1024]), osb_s)
                dk = dbg_out("kcbT", [64, 4, 256], BF16)
                S.dma('sp', dk, kcbT[:])
                dv = dbg_out("vcb", [128, 4, 2, 129], BF16)
                S.dma('sp', dv, vcb[:])
                dsel = dbg_out("selT", [64, 4, NS, 128], BF16)
                S.dma('sp', dsel, selT[64:128])

        if stages >= 8:
            with ExitStack() as st8:
                mTall = st8.enter_context(nc.sbuf_tensor("p8_mTall", [128, 16, OWN], BF16))
                with ExitStack() as st:
                    sb = lambda n, s, d: st.enter_context(nc.sbuf_tensor(n, list(s), d))
                    Wbn = sb("p8_Wbn", [128, 8, D], BF16)
                    Wbs = sb("p8_Wbs", [128, 8, D], BF16)
                    ob2 = [[sb("p8_ob%d_%d" % (i, j), [128, 16, 64], F32) for i in range(3)] for j in range(2)]
                    osb_t2 = [sb("p8_osb%d" % j, [128, 1024], F32) for j in range(2)]
                    gbrt2 = [sb("p8_gbr%d" % j, [128, 48], F32) for j in range(2)]
                    onsa = sb("p8_onsa", [128, 16, 64], F32)
                    tmp = sb("p8_tmp", [128, 16, 64], F32)
                    onb = sb("p8_onb", [128, 1024], BF16)
                    osb16 = sb("p8_osb16", [128, 1024], BF16)
                    onT = sb("p8_onT", [128, 8, 128], BF16)
                    osT = sb("p8_osT", [128, 8, 128], BF16)
                    gab2 = [sb("p8_gab%d" % j, [128, 4096], F32) for j in range(2)]
                    m1 = sb("p8_m1", [128, 512], F32)
                    m2 = sb("p8_m2", [128, 512], F32)
                    mb = sb("p8_mb", [128, D], BF16)
                    wbn_v = wbn_d.rearrange("(c p) n -> p c n", p=128)
                    wbs_v = wbs_d.rearrange("(c p) n -> p c n", p=128)
                    for c4 in range(0, 8, 4):
                        S.dma('pool', Wbn[:, c4:c4 + 4, :], wbn_v[:, c4:c4 + 4, :], writes=['Wbn'])
                        S.dma('pool', Wbs[:, c4:c4 + 4, :], wbs_v[:, c4:c4 + 4, :], writes=['Wbs'])
                    for ti in range(8):
                        rows = slice(ti * 128, (ti + 1) * 128)
                        jb_ = ti % 2
                        ob, osb_t, gbrt, gab = ob2[jb_], osb_t2[jb_], gbrt2[jb_], gab2[jb_]
                        kx = '_%d' % jb_
                        for i in range(3):
                            S.dma('sp', ob[i][:].rearrange("p h d -> p (h d)"), obr_s[i, rows, :], writes=['ob%d' % i + kx])
                        S.dma('sp', osb_t[:], osb_s[rows, :], writes=['osb_t' + kx])
                        S.dma('sp', gbrt[:], gbr_s[rows, :], writes=['gbrt' + kx])
                        S.dma('sp', gab[:], gab_s[rows, :], writes=['gab' + kx])
                        gv = lambda i: bc(gbrt[:, i * 16:(i + 1) * 16].unsqueeze(2), [128, 16, 64])
                        S.op('dve', lambda e: e.tensor_tensor(onsa[:], ob[0][:], gv(0), ALU.mult), reads=['ob0' + kx, 'gbrt' + kx], writes=['onsa'])
                        S.op('dve', lambda e: e.tensor_tensor(tmp[:], ob[1][:], gv(1), ALU.mult), reads=['ob1' + kx, 'gbrt' + kx], writes=['tmp'])
                        S.op('dve', lambda e: e.tensor_tensor(onsa[:], onsa[:], tmp[:], ALU.add), reads=['onsa', 'tmp'], writes=['onsa'])
                        S.op('dve', lambda e: e.tensor_tensor(tmp[:], ob[2][:], gv(2), ALU.mult), reads=['ob2' + kx, 'gbrt' + kx], writes=['tmp'])
                        S.op('dve', lambda e: e.tensor_tensor(onb[:].rearrange("p (h d) -> p h d", d=64), onsa[:], tmp[:], ALU.add),
                             reads=['onsa', 'tmp'], writes=['onb'])
                        S.op('act', lambda e: e.activation(osb16[:], osb_t[:], AF.Copy), reads=['osb_t' + kx], writes=['osb16'])
                        for (src, sk, dstT, dk) in ((onb, 'onb', onT, 'onT'), (osb16, 'osb16', osT, 'osT')):
                            for cc in range(8):
                                S.op('pe', lambda e, cc=cc, src=src: e.transpose(pT[:, cc * 128:(cc + 1) * 128], src[:, cc * 128:(cc + 1) * 128], identb[:]),
                                     reads=[sk, 'identb'], writes=['pT'])
                            S.op('dve', lambda e, dstT=dstT: e.tensor_copy(dstT[:], pT[:].rearrange("p (c t) -> p c t", c=8)),
                                 reads=['pT'], writes=[dk])
                        for cb in range(4):
                            cs = slice(cb * 512, (cb + 1) * 512)
                            for c in range(8):
                                S.op('pe', lambda e, c=c, cs=cs: e.matmul(pA[0][:], onT[:, c, :], Wbn[:, c, cs], start=(c == 0), stop=(c == 7)),
                                     reads=['onT', 'Wbn'], writes=['pA0'])
                            for c in range(8):
                                S.op('pe', lambda e, c=c, cs=cs: e.matmul(pA[1][:], osT[:, c, :], Wbs[:, c, cs], start=(c == 0), stop=(c == 7)),
                                     reads=['osT', 'Wbs'], writes=['pA1'])
                            S.op('dve', lambda e, cs=cs: e.tensor_tensor(m1[:], pA[0][:], gab[:, cs], ALU.mult), reads=['pA0', 'gab' + kx], writes=['m1'])
                            S.op('dve', lambda e, cb=cb: e.tensor_tensor(m2[:], pA[1][:], gab[:, 2048 + cb * 512:2048 + (cb + 1) * 512], ALU.mult),
                                 reads=['pA1', 'gab' + kx], writes=['m2'])
                            S.op('dve', lambda e, cs=cs: e.tensor_tensor(mb[:, cs], m1[:], m2[:], ALU.add), reads=['m1', 'm2'], writes=['mb'])
                        for half in range(2):
                            for cc in range(8):
                                c = half * 8 + cc
                                S.op('pe', lambda e, cc=cc, c=c: e.transpose(pT[:, cc * 128:(cc + 1) * 128], mb[:, c * 128:(c + 1) * 128], identb[:]),
                                     reads=['mb', 'identb'], writes=['pT'])
                            S.op('dve', lambda e, half=half, ti=ti: e.tensor_copy(
                                mTall[:, half * 8:(half + 1) * 8, ti * 128:(ti + 1) * 128], pT[:].rearrange("p (c t) -> p c t", c=8)),
                                reads=['pT'], writes=['mTall'])
                S.barrier()
                with ExitStack() as st:
                    sb = lambda n, s, d: st.enter_context(nc.sbuf_tensor(n, list(s), d))
                    Wout = sb("p8_Wout", [128, 16, D], BF16)
                    xo = [sb("p8_xo%d" % i, [128, D], F32) for i in range(2)]
                    h1t = [sb("p8_h1t%d" % i, [128, D], F32) for i in range(2)]
                    wo_v = wout_d.rearrange("(c p) n -> p c n", p=128)
                    for c4 in range(0, 16, 4):
                        S.dma('pool', Wout[:, c4:c4 + 4, :], wo_v[:, c4:c4 + 4, :], writes=['Wout'])
                    for ti in range(8):
                        rows = slice(ti * 128, (ti + 1) * 128)
                        x_ = xo[ti % 2]
                        xk = 'xo%d' % (ti % 2)
                        h_ = h1t[ti % 2]
                        hk = 'h1t%d' % (ti % 2)
                        S.dma('sp', x_[:], x_own[rows, :], writes=[xk])
                        for cb in range(4):
                            cs = slice(cb * 512, (cb + 1) * 512)
                            pa = pA[cb % 2]
                            pk = 'pA%d' % (cb % 2)
                            for c in range(16):
                                S.op('pe', lambda e, c=c, cs=cs, pa=pa, ti=ti: e.matmul(pa[:], mTall[:, c, ti * 128:(ti + 1) * 128], Wout[:, c, cs],
                                                                                 start=(c == 0), stop=(c == 15)),
                                     reads=['mTall', 'Wout'], writes=[pk])
                            S.op('dve', lambda e, cs=cs, pa=pa, h_=h_, x_=x_: e.tensor_tensor(h_[:, cs], pa[:], x_[:, cs], ALU.add),
                                 reads=[pk, xk], writes=[hk])
                        S.dma('sp', h1_s[rows, :], h_[:], reads=[hk], writes=['sc_h1'])
            S.barrier()

        if 'p8' in DEBUG:
            S.dma('sp', dbg_out("h1", [OWN, D]), h1_s)

        if stages >= 9:
            with ExitStack() as st:
                sb = lambda n, s, d: st.enter_context(nc.sbuf_tensor(n, list(s), d))
                Wq = sb("p9_Wq", [128, 16, D], BF16)
                skt = sb("p9_skt", [128, 16, 128], BF16)
                gffn = sb("p9_gffn", [128, D], F32)
                gfin = sb("p9_gfin", [128, D], F32)
                iota16 = sb("p9_iota", [128, 16], F32)
                h1t = sb("p9_h1t", [128, D], F32)
                ss = sb("p9_ss", [128, 1], F32)
                rstd = sb("p9_rstd", [128, 1], F32)
                hn = sb("p9_hn", [128, D], F32)
                hnb = sb("p9_hnb", [128, D], BF16)
                hnT = sb("p9_hnT", [128, 16, 128], BF16)
                qTs = sb("p9_qTs", [128, 16, 128], BF16)
                sc = sb("p9_sc", [128, 16, 128], F32)
                sc2 = sb("p9_sc2", [128, 256], F32)
                tv = sb("p9_tv", [128, 16, 16], F32)
                tiu = sb("p9_tiu", [128, 16, 16], U32)
                tif = sb("p9_tif", [128, 16, 16], F32)
                cand = sb("p9_cand", [128, 8, 256], F32)
                bs = sb("p9_bs", [128, 8, 16], F32)
                bju = sb("p9_bju", [128, 8, 16], U32)
                bjf = sb("p9_bjf", [128, 8, 16], F32)
                ja = sb("p9_ja", [128, 8, 16], F32)
                jb = sb("p9_jb", [128, 8, 16], F32)
                eq = sb("p9_eq", [128, 8, 16, 16], BF16)
                i0 = sb("p9_i0", [128, 8, 16], F32)
                i1 = sb("p9_i1", [128, 8, 16], F32)
                exf = sb("p9_exf", [128, 128], F32)
                exi = sb("p9_exi", [128, 128], I32)
                gm = sb("p9_gm", [128, 8], F32)
                ge = sb("p9_ge", [128, 8, 16], F32)
                gz = sb("p9_gz", [128, 8], F32)
                pre = sb("p9_pre", [128, 128], F32)
                t1 = sb("p9_t1", [128, 128], F32)
                t2 = sb("p9_t2", [128, 128], F32)
                coef = sb("p9_coef", [128, 128], F32)
                gbuf = [sb("p9_gb%d" % i, [128, D], BF16) for i in range(4)]
                DB = 16
                dgb = [sb("p9_dgb%d" % i, [128, DB, 128], BF16) for i in range(2)]
                junkb = sb("p9_junkb", [128, D], BF16)
                wq_v = wq_d.rearrange("(c p) n -> p c n", p=128)
                for c4 in range(0, 16, 4):
                    S.dma('pool', Wq[:, c4:c4 + 4, :], wq_v[:, c4:c4 + 4, :], writes=['Wq'])
                S.dma('pool', skt[:], skT_d.rearrange("a c k -> c a k"), writes=['skt'])
                S.dma('sp', gffn[:], g_ffn.partition_broadcast(128), writes=['gffn'])
                S.dma('sp', gfin[:], g_fin.partition_broadcast(128), writes=['gfin'])
                S.dma('sp', iota16[:], iota_d, writes=['iota16'])
                thr16 = sb("p9_thr16", [128, 16], F32)
                S.op('dve', lambda e: e.tensor_scalar(thr16[:], iota16[:], 16.0, 16.0, ALU.mult, ALU.add), reads=['iota16'], writes=['thr16'])
                ng = 0
                pvacc = [pO[0], pO[1], pC[0], pC[1]]
                pvk = ['pO0', 'pO1', 'pC0', 'pC1']
                issue_casts(len(cast_jobs))
                S.wait_bg(['pool'])
                NGX = 5
                h1t2 = [h1t, sb("p9_h1tb", [128, D], F32)]
                hnb2 = [hnb, sb("p9_hnbb", [128, D], BF16)]
                exi2 = [exi, sb("p9_exib", [128, 128], I32)]
                ge2 = [ge, sb("p9_geb", [128, 8, 16], F32)]
                pre2 = [pre, sb("p9_preb", [128, 128], F32)]
                coef2 = [coef, sb("p9_coefb", [128, 128], F32)]
                ssf = sb("p9_ssf", [128, 1], F32)
                rstdf = sb("p9_rstdf", [128, 1], F32)
                gbuf.extend([sb("p9_gbx%d" % i, [128, D], BF16) for i in range(NGX)])
                NG = len(gbuf)
                ngc = [0]

                def top16(vals, vkey, width, out_v, out_i, okey):
                    S.op('dve', lambda e: e.max(out_v[:, 0:8], vals), reads=[vkey], writes=[okey])
                    S.op('dve', lambda e: e.max_index(out_i[:, 0:8], out_v[:, 0:8], vals), reads=[vkey, okey], writes=[okey + 'i'])
                    S.op('dve', lambda e: e.match_replace(sc2[:, 0:width], out_v[:, 0:8], vals, -3.0e38), reads=[vkey, okey], writes=['sc2'])
                    S.op('dve', lambda e: e.max(out_v[:, 8:16], sc2[:, 0:width]), reads=['sc2'], writes=[okey])
                    S.op('dve', lambda e: e.max_index(out_i[:, 8:16], out_v[:, 8:16], sc2[:, 0:width]), reads=['sc2', okey], writes=[okey + 'i'])

                def front(t):
                    b = t % 2
                    h1t, exi, ge, hnb = h1t2[b], exi2[b], ge2[b], hnb2[b]
                    hk, nk, xk, gek, hbk = 'h1t%d' % b, 'hn', 'exi%d' % b, 'ge%d' % b, 'hnb%d' % b
                    rows = slice(t * 128, (t + 1) * 128)
                    S.dma('sp', h1t[:], h1_s[rows, :], writes=[hk])
                    norm_rows(h1t[:], hk, ss, rstd, hnb, 'p9', jkey=hbk)
                    S.op('dve', lambda e: e.scalar_tensor_tensor(hn[:], h1t[:], rstd[:, 0:1], gffn[:], ALU.mult, ALU.mult),
                         reads=[hk, 'p9rstd', 'gffn'], writes=[nk])
                    S.op('act', lambda e: e.activation(hnb[:], hn[:], AF.Copy), reads=[nk], writes=[hbk])
                    yield
                    for half in range(2):
                        for cc in range(8):
                            c = half * 8 + cc
                            S.op('pe', lambda e, cc=cc, c=c: e.transpose(pT[:, cc * 128:(cc + 1) * 128], hnb[:, c * 128:(c + 1) * 128], identb[:]),
                                 reads=[hbk, 'identb'], writes=['pT'])
                        S.op('dve', lambda e, half=half: e.tensor_copy(hnT[:, half * 8:(half + 1) * 8, :], pT[:].rearrange("p (c t) -> p c t", c=8)),
                             reads=['pT'], writes=['hnT'])
                        yield
                    for b4 in range(4):
                        pa = pA[b4 % 2]
                        pk = 'pA%d' % (b4 % 2)
                        for bb in range(4):
                            blk = b4 * 4 + bb
                            for c in range(16):
                                S.op('pe', lambda e, c=c, blk=blk, bb=bb, pa=pa: e.matmul(
                                    pa[:, bb * 128:(bb + 1) * 128], Wq[:, c, blk * 128:(blk + 1) * 128], hnT[:, c, :],
                                    start=(c == 0), stop=(c == 15)), reads=['Wq', 'hnT'], writes=[pk])
                        evac(qTs[:, b4 * 4:(b4 + 1) * 4, :], pa[:].rearrange("p (b t) -> p b t", b=4), [pk], ['qTs'])
                        yield
                    for b4 in range(4):
                        pa = pA[b4 % 2]
                        pk = 'pA%d' % (b4 % 2)
                        for bb in range(4):
                            blk = b4 * 4 + bb
                            S.op('pe', lambda e, blk=blk, bb=bb, pa=pa: e.matmul(
                                pa[:, bb * 128:(bb + 1) * 128], qTs[:, blk, :], skt[:, blk, :], start=True, stop=True),
                                reads=['qTs', 'skt'], writes=[pk])
                        evac(sc[:, b4 * 4:(b4 + 1) * 4, :], pa[:].rearrange("p (b t) -> p b t", b=4), [pk], ['sc'])
                        yield
                    for blk in range(16):
                        top16(sc[:, blk, :], 'sc', 128, tv[:, blk, :], tiu[:, blk, :], 'tv')
                        yield
                    S.op('dve', lambda e: e.tensor_copy(tif[:], tiu[:]), reads=['tvi'], writes=['tif'])
                    tvv = tv[:].rearrange("p (h two) k -> p h two k", two=2)
                    tfv = tif[:].rearrange("p (h two) k -> p h two k", two=2)
                    S.op('dve', lambda e: e.tensor_tensor(
                        cand[:].rearrange("p h (a b) -> p h a b", b=16),
                        bc(tvv[:, :, 0, :].unsqueeze(3), [128, 8, 16, 16]),
                        bc(tvv[:, :, 1, :].unsqueeze(2), [128, 8, 16, 16]), ALU.add), reads=['tv'], writes=['cand'])
                    yield
                    for h in range(8):
                        top16(cand[:, h, :], 'cand', 256, bs[:, h, :], bju[:, h, :], 'bs')
                        yield
                    S.op('dve', lambda e: e.tensor_copy(bjf[:], bju[:]), reads=['bsi'], writes=['bjf'])
                    S.op('dve', lambda e: e.tensor_tensor(eq[:], bc(bjf[:].unsqueeze(3), [128, 8, 16, 16]),
                                                          bc(thr16[:].unsqueeze(1).unsqueeze(1), [128, 8, 16, 16]), ALU.is_ge),
                         reads=['bjf', 'thr16'], writes=['eq'])
                    S.op('dve', lambda e: e.tensor_reduce(ja[:], eq[:], AX.X, ALU.add), reads=['eq'], writes=['ja'])
                    S.op('dve', lambda e: e.scalar_tensor_tensor(jb[:], ja[:], -16.0, bjf[:], ALU.mult, ALU.add), reads=['ja', 'bjf'], writes=['jb'])
                    yield
                    iob = bc(iota16[:].unsqueeze(1).unsqueeze(1), [128, 8, 16, 16])
                    for (jx, jk, two, ix, ik) in ((ja, 'ja', 0, i0, 'i0'), (jb, 'jb', 1, i1, 'i1')):
                        S.op('dve', lambda e, jx=jx: e.tensor_tensor(eq[:], iob, bc(jx[:].unsqueeze(3), [128, 8, 16, 16]), ALU.is_equal),
                             reads=['iota16', jk], writes=['eq'])
                        S.op('dve', lambda e, two=two: e.tensor_tensor(eq[:], eq[:], bc(tfv[:, :, two, :].unsqueeze(2), [128, 8, 16, 16]), ALU.mult),
                             reads=['eq', 'tif'], writes=['eq'])
                        S.op('dve', lambda e, ix=ix: e.tensor_reduce(ix[:], eq[:], AX.X, ALU.add), reads=['eq'], writes=[ik])
                        yield
                    S.op('dve', lambda e: e.scalar_tensor_tensor(exf[:].rearrange("p (h k) -> p h k", k=16), i0[:], 128.0, i1[:], ALU.mult, ALU.add),
                         reads=['i0', 'i1'], writes=['exf'])
                    S.op('dve', lambda e: e.tensor_copy(exi[:], exf[:]), reads=['exf'], writes=[xk])
                    yield
                    S.op('dve', lambda e: e.tensor_reduce(gm[:], bs[:], AX.X, ALU.max), reads=['bs'], writes=['gm'])
                    S.op('dve', lambda e: e.tensor_tensor(ge[:], bs[:], bc(gm[:].unsqueeze(2), [128, 8, 16]), ALU.subtract), reads=['bs', 'gm'], writes=[gek])
                    S.op('act', lambda e: e.activation(ge[:], ge[:], AF.Exp), reads=[gek], writes=[gek])
                    S.op('dve', lambda e: e.tensor_reduce(gz[:], ge[:], AX.X, ALU.add), reads=[gek], writes=['gz'])
                    S.op('dve', lambda e: e.reciprocal(gz[:], gz[:]), reads=['gz'], writes=['gz'])
                    S.op('dve', lambda e: e.tensor_tensor(ge[:], ge[:], bc(gz[:].unsqueeze(2), [128, 8, 16]), ALU.mult), reads=[gek, 'gz'], writes=[gek])

                def ustep(t, slot):
                    b = t % 2
                    gb_ = gbuf[ngc[0] % NG]
                    gk = 'gb%d' % (ngc[0] % NG)
                    ngc[0] += 1
                    S.dma('pool', None, None, reads=['exi%d' % b], writes=[gk],
                          fn=lambda e: e.indirect_dma_start(
                              out=gb_[:], out_offset=None, in_=pu16[:, :],
                              in_offset=bass.IndirectOffsetOnAxis(ap=exi2[b][:, slot:slot + 1], axis=0)))
                    S.op('dve', lambda e: e.scalar_tensor_tensor(
                        junkb[:], gb_[:], 1.0, hnb2[b][:], ALU.mult, ALU.mult,
                        accum_out=pre2[b][:, slot:slot + 1]), reads=[gk, 'hnb%d' % b], writes=['junkb', 'pre%d' % b])

                def midstep(t):
                    b = t % 2
                    pre, coef, ge = pre2[b], coef2[b], ge2[b]
                    pk_, ck_ = 'pre%d' % b, 'coef%d' % b
                    S.op('dve', lambda e: e.tensor_tensor(t1[:], pre[:], pre[:], ALU.mult), reads=[pk_], writes=['t1'])
                    S.op('dve', lambda e: e.tensor_scalar(t1[:], t1[:], 0.044715, 1.0, ALU.mult, ALU.add), reads=['t1'], writes=['t1'])
                    S.op('dve', lambda e: e.tensor_tensor(t1[:], t1[:], pre[:], ALU.mult), reads=['t1', pk_], writes=['t1'])
                    S.op('act', lambda e: e.activation(t2[:], t1[:], AF.Sigmoid, scale=1.5957691216057308), reads=['t1'], writes=['t2'])
                    S.op('dve', lambda e: e.tensor_tensor(t2[:], t2[:], pre[:], ALU.mult), reads=['t2', pk_], writes=['t2'])
                    S.op('dve', lambda e: e.tensor_tensor(coef[:], t2[:], ge[:].rearrange("p h k -> p (h k)"), ALU.mult),
                         reads=['t2', 'ge%d' % b], writes=[ck_])
                    build_diag(t, 0)

                def build_diag(t, k):
                    b = t % 2
                    S.op('dve', lambda e: e.tensor_tensor(
                        dgb[k % 2][:], bc(identf[:].unsqueeze(1), [128, DB, 128]),
                        bc(coef2[b][:, k * DB:(k + 1) * DB].unsqueeze(2), [128, DB, 128]), ALU.mult),
                        reads=['identf', 'coef%d' % b], writes=['dgb%d' % (k % 2)])

                def vstep(t, slot):
                    b = t % 2
                    gb_ = gbuf[ngc[0] % NG]
                    gk = 'gb%d' % (ngc[0] % NG)
                    ngc[0] += 1
                    S.dma('pool', None, None, reads=['exi%d' % b], writes=[gk],
                          fn=lambda e: e.indirect_dma_start(
                              out=gb_[:], out_offset=None, in_=pv16[:, :],
                              in_offset=bass.IndirectOffsetOnAxis(ap=exi2[b][:, slot:slot + 1], axis=0)))
                    if slot % DB == 0 and slot + DB < 128:
                        build_diag(t, slot // DB + 1)
                    dgt = dgb[(slot // DB) % 2]
                    dk = 'dgb%d' % ((slot // DB) % 2)
                    for cb in range(4):
                        S.op('pe', lambda e, cb=cb: e.matmul(
                            pvacc[cb][:], dgt[:, slot % DB, :], gb_[:, cb * 512:(cb + 1) * 512], start=(slot == 0), stop=(slot == 127)),
                            reads=[dk, gk], writes=[pvk[cb]])

                def finstep(t):
                    b = t % 2
                    rows = slice(t * 128, (t + 1) * 128)
                    acc = h1t2[b]
                    ak = 'h1t%d' % b
                    for cb in range(4):
                        S.op('dve', lambda e, cb=cb: e.tensor_tensor(acc[:, cb * 512:(cb + 1) * 512], pvacc[cb][:], acc[:, cb * 512:(cb + 1) * 512], ALU.add),
                             reads=[pvk[cb], ak], writes=[ak])
                    norm_rows(acc[:], ak, ssf, rstdf, hnb2[b], 'p9f', jkey='hnb%d' % b)
                    S.op('dve', lambda e: e.scalar_tensor_tensor(acc[:], acc[:], rstdf[:, 0:1], gfin[:], ALU.mult, ALU.mult),
                         reads=[ak, 'p9frstd', 'gfin'], writes=[ak])
                    S.dma('sp', out_d[rows, :], acc[:], reads=[ak], writes=['out'])

                for _ in front(0):
                    pass
                for slot in range(128):
                    ustep(0, slot)
                midstep(0)
                for t in range(8):
                    if t + 1 < 8:
                        fg = front(t + 1)
                        HEAD = 96
                        for slot in range(HEAD):
                            vstep(t, slot)
                            next(fg, None)
                        for _ in fg:
                            pass
                        vs_, us_ = HEAD, 0
                        while vs_ < 128 or us_ < 128:
                            if us_ < 128:
                                ustep(t + 1, us_)
                                us_ += 1
                            if vs_ < 128 and (vs_ - HEAD) * 128 < us_ * (128 - HEAD):
                                vstep(t, vs_)
                                vs_ += 1
                    else:
                        for slot in range(128):
                            vstep(t, slot)
                    finstep(t)
                    if t + 1 < 8:
                        midstep(t + 1)

        S.finish()
    return nc, dbg


def host_consts(j, rel_table):
    tab = np.asarray(rel_table, np.float32)
    k = np.arange(128)[:, None]
    q = np.arange(128)[None, :]
    abs_t = np.empty((12, 128, 16, 128), np.float32)
    for u in range(12):
        if u == 11:
            abs_t[u] = tab[31][None, :, None]
            continue
        dist = 128 * (u + j - 3) + q - k
        val = tab[_bucket(dist)]
        val = np.where((dist >= 0)[:, :, None], val, np.float32(NEGM))
        abs_t[u] = val.transpose(0, 2, 1)
    abw_t = np.empty((8, 128, 16, 128), np.float32)
    for u in range(8):
        dist = 128 * (u + j - 3) + q - k
        val = tab[_bucket(dist)]
        ok = (dist >= 0) & (dist < 512)
        val = np.where(ok[:, :, None], val, np.float32(NEGM))
        abw_t[u] = val.transpose(0, 2, 1)
    msb = np.empty((4, 128, 128), np.float32)
    for u in range(4):
        dist = 128 * (u + j - 3) + q - k
        msb[u] = np.where(dist >= 1, np.float32(0), np.float32(NEGM))
    cb = np.empty((NS, 2, 128, 16, 128), np.float32)
    selc = np.empty((NS, 128, 64), np.float32)
    for s in range(NS):
        t = (4 * s + j) * 128 + np.arange(128)
        n = np.arange(256)
        dist = t[None, :] - (16 * n[:, None] + 31)
        val = tab[_bucket(dist)]
        ok = (dist >= 0) & (n[:, None] < 255)
        val = np.where(ok[:, :, None], val, np.float32(NEGM)).transpose(0, 2, 1)
        cb[s] = val.reshape(2, 128, 16, 128)
        cur = t[:, None] // 64
        jb = np.arange(64)[None, :]
        valid = jb <= cur
        forced = (jb == 0) | ((cur - jb >= 0) & (cur - jb < 2))
        selc[s] = np.where(valid, np.where(forced, np.float32(1e4), np.float32(0)), np.float32(-1e30))
    return dict(abs=abs_t, abw=abw_t, msb=msb, cbias=cb, selc=selc)


def shared_consts():
    n_cmp = 255
    c_start = np.arange(n_cmp) * 16
    s_start = np.arange(64) * 64
    ov = np.maximum(np.minimum(c_start[:, None] + 32, s_start[None, :] + 64)
                    - np.maximum(c_start[:, None], s_start[None, :]), 0).astype(np.float32) / 32
    ovl = np.zeros((256, 64), np.float32)
    ovl[:255] = ov
    expand = np.zeros((NB, 64, 128), np.float32)
    for kb in range(NB):
        for kk in range(128):
            expand[kb, (128 * kb + kk) // 64, kk] = 1.0
    jj = np.arange(128)[:, None]
    ss = np.arange(128)[None, :]
    tri = (jj >= ss).astype(np.float32)
    iota = np.tile(np.arange(16, dtype=np.float32)[None, :], (128, 1))
    return dict(ovl=ovl.reshape(2, 128, 64), expand=expand, tri=tri, ident=np.eye(128, dtype=np.float32), iota16=iota)


def prep_inputs(inp):
    f = lambda a: np.ascontiguousarray(np.asarray(a, dtype=np.float32))
    x = f(inp['x'])
    shared = shared_consts()
    shared.update(
        w_in=f(inp['w_in'][0]), g_attn=f(inp['attn_norm_g'][0]), g_ffn=f(inp['ffn_norm_g'][0]),
        g_fin=f(inp['final_norm_g']),
        ckw1=f(np.stack([inp['cmp_k_w1'][0], inp['cmp_v_w1'][0]])),
        ckpeT=f(np.stack([np.asarray(inp['cmp_k_pe'][0]).T, np.asarray(inp['cmp_v_pe'][0]).T])),
        ckw2=f(np.stack([inp['cmp_k_w2'][0], inp['cmp_v_w2'][0]])),
        wbn=f(inp['w_branch_nsa'][0]), wbs=f(inp['w_branch_sb'][0]), wout=f(inp['w_out'][0]),
        wq=f(inp['peer_w_q'][0]),
        skT=f(np.asarray(inp['peer_sub_keys'][0]).reshape(16, 128, 128).transpose(0, 2, 1)),
        pu=f(inp['peer_u'][0]), pv=f(inp['peer_v'][0]),
    )
    per_j = [host_consts(j, inp['rel_bias_table']) for j in range(4)]
    in_maps = []
    for c in range(8):
        b, j = c // 4, c % 4
        own = np.concatenate([np.arange((4 * s + j) * 128, (4 * s + j + 1) * 128) for s in range(NS)])
        m = dict(shared)
        m.update(per_j[j])
        m['x_full'] = x[b]
        m['x_own'] = np.ascontiguousarray(x[b][own])
        in_maps.append(m)
    return in_maps


def own_index(j):
    return np.concatenate([np.arange((4 * s + j) * 128, (4 * s + j + 1) * 128) for s in range(NS)])


def kernel(**inputs):
    in_maps = prep_inputs(inputs)
    nc, dbg = build_program()
    res = run_bass_kernel_spmd(nc, in_maps, core_ids=list(range(8)))
    out = np.empty((2, T, D), np.float32)
    for c in range(8):
        b, j = c // 4, c % 4
        out[b, own_index(j)] = res.results[c]["out"]
    return out
```
